# Optimizing a Trainium2 kernel written in Bass

```python
import math
import jax, jax.numpy as jnp
from jax import lax
import numpy as np

D_MODEL = 1024
BATCH = 8
SEQ = 4096
DEPTH = 2

DA_HEADS = 4
DA_HEAD_DIM = 64
DA_WIDTH = DA_HEADS * 2 * DA_HEAD_DIM
MLA_HEADS = 4
MLA_Q_RANK = 256
MLA_KV_RANK = 128
MLA_NOPE_DIM = 64
MLA_ROPE_DIM = 32
MLA_V_DIM = 128
ROPE_THETA = 10000.0
AB_IN_WIDTH = 3 * DA_WIDTH + MLA_Q_RANK + MLA_KV_RANK + MLA_ROPE_DIM
AB_MIX_WIDTH = DA_WIDTH + MLA_HEADS * MLA_V_DIM
DIL_HEADS = 16
DIL_HEAD_DIM = 64
DIL_PATTERNS = ((128, 1), (512, 4), (2048, 16))
DIL_Q_BLOCK = 64
Q_BLOCK = 128
MOE_GROUPS = 4
MOE_EXPERTS_PER_GROUP = 8
MOE_EXPERTS = MOE_GROUPS * MOE_EXPERTS_PER_GROUP
MOE_TOP_K = 2
MOE_HIDDEN = 512
MOE_BLOCK = 256
N_EVEN = (DEPTH + 1) // 2
N_ODD = DEPTH // 2
DEEPNORM_ALPHA = (2 * DEPTH) ** 0.25
DEEPNORM_BETA = (8 * DEPTH) ** -0.25
NORM_EPS = 1e-5

kernel_name = 'hybrid_diff_mla_dilated_hmoe_encoder'


def _layer_norm(x, g, b):
    xf = x.astype(jnp.float32)
    mu = jnp.mean(xf, -1, keepdims=True)
    var = jnp.mean(jnp.square(xf - mu), -1, keepdims=True)
    return ((xf - mu) * lax.rsqrt(var + NORM_EPS)).astype(x.dtype) * g + b


def _rms_norm(x, g):
    xf = x.astype(jnp.float32)
    return (xf * lax.rsqrt(jnp.mean(jnp.square(xf), -1, keepdims=True) + NORM_EPS)).astype(x.dtype) * g


def _alibi_slopes(n_heads):
    start = 2.0 ** (-8.0 / n_heads)
    return jnp.asarray([start ** (i + 1) for i in range(n_heads)], dtype=jnp.float32)


def _rope(t):
    s, r = t.shape[1], t.shape[-1]
    half = r // 2
    inv_freq = jnp.power(ROPE_THETA, -jnp.arange(half, dtype=jnp.float32) * 2.0 / r)
    ang = jnp.arange(s, dtype=jnp.float32)[:, None] * inv_freq[None, :]
    cos = jnp.cos(ang)[None, :, None, :]
    sin = jnp.sin(ang)[None, :, None, :]
    tf = t.astype(jnp.float32)
    t1, t2 = tf[..., :half], tf[..., half:]
    return jnp.concatenate([t1 * cos - t2 * sin, t1 * sin + t2 * cos], -1).astype(t.dtype)


def _to_blocks(t, blk):
    return jnp.swapaxes(t.reshape(t.shape[0], t.shape[1] // blk, blk, *t.shape[2:]), 0, 1)


def _from_blocks(t):
    t = jnp.swapaxes(t, 0, 1)
    return t.reshape(t.shape[0], t.shape[1] * t.shape[2], *t.shape[3:])


def _diff_attention(q1, q2, k1, k2, v, lam, slopes):
    s = q1.shape[1]
    scale = q1.shape[-1] ** -0.5
    pos = jnp.arange(s, dtype=jnp.float32)

    def block(args):
        q1b, q2b, qpos = args
        bias = -slopes[:, None, None] * jnp.abs(qpos[:, None] - pos[None, :])[None]
        s1 = jnp.einsum('bqhd,bkhd->bhqk', q1b, k1).astype(jnp.float32) * scale + bias
        s2 = jnp.einsum('bqhd,bkhd->bhqk', q2b, k2).astype(jnp.float32) * scale + bias
        attn = jax.nn.softmax(s1, -1) - lam * jax.nn.softmax(s2, -1)
        return jnp.einsum('bhqk,bkhe->bqhe', attn.astype(v.dtype), v)

    out = lax.map(block, (_to_blocks(q1, Q_BLOCK), _to_blocks(q2, Q_BLOCK), pos.reshape(s // Q_BLOCK, Q_BLOCK)))
    return _from_blocks(out)


def _mla_attention(q_nope, q_rope, k_nope, k_rope, v):
    scale = (q_nope.shape[-1] + q_rope.shape[-1]) ** -0.5

    def block(args):
        qnb, qrb = args
        sc = (jnp.einsum('bqhd,bkhd->bhqk', qnb, k_nope)
              + jnp.einsum('bqhr,bkr->bhqk', qrb, k_rope)).astype(jnp.float32) * scale
        p = jax.nn.softmax(sc, -1)
        return jnp.einsum('bhqk,bkhe->bqhe', p.astype(v.dtype), v)

    out = lax.map(block, (_to_blocks(q_nope, Q_BLOCK), _to_blocks(q_rope, Q_BLOCK)))
    return _from_blocks(out)


def _dilated_attention(q, k, v, slopes):
    s = q.shape[1]
    scale = q.shape[-1] ** -0.5

    def block(args):
        qb, t0 = args
        t = t0 + jnp.arange(DIL_Q_BLOCK, dtype=jnp.int32)
        outs, lses = [], []
        for window, dilation in DIL_PATTERNS:
            n_side = window // (2 * dilation)
            off = dilation * (jnp.arange(2 * n_side + 1, dtype=jnp.int32) - n_side)
            idx = t[:, None] + off[None, :]
            valid = (idx >= 0) & (idx < s)
            idx = jnp.clip(idx, 0, s - 1)
            kg = k[:, idx]
            vg = v[:, idx]
            sc = jnp.einsum('bqhd,bqjhd->bhqj', qb, kg).astype(jnp.float32) * scale
            sc = sc - slopes[:, None, None] * jnp.abs(off).astype(jnp.float32)
            sc = jnp.where(valid, sc, -jnp.inf)
            lse = jax.nn.logsumexp(sc, -1)
            p = jnp.exp(sc - lse[..., None])
            outs.append(jnp.einsum('bhqj,bqjhd->bqhd', p.astype(v.dtype), vg))
            lses.append(lse)
        mix = jax.nn.softmax(jnp.stack(lses), axis=0)
        mix = jnp.swapaxes(mix, 2, 3)[..., None].astype(v.dtype)
        return sum(mix[i] * o for i, o in enumerate(outs))

    starts = jnp.arange(s // DIL_Q_BLOCK, dtype=jnp.int32) * DIL_Q_BLOCK
    out = lax.map(block, (_to_blocks(q, DIL_Q_BLOCK), starts))
    return _from_blocks(out)


def _mixer_ab(x, w_in, lam_q1, lam_k1, lam_q2, lam_k2, diff_g, q_norm_g, w_uq, kv_norm_g, w_ukv, w_out, layer_idx):
    b, s, _ = x.shape
    h = x @ w_in
    cuts = [DA_WIDTH, 2 * DA_WIDTH, 3 * DA_WIDTH, 3 * DA_WIDTH + MLA_Q_RANK, 3 * DA_WIDTH + MLA_Q_RANK + MLA_KV_RANK]
    qa, ka, va, c_q, c_kv, k_r = jnp.split(h, cuts, axis=-1)
    qa = qa.reshape(b, s, DA_HEADS, 2, DA_HEAD_DIM)
    ka = ka.reshape(b, s, DA_HEADS, 2, DA_HEAD_DIM)
    va = va.reshape(b, s, DA_HEADS, 2 * DA_HEAD_DIM)
    lam_init = 0.8 - 0.6 * math.exp(-0.3 * layer_idx)
    lam = (jnp.exp(jnp.sum(lam_q1 * lam_k1).astype(jnp.float32))
           - jnp.exp(jnp.sum(lam_q2 * lam_k2).astype(jnp.float32)) + lam_init)
    oa = _diff_attention(qa[:, :, :, 0], qa[:, :, :, 1], ka[:, :, :, 0], ka[:, :, :, 1], va, lam,
                         _alibi_slopes(DA_HEADS))
    oa = _rms_norm(oa, diff_g) * (1.0 - lam_init)
    q = (_rms_norm(c_q, q_norm_g) @ w_uq).reshape(b, s, MLA_HEADS, MLA_NOPE_DIM + MLA_ROPE_DIM)
    q_nope, q_rope = q[..., :MLA_NOPE_DIM], _rope(q[..., MLA_NOPE_DIM:])
    kv = (_rms_norm(c_kv, kv_norm_g) @ w_ukv).reshape(b, s, MLA_HEADS, MLA_NOPE_DIM + MLA_V_DIM)
    k_nope, vb = kv[..., :MLA_NOPE_DIM], kv[..., MLA_NOPE_DIM:]
    k_rope = _rope(k_r[:, :, None, :])[:, :, 0]
    ob = _mla_attention(q_nope, q_rope, k_nope, k_rope, vb)
    o = jnp.concatenate([oa.reshape(b, s, -1), ob.reshape(b, s, -1)], -1)
    return o @ w_out


def _mixer_c(x, w_in, w_out):
    b, s, _ = x.shape
    h = (x @ w_in).reshape(b, s, 3, DIL_HEADS, DIL_HEAD_DIM)
    o = _dilated_attention(h[:, :, 0], h[:, :, 1], h[:, :, 2], _alibi_slopes(DIL_HEADS))
    return o.reshape(b, s, -1) @ w_out


def _hier_moe(x, w_group, b_group, w_route, b_route, w_gate, w_up, w_down):
    b, s, d = x.shape
    n_tok = b * s
    xf = x.reshape(n_tok, d)
    coarse = (xf @ w_group + b_group).astype(jnp.float32)
    g_sel = jnp.argmax(coarse, -1)
    p_group = jnp.take_along_axis(jax.nn.softmax(coarse, -1), g_sel[:, None], -1)[:, 0]
    fine = (xf @ w_route + b_route).astype(jnp.float32).reshape(n_tok, MOE_GROUPS, MOE_EXPERTS_PER_GROUP)
    fine = jnp.take_along_axis(fine, g_sel[:, None, None], axis=1)[:, 0]
    top_v, top_i = lax.top_k(fine, MOE_TOP_K)
    gate = (p_group[:, None] * jax.nn.softmax(top_v, -1)).astype(x.dtype)
    eid = g_sel[:, None].astype(jnp.int32) * MOE_EXPERTS_PER_GROUP + top_i.astype(jnp.int32)
    n_asg = n_tok * MOE_TOP_K
    a_eid = eid.reshape(-1)
    a_tok = jnp.repeat(jnp.arange(n_tok, dtype=jnp.int32), MOE_TOP_K)
    a_w = gate.reshape(-1)
    order = jnp.argsort(a_eid)
    s_eid, s_tok, s_w = a_eid[order], a_tok[order], a_w[order]
    counts = jnp.bincount(a_eid, length=MOE_EXPERTS).astype(jnp.int32)
    padded = (counts + MOE_BLOCK - 1) // MOE_BLOCK * MOE_BLOCK
    raw_start = jnp.cumsum(counts) - counts
    pad_end = jnp.cumsum(padded)
    pad_start = pad_end - padded
    n_slot = n_asg + MOE_EXPERTS * MOE_BLOCK
    n_blk = n_slot // MOE_BLOCK
    dest = pad_start[s_eid] + jnp.arange(n_asg, dtype=jnp.int32) - raw_start[s_eid]
    slot_tok = jnp.full((n_slot,), n_tok, jnp.int32).at[dest].set(s_tok)
    slot_w = jnp.zeros((n_slot,), x.dtype).at[dest].set(s_w)
    blk_eid = jnp.minimum(jnp.searchsorted(pad_end, jnp.arange(n_blk, dtype=jnp.int32) * MOE_BLOCK, side='right'),
                          MOE_EXPERTS - 1)
    x_pad = jnp.concatenate([xf, jnp.zeros((1, d), xf.dtype)], 0)

    def expert_block(args):
        e, tok = args
        xb = x_pad[tok]
        hb = jax.nn.silu(xb @ w_gate[e]) * (xb @ w_up[e])
        return hb @ w_down[e]

    ys = lax.map(expert_block, (blk_eid, slot_tok.reshape(n_blk, MOE_BLOCK)))
    ys = ys.reshape(n_slot, d) * slot_w[:, None]
    out = jnp.zeros((n_tok + 1, d), x.dtype).at[slot_tok].add(ys)[:n_tok]
    return out.reshape(b, s, d)


def setup_inputs(seed: int = 0) -> dict:
    key = jax.random.key(seed)
    ks = iter(jax.random.split(key, 32))

    def nrm(shape, scale):
        return jax.random.normal(next(ks), shape, jnp.float32) * scale

    def gain(shape):
        return 1.0 + nrm(shape, 0.02)

    d = D_MODEL
    dil_w = DIL_HEADS * DIL_HEAD_DIM
    return {
        'x': nrm((BATCH, SEQ, d), 1.0),
        'w_in_ab': nrm((N_EVEN, d, AB_IN_WIDTH), d ** -0.5),
        'lam_q1': nrm((N_EVEN, DA_HEAD_DIM), 0.1),
        'lam_k1': nrm((N_EVEN, DA_HEAD_DIM), 0.1),
        'lam_q2': nrm((N_EVEN, DA_HEAD_DIM), 0.1),
        'lam_k2': nrm((N_EVEN, DA_HEAD_DIM), 0.1),
        'diff_norm_g': gain((N_EVEN, 2 * DA_HEAD_DIM)),
        'mla_q_norm_g': gain((N_EVEN, MLA_Q_RANK)),
        'w_uq': nrm((N_EVEN, MLA_Q_RANK, MLA_HEADS * (MLA_NOPE_DIM + MLA_ROPE_DIM)), MLA_Q_RANK ** -0.5),
        'mla_kv_norm_g': gain((N_EVEN, MLA_KV_RANK)),
        'w_ukv': nrm((N_EVEN, MLA_KV_RANK, MLA_HEADS * (MLA_NOPE_DIM + MLA_V_DIM)), MLA_KV_RANK ** -0.5),
        'w_out_ab': nrm((N_EVEN, AB_MIX_WIDTH, d), AB_MIX_WIDTH ** -0.5 * DEEPNORM_BETA),
        'w_in_c': nrm((N_ODD, d, 3 * dil_w), d ** -0.5),
        'w_out_c': nrm((N_ODD, dil_w, d), dil_w ** -0.5 * DEEPNORM_BETA),
        'ln_mix_g': gain((DEPTH, d)),
        'ln_mix_b': nrm((DEPTH, d), 0.02),
        'moe_w_group': nrm((DEPTH, d, MOE_GROUPS), d ** -0.5),
        'moe_b_group': nrm((DEPTH, MOE_GROUPS), 0.01),
        'moe_w_route': nrm((DEPTH, d, MOE_EXPERTS), d ** -0.5),
        'moe_b_route': nrm((DEPTH, MOE_EXPERTS), 0.01),
        'moe_w_gate': nrm((DEPTH, MOE_EXPERTS, d, MOE_HIDDEN), d ** -0.5),
        'moe_w_up': nrm((DEPTH, MOE_EXPERTS, d, MOE_HIDDEN), d ** -0.5),
        'moe_w_down': nrm((DEPTH, MOE_EXPERTS, MOE_HIDDEN, d), MOE_HIDDEN ** -0.5 * DEEPNORM_BETA),
        'ln_ffn_g': gain((DEPTH, d)),
        'ln_ffn_b': nrm((DEPTH, d), 0.02),
    }


def reference(x, w_in_ab, lam_q1, lam_k1, lam_q2, lam_k2, diff_norm_g, mla_q_norm_g, w_uq, mla_kv_norm_g, w_ukv,
              w_out_ab, w_in_c, w_out_c, ln_mix_g, ln_mix_b, moe_w_group, moe_b_group, moe_w_route, moe_b_route,
              moe_w_gate, moe_w_up, moe_w_down, ln_ffn_g, ln_ffn_b):
    for layer in range(DEPTH):
        i = layer // 2
        if layer % 2 == 0:
            mixed = _mixer_ab(x, w_in_ab[i], lam_q1[i], lam_k1[i], lam_q2[i], lam_k2[i], diff_norm_g[i],
                              mla_q_norm_g[i], w_uq[i], mla_kv_norm_g[i], w_ukv[i], w_out_ab[i], layer)
        else:
            mixed = _mixer_c(x, w_in_c[i], w_out_c[i])
        x = _layer_norm(DEEPNORM_ALPHA * x + mixed, ln_mix_g[layer], ln_mix_b[layer])
        ffn = _hier_moe(x, moe_w_group[layer], moe_b_group[layer], moe_w_route[layer], moe_b_route[layer],
                        moe_w_gate[layer], moe_w_up[layer], moe_w_down[layer])
        x = _layer_norm(DEEPNORM_ALPHA * x + ffn, ln_ffn_g[layer], ln_ffn_b[layer])
    return x
```

```python
import math
import numpy as np
from contextlib import ExitStack
import concourse.bass as bass
import concourse.mybir as mybir
from concourse.bass_utils import run_bass_kernel_spmd

F32 = mybir.dt.float32
BF16 = mybir.dt.bfloat16
I32 = mybir.dt.int32
ALU = mybir.AluOpType
AF = mybir.ActivationFunctionType
AX = mybir.AxisListType

S = 4096
D = 1024
NT = S // 128
DEPTH = 2
ALPHA = (2 * DEPTH) ** 0.25
EPS = 1e-5
NPOOL = 16
E_ = 32
HID = 512
BLK = 256
NB = 63
NSLOT = NB * BLK
NEGD_C = 3968
NEGD_W = 8064


class Buf:
    __slots__ = ("t", "w", "r", "name", "multi", "wm")

    def __init__(self, t, name="", multi=False):
        self.t = t
        self.w = None
        self.r = {}
        self.name = name
        self.multi = multi
        self.wm = {}

    def __getitem__(self, k):
        return self.t[k]

    def ap(self):
        return self.t.ap()


class FW:
    def __init__(self, nc, stack):
        self.nc = nc
        self.stack = stack
        self.engs = {"pe": nc.tensor, "act": nc.scalar, "dve": nc.vector, "pool": nc.gpsimd, "sp": nc.sync}
        self.esem, self.cnt, self.seen, self.dq = {}, {}, {}, {}
        for e in self.engs:
            self.esem[e] = stack.enter_context(nc.semaphore("es_" + e))
            self.cnt[e] = 0
            self.seen[e] = {}
        for q in ("sp", "pool", "act"):
            sems = [stack.enter_context(nc.semaphore(f"dq_{q}_{i}")) for i in range(NPOOL)]
            self.dq[q] = {"sems": sems, "n": 0}
        self.n_inst = 0
        self.uid = 0

    def sb(self, name, shape, dtype, stack=None, multi=False):
        st = stack if stack is not None else self.stack
        self.uid += 1
        return Buf(st.enter_context(self.nc.sbuf_tensor(f"s{self.uid}_{name}", list(shape), dtype)), name, multi)

    def ps(self, name, shape, dtype=F32, stack=None):
        st = stack if stack is not None else self.stack
        return Buf(st.enter_context(self.nc.psum_tensor("p_" + name, list(shape), dtype)), name)

    def dram(self, name, shape, dtype, kind="Internal"):
        return Buf(self.nc.dram_tensor(name, list(shape), dtype, kind=kind), name, True)

    def op(self, eng, fn, reads=(), writes=(), dma=False):
        waits = {}
        own = self.esem[eng]
        seen = self.seen[eng]

        def need(sem, val):
            if eng == "pe" and sem is own:
                return
            if seen.get(sem, 0) >= val:
                return
            if waits.get(sem, 0) < val:
                waits[sem] = val

        for b in reads:
            if b.multi:
                for s_, v_ in b.wm.items():
                    need(s_, v_)
            elif b.w is not None:
                need(*b.w)
        for b in writes:
            if not b.multi and b.w is not None:
                need(*b.w)
            for s_, v_ in b.r.items():
                need(s_, v_)
        if dma:
            q = self.dq[eng]
            j = q["n"]
            s = q["sems"][j % NPOOL]
            if j >= NPOOL:
                need(s, 16 * (j // NPOOL))
        e = self.engs[eng]
        for sem, val in waits.items():
            e.wait_ge(sem, val)
            seen[sem] = val
        ins = fn(e)
        self.n_inst += 1
        if dma:
            ins.then_inc(s, 16)
            ev = (s, 16 * (j // NPOOL + 1))
            q["n"] += 1
        else:
            self.cnt[eng] += 1
            ins.then_inc(own, 1)
            ev = (own, self.cnt[eng])
        for b in reads:
            if b.r.get(ev[0], 0) < ev[1]:
                b.r[ev[0]] = ev[1]
        for b in writes:
            if b.multi:
                if b.wm.get(ev[0], 0) < ev[1]:
                    b.wm[ev[0]] = ev[1]
            else:
                b.w = ev
                b.r = {}
        return ev

    def barrier(self):
        evs = []
        for e in self.engs:
            if self.cnt[e] > 0:
                evs.append((self.esem[e], self.cnt[e]))
        for q in self.dq.values():
            n = q["n"]
            for i, s in enumerate(q["sems"]):
                k = (n - i + NPOOL - 1) // NPOOL
                if k > 0:
                    evs.append((s, 16 * k))
        for e, eo in self.engs.items():
            for sem, val in evs:
                if sem is self.esem[e]:
                    continue
                if self.seen[e].get(sem, 0) >= val:
                    continue
                eo.wait_ge(sem, val)
                self.seen[e][sem] = val


class Rot:
    def __init__(self, bufs):
        self.bufs = bufs
        self.i = 0

    def next(self):
        b = self.bufs[self.i % len(self.bufs)]
        self.i += 1
        return b


def ssl(start, n, step):
    return slice(start, start + step * (n - 1) + 1, step)


def alibi_slopes(n):
    start = 2.0 ** (-8.0 / n)
    return [start ** (i + 1) for i in range(n)]


def build_program(upto=None, dumps=()):
    nc = bass.Bass("TRN2", target_bir_lowering=False)
    ext = {}

    def ein(name, shape, dtype=F32):
        ext[name] = Buf(nc.dram_tensor(name, list(shape), dtype, kind="ExternalInput"), name)
        return ext[name]

    x_in = ein("x", [S, D])
    ident_d = ein("ident", [128, 128])
    negd_d = ein("negd", [128, NEGD_W])
    cos_d = ein("ropecos", [128, S])
    sin_d = ein("ropesin", [128, S])
    w_in_ab = ein("w_in_ab", [D, 1984])
    w_uq = ein("w_uq", [256, 512])
    w_ukv = ein("w_ukv", [128, 768])
    w_out_ab = ein("w_out_ab", [D, D])
    lam4 = ein("lam4", [4, 64])
    diff_g = ein("diff_g", [128, 1])
    qn_g = ein("qn_g", [128, 2])
    kvn_g = ein("kvn_g", [128, 1])
    ln_mix_g = ein("ln_mix_g", [2, D])
    ln_mix_b = ein("ln_mix_b", [2, D])
    ln_ffn_g = ein("ln_ffn_g", [2, D])
    ln_ffn_b = ein("ln_ffn_b", [2, D])
    w_router = ein("w_router", [2, D, 36])
    b_router = ein("b_router", [2, 36])
    wg_l = ein("wg_l", [2 * E_ * 128, 8 * HID])
    wu_l = ein("wu_l", [2 * E_ * 128, 8 * HID])
    wd_l = ein("wd_l", [2 * E_ * 128, 4 * D])
    w_in_c = ein("w_in_c", [D, 3 * D])
    w_out_c = ein("w_out_c", [D, D])
    dil_negd_d = ein("dil_negd", [128, 3, 512])
    moe_c = ein("moe_c", [128, 4, 64])
    triu_d = ein("triu", [128, 128])
    tokid_d = ein("tokid", [128, NT])
    sel_d = ein("sel", [128, 64])
    out_d = Buf(nc.dram_tensor("out", [S, D], F32, kind="ExternalOutput"), "out", True)

    dump_bufs = {}

    with ExitStack() as st:
        fw = FW(nc, st)
        op = fw.op

        def dma(q, out_ap, in_ap, reads, writes):
            return op(q, lambda e: e.dma_start(out=out_ap, in_=in_ap), reads, writes, dma=True)

        _dram0 = fw.dram
        fw.dram = lambda name, shape, dtype: _dram0(name, shape, dtype, kind=("ExternalOutput" if name in dumps else "Internal"))
        qkA = fw.dram("qkA", [8, 128, S], BF16)
        vA = fw.dram("vA", [S, 512], BF16)
        cq_d = fw.dram("cq_d", [256, S], F32)
        ckv_d = fw.dram("ckv_d", [128, S], F32)
        qmT = fw.dram("qmT", [4, 96, S], BF16)
        kmT = fw.dram("kmT", [4, 96, S], BF16)
        vM = fw.dram("vM", [S, 512], BF16)
        oT_d = fw.dram("oT_d", [8, 128, S], BF16)
        x1_d = fw.dram("x1_d", [S, D], F32)
        x1b_d = fw.dram("x1b_d", [S + 1, D], BF16)
        x2_d = fw.dram("x2_d", [S, D], F32)
        slot_d = fw.dram("slot_d", [NSLOT + 128, 4], F32)
        y_d = fw.dram("y_d", [2 * S + 128, D], F32)
        wgb = fw.dram("wgb", [2 * E_ * 128, 8 * HID], BF16)
        wub = fw.dram("wub", [2 * E_ * 128, 8 * HID], BF16)
        wdb = fw.dram("wdb", [2 * E_ * 128, 4 * D], BF16)
        cast_list = []
        CR = 512
        for l_ in range(2):
            for src_, dst_ in ((wg_l, wgb), (wu_l, wub), (wd_l, wdb)):
                for r_ in range(l_ * E_ * 128, (l_ + 1) * E_ * 128, CR):
                    cast_list.append((src_, dst_, r_))
        cast_bufs = []

        def issue_cast(n=1):
            for _ in range(n):
                if not cast_list:
                    return
                src_, dst_, r_ = cast_list.pop(0)
                cb = Buf(None, "castchunk")
                cast_bufs.append(cb)
                dma("pool", dst_[r_:r_ + CR, :], src_[r_:r_ + CR, :], [src_], [cb])
        qkC = fw.dram("qkC", [16, 128, S], BF16)
        vC = fw.dram("vC", [3, 4, 128, NT, 260], BF16)

        ident = fw.sb("ident", [128, 128], F32)
        dma("sp", ident[:], ident_d.ap(), [ident_d], [ident])
        ones_bf = fw.sb("ones_bf", [128, 128], BF16)
        op("dve", lambda e: e.memset(ones_bf[:], 1.0), [], [ones_bf])
        ones_f = fw.sb("ones_f", [128, 128], F32)
        op("dve", lambda e: e.memset(ones_f[:], 1.0), [], [ones_f])
        eps_t = fw.sb("eps_t", [128, 1], F32)
        op("dve", lambda e: e.memset(eps_t[:], EPS), [], [eps_t])
        psb = [fw.ps(f"psb{i}", [128, 512], F32) for i in range(8)]

        def dump(name, buf_ap, shape, dtype, reads):
            if name in dumps:
                d = Buf(nc.dram_tensor("dbg_" + name, list(shape), dtype, kind="ExternalOutput"), name)
                dump_bufs[name] = d
                dma("sp", d.ap(), buf_ap, reads, [d])

        def build_xT(src_d, xT, pst, stg):
            for i in range(NT):
                xt = stg.next()
                dma("sp", xt[:], src_d[i * 128:(i + 1) * 128, :], [src_d], [xt])
                for hf in range(2):
                    ps = pst.next()
                    for c4 in range(4):
                        c = hf * 4 + c4
                        op("pe", lambda e: e.transpose(out=ps[:, c4 * 128:(c4 + 1) * 128], in_=xt[:, c * 128:(c + 1) * 128], identity=ident[:]), [xt, ident], [ps])
                    eng = "act" if hf == 0 else "dve"
                    o_ap = xT[:, hf * 4:(hf + 1) * 4, i * 128:(i + 1) * 128]
                    i_ap = ps[:, :].rearrange("p (c n) -> p c n", c=4)
                    if eng == "act":
                        op("act", lambda e: e.copy(out=o_ap, in_=i_ap), [ps], [xT])
                    else:
                        op("dve", lambda e: e.tensor_copy(out=o_ap, in_=i_ap), [ps], [xT])

        def attn_core(QT, KT, Vsb, krows, q0, po, pss, pscore, slope, negd, tmps, es, band=None):
            qb_, qr = QT
            kb_, kr = KT
            kbs = list(range(NT))
            for n, kb in enumerate(kbs):
                ps = pscore.next()
                op("pe", lambda e: e.matmul(ps[:, :], lhsT=kb_[kr:kr + krows, kb * 128:(kb + 1) * 128], rhs=qb_[qr:qr + krows, q0:q0 + 512], start=True, stop=True), [kb_, qb_], [ps])
                E = es.next()
                if slope is not None:
                    tmp = tmps.next()
                    n0 = q0 - kb * 128 + NEGD_C
                    op("dve", lambda e: e.scalar_tensor_tensor(out=tmp[:, :], in0=negd[:, n0:n0 + 512], scalar=float(slope), in1=ps[:, :], op0=ALU.mult, op1=ALU.add), [negd, ps], [tmp])
                    op("act", lambda e: e.activation(out=E[:, :], in_=tmp[:, :], func=AF.Exp), [tmp], [E])
                else:
                    op("act", lambda e: e.activation(out=E[:, :], in_=ps[:, :], func=AF.Exp), [ps], [E])
                first, last = (n == 0), (n == len(kbs) - 1)
                op("pe", lambda e: e.matmul(po[:, :], lhsT=Vsb[:, kb, :], rhs=E[:, :], start=first, stop=last), [Vsb, E], [po])
                op("pe", lambda e: e.matmul(pss[:, :], lhsT=ones_bf[:, :], rhs=E[:, :], start=first, stop=last), [ones_bf, E], [pss])

        def rstd_from_ssq(ps_ssq, n, out_t, tmp_t):
            op("act", lambda e: e.activation(out=tmp_t[:, :], in_=ps_ssq[:, :], func=AF.Sqrt, bias=eps_t[:, 0:1], scale=1.0 / n), [ps_ssq, eps_t], [tmp_t])
            op("dve", lambda e: e.reciprocal(out=out_t[:, :], in_=tmp_t[:, :]), [tmp_t], [out_t])

        with ExitStack() as ph:
            xT = fw.sb("xT", [128, 8, S], BF16, ph)
            Win = fw.sb("Win", [128, 8, 1984], BF16, ph, multi=True)
            for c in range(8):
                dma("pool", Win[:, c, :], w_in_ab[c * 128:(c + 1) * 128, :], [w_in_ab], [Win])
            cosT = fw.sb("cosT", [128, S], F32, ph)
            sinT = fw.sb("sinT", [128, S], F32, ph)
            dma("sp", cosT[:], cos_d.ap(), [cos_d], [cosT])
            dma("sp", sinT[:], sin_d.ap(), [sin_d], [sinT])
            stg = Rot([fw.sb(f"a0_x{i}", [128, D], F32, ph) for i in range(2)])
            pst = Rot(psb[0:2])
            build_xT(x_in, xT, pst, stg)
            dump("xT", xT[:, :, :], [128, 8, S], BF16, [xT])
            psr = Rot(psb[2:8])
            sbf = Rot([fw.sb(f"a0_sb{i}", [128, 512], BF16, ph) for i in range(4)])
            sf = Rot([fw.sb(f"a0_sf{i}", [128, 512], F32, ph) for i in range(4)])
            tog = [0]

            def evac(out_ap, in_ap, reads, writes, scale=None):
                tog[0] ^= 1
                if tog[0] or scale is not None:
                    if scale is None:
                        op("act", lambda e: e.copy(out=out_ap, in_=in_ap), reads, writes)
                    else:
                        op("act", lambda e: e.activation(out=out_ap, in_=in_ap, func=AF.Copy, scale=float(scale)), reads, writes)
                else:
                    op("dve", lambda e: e.tensor_copy(out=out_ap, in_=in_ap), reads, writes)

            def proj_fm(Wsb, nchunk, col0, M, rhs_fn, rhs_reads, tb):
                ps = psr.next()
                for c in range(nchunk):
                    op("pe", lambda e: e.matmul(ps[0:M, :], lhsT=Wsb[:, c, col0:col0 + M], rhs=rhs_fn(c), start=(c == 0), stop=(c == nchunk - 1)), [Wsb] + rhs_reads, [ps])
                return ps

            for tb in range(8):
                tsl = slice(tb * 512, (tb + 1) * 512)
                rf = lambda c: xT[:, c, tsl]
                for h in range(8):
                    ps = proj_fm(Win, 8, h * 128, 128, rf, [xT], tb)
                    s_ = sbf.next()
                    evac(s_[:, :], ps[:, :], [ps], [s_], scale=(0.125 if h < 4 else None))
                    dma("pool", qkA[h, :, tsl], s_[:, :], [s_], [qkA])
                for j in range(3):
                    ps = proj_fm(Win, 8, 1536 + j * 128, 128, rf, [xT], tb)
                    s_ = sf.next()
                    evac(s_[:, :], ps[:, :], [ps], [s_])
                    if j < 2:
                        dma("pool", cq_d[j * 128:(j + 1) * 128, tsl], s_[:, :], [s_], [cq_d])
                    else:
                        dma("pool", ckv_d[:, tsl], s_[:, :], [s_], [ckv_d])
                ps1 = proj_fm(Win, 8, 1920, 32, rf, [xT], tb)
                ps2 = proj_fm(Win, 8, 1952, 32, rf, [xT], tb)
                t1, t2 = sf.next(), sf.next()
                op("dve", lambda e: e.tensor_tensor(out=t1[0:32, :], in0=ps1[0:32, :], in1=cosT[0:32, tsl], op=ALU.mult), [ps1, cosT], [t1])
                op("dve", lambda e: e.tensor_tensor(out=t2[0:32, :], in0=ps2[0:32, :], in1=sinT[0:32, tsl], op=ALU.mult), [ps2, sinT], [t2])
                s_ = sbf.next()
                op("dve", lambda e: e.tensor_tensor(out=s_[0:32, :], in0=t1[0:32, :], in1=t2[0:32, :], op=ALU.add), [t1, t2], [s_])
                for h in range(4):
                    dma("pool", kmT[h, 64:96, tsl], s_[0:32, :], [s_], [kmT])
            for i in range(NT):
                ps = psr.next()
                for c in range(8):
                    op("pe", lambda e: e.matmul(ps[:, :], lhsT=xT[:, c, i * 128:(i + 1) * 128], rhs=Win[:, c, 1024:1536], start=(c == 0), stop=(c == 7)), [xT, Win], [ps])
                s_ = sbf.next()
                evac(s_[:, :], ps[:, :], [ps], [s_])
                dma("pool", vA[i * 128:(i + 1) * 128, :], s_[:, :], [s_], [vA])
            fw.barrier()
        if upto == "A0":
            return nc, ext, out_d, dump_bufs

        with ExitStack() as ph:
            cq = fw.sb("cq", [128, 2, S], F32, ph, multi=True)
            ckv = fw.sb("ckv", [128, S], F32, ph)
            for j in range(2):
                dma("sp", cq[:, j, :], cq_d[j * 128:(j + 1) * 128, :], [cq_d], [cq])
            dma("sp", ckv[:, :], ckv_d.ap(), [ckv_d], [ckv])
            Wuq = fw.sb("Wuq", [128, 2, 512], BF16, ph, multi=True)
            for j in range(2):
                dma("pool", Wuq[:, j, :], w_uq[j * 128:(j + 1) * 128, :], [w_uq], [Wuq])
            Wukv = fw.sb("Wukv", [128, 1, 768], BF16, ph)
            dma("pool", Wukv[:, 0, :], w_ukv.ap(), [w_ukv], [Wukv])
            gq = fw.sb("gq", [128, 2], F32, ph)
            gkv = fw.sb("gkv", [128, 1], F32, ph)
            dma("sp", gq[:], qn_g.ap(), [qn_g], [gq])
            dma("sp", gkv[:], kvn_g.ap(), [kvn_g], [gkv])
            cosT = fw.sb("cosT1", [128, S], F32, ph)
            sinT = fw.sb("sinT1", [128, S], F32, ph)
            dma("sp", cosT[:], cos_d.ap(), [cos_d], [cosT])
            dma("sp", sinT[:], sin_d.ap(), [sin_d], [sinT])
            sq = Rot([fw.sb(f"a1_sq{i}", [128, 512], F32, ph) for i in range(2)])
            tmpf = Rot([fw.sb(f"a1_t{i}", [128, 512], F32, ph) for i in range(4)])
            cqn = Rot([fw.sb(f"a1_cqn{i}", [128, 2, 512], BF16, ph) for i in range(2)])
            ckvn = Rot([fw.sb(f"a1_ckvn{i}", [128, 512], BF16, ph) for i in range(2)])
            sbf = Rot([fw.sb(f"a1_sb{i}", [128, 512], BF16, ph) for i in range(4)])
            psr = Rot(psb[0:8])
            SCALE_M = 96.0 ** -0.5
            for tb in range(8):
                tsl = slice(tb * 512, (tb + 1) * 512)
                pss_ = psr.next()
                for j in range(2):
                    s_ = sq.next()
                    op("act", lambda e: e.activation(out=s_[:, :], in_=cq[:, j, tsl], func=AF.Square), [cq], [s_])
                    op("pe", lambda e: e.matmul(pss_[:, :], lhsT=ones_f[:, :], rhs=s_[:, :], start=(j == 0), stop=(j == 1)), [ones_f, s_], [pss_])
                rs, tt = tmpf.next(), tmpf.next()
                rstd_from_ssq(pss_, 256.0, rs, tt)
                cn = cqn.next()
                for j in range(2):
                    op("dve", lambda e: e.scalar_tensor_tensor(out=cn[:, j, :], in0=cq[:, j, tsl], scalar=gq[:, j:j + 1], in1=rs[:, :], op0=ALU.mult, op1=ALU.mult), [cq, gq, rs], [cn])
                for h in range(4):
                    ps = psr.next()
                    for j in range(2):
                        op("pe", lambda e: e.matmul(ps[0:64, :], lhsT=Wuq[:, j, h * 64:(h + 1) * 64], rhs=cn[:, j, :], start=(j == 0), stop=(j == 1)), [Wuq, cn], [ps])
                    s_ = sbf.next()
                    op("act", lambda e: e.activation(out=s_[0:64, :], in_=ps[0:64, :], func=AF.Copy, scale=SCALE_M), [ps], [s_])
                    dma("pool", qmT[h, 0:64, tsl], s_[0:64, :], [s_], [qmT])
                ps1, ps2 = psr.next(), psr.next()
                for j in range(2):
                    op("pe", lambda e: e.matmul(ps1[:, :], lhsT=Wuq[:, j, 256:384], rhs=cn[:, j, :], start=(j == 0), stop=(j == 1)), [Wuq, cn], [ps1])
                for j in range(2):
                    op("pe", lambda e: e.matmul(ps2[:, :], lhsT=Wuq[:, j, 384:512], rhs=cn[:, j, :], start=(j == 0), stop=(j == 1)), [Wuq, cn], [ps2])
                t1, t2 = tmpf.next(), tmpf.next()
                op("dve", lambda e: e.tensor_tensor(out=t1[:, :], in0=ps1[:, :], in1=cosT[:, tsl], op=ALU.mult), [ps1, cosT], [t1])
                op("dve", lambda e: e.tensor_tensor(out=t2[:, :], in0=ps2[:, :], in1=sinT[:, tsl], op=ALU.mult), [ps2, sinT], [t2])
                op("dve", lambda e: e.tensor_tensor(out=t1[:, :], in0=t1[:, :], in1=t2[:, :], op=ALU.add), [t1, t2], [t1])
                s_ = sbf.next()
                op("act", lambda e: e.activation(out=s_[:, :], in_=t1[:, :], func=AF.Copy, scale=SCALE_M), [t1], [s_])
                for h in range(4):
                    dma("pool", qmT[h, 64:96, tsl], s_[h * 32:(h + 1) * 32, :], [s_], [qmT])
                pss_ = psr.next()
                s_ = sq.next()
                op("act", lambda e: e.activation(out=s_[:, :], in_=ckv[:, tsl], func=AF.Square), [ckv], [s_])
                op("pe", lambda e: e.matmul(pss_[:, :], lhsT=ones_f[:, :], rhs=s_[:, :], start=True, stop=True), [ones_f, s_], [pss_])
                rs, tt = tmpf.next(), tmpf.next()
                rstd_from_ssq(pss_, 128.0, rs, tt)
                kn = ckvn.next()
                op("dve", lambda e: e.scalar_tensor_tensor(out=kn[:, :], in0=ckv[:, tsl], scalar=gkv[:, 0:1], in1=rs[:, :], op0=ALU.mult, op1=ALU.mult), [ckv, gkv, rs], [kn])
                for h in range(4):
                    ps = psr.next()
                    op("pe", lambda e: e.matmul(ps[0:64, :], lhsT=Wukv[:, 0, h * 64:(h + 1) * 64], rhs=kn[:, :], start=True, stop=True), [Wukv, kn], [ps])
                    s_ = sbf.next()
                    op("dve", lambda e: e.tensor_copy(out=s_[0:64, :], in_=ps[0:64, :]), [ps], [s_])
                    dma("pool", kmT[h, 0:64, tsl], s_[0:64, :], [s_], [kmT])
                for i4 in range(4):
                    i = tb * 4 + i4
                    ps = psr.next()
                    op("pe", lambda e: e.matmul(ps[:, :], lhsT=kn[:, i4 * 128:(i4 + 1) * 128], rhs=Wukv[:, 0, 256:768], start=True, stop=True), [kn, Wukv], [ps])
                    s_ = sbf.next()
                    op("act", lambda e: e.copy(out=s_[:, :], in_=ps[:, :]), [ps], [s_])
                    dma("pool", vM[i * 128:(i + 1) * 128, :], s_[:, :], [s_], [vM])
            fw.barrier()
        if upto == "A1":
            return nc, ext, out_d, dump_bufs

        LAM_INIT0 = 0.8 - 0.6 * math.exp(-0.3 * 0)
        with ExitStack() as ph:
            negd = fw.sb("negd", [128, NEGD_W], F32, ph)
            dma("sp", negd[:], negd_d.ap(), [negd_d], [negd])
            lamt = fw.sb("lamt", [128, 4, 64], F32, ph)
            dma("sp", lamt[:], lam4.ap().rearrange("(o a) b -> o a b", o=1).broadcast_to([128, 4, 64]), [lam4], [lamt])
            lw = fw.sb("lw", [128, 8], F32, ph)
            lprod = fw.sb("lprod", [128, 2, 64], F32, ph)
            op("dve", lambda e: e.tensor_tensor(out=lprod[:, 0, :], in0=lamt[:, 0, :], in1=lamt[:, 1, :], op=ALU.mult), [lamt], [lprod])
            op("dve", lambda e: e.tensor_tensor(out=lprod[:, 1, :], in0=lamt[:, 2, :], in1=lamt[:, 3, :], op=ALU.mult), [lamt], [lprod])
            op("dve", lambda e: e.reduce_sum(out=lw[:, 0:2], in_=lprod[:, :, :], axis=AX.X), [lprod], [lw])
            op("act", lambda e: e.activation(out=lw[:, 2:4], in_=lw[:, 0:2], func=AF.Exp), [lw], [lw])
            op("dve", lambda e: e.tensor_tensor(out=lw[:, 4:5], in0=lw[:, 3:4], in1=lw[:, 2:3], op=ALU.subtract), [lw], [lw])
            op("dve", lambda e: e.tensor_scalar_add(out=lw[:, 5:6], in0=lw[:, 4:5], scalar1=-LAM_INIT0), [lw], [lw])
            neg_lam = lw[:, 5:6]
            dg = fw.sb("dg", [128, 1], F32, ph)
            dma("sp", dg[:], diff_g.ap(), [diff_g], [dg])
            dg2 = fw.sb("dg2", [128, 1], F32, ph)
            op("dve", lambda e: e.tensor_scalar_mul(out=dg2[:, :], in0=dg[:, :], scalar1=(1.0 - LAM_INIT0)), [dg], [dg2])

            QTs = Rot([fw.sb(f"b_q{i}", [128, S], BF16, ph) for i in range(2)])
            QZ = [Rot([fw.sb(f"b_qz{m}{i}", [128, S], BF16, ph) for i in range(2)]) for m in range(2)]
            for m in range(2):
                for qz in QZ[m].bufs:
                    op("dve", lambda e: e.memset(qz[:, :], 0.0), [], [qz])
            KTs = Rot([fw.sb(f"b_k{i}", [128, S], BF16, ph) for i in range(2)])
            Vs = Rot([fw.sb(f"b_v{i}", [128, NT, 128], BF16, ph) for i in range(2)])
            tmps = Rot([fw.sb(f"b_t{i}", [128, 512], BF16, ph) for i in range(5)])
            es = Rot([fw.sb(f"b_e{i}", [128, 512], BF16, ph) for i in range(6)])
            ETs = Rot([fw.sb(f"b_et{i}", [128, NEGD_W], BF16, ph) for i in range(2)])
            pscore = Rot(psb[0:4])
            pacc = Rot([(psb[4], psb[5]), (psb[6], psb[7])])
            of = Rot([fw.sb(f"b_of{i}", [128, 512], F32, ph) for i in range(4)])
            rsf = Rot([fw.sb(f"b_rs{i}", [128, 512], F32, ph) for i in range(6)])
            ob = Rot([fw.sb(f"b_ob{i}", [128, 512], BF16, ph) for i in range(2)])
            slopes = alibi_slopes(4)
            LA = 3
            jobs = []

            def mk_loader(kind, h, QT, KT, V):
                def ld():
                    if kind == "diff":
                        ET = cur_et[h]
                        for c4 in range(4):
                            csl = slice(c4 * 2016, (c4 + 1) * 2016)
                            op("act", lambda e: e.activation(out=ET[:, csl], in_=negd[:, csl], func=AF.Exp, scale=float(slopes[h])), [negd], [ET])
                        for m in range(2):
                            dma("sp", QT[m][m * 64:(m + 1) * 64, :], qkA[h, m * 64:(m + 1) * 64, :], [qkA], [QT[m]])
                        dma("sp", KT[:, :], qkA[4 + h, :, :], [qkA], [KT])
                        dma("sp", V[:, :, :], vA[:, h * 128:(h + 1) * 128].rearrange("(t p) e -> p t e", p=128), [vA], [V])
                    else:
                        dma("sp", QT[0:96, :], qmT[h, :, :], [qmT], [QT])
                        dma("sp", KT[0:96, :], kmT[h, :, :], [kmT], [KT])
                        dma("sp", V[:, :, :], vM[:, h * 128:(h + 1) * 128].rearrange("(t p) e -> p t e", p=128), [vM], [V])
                return ld

            def fin_diff(h, q0, st_):
                def fin_map(po, pss_):
                    rs, r2 = rsf.next(), rsf.next()
                    op("act", lambda e: e.activation(out=r2[:, :], in_=pss_[:, :], func=AF.Ln), [pss_], [r2])
                    op("act", lambda e: e.activation(out=rs[:, :], in_=r2[:, :], func=AF.Exp, scale=-1.0), [r2], [rs])
                    op("dve", lambda e: e.tensor_tensor(out=r2[:, :], in0=rs[:, :], in1=pss_[:, :], op=ALU.mult), [rs, pss_], [r2])
                    op("dve", lambda e: e.tensor_scalar(out=r2[:, :], in0=r2[:, :], scalar1=-1.0, scalar2=2.0, op0=ALU.mult, op1=ALU.add), [r2], [r2])
                    op("dve", lambda e: e.tensor_tensor(out=rs[:, :], in0=rs[:, :], in1=r2[:, :], op=ALU.mult), [rs, r2], [rs])
                    o_ = of.next()
                    op("dve", lambda e: e.tensor_tensor(out=o_[:, :], in0=po[:, :], in1=rs[:, :], op=ALU.mult), [po, rs], [o_])
                    st_.append(o_)
                    if len(st_) == 2:
                        om = st_
                        oa = of.next()
                        op("dve", lambda e: e.scalar_tensor_tensor(out=oa[:, :], in0=om[1][:, :], scalar=neg_lam, in1=om[0][:, :], op0=ALU.mult, op1=ALU.add), [om[0], om[1], lw], [oa])
                        sq_ = of.next()
                        op("act", lambda e: e.activation(out=sq_[:, :], in_=oa[:, :], func=AF.Square), [oa], [sq_])
                        pssq = pscore.next()
                        op("pe", lambda e: e.matmul(pssq[:, :], lhsT=ones_f[:, :], rhs=sq_[:, :], start=True, stop=True), [ones_f, sq_], [pssq])
                        rs2, tt = rsf.next(), rsf.next()
                        op("act", lambda e: e.activation(out=tt[:, :], in_=pssq[:, :], func=AF.Ln, bias=eps_t[:, 0:1], scale=1.0 / 128.0), [pssq, eps_t], [tt])
                        op("act", lambda e: e.activation(out=rs2[:, :], in_=tt[:, :], func=AF.Exp, scale=-0.5), [tt], [rs2])
                        o_b = ob.next()
                        op("dve", lambda e: e.scalar_tensor_tensor(out=o_b[:, :], in0=oa[:, :], scalar=dg2[:, 0:1], in1=rs2[:, :], op0=ALU.mult, op1=ALU.mult), [oa, dg2, rs2], [o_b])
                        dma("pool", oT_d[h, :, q0:q0 + 512], o_b[:, :], [o_b], [oT_d])
                return fin_map

            def fin_mla(h, q0):
                def fin_map(po, pss_):
                    rs = rsf.next()
                    op("dve", lambda e: e.reciprocal(out=rs[:, :], in_=pss_[:, :]), [pss_], [rs])
                    o_b = ob.next()
                    op("dve", lambda e: e.tensor_tensor(out=o_b[:, :], in0=po[:, :], in1=rs[:, :], op=ALU.mult), [po, rs], [o_b])
                    dma("pool", oT_d[4 + h, :, q0:q0 + 512], o_b[:, :], [o_b], [oT_d])
                return fin_map

            cur_et = {}
            SKIP_THR = {0: 512, 1: 2048}
            for h in range(4):
                QT, KT, V = (QZ[0].next(), QZ[1].next()), KTs.next(), Vs.next()
                cur_et[h] = ETs.next()
                first_of_head = True
                for qb in range(8):
                    st_ = []
                    fin = fin_diff(h, qb * 512, st_)
                    kbs = []
                    for kb in range(NT):
                        md = max(0, kb * 128 - (qb * 512 + 511), qb * 512 - (kb * 128 + 127))
                        if h in SKIP_THR and md >= SKIP_THR[h]:
                            continue
                        kbs.append(kb)
                    for m in range(2):
                        for kb in kbs:
                            jobs.append(dict(QT=QT[m], KT=KT, V=V, r0=0, kr=128, q0=qb * 512, kb=kb, slope=slopes[h], ET=cur_et[h], first=(kb == kbs[0]), last=(kb == kbs[-1]), fin=fin,
                                             pre=(mk_loader("diff", h, QT, KT, V) if first_of_head else None)))
                            first_of_head = False
            for h in range(4):
                QT, KT, V = QTs.next(), KTs.next(), Vs.next()
                first_of_head = True
                for qb in range(8):
                    fin = fin_mla(h, qb * 512)
                    for kb in range(NT):
                        jobs.append(dict(QT=QT, KT=KT, V=V, r0=0, kr=96, q0=qb * 512, kb=kb, slope=None, first=(kb == 0), last=(kb == NT - 1), fin=fin,
                                         pre=(mk_loader("mla", h, QT, KT, V) if first_of_head else None)))
                        first_of_head = False

            def stage1(j):
                if j["pre"] is not None:
                    j["pre"]()
                QT, KT, r0, kr, q0, kb = j["QT"], j["KT"], j["r0"], j["kr"], j["q0"], j["kb"]
                ps = pscore.next()
                op("pe", lambda e: e.matmul(ps[:, :], lhsT=KT[r0:r0 + kr, kb * 128:(kb + 1) * 128], rhs=QT[r0:r0 + kr, q0:q0 + 512], start=True, stop=True), [KT, QT], [ps])
                E = es.next()
                if j["slope"] is not None:
                    tmp = tmps.next()
                    n0 = q0 - kb * 128 + NEGD_C
                    ET = j["ET"]
                    op("act", lambda e: e.activation(out=tmp[:, :], in_=ps[:, :], func=AF.Exp), [ps], [tmp])
                    op("dve", lambda e: e.tensor_tensor(out=E[:, :], in0=tmp[:, :], in1=ET[:, n0:n0 + 512], op=ALU.mult), [tmp, ET], [E])
                else:
                    op("act", lambda e: e.activation(out=E[:, :], in_=ps[:, :], func=AF.Exp), [ps], [E])
                j["E"] = E

            cur = [None]

            def stage2(j):
                if j["first"]:
                    cur[0] = pacc.next()
                po, pss_ = cur[0]
                V, kb, E = j["V"], j["kb"], j["E"]
                op("pe", lambda e: e.matmul(po[:, :], lhsT=V[:, kb, :], rhs=E[:, :], start=j["first"], stop=j["last"]), [V, E], [po])
                op("pe", lambda e: e.matmul(pss_[:, :], lhsT=ones_bf[:, :], rhs=E[:, :], start=j["first"], stop=j["last"]), [ones_bf, E], [pss_])
                if j["last"]:
                    j["fin"](po, pss_)

            for idx in range(len(jobs) + LA):
                if idx % 50 == 10:
                    issue_cast()
                if idx < len(jobs):
                    stage1(jobs[idx])
                if idx >= LA:
                    stage2(jobs[idx - LA])
            issue_cast(len(cast_list))
            fw.barrier()
        if upto == "B":
            return nc, ext, out_d, dump_bufs

        LOG = fw.sb("LOG", [128, NT, 36], F32)

        def ln_stage1(y, stt, junk):
            op("act", lambda e: e.activation(out=junk[:, :], in_=y[:, :], func=AF.Identity, accum_out=stt[:, 0:1]), [y], [junk, stt])
            op("act", lambda e: e.activation(out=junk[:, :], in_=y[:, :], func=AF.Square, accum_out=stt[:, 1:2]), [y], [junk, stt])

        def ln_stage2(y, g_t, b_t, stt, out_t, junk):
            ln_front(y, stt, junk)
            ln_back(g_t, b_t, out_t, junk)

        def ln_back(g_t, b_t, out_t, junk):
            op("dve", lambda e: e.tensor_tensor(out=junk[:, :], in0=junk[:, :], in1=g_t[:, :], op=ALU.mult), [junk, g_t], [junk])
            op("pool", lambda e: e.tensor_tensor(out=out_t[:, :], in0=junk[:, :], in1=b_t[:, :], op=ALU.add), [junk, b_t], [out_t])

        def ln_front(y, stt, junk):
            op("dve", lambda e: e.tensor_scalar_mul(out=stt[:, 2:3], in0=stt[:, 0:1], scalar1=1.0 / D), [stt], [stt])
            op("dve", lambda e: e.tensor_tensor(out=stt[:, 3:4], in0=stt[:, 2:3], in1=stt[:, 2:3], op=ALU.mult), [stt], [stt])
            op("dve", lambda e: e.scalar_tensor_tensor(out=stt[:, 4:5], in0=stt[:, 1:2], scalar=1.0 / D, in1=stt[:, 3:4], op0=ALU.mult, op1=ALU.subtract), [stt], [stt])
            op("act", lambda e: e.activation(out=stt[:, 5:6], in_=stt[:, 4:5], func=AF.Sqrt, bias=eps_t[:, 0:1], scale=1.0), [stt, eps_t], [stt])
            op("dve", lambda e: e.reciprocal(out=stt[:, 6:7], in_=stt[:, 5:6]), [stt], [stt])
            op("dve", lambda e: e.scalar_tensor_tensor(out=stt[:, 7:8], in0=stt[:, 2:3], scalar=-1.0, in1=stt[:, 6:7], op0=ALU.mult, op1=ALU.mult), [stt], [stt])
            op("act", lambda e: e.activation(out=junk[:, :], in_=y[:, :], func=AF.Identity, bias=stt[:, 7:8], scale=stt[:, 6:7]), [y, stt], [junk])

        def phase_C(layer, w_out_dram, xsrc):
            with ExitStack() as ph:
                oT = fw.sb("c_oT", [128, 8, S], BF16, ph, multi=True)
                for c in range(8):
                    dma("sp", oT[:, c, :], oT_d[c, :, :], [oT_d], [oT])
                Wo = fw.sb("c_Wo", [128, 8, D], BF16, ph, multi=True)
                for c in range(8):
                    dma("pool", Wo[:, c, :], w_out_dram[c * 128:(c + 1) * 128, :], [w_out_dram], [Wo])
                Wr = fw.sb("c_Wr", [128, 8, 36], F32, ph)
                dma("sp", Wr[:], w_router[layer].rearrange("(c p) n -> p c n", p=128), [w_router], [Wr])
                g_t = fw.sb("c_g", [128, D], F32, ph)
                b_t = fw.sb("c_b", [128, D], F32, ph)
                dma("sp", g_t[:], ln_mix_g[layer:layer + 1, :].broadcast_to([128, D]), [ln_mix_g], [g_t])
                dma("sp", b_t[:], ln_mix_b[layer:layer + 1, :].broadcast_to([128, D]), [ln_mix_b], [b_t])
                brt = fw.sb("c_brt", [128, 36], F32, ph)
                dma("sp", brt[:], b_router[layer:layer + 1, :].broadcast_to([128, 36]), [b_router], [brt])
                zr = fw.sb("c_zr", [1, D], BF16, ph)
                op("dve", lambda e: e.memset(zr[:], 0.0), [], [zr])
                dma("sp", x1b_d[S:S + 1, :], zr[:], [zr], [x1b_d])
                xts = Rot([fw.sb(f"c_x{i}", [128, D], F32, ph) for i in range(3)])
                ys = Rot([fw.sb(f"c_y{i}", [128, D], F32, ph) for i in range(4)])
                junks = Rot([fw.sb(f"c_j{i}", [128, D], F32, ph) for i in range(4)])
                x1s = Rot([fw.sb(f"c_x1{i}", [128, D], F32, ph) for i in range(4)])
                x1bs = Rot([fw.sb(f"c_x1b{i}", [128, D], BF16, ph) for i in range(2)])
                x1Ts = Rot([fw.sb(f"c_x1T{i}", [128, 8, 128], F32, ph) for i in range(2)])
                stts = Rot([fw.sb(f"c_st{i}", [128, 8], F32, ph) for i in range(4)])
                pmm = Rot(psb[0:4])
                ptr = Rot(psb[4:6])
                prt = Rot(psb[6:8])
                def c_A(i):
                    isl = slice(i * 128, (i + 1) * 128)
                    xt = xts.next()
                    dma("sp", xt[:], xsrc[isl, :], [xsrc], [xt])
                    y = ys.next()
                    for n in range(2):
                        ps = pmm.next()
                        for c in range(8):
                            op("pe", lambda e: e.matmul(ps[:, :], lhsT=oT[:, c, isl], rhs=Wo[:, c, n * 512:(n + 1) * 512], start=(c == 0), stop=(c == 7)), [oT, Wo], [ps])
                        op("dve", lambda e: e.scalar_tensor_tensor(out=y[:, n * 512:(n + 1) * 512], in0=xt[:, n * 512:(n + 1) * 512], scalar=float(ALPHA), in1=ps[:, :], op0=ALU.mult, op1=ALU.add), [xt, ps], [y])
                    junk, stt = junks.next(), stts.next()
                    ln_stage1(y, stt, junk)
                    return (y, junk, stt)

                def c_B1f(i, st3):
                    y, junk, stt = st3
                    ln_front(y, stt, junk)

                def c_B1b(i, st3):
                    isl = slice(i * 128, (i + 1) * 128)
                    y, junk, stt = st3
                    x1t = x1s.next()
                    ln_back(g_t, b_t, x1t, junk)
                    dma("pool", x1_d[isl, :], x1t[:, :], [x1t], [x1_d])
                    x1b = x1bs.next()
                    op("act", lambda e: e.copy(out=x1b[:, :], in_=x1t[:, :]), [x1t], [x1b])
                    dma("pool", x1b_d[isl, :], x1b[:, :], [x1b], [x1b_d])
                    return x1t

                def c_B2(i, x1t):
                    x1T = x1Ts.next()
                    for hf in range(2):
                        ps = ptr.next()
                        for c4 in range(4):
                            c = hf * 4 + c4
                            op("pe", lambda e: e.transpose(out=ps[:, c4 * 128:(c4 + 1) * 128], in_=x1t[:, c * 128:(c + 1) * 128], identity=ident[:]), [x1t, ident], [ps])
                        o_ap = x1T[:, hf * 4:(hf + 1) * 4, :]
                        i_ap = ps[:, :].rearrange("p (c n) -> p c n", c=4)
                        if hf == 0:
                            op("act", lambda e: e.copy(out=o_ap, in_=i_ap), [ps], [x1T])
                        else:
                            op("dve", lambda e: e.tensor_copy(out=o_ap, in_=i_ap), [ps], [x1T])
                    ps = prt.next()
                    for c in range(8):
                        op("pe", lambda e: e.matmul(ps[:, 0:36], lhsT=x1T[:, c, :], rhs=Wr[:, c, :], start=(c == 0), stop=(c == 7)), [x1T, Wr], [ps])
                    op("dve", lambda e: e.tensor_tensor(out=LOG[:, i, :], in0=ps[:, 0:36], in1=brt[:, :], op=ALU.add), [ps, brt], [LOG])

                stA = {}
                x1t_of = {}
                for i in range(-2, NT + 1):
                    if 0 <= i + 2 < NT:
                        stA[i + 2] = c_A(i + 2)
                    if 0 <= i + 1 < NT:
                        c_B1f(i + 1, stA[i + 1])
                    if 0 <= i - 1 < NT:
                        c_B2(i - 1, x1t_of.pop(i - 1))
                    if 0 <= i + 1 < NT:
                        x1t_of[i + 1] = c_B1b(i + 1, stA.pop(i + 1))
                fw.barrier()

        def phase_M(layer, dst):
            with ExitStack() as ph:
                bs = ExitStack()
                tstack = [ph]

                def T(name, shape, dt=F32):
                    return fw.sb("m_" + name, shape, dt, tstack[0])
                mc = T("mc", [128, 4, 64])
                dma("sp", mc[:], moe_c.ap(), [moe_c], [mc])
                tokid = T("tokid", [128, NT])
                dma("sp", tokid[:], tokid_d.ap(), [tokid_d], [tokid])
                triu = T("triu", [128, 128])
                dma("sp", triu[:], triu_d.ap(), [triu_d], [triu])
                triu_b = T("triu_b", [128, 128], BF16)
                op("dve", lambda e: e.tensor_copy(out=triu_b[:, :], in_=triu[:, :]), [triu], [triu_b])
                coarse = LOG[:, :, 0:4]
                fine4 = LOG[:, :, 4:36].rearrange("p j (g i) -> p j g i", g=4)
                gmax = T("gmax", [128, NT])
                op("dve", lambda e: e.tensor_reduce(out=gmax[:, :], in_=coarse, axis=AX.X, op=ALU.max), [LOG], [gmax])
                ohg = T("ohg", [128, NT, 4])
                op("dve", lambda e: e.tensor_tensor(out=ohg[:, :, :], in0=coarse, in1=gmax[:, :].unsqueeze(2).to_broadcast([128, NT, 4]), op=ALU.is_equal), [LOG, gmax], [ohg])
                ex = T("ex", [128, NT, 4])
                op("dve", lambda e: e.tensor_tensor(out=ex[:, :, :], in0=coarse, in1=gmax[:, :].unsqueeze(2).to_broadcast([128, NT, 4]), op=ALU.subtract), [LOG, gmax], [ex])
                op("act", lambda e: e.activation(out=ex[:, :, :], in_=ex[:, :, :], func=AF.Exp), [ex], [ex])
                pg = T("pg", [128, NT])
                op("dve", lambda e: e.reduce_sum(out=pg[:, :], in_=ex[:, :, :], axis=AX.X), [ex], [pg])
                op("dve", lambda e: e.reciprocal(out=pg[:, :], in_=pg[:, :]), [pg], [pg])
                t48 = T("t48", [128, NT, 4, 8])
                op("dve", lambda e: e.tensor_tensor(out=t48[:, :, :, :], in0=fine4, in1=ohg[:, :, :].unsqueeze(3).to_broadcast([128, NT, 4, 8]), op=ALU.mult), [LOG, ohg], [t48])
                fsel = T("fsel", [128, NT, 8])
                op("dve", lambda e: e.reduce_sum(out=fsel[:, :, :], in_=t48[:, :, :, :].rearrange("p j g i -> p j i g"), axis=AX.X), [t48], [fsel])
                v1 = T("v1", [128, NT])
                op("dve", lambda e: e.tensor_reduce(out=v1[:, :], in_=fsel[:, :, :], axis=AX.X, op=ALU.max), [fsel], [v1])
                oh1 = T("oh1", [128, NT, 8])
                op("dve", lambda e: e.tensor_tensor(out=oh1[:, :, :], in0=fsel[:, :, :], in1=v1[:, :].unsqueeze(2).to_broadcast([128, NT, 8]), op=ALU.is_equal), [fsel, v1], [oh1])
                msk = T("msk", [128, NT, 8])
                op("dve", lambda e: e.scalar_tensor_tensor(out=msk[:, :, :], in0=oh1[:, :, :], scalar=-1.0e30, in1=fsel[:, :, :], op0=ALU.mult, op1=ALU.add), [oh1, fsel], [msk])
                v2 = T("v2", [128, NT])
                op("dve", lambda e: e.tensor_reduce(out=v2[:, :], in_=msk[:, :, :], axis=AX.X, op=ALU.max), [msk], [v2])
                oh2 = T("oh2", [128, NT, 8])
                op("dve", lambda e: e.tensor_tensor(out=oh2[:, :, :], in0=msk[:, :, :], in1=v2[:, :].unsqueeze(2).to_broadcast([128, NT, 8]), op=ALU.is_equal), [msk, v2], [oh2])
                ed = T("ed", [128, NT])
                op("dve", lambda e: e.tensor_tensor(out=ed[:, :], in0=v2[:, :], in1=v1[:, :], op=ALU.subtract), [v1, v2], [ed])
                op("act", lambda e: e.activation(out=ed[:, :], in_=ed[:, :], func=AF.Exp), [ed], [ed])
                w1 = T("w1", [128, NT])
                op("dve", lambda e: e.tensor_scalar_add(out=w1[:, :], in0=ed[:, :], scalar1=1.0), [ed], [w1])
                op("dve", lambda e: e.reciprocal(out=w1[:, :], in_=w1[:, :]), [w1], [w1])
                gates = T("gates", [128, 2, NT])
                op("dve", lambda e: e.tensor_tensor(out=gates[:, 0, :], in0=w1[:, :], in1=pg[:, :], op=ALU.mult), [w1, pg], [gates])
                op("dve", lambda e: e.tensor_tensor(out=w1[:, :], in0=w1[:, :], in1=ed[:, :], op=ALU.mult), [w1, ed], [w1])
                op("dve", lambda e: e.tensor_tensor(out=gates[:, 1, :], in0=w1[:, :], in1=pg[:, :], op=ALU.mult), [w1, pg], [gates])
                Ak = [T(f"A{k}", [128, NT, 4, 8]) for k in range(2)]
                for k, ohk in enumerate((oh1, oh2)):
                    op("dve", lambda e: e.tensor_tensor(out=Ak[k][:, :, :, :], in0=ohg[:, :, :].unsqueeze(3).to_broadcast([128, NT, 4, 8]), in1=ohk[:, :, :].unsqueeze(2).to_broadcast([128, NT, 4, 8]), op=ALU.mult), [ohg, ohk], [Ak[k]])
                A_bf = T("A_bf", [128, NT * 32], BF16)
                op("dve", lambda e: e.tensor_tensor(out=A_bf[:, :], in0=Ak[0][:, :, :, :].rearrange("p j g i -> p (j g i)"), in1=Ak[1][:, :, :, :].rearrange("p j g i -> p (j g i)"), op=ALU.add), [Ak[0], Ak[1]], [A_bf])
                sa = T("sa", [128, NT, 32])
                sb_ = T("sb", [128, NT, 32])
                tots = T("tots", [128, NT, 32])
                rank = T("rank", [128, NT, 32])
                for hf in range(2):
                    ps = psb[hf]
                    op("pe", lambda e: e.matmul(ps[:, :], lhsT=ones_bf[:, :], rhs=A_bf[:, hf * 512:(hf + 1) * 512], start=True, stop=True), [ones_bf, A_bf], [ps])
                    op("dve", lambda e: e.tensor_copy(out=tots[:, hf * 16:(hf + 1) * 16, :], in_=ps[:, :].rearrange("p (j e) -> p j e", e=32)), [ps], [tots])
                    ps2 = psb[2 + hf]
                    op("pe", lambda e: e.matmul(ps2[:, :], lhsT=triu_b[:, :], rhs=A_bf[:, hf * 512:(hf + 1) * 512], start=True, stop=True), [triu_b, A_bf], [ps2])
                    op("dve", lambda e: e.tensor_copy(out=rank[:, hf * 16:(hf + 1) * 16, :], in_=ps2[:, :].rearrange("p (j e) -> p j e", e=32)), [ps2], [rank])
                op("dve", lambda e: e.tensor_copy(out=sa[:, :, :], in_=tots[:, :, :]), [tots], [sa])
                a_, b_ = sa, sb_
                for s_ in (1, 2, 4, 8, 16):
                    op("dve", lambda e: e.tensor_tensor(out=b_[:, s_:, :], in0=a_[:, s_:, :], in1=a_[:, :NT - s_, :], op=ALU.add), [a_], [b_])
                    op("dve", lambda e: e.tensor_copy(out=b_[:, :s_, :], in_=a_[:, :s_, :]), [a_], [b_])
                    a_, b_ = b_, a_
                inc = a_
                cnt = T("cnt", [128, 32])
                op("dve", lambda e: e.tensor_copy(out=cnt[:, :], in_=inc[:, NT - 1, :]), [inc], [cnt])
                op("dve", lambda e: e.tensor_tensor(out=rank[:, :, :], in0=rank[:, :, :], in1=inc[:, :, :], op=ALU.add), [rank, inc], [rank])
                op("dve", lambda e: e.tensor_tensor(out=rank[:, :, :], in0=rank[:, :, :], in1=tots[:, :, :], op=ALU.subtract), [rank, tots], [rank])
                cmp_ = T("cmp", [128, 64, 32])
                op("dve", lambda e: e.tensor_tensor(out=cmp_[:, 0:32, :], in0=cnt[:, :].unsqueeze(2).to_broadcast([128, 32, 32]), in1=mc[:, 1, 0:32].unsqueeze(1).to_broadcast([128, 32, 32]), op=ALU.is_gt), [cnt, mc], [cmp_])
                nblk = T("nblk", [128, 32])
                op("dve", lambda e: e.reduce_sum(out=nblk[:, :], in_=cmp_[:, 0:32, :], axis=AX.X), [cmp_], [nblk])
                na = T("na", [128, 32])
                nb_ = T("nb", [128, 32])
                op("dve", lambda e: e.tensor_copy(out=na[:, :], in_=nblk[:, :]), [nblk], [na])
                a_, b_ = na, nb_
                for s_ in (1, 2, 4, 8, 16):
                    op("dve", lambda e: e.tensor_tensor(out=b_[:, s_:], in0=a_[:, s_:], in1=a_[:, :32 - s_], op=ALU.add), [a_], [b_])
                    op("dve", lambda e: e.tensor_copy(out=b_[:, :s_], in_=a_[:, :s_]), [a_], [b_])
                    a_, b_ = b_, a_
                pend = T("pend", [128, 32])
                pstart = T("pstart", [128, 32])
                op("dve", lambda e: e.tensor_scalar_mul(out=pend[:, :], in0=a_[:, :], scalar1=float(BLK)), [a_], [pend])
                op("dve", lambda e: e.scalar_tensor_tensor(out=pstart[:, :], in0=nblk[:, :], scalar=-float(BLK), in1=pend[:, :], op0=ALU.mult, op1=ALU.add), [nblk, pend], [pstart])
                op("dve", lambda e: e.tensor_tensor(out=rank[:, :, :], in0=rank[:, :, :], in1=pstart[:, :].unsqueeze(1).to_broadcast([128, NT, 32]), op=ALU.add), [rank, pstart], [rank])
                dest = T("dest", [128, 2, NT])
                for k in range(2):
                    op("dve", lambda e: e.tensor_tensor(out=sa[:, :, :], in0=Ak[k][:, :, :, :].rearrange("p j g i -> p j (g i)"), in1=rank[:, :, :], op=ALU.mult), [Ak[k], rank], [sa])
                    op("dve", lambda e: e.reduce_sum(out=dest[:, k, :], in_=sa[:, :, :], axis=AX.X), [sa], [dest])
                dest_i = T("dest_i", [128, 2, NT], I32)
                op("dve", lambda e: e.tensor_copy(out=dest_i[:, :, :], in_=dest[:, :, :]), [dest], [dest_i])
                op("dve", lambda e: e.tensor_tensor(out=cmp_[:, 0:NB, :], in0=pend[:, :].unsqueeze(1).to_broadcast([128, NB, 32]), in1=mc[:, 2, 0:NB].unsqueeze(2).to_broadcast([128, NB, 32]), op=ALU.is_le), [pend, mc], [cmp_])
                beid = T("beid", [128, 64])
                op("dve", lambda e: e.reduce_sum(out=beid[:, 0:NB], in_=cmp_[:, 0:NB, :], axis=AX.X), [cmp_], [beid])
                op("dve", lambda e: e.tensor_scalar_min(out=beid[:, 0:NB], in0=beid[:, 0:NB], scalar1=31.0), [beid], [beid])
                op("dve", lambda e: e.tensor_scalar(out=beid[:, 0:NB], in0=beid[:, 0:NB], scalar1=128.0, scalar2=float(layer * E_ * 128), op0=ALU.mult, op1=ALU.add), [beid], [beid])
                op("dve", lambda e: e.tensor_tensor(out=beid[:, 0:NB], in0=beid[:, 0:NB], in1=mc[:, 3, 0:1].to_broadcast([128, NB]), op=ALU.add), [beid, mc], [beid])
                widx = T("widx", [128, 64], I32)
                op("dve", lambda e: e.tensor_copy(out=widx[:, 0:NB], in_=beid[:, 0:NB]), [beid], [widx])
                NA = NSLOT // 128 + 1
                padrec = T("padrec", [128, NA, 4])
                op("dve", lambda e: e.memset(padrec[:, :, :], 0.0), [], [padrec])
                op("dve", lambda e: e.memset(padrec[:, :, 0:1], float(S)), [], [padrec])
                op("dve", lambda e: e.tensor_scalar_add(out=padrec[:, :, 2], in0=mc[:, 3, 0:1].to_broadcast([128, NA]), scalar1=float(2 * S)), [mc], [padrec])
                dma("sp", slot_d.ap().rearrange("(a p) c -> p a c", p=128), padrec[:, :, :], [padrec], [slot_d])
                rec = T("rec", [128, 2, NT, 4])
                op("dve", lambda e: e.memset(rec[:, :, :, :], 0.0), [], [rec])
                for k in range(2):
                    op("dve", lambda e: e.tensor_copy(out=rec[:, k, :, 0], in_=tokid[:, :]), [tokid], [rec])
                    op("dve", lambda e: e.tensor_copy(out=rec[:, k, :, 1], in_=gates[:, k, :]), [gates], [rec])
                    op("dve", lambda e: e.tensor_scalar_add(out=rec[:, k, :, 2], in0=tokid[:, :], scalar1=float(k * S)), [tokid], [rec])
                sc_bufs = []
                for k in range(2):
                    for j in range(NT):
                        sc_b = Buf(None, "slotscatter")
                        sc_bufs.append(sc_b)
                        op("pool", lambda e: e.indirect_dma_start(out=slot_d[:, :], out_offset=bass.IndirectOffsetOnAxis(ap=dest_i[:, k, j:j + 1], axis=0), in_=rec[:, k, j, :], in_offset=None), [rec, dest_i, slot_d], [sc_b], dma=True)
                SL = T("SL", [128, NA - 1, 4])
                dma("sp", SL[:, :, :], slot_d[0:NSLOT, :].rearrange("(a p) c -> p a c", p=128), [slot_d] + sc_bufs, [SL])
                tok_i = T("tok_i", [128, NA - 1], I32)
                row_i = T("row_i", [128, NA - 1], I32)
                gate_s = T("gate_s", [128, NA - 1])
                op("dve", lambda e: e.tensor_copy(out=tok_i[:, :], in_=SL[:, :, 0]), [SL], [tok_i])
                op("dve", lambda e: e.tensor_copy(out=row_i[:, :], in_=SL[:, :, 2]), [SL], [row_i])
                op("dve", lambda e: e.tensor_copy(out=gate_s[:, :], in_=SL[:, :, 1]), [SL], [gate_s])
                if "moe_dbg" in dumps:
                    dump("m_dest", dest[:, :, :], [128, 2, NT], F32, [dest])
                    dump("m_gates", gates[:, :, :], [128, 2, NT], F32, [gates])
                    dump("m_beid", beid[:, :], [128, 64], F32, [beid])
                    dump("m_SL", SL[:, :, :], [128, NA - 1, 4], F32, [SL])
                    dump("m_cnt", cnt[:, :], [128, 32], F32, [cnt])
                tstack[0] = bs
                Wgs = Rot([T(f"Wg{i}", [128, 8 * HID], BF16) for i in range(2)])
                Wus = Rot([T(f"Wu{i}", [128, 8 * HID], BF16) for i in range(2)])
                Wds = Rot([T(f"Wd{i}", [128, 4 * D], BF16) for i in range(2)])
                xgs = Rot([T(f"xg{i}", [128, D], BF16) for i in range(4)])
                xgTs = Rot([T(f"xgT{i}", [128, 8, BLK], BF16) for i in range(2)])
                acts = Rot([T(f"act{i}", [128, 4, BLK], BF16) for i in range(2)])
                sgs = Rot([T(f"sg{i}", [128, BLK], F32) for i in range(3)])
                ysbs = Rot([T(f"ysb{i}", [128, D], F32) for i in range(3)])
                identb = T("identb", [128, 128], BF16)
                op("dve", lambda e: e.tensor_copy(out=identb[:, :], in_=ident[:, :]), [ident], [identb])
                ptr = Rot(psb[0:2])
                pgu = Rot(psb[2:6])
                pyy = Rot(psb[6:8])
                def gathers(b):
                    Wg, Wu, Wd = Wgs.next(), Wus.next(), Wds.next()
                    for Wt, src in ((Wg, wgb), (Wu, wub), (Wd, wdb)):
                        op("pool", lambda e: e.indirect_dma_start(out=Wt[:, :], out_offset=None, in_=src[:, :], in_offset=bass.IndirectOffsetOnAxis(ap=widx[:, b:b + 1], axis=0)), [src, widx], [Wt], dma=True)
                    xg2 = []
                    for hf in range(2):
                        a = 2 * b + hf
                        xg = xgs.next()
                        op("pool", lambda e: e.indirect_dma_start(out=xg[:, :], out_offset=None, in_=x1b_d[:, :], in_offset=bass.IndirectOffsetOnAxis(ap=tok_i[:, a:a + 1], axis=0)), [x1b_d, tok_i], [xg], dma=True)
                        xg2.append(xg)
                    return Wg, Wu, Wd, xg2

                nxt = gathers(0)
                for b in range(NB):
                    Wg, Wu, Wd, xg2 = nxt
                    if b + 1 < NB:
                        nxt = gathers(b + 1)
                    xgT = xgTs.next()
                    for hf in range(2):
                        xg = xg2[hf]
                        ps = ptr.next()
                        psv = ps[:, :].bitcast(BF16)
                        for c in range(8):
                            op("pe", lambda e: e.transpose(out=psv[:, c * 128:(c + 1) * 128], in_=xg[:, c * 128:(c + 1) * 128], identity=identb[:]), [xg, identb], [ps])
                        o_ap = xgT[:, :, hf * 128:(hf + 1) * 128]
                        i_ap = psv.rearrange("p (c n) -> p c n", c=8)
                        if hf == 0:
                            op("act", lambda e: e.copy(out=o_ap, in_=i_ap), [ps], [xgT])
                        else:
                            op("dve", lambda e: e.tensor_copy(out=o_ap, in_=i_ap), [ps], [xgT])
                    act_ = acts.next()
                    for m in range(4):
                        pg_, pu_ = pgu.next(), pgu.next()
                        for c in range(8):
                            op("pe", lambda e: e.matmul(pg_[:, 0:BLK], lhsT=Wg[:, c * HID + m * 128:c * HID + (m + 1) * 128], rhs=xgT[:, c, :], start=(c == 0), stop=(c == 7)), [Wg, xgT], [pg_])
                        for c in range(8):
                            op("pe", lambda e: e.matmul(pu_[:, 0:BLK], lhsT=Wu[:, c * HID + m * 128:c * HID + (m + 1) * 128], rhs=xgT[:, c, :], start=(c == 0), stop=(c == 7)), [Wu, xgT], [pu_])
                        sg = sgs.next()
                        op("act", lambda e: e.activation(out=sg[:, :], in_=pg_[:, 0:BLK], func=AF.Silu), [pg_], [sg])
                        op("dve", lambda e: e.tensor_tensor(out=act_[:, m, :], in0=sg[:, :], in1=pu_[:, 0:BLK], op=ALU.mult), [sg, pu_], [act_])
                    for hf in range(2):
                        a = 2 * b + hf
                        ysb = ysbs.next()
                        for n in range(2):
                            py = pyy.next()
                            for m in range(4):
                                op("pe", lambda e: e.matmul(py[:, :], lhsT=act_[:, m, hf * 128:(hf + 1) * 128], rhs=Wd[:, m * D + n * 512:m * D + (n + 1) * 512], start=(m == 0), stop=(m == 3)), [act_, Wd], [py])
                            if n == 0:
                                op("act", lambda e: e.activation(out=ysb[:, 0:512], in_=py[:, :], func=AF.Copy, scale=gate_s[:, a:a + 1]), [py, gate_s], [ysb])
                            else:
                                op("dve", lambda e: e.tensor_scalar_mul(out=ysb[:, 512:1024], in0=py[:, :], scalar1=gate_s[:, a:a + 1]), [py, gate_s], [ysb])
                        op("pool", lambda e: e.indirect_dma_start(out=y_d[:, :], out_offset=bass.IndirectOffsetOnAxis(ap=row_i[:, a:a + 1], axis=0), in_=ysb[:, :], in_offset=None), [ysb, row_i], [y_d], dma=True)
                fw.barrier()
                bs.close()
                tstack[0] = ph
                g_t = T("g", [128, D])
                b_t = T("b", [128, D])
                dma("sp", g_t[:], ln_ffn_g[layer:layer + 1, :].broadcast_to([128, D]), [ln_ffn_g], [g_t])
                dma("sp", b_t[:], ln_ffn_b[layer:layer + 1, :].broadcast_to([128, D]), [ln_ffn_b], [b_t])
                xts = Rot([T(f"cx{i}", [128, D]) for i in range(4)])
                y0s = Rot([T(f"cy0{i}", [128, D]) for i in range(4)])
                y1s = Rot([T(f"cy1{i}", [128, D]) for i in range(4)])
                junks = Rot([T(f"cj{i}", [128, D]) for i in range(4)])
                outs = Rot([T(f"co{i}", [128, D]) for i in range(4)])
                stts = Rot([T(f"cst{i}", [128, 8]) for i in range(4)])
                def m_A(i):
                    isl = slice(i * 128, (i + 1) * 128)
                    xt, y0, y1 = xts.next(), y0s.next(), y1s.next()
                    dma("sp", xt[:], x1_d[isl, :], [x1_d], [xt])
                    dma("sp", y0[:], y_d[isl, :], [y_d], [y0])
                    dma("sp", y1[:], y_d[S + i * 128:S + (i + 1) * 128, :], [y_d], [y1])
                    op("pool", lambda e: e.tensor_tensor(out=y1[:, :], in0=y0[:, :], in1=y1[:, :], op=ALU.add), [y0, y1], [y1])
                    op("dve", lambda e: e.scalar_tensor_tensor(out=y0[:, :], in0=xt[:, :], scalar=float(ALPHA), in1=y1[:, :], op0=ALU.mult, op1=ALU.add), [xt, y1], [y0])
                    junk, stt = junks.next(), stts.next()
                    ln_stage1(y0, stt, junk)
                    return (y0, junk, stt)

                def m_B1f(i, st3):
                    y0, junk, stt = st3
                    ln_front(y0, stt, junk)

                def m_B1b(i, st3):
                    isl = slice(i * 128, (i + 1) * 128)
                    y0, junk, stt = st3
                    o_t = outs.next()
                    ln_back(g_t, b_t, o_t, junk)
                    dma("pool", dst[isl, :], o_t[:, :], [o_t], [dst])

                stA = {}
                for i in range(-2, NT):
                    if 0 <= i + 1 < NT:
                        m_B1f(i + 1, stA[i + 1])
                    if 0 <= i + 2 < NT:
                        stA[i + 2] = m_A(i + 2)
                    if 0 <= i + 1 < NT:
                        m_B1b(i + 1, stA.pop(i + 1))
                fw.barrier()

        phase_C(0, w_out_ab, x_in)
        if upto == "C0":
            return nc, ext, out_d, dump_bufs
        phase_M(0, x2_d)
        if upto == "M0":
            return nc, ext, out_d, dump_bufs
        PATS = (1, 4, 16)
        with ExitStack() as ph:
            xT = fw.sb("l1_xT", [128, 8, S], BF16, ph)
            Wc = fw.sb("l1_W", [128, 8, 3 * D], BF16, ph, multi=True)
            for c in range(8):
                dma("pool", Wc[:, c, :], w_in_c[c * 128:(c + 1) * 128, :], [w_in_c], [Wc])
            stg = Rot([fw.sb(f"l1_x{i}", [128, D], F32, ph) for i in range(2)])
            build_xT(x2_d, xT, Rot(psb[0:2]), stg)
            psr = Rot(psb[2:8])
            sbf = Rot([fw.sb(f"l1_sb{i}", [128, 512], BF16, ph) for i in range(4)])
            vst = Rot([fw.sb(f"l1_v{i}", [128, 16, 65], BF16, ph) for i in range(6)])
            for v_ in vst.bufs:
                op("dve", lambda e: e.memset(v_[:, :, :], 1.0), [], [v_])
            for tb in range(8):
                tsl = slice(tb * 512, (tb + 1) * 512)
                for ch in range(16):
                    ps = psr.next()
                    for c in range(8):
                        op("pe", lambda e: e.matmul(ps[:, :], lhsT=Wc[:, c, ch * 128:(ch + 1) * 128], rhs=xT[:, c, tsl], start=(c == 0), stop=(c == 7)), [Wc, xT], [ps])
                    s_ = sbf.next()
                    if ch < 8:
                        op("act", lambda e: e.activation(out=s_[:, :], in_=ps[:, :], func=AF.Copy, scale=0.125), [ps], [s_])
                    else:
                        op("dve", lambda e: e.tensor_copy(out=s_[:, :], in_=ps[:, :]), [ps], [s_])
                    dma("pool", qkC[ch, :, tsl], s_[:, :], [s_], [qkC])
            for ri, r in enumerate(PATS):
                nqb = S // r // 128
                for res in range(r):
                    for kt in range(nqb):
                        t = res * nqb + kt
                        tok = ssl(res + r * 128 * kt, 128, r)
                        v_ = vst.next()
                        for n in range(2):
                            ps = psr.next()
                            for c in range(8):
                                op("pe", lambda e: e.matmul(ps[:, :], lhsT=xT[:, c, tok], rhs=Wc[:, c, 2048 + n * 512:2048 + (n + 1) * 512], start=(c == 0), stop=(c == 7)), [xT, Wc], [ps])
                            o_ap = v_[:, n * 8:(n + 1) * 8, 0:64]
                            i_ap = ps[:, :].rearrange("p (h e) -> p h e", e=64)
                            if n == 0:
                                op("act", lambda e: e.copy(out=o_ap, in_=i_ap), [ps], [v_])
                            else:
                                op("dve", lambda e: e.tensor_copy(out=o_ap, in_=i_ap), [ps], [v_])
                        for g in range(4):
                            dma("sp" if g % 2 == 0 else "pool", vC[ri, g, :, t, :], v_[:, g * 4:(g + 1) * 4, :].rearrange("p h e -> p (h e)"), [v_], [vC])
            fw.barrier()
        if upto == "A1x":
            return nc, ext, out_d, dump_bufs

        with ExitStack() as ph:
            dng = fw.sb("d_negd", [128, 3, 512], F32, ph)
            dma("sp", dng[:], dil_negd_d.ap(), [dil_negd_d], [dng])
            sel = fw.sb("d_sel", [128, 64], F32, ph)
            dma("sp", sel[:], sel_d.ap(), [sel_d], [sel])
            QZ = [fw.sb(f"d_QZ{par}", [128, 2, S], BF16, ph) for par in range(2)]
            for par in range(2):
                op("pool", lambda e: e.memset(QZ[par][:, :, :], 0.0), [], [QZ[par]])
            KT = fw.sb("d_KT", [128, 2, S], BF16, ph, multi=True)
            Vr = [fw.sb(f"d_V{ri}", [128, NT, 260], BF16, ph) for ri in range(3)]
            Oacc = fw.sb("d_O", [65, 4, S], F32, ph)
            tmps = Rot([fw.sb(f"d_t{i}", [128, 512], F32, ph) for i in range(4)])
            Es = Rot([fw.sb(f"d_e{i}", [128, 512], BF16, ph) for i in range(8)])
            rcp = Rot([fw.sb(f"d_r{i}", [64, 512], F32, ph) for i in range(4)])
            obs = Rot([fw.sb(f"d_ob{i}", [64, 512], BF16, ph) for i in range(2)])
            pscore = Rot(psb[0:6])
            pov = Rot(psb[6:8])
            dslopes = alibi_slopes(16)
            LAG = 1
            gjobs = []
            for g in range(4):
                for hl in range(4):
                    h = 4 * g + hl
                    for ri, r in enumerate(PATS):
                        nqb = S // r // 128
                        G = min(4, nqb)
                        for res in range(r):
                            for qg in range(0, nqb, G):
                                gjobs.append(dict(g=g, hl=hl, h=h, ri=ri, r=r, nqb=nqb, G=G, res=res, qg=qg, first_g=False, last_h=False))
                    gjobs[-1]["last_h"] = True
            seen_g = set()
            for j in gjobs:
                if j["g"] not in seen_g:
                    seen_g.add(j["g"])
                    j["first_g"] = True

            def load_group(g):
                for cl in range(2):
                    for par in range(2):
                        dma("sp", QZ[par][par * 64:(par + 1) * 64, cl, :], qkC[2 * g + cl, par * 64:(par + 1) * 64, :], [qkC], [QZ[par]])
                    dma("sp", KT[:, cl, :], qkC[8 + 2 * g + cl, :, :], [qkC], [KT])
                for ri in range(3):
                    dma("sp", Vr[ri][:, :, :], vC[ri, g, :, :, :], [vC], [Vr[ri]])

            def d_stage1(j):
                if j["first_g"]:
                    load_group(j["g"])
                hl, h, r, nqb, G, res, qg = j["hl"], j["h"], j["r"], j["nqb"], j["G"], j["res"], j["qg"]
                cl, r0 = hl // 2, (hl % 2) * 64
                blocks = list(range(qg, qg + G))
                Et, rng = [], []
                for typ in range(3):
                    ps = pscore.next()
                    val = [il for il, i in enumerate(blocks) if 0 <= i + typ - 1 < nqb]
                    lo, hi = val[0], val[-1] + 1
                    for il in val:
                        i = blocks[il]
                        kt = i + typ - 1
                        ksl = ssl(res + r * 128 * kt, 128, r)
                        qsl = ssl(res + r * 128 * i, 128, r)
                        op("pe", lambda e: e.matmul(ps[:, il * 128:(il + 1) * 128], lhsT=KT[:, cl, ksl], rhs=QZ[hl % 2][:, cl, qsl], start=True, stop=True), [KT, QZ[hl % 2]], [ps])
                    tmp, E = tmps.next(), Es.next()
                    op("dve", lambda e: e.scalar_tensor_tensor(out=tmp[:, lo * 128:hi * 128], in0=dng[:, typ, lo * 128:hi * 128], scalar=float(dslopes[h] * r), in1=ps[:, lo * 128:hi * 128], op0=ALU.mult, op1=ALU.add), [dng, ps], [tmp])
                    op("act", lambda e: e.activation(out=E[:, lo * 128:hi * 128], in_=tmp[:, lo * 128:hi * 128], func=AF.Exp), [tmp], [E])
                    Et.append(E)
                    rng.append(val)
                j["Et"], j["rng"], j["blocks"] = Et, rng, blocks

            def d_stage2(j):
                hl, h, ri, r, nqb, G, res, qg = j["hl"], j["h"], j["ri"], j["r"], j["nqb"], j["G"], j["res"], j["qg"]
                r0 = (hl % 2) * 64
                Et, rng, blocks = j["Et"], j["rng"], j["blocks"]
                po = pov.next()
                for il, i in enumerate(blocks):
                    typs = [typ for typ in range(3) if il in rng[typ]]
                    for n_, typ in enumerate(typs):
                        kt = i + typ - 1
                        t = res * nqb + kt
                        op("pe", lambda e: e.matmul(po[0:65, il * 128:(il + 1) * 128], lhsT=Vr[ri][:, t, hl * 65:(hl + 1) * 65], rhs=Et[typ][:, il * 128:(il + 1) * 128], start=(n_ == 0), stop=(n_ == len(typs) - 1)), [Vr[ri], Et[typ]], [po])
                osl = ssl(res + r * 128 * qg, 128 * G, r)
                if ri == 0:
                    op("act", lambda e: e.copy(out=Oacc[0:65, hl, osl], in_=po[0:65, 0:G * 128]), [po], [Oacc])
                else:
                    op("dve", lambda e: e.tensor_tensor(out=Oacc[0:65, hl, osl], in0=Oacc[0:65, hl, osl], in1=po[0:65, 0:G * 128], op=ALU.add), [Oacc, po], [Oacc])
                if j["last_h"]:
                    for tb in range(8):
                        tsl = slice(tb * 512, (tb + 1) * 512)
                        pn = pov.next()
                        op("pe", lambda e: e.matmul(pn[0:64, :], lhsT=sel[0:65, :], rhs=Oacc[0:65, hl, tsl], start=True, stop=True), [sel, Oacc], [pn])
                        rc, rc2, o_b = rcp.next(), rcp.next(), obs.next()
                        op("act", lambda e: e.activation(out=rc2[:, :], in_=pn[0:64, :], func=AF.Ln), [pn], [rc2])
                        op("act", lambda e: e.activation(out=rc[:, :], in_=rc2[:, :], func=AF.Exp, scale=-1.0), [rc2], [rc])
                        op("dve", lambda e: e.tensor_tensor(out=rc2[:, :], in0=rc[:, :], in1=pn[0:64, :], op=ALU.mult), [rc, pn], [rc2])
                        op("dve", lambda e: e.tensor_scalar(out=rc2[:, :], in0=rc2[:, :], scalar1=-1.0, scalar2=2.0, op0=ALU.mult, op1=ALU.add), [rc2], [rc2])
                        op("dve", lambda e: e.tensor_tensor(out=rc[:, :], in0=rc[:, :], in1=rc2[:, :], op=ALU.mult), [rc, rc2], [rc])
                        op("dve", lambda e: e.tensor_tensor(out=o_b[:, :], in0=Oacc[0:64, hl, tsl], in1=rc[:, :], op=ALU.mult), [Oacc, rc], [o_b])
                        dma("pool", oT_d[h // 2, r0:r0 + 64, tsl], o_b[:, :], [o_b], [oT_d])

            pend = []
            for j in gjobs:
                if j["first_g"]:
                    while pend:
                        d_stage2(pend.pop(0))
                d_stage1(j)
                pend.append(j)
                if len(pend) > LAG:
                    d_stage2(pend.pop(0))
            while pend:
                d_stage2(pend.pop(0))
            fw.barrier()
        if upto == "B1":
            return nc, ext, out_d, dump_bufs
        phase_C(1, w_out_c, x2_d)
        phase_M(1, out_d)
    return nc, ext, out_d, dump_bufs


def host_consts():
    c = {}
    c["ident"] = np.eye(128, dtype=np.float32)
    k = np.arange(128, dtype=np.float32)[:, None]
    n = np.arange(NEGD_W, dtype=np.float32)[None, :]
    c["negd"] = (-np.abs(n - k - NEGD_C)).astype(np.float32)
    half = 16
    inv_freq = np.power(np.float32(10000.0), -np.arange(half, dtype=np.float32) * np.float32(2.0) / np.float32(32)).astype(np.float32)
    ang = (np.arange(S, dtype=np.float32)[:, None] * inv_freq[None, :]).astype(np.float32)
    cos = np.cos(ang).astype(np.float32).T
    sin = np.sin(ang).astype(np.float32).T
    c32 = np.concatenate([cos, cos], 0)
    s32 = np.concatenate([-sin, sin], 0)
    c["ropecos"] = np.ascontiguousarray(np.tile(c32, (4, 1)))
    c["ropesin"] = np.ascontiguousarray(np.tile(s32, (4, 1)))
    kk = np.arange(128, dtype=np.float32)[:, None]
    qq = np.arange(128, dtype=np.float32)[None, :]
    tabs = []
    for typ in range(3):
        d = kk - qq + 128.0 * (typ - 1)
        t = np.where(np.abs(d) <= 64, -np.abs(d), -1.0e9).astype(np.float32)
        tabs.append(np.tile(t, (1, 4)))
    c["dil_negd"] = np.ascontiguousarray(np.stack(tabs, 1))
    mc = np.zeros((128, 4, 64), np.float32)
    mc[:, 0, :32] = np.arange(32, dtype=np.float32)[None, :]
    mc[:, 1, :32] = 256.0 * np.arange(32, dtype=np.float32)[None, :]
    mc[:, 2, :63] = 256.0 * np.arange(63, dtype=np.float32)[None, :]
    c["moe_c"] = mc
    mc[:, 3, :] = np.arange(128, dtype=np.float32)[:, None]
    c["tokid"] = (np.arange(NT, dtype=np.float32)[None, :] * 128 + np.arange(128, dtype=np.float32)[:, None]).astype(np.float32)
    sl = np.zeros((128, 64), np.float32)
    sl[64, :] = 1.0
    c["sel"] = sl
    c["triu"] = np.triu(np.ones((128, 128), np.float32), 1)
    return c


def host_weights(inp):
    w = {}
    wi = inp["w_in_ab"][0]
    w["w_in_ab"] = np.ascontiguousarray(np.concatenate([wi, wi[:, 1936:1952], wi[:, 1920:1936]], 1))
    wq = inp["w_uq"][0]
    nope = [wq[:, h * 96:h * 96 + 64] for h in range(4)]
    rope = [wq[:, h * 96 + 64:h * 96 + 96] for h in range(4)]
    rsw = [np.concatenate([wq[:, h * 96 + 80:h * 96 + 96], wq[:, h * 96 + 64:h * 96 + 80]], 1) for h in range(4)]
    w["w_uq"] = np.ascontiguousarray(np.concatenate(nope + rope + rsw, 1))
    wk = inp["w_ukv"][0]
    kn = [wk[:, h * 192:h * 192 + 64] for h in range(4)]
    vv = [wk[:, h * 192 + 64:h * 192 + 192] for h in range(4)]
    w["w_ukv"] = np.ascontiguousarray(np.concatenate(kn + vv, 1))
    w["w_out_ab"] = np.ascontiguousarray(inp["w_out_ab"][0])
    w["lam4"] = np.ascontiguousarray(np.stack([inp["lam_q1"][0], inp["lam_k1"][0], inp["lam_q2"][0], inp["lam_k2"][0]], 0))
    w["diff_g"] = np.ascontiguousarray(inp["diff_norm_g"][0].reshape(128, 1))
    w["qn_g"] = np.ascontiguousarray(inp["mla_q_norm_g"][0].reshape(2, 128).T)
    w["kvn_g"] = np.ascontiguousarray(inp["mla_kv_norm_g"][0].reshape(128, 1))
    for k in ("ln_mix_g", "ln_mix_b", "ln_ffn_g", "ln_ffn_b"):
        w[k] = np.ascontiguousarray(inp[k])
    w["w_router"] = np.ascontiguousarray(np.concatenate([inp["moe_w_group"], inp["moe_w_route"]], 2))
    w["b_router"] = np.ascontiguousarray(np.concatenate([inp["moe_b_group"], inp["moe_b_route"]], 1))
    g = inp["moe_w_gate"].reshape(2, E_, 8, 128, HID).transpose(0, 1, 3, 2, 4)
    w["wg_l"] = np.ascontiguousarray(g).reshape(2 * E_ * 128, 8 * HID)
    u = inp["moe_w_up"].reshape(2, E_, 8, 128, HID).transpose(0, 1, 3, 2, 4)
    w["wu_l"] = np.ascontiguousarray(u).reshape(2 * E_ * 128, 8 * HID)
    dd = inp["moe_w_down"].reshape(2, E_, 4, 128, D).transpose(0, 1, 3, 2, 4)
    w["wd_l"] = np.ascontiguousarray(dd).reshape(2 * E_ * 128, 4 * D)
    w["w_in_c"] = np.ascontiguousarray(inp["w_in_c"][0])
    w["w_out_c"] = np.ascontiguousarray(inp["w_out_c"][0])
    return w


def kernel(**inputs):
    inp = {k: np.asarray(v) for k, v in inputs.items()}
    nc, ext, out_d, _ = build_program()
    shared = host_consts()
    shared.update(host_weights(inp))
    x = np.ascontiguousarray(inp["x"], dtype=np.float32)
    in_maps = []
    for c in range(8):
        m = dict(shared)
        m["x"] = x[c]
        in_maps.append(m)
    res = run_bass_kernel_spmd(nc, in_maps, core_ids=list(range(8)))
    return np.stack([np.asarray(r["out"]) for r in res.results], 0).astype(np.float32)
```

```python
import math
import numpy as np
from contextlib import ExitStack
import concourse.bass as bass
import concourse.mybir as mybir
from concourse.bass_utils import run_bass_kernel_spmd

F32 = mybir.dt.float32
BF16 = mybir.dt.bfloat16
I32 = mybir.dt.int32
ALU = mybir.AluOpType
AF = mybir.ActivationFunctionType
AX = mybir.AxisListType

S = 4096
D = 1024
NT = S // 128
DEPTH = 2
ALPHA = (2 * DEPTH) ** 0.25
EPS = 1e-5
NPOOL = 16
E_ = 32
HID = 512
BLK = 256
NB = 63
NSLOT = NB * BLK
NEGD_C = 3968
NEGD_W = 8064


class Buf:
    __slots__ = ("t", "w", "r", "name", "multi", "wm")

    def __init__(self, t, name="", multi=False):
        self.t = t
        self.w = None
        self.r = {}
        self.name = name
        self.multi = multi
        self.wm = {}

    def __getitem__(self, k):
        return self.t[k]

    def ap(self):
        return self.t.ap()


class FW:
    def __init__(self, nc, stack):
        self.nc = nc
        self.stack = stack
        self.engs = {"pe": nc.tensor, "act": nc.scalar, "dve": nc.vector, "pool": nc.gpsimd, "sp": nc.sync}
        self.esem, self.cnt, self.seen, self.dq = {}, {}, {}, {}
        for e in self.engs:
            self.esem[e] = stack.enter_context(nc.semaphore("es_" + e))
            self.cnt[e] = 0
            self.seen[e] = {}
        for q in ("sp", "pool", "act"):
            sems = [stack.enter_context(nc.semaphore(f"dq_{q}_{i}")) for i in range(NPOOL)]
            self.dq[q] = {"sems": sems, "n": 0}
        self.n_inst = 0
        self.uid = 0

    def sb(self, name, shape, dtype, stack=None, multi=False):
        st = stack if stack is not None else self.stack
        self.uid += 1
        return Buf(st.enter_context(self.nc.sbuf_tensor(f"s{self.uid}_{name}", list(shape), dtype)), name, multi)

    def ps(self, name, shape, dtype=F32, stack=None):
        st = stack if stack is not None else self.stack
        return Buf(st.enter_context(self.nc.psum_tensor("p_" + name, list(shape), dtype)), name)

    def dram(self, name, shape, dtype, kind="Internal"):
        return Buf(self.nc.dram_tensor(name, list(shape), dtype, kind=kind), name, True)

    def op(self, eng, fn, reads=(), writes=(), dma=False):
        waits = {}
        own = self.esem[eng]
        seen = self.seen[eng]

        def need(sem, val):
            if eng == "pe" and sem is own:
                return
            if seen.get(sem, 0) >= val:
                return
            if waits.get(sem, 0) < val:
                waits[sem] = val

        for b in reads:
            if b.multi:
                for s_, v_ in b.wm.items():
                    need(s_, v_)
            elif b.w is not None:
                need(*b.w)
        for b in writes:
            if not b.multi and b.w is not None:
                need(*b.w)
            for s_, v_ in b.r.items():
                need(s_, v_)
        if dma:
            q = self.dq[eng]
            j = q["n"]
            s = q["sems"][j % NPOOL]
            if j >= NPOOL:
                need(s, 16 * (j // NPOOL))
        e = self.engs[eng]
        for sem, val in waits.items():
            e.wait_ge(sem, val)
            seen[sem] = val
        ins = fn(e)
        self.n_inst += 1
        if dma:
            ins.then_inc(s, 16)
            ev = (s, 16 * (j // NPOOL + 1))
            q["n"] += 1
        else:
            self.cnt[eng] += 1
            ins.then_inc(own, 1)
            ev = (own, self.cnt[eng])
        for b in reads:
            if b.r.get(ev[0], 0) < ev[1]:
                b.r[ev[0]] = ev[1]
        for b in writes:
            if b.multi:
                if b.wm.get(ev[0], 0) < ev[1]:
                    b.wm[ev[0]] = ev[1]
            else:
                b.w = ev
                b.r = {}
        return ev

    def barrier(self):
        evs = []
        for e in self.engs:
            if self.cnt[e] > 0:
                evs.append((self.esem[e], self.cnt[e]))
        for q in self.dq.values():
            n = q["n"]
            for i, s in enumerate(q["sems"]):
                k = (n - i + NPOOL - 1) // NPOOL
                if k > 0:
                    evs.append((s, 16 * k))
        for e, eo in self.engs.items():
            for sem, val in evs:
                if sem is self.esem[e]:
                    continue
                if self.seen[e].get(sem, 0) >= val:
                    continue
                eo.wait_ge(sem, val)
                self.seen[e][sem] = val


class Rot:
    def __init__(self, bufs):
        self.bufs = bufs
        self.i = 0

    def next(self):
        b = self.bufs[self.i % len(self.bufs)]
        self.i += 1
        return b


def ssl(start, n, step):
    return slice(start, start + step * (n - 1) + 1, step)


def alibi_slopes(n):
    start = 2.0 ** (-8.0 / n)
    return [start ** (i + 1) for i in range(n)]


def build_program(upto=None, dumps=()):
    nc = bass.Bass("TRN2", target_bir_lowering=False)
    ext = {}

    def ein(name, shape, dtype=F32):
        ext[name] = Buf(nc.dram_tensor(name, list(shape), dtype, kind="ExternalInput"), name)
        return ext[name]

    x_in = ein("x", [S, D])
    ident_d = ein("ident", [128, 128])
    negd_d = ein("negd", [128, NEGD_W])
    cos_d = ein("ropecos", [128, S])
    sin_d = ein("ropesin", [128, S])
    w_in_ab = ein("w_in_ab", [D, 1984])
    w_uq = ein("w_uq", [256, 512])
    w_ukv = ein("w_ukv", [128, 768])
    w_out_ab = ein("w_out_ab", [D, D])
    lam4 = ein("lam4", [4, 64])
    diff_g = ein("diff_g", [128, 1])
    qn_g = ein("qn_g", [128, 2])
    kvn_g = ein("kvn_g", [128, 1])
    ln_mix_g = ein("ln_mix_g", [2, D])
    ln_mix_b = ein("ln_mix_b", [2, D])
    ln_ffn_g = ein("ln_ffn_g", [2, D])
    ln_ffn_b = ein("ln_ffn_b", [2, D])
    w_router = ein("w_router", [2, D, 36])
    b_router = ein("b_router", [2, 36])
    wg_l = ein("wg_l", [2 * E_ * 128, 8 * HID])
    wu_l = ein("wu_l", [2 * E_ * 128, 8 * HID])
    wd_l = ein("wd_l", [2 * E_ * 128, 4 * D])
    w_in_c = ein("w_in_c", [D, 3 * D])
    w_out_c = ein("w_out_c", [D, D])
    dil_negd_d = ein("dil_negd", [128, 3, 512])
    moe_c = ein("moe_c", [128, 4, 64])
    triu_d = ein("triu", [128, 128])
    tokid_d = ein("tokid", [128, NT])
    sel_d = ein("sel", [128, 64])
    out_d = Buf(nc.dram_tensor("out", [S, D], F32, kind="ExternalOutput"), "out", True)

    dump_bufs = {}

    with ExitStack() as st:
        fw = FW(nc, st)
        op = fw.op

        def dma(q, out_ap, in_ap, reads, writes):
            return op(q, lambda e: e.dma_start(out=out_ap, in_=in_ap), reads, writes, dma=True)

        _dram0 = fw.dram
        fw.dram = lambda name, shape, dtype: _dram0(name, shape, dtype, kind=("ExternalOutput" if name in dumps else "Internal"))
        qkA = fw.dram("qkA", [8, 128, S], BF16)
        vA = fw.dram("vA", [S, 512], BF16)
        cq_d = fw.dram("cq_d", [256, S], F32)
        ckv_d = fw.dram("ckv_d", [128, S], F32)
        qmT = fw.dram("qmT", [4, 96, S], BF16)
        kmT = fw.dram("kmT", [4, 96, S], BF16)
        vM = fw.dram("vM", [S, 512], BF16)
        oT_d = fw.dram("oT_d", [8, 128, S], BF16)
        x1_d = fw.dram("x1_d", [S, D], F32)
        x1b_d = fw.dram("x1b_d", [S + 1, D], BF16)
        x2_d = fw.dram("x2_d", [S, D], F32)
        slot_d = fw.dram("slot_d", [NSLOT + 128, 4], F32)
        y_d = fw.dram("y_d", [2 * S + 128, D], F32)
        wgb = fw.dram("wgb", [2 * E_ * 128, 8 * HID], BF16)
        wub = fw.dram("wub", [2 * E_ * 128, 8 * HID], BF16)
        wdb = fw.dram("wdb", [2 * E_ * 128, 4 * D], BF16)
        cast_list = []
        CR = 512
        for l_ in range(2):
            for src_, dst_ in ((wg_l, wgb), (wu_l, wub), (wd_l, wdb)):
                for r_ in range(l_ * E_ * 128, (l_ + 1) * E_ * 128, CR):
                    cast_list.append((src_, dst_, r_))
        cast_bufs = []

        def issue_cast(n=1):
            for _ in range(n):
                if not cast_list:
                    return
                src_, dst_, r_ = cast_list.pop(0)
                cb = Buf(None, "castchunk")
                cast_bufs.append(cb)
                dma("pool", dst_[r_:r_ + CR, :], src_[r_:r_ + CR, :], [src_], [cb])
        qkC = fw.dram("qkC", [16, 128, S], BF16)
        vC = fw.dram("vC", [3, 4, 128, NT, 260], BF16)

        ident = fw.sb("ident", [128, 128], F32)
        dma("sp", ident[:], ident_d.ap(), [ident_d], [ident])
        ones_bf = fw.sb("ones_bf", [128, 128], BF16)
        op("dve", lambda e: e.memset(ones_bf[:], 1.0), [], [ones_bf])
        ones_f = fw.sb("ones_f", [128, 128], F32)
        op("dve", lambda e: e.memset(ones_f[:], 1.0), [], [ones_f])
        eps_t = fw.sb("eps_t", [128, 1], F32)
        op("dve", lambda e: e.memset(eps_t[:], EPS), [], [eps_t])
        psb = [fw.ps(f"psb{i}", [128, 512], F32) for i in range(8)]

        def dump(name, buf_ap, shape, dtype, reads):
            if name in dumps:
                d = Buf(nc.dram_tensor("dbg_" + name, list(shape), dtype, kind="ExternalOutput"), name)
                dump_bufs[name] = d
                dma("sp", d.ap(), buf_ap, reads, [d])

        def build_xT(src_d, xT, pst, stg):
            for i in range(NT):
                xt = stg.next()
                dma("sp", xt[:], src_d[i * 128:(i + 1) * 128, :], [src_d], [xt])
                for hf in range(2):
                    ps = pst.next()
                    for c4 in range(4):
                        c = hf * 4 + c4
                        op("pe", lambda e: e.transpose(out=ps[:, c4 * 128:(c4 + 1) * 128], in_=xt[:, c * 128:(c + 1) * 128], identity=ident[:]), [xt, ident], [ps])
                    eng = "act" if hf == 0 else "dve"
                    o_ap = xT[:, hf * 4:(hf + 1) * 4, i * 128:(i + 1) * 128]
                    i_ap = ps[:, :].rearrange("p (c n) -> p c n", c=4)
                    if eng == "act":
                        op("act", lambda e: e.copy(out=o_ap, in_=i_ap), [ps], [xT])
                    else:
                        op("dve", lambda e: e.tensor_copy(out=o_ap, in_=i_ap), [ps], [xT])

        def attn_core(QT, KT, Vsb, krows, q0, po, pss, pscore, slope, negd, tmps, es, band=None):
            qb_, qr = QT
            kb_, kr = KT
            kbs = list(range(NT))
            for n, kb in enumerate(kbs):
                ps = pscore.next()
                op("pe", lambda e: e.matmul(ps[:, :], lhsT=kb_[kr:kr + krows, kb * 128:(kb + 1) * 128], rhs=qb_[qr:qr + krows, q0:q0 + 512], start=True, stop=True), [kb_, qb_], [ps])
                E = es.next()
                if slope is not None:
                    tmp = tmps.next()
                    n0 = q0 - kb * 128 + NEGD_C
                    op("dve", lambda e: e.scalar_tensor_tensor(out=tmp[:, :], in0=negd[:, n0:n0 + 512], scalar=float(slope), in1=ps[:, :], op0=ALU.mult, op1=ALU.add), [negd, ps], [tmp])
                    op("act", lambda e: e.activation(out=E[:, :], in_=tmp[:, :], func=AF.Exp), [tmp], [E])
                else:
                    op("act", lambda e: e.activation(out=E[:, :], in_=ps[:, :], func=AF.Exp), [ps], [E])
                first, last = (n == 0), (n == len(kbs) - 1)
                op("pe", lambda e: e.matmul(po[:, :], lhsT=Vsb[:, kb, :], rhs=E[:, :], start=first, stop=last), [Vsb, E], [po])
                op("pe", lambda e: e.matmul(pss[:, :], lhsT=ones_bf[:, :], rhs=E[:, :], start=first, stop=last), [ones_bf, E], [pss])

        def rstd_from_ssq(ps_ssq, n, out_t, tmp_t):
            op("act", lambda e: e.activation(out=tmp_t[:, :], in_=ps_ssq[:, :], func=AF.Sqrt, bias=eps_t[:, 0:1], scale=1.0 / n), [ps_ssq, eps_t], [tmp_t])
            op("dve", lambda e: e.reciprocal(out=out_t[:, :], in_=tmp_t[:, :]), [tmp_t], [out_t])

        with ExitStack() as ph:
            xT = fw.sb("xT", [128, 8, S], BF16, ph)
            Win = fw.sb("Win", [128, 8, 1984], BF16, ph, multi=True)
            for c in range(8):
                dma("pool", Win[:, c, :], w_in_ab[c * 128:(c + 1) * 128, :], [w_in_ab], [Win])
            cosT = fw.sb("cosT", [128, S], F32, ph)
            sinT = fw.sb("sinT", [128, S], F32, ph)
            dma("sp", cosT[:], cos_d.ap(), [cos_d], [cosT])
            dma("sp", sinT[:], sin_d.ap(), [sin_d], [sinT])
            stg = Rot([fw.sb(f"a0_x{i}", [128, D], F32, ph) for i in range(2)])
            pst = Rot(psb[0:2])
            build_xT(x_in, xT, pst, stg)
            dump("xT", xT[:, :, :], [128, 8, S], BF16, [xT])
            psr = Rot(psb[2:8])
            sbf = Rot([fw.sb(f"a0_sb{i}", [128, 512], BF16, ph) for i in range(4)])
            sf = Rot([fw.sb(f"a0_sf{i}", [128, 512], F32, ph) for i in range(4)])
            tog = [0]

            def evac(out_ap, in_ap, reads, writes, scale=None):
                tog[0] ^= 1
                if tog[0] or scale is not None:
                    if scale is None:
                        op("act", lambda e: e.copy(out=out_ap, in_=in_ap), reads, writes)
                    else:
                        op("act", lambda e: e.activation(out=out_ap, in_=in_ap, func=AF.Copy, scale=float(scale)), reads, writes)
                else:
                    op("dve", lambda e: e.tensor_copy(out=out_ap, in_=in_ap), reads, writes)

            def proj_fm(Wsb, nchunk, col0, M, rhs_fn, rhs_reads, tb):
                ps = psr.next()
                for c in range(nchunk):
                    op("pe", lambda e: e.matmul(ps[0:M, :], lhsT=Wsb[:, c, col0:col0 + M], rhs=rhs_fn(c), start=(c == 0), stop=(c == nchunk - 1)), [Wsb] + rhs_reads, [ps])
                return ps

            for tb in range(8):
                tsl = slice(tb * 512, (tb + 1) * 512)
                rf = lambda c: xT[:, c, tsl]
                for h in range(8):
                    ps = proj_fm(Win, 8, h * 128, 128, rf, [xT], tb)
                    s_ = sbf.next()
                    evac(s_[:, :], ps[:, :], [ps], [s_], scale=(0.125 if h < 4 else None))
                    dma("pool", qkA[h, :, tsl], s_[:, :], [s_], [qkA])
                for j in range(3):
                    ps = proj_fm(Win, 8, 1536 + j * 128, 128, rf, [xT], tb)
                    s_ = sf.next()
                    evac(s_[:, :], ps[:, :], [ps], [s_])
                    if j < 2:
                        dma("pool", cq_d[j * 128:(j + 1) * 128, tsl], s_[:, :], [s_], [cq_d])
                    else:
                        dma("pool", ckv_d[:, tsl], s_[:, :], [s_], [ckv_d])
                ps1 = proj_fm(Win, 8, 1920, 32, rf, [xT], tb)
                ps2 = proj_fm(Win, 8, 1952, 32, rf, [xT], tb)
                t1, t2 = sf.next(), sf.next()
                op("dve", lambda e: e.tensor_tensor(out=t1[0:32, :], in0=ps1[0:32, :], in1=cosT[0:32, tsl], op=ALU.mult), [ps1, cosT], [t1])
                op("dve", lambda e: e.tensor_tensor(out=t2[0:32, :], in0=ps2[0:32, :], in1=sinT[0:32, tsl], op=ALU.mult), [ps2, sinT], [t2])
                s_ = sbf.next()
                op("dve", lambda e: e.tensor_tensor(out=s_[0:32, :], in0=t1[0:32, :], in1=t2[0:32, :], op=ALU.add), [t1, t2], [s_])
                for h in range(4):
                    dma("pool", kmT[h, 64:96, tsl], s_[0:32, :], [s_], [kmT])
            for i in range(NT):
                ps = psr.next()
                for c in range(8):
                    op("pe", lambda e: e.matmul(ps[:, :], lhsT=xT[:, c, i * 128:(i + 1) * 128], rhs=Win[:, c, 1024:1536], start=(c == 0), stop=(c == 7)), [xT, Win], [ps])
                s_ = sbf.next()
                evac(s_[:, :], ps[:, :], [ps], [s_])
                dma("pool", vA[i * 128:(i + 1) * 128, :], s_[:, :], [s_], [vA])
            fw.barrier()
        if upto == "A0":
            return nc, ext, out_d, dump_bufs

        with ExitStack() as ph:
            cq = fw.sb("cq", [128, 2, S], F32, ph, multi=True)
            ckv = fw.sb("ckv", [128, S], F32, ph)
            for j in range(2):
                dma("sp", cq[:, j, :], cq_d[j * 128:(j + 1) * 128, :], [cq_d], [cq])
            dma("sp", ckv[:, :], ckv_d.ap(), [ckv_d], [ckv])
            Wuq = fw.sb("Wuq", [128, 2, 512], BF16, ph, multi=True)
            for j in range(2):
                dma("pool", Wuq[:, j, :], w_uq[j * 128:(j + 1) * 128, :], [w_uq], [Wuq])
            Wukv = fw.sb("Wukv", [128, 1, 768], BF16, ph)
            dma("pool", Wukv[:, 0, :], w_ukv.ap(), [w_ukv], [Wukv])
            gq = fw.sb("gq", [128, 2], F32, ph)
            gkv = fw.sb("gkv", [128, 1], F32, ph)
            dma("sp", gq[:], qn_g.ap(), [qn_g], [gq])
            dma("sp", gkv[:], kvn_g.ap(), [kvn_g], [gkv])
            cosT = fw.sb("cosT1", [128, S], F32, ph)
            sinT = fw.sb("sinT1", [128, S], F32, ph)
            dma("sp", cosT[:], cos_d.ap(), [cos_d], [cosT])
            dma("sp", sinT[:], sin_d.ap(), [sin_d], [sinT])
            sq = Rot([fw.sb(f"a1_sq{i}", [128, 512], F32, ph) for i in range(2)])
            tmpf = Rot([fw.sb(f"a1_t{i}", [128, 512], F32, ph) for i in range(4)])
            cqn = Rot([fw.sb(f"a1_cqn{i}", [128, 2, 512], BF16, ph) for i in range(2)])
            ckvn = Rot([fw.sb(f"a1_ckvn{i}", [128, 512], BF16, ph) for i in range(2)])
            sbf = Rot([fw.sb(f"a1_sb{i}", [128, 512], BF16, ph) for i in range(4)])
            psr = Rot(psb[0:8])
            SCALE_M = 96.0 ** -0.5
            for tb in range(8):
                tsl = slice(tb * 512, (tb + 1) * 512)
                pss_ = psr.next()
                for j in range(2):
                    s_ = sq.next()
                    op("act", lambda e: e.activation(out=s_[:, :], in_=cq[:, j, tsl], func=AF.Square), [cq], [s_])
                    op("pe", lambda e: e.matmul(pss_[:, :], lhsT=ones_f[:, :], rhs=s_[:, :], start=(j == 0), stop=(j == 1)), [ones_f, s_], [pss_])
                rs, tt = tmpf.next(), tmpf.next()
                rstd_from_ssq(pss_, 256.0, rs, tt)
                cn = cqn.next()
                for j in range(2):
                    op("dve", lambda e: e.scalar_tensor_tensor(out=cn[:, j, :], in0=cq[:, j, tsl], scalar=gq[:, j:j + 1], in1=rs[:, :], op0=ALU.mult, op1=ALU.mult), [cq, gq, rs], [cn])
                for h in range(4):
                    ps = psr.next()
                    for j in range(2):
                        op("pe", lambda e: e.matmul(ps[0:64, :], lhsT=Wuq[:, j, h * 64:(h + 1) * 64], rhs=cn[:, j, :], start=(j == 0), stop=(j == 1)), [Wuq, cn], [ps])
                    s_ = sbf.next()
                    op("act", lambda e: e.activation(out=s_[0:64, :], in_=ps[0:64, :], func=AF.Copy, scale=SCALE_M), [ps], [s_])
                    dma("pool", qmT[h, 0:64, tsl], s_[0:64, :], [s_], [qmT])
                ps1, ps2 = psr.next(), psr.next()
                for j in range(2):
                    op("pe", lambda e: e.matmul(ps1[:, :], lhsT=Wuq[:, j, 256:384], rhs=cn[:, j, :], start=(j == 0), stop=(j == 1)), [Wuq, cn], [ps1])
                for j in range(2):
                    op("pe", lambda e: e.matmul(ps2[:, :], lhsT=Wuq[:, j, 384:512], rhs=cn[:, j, :], start=(j == 0), stop=(j == 1)), [Wuq, cn], [ps2])
                t1, t2 = tmpf.next(), tmpf.next()
                op("dve", lambda e: e.tensor_tensor(out=t1[:, :], in0=ps1[:, :], in1=cosT[:, tsl], op=ALU.mult), [ps1, cosT], [t1])
                op("dve", lambda e: e.tensor_tensor(out=t2[:, :], in0=ps2[:, :], in1=sinT[:, tsl], op=ALU.mult), [ps2, sinT], [t2])
                op("dve", lambda e: e.tensor_tensor(out=t1[:, :], in0=t1[:, :], in1=t2[:, :], op=ALU.add), [t1, t2], [t1])
                s_ = sbf.next()
                op("act", lambda e: e.activation(out=s_[:, :], in_=t1[:, :], func=AF.Copy, scale=SCALE_M), [t1], [s_])
                for h in range(4):
                    dma("pool", qmT[h, 64:96, tsl], s_[h * 32:(h + 1) * 32, :], [s_], [qmT])
                pss_ = psr.next()
                s_ = sq.next()
                op("act", lambda e: e.activation(out=s_[:, :], in_=ckv[:, tsl], func=AF.Square), [ckv], [s_])
                op("pe", lambda e: e.matmul(pss_[:, :], lhsT=ones_f[:, :], rhs=s_[:, :], start=True, stop=True), [ones_f, s_], [pss_])
                rs, tt = tmpf.next(), tmpf.next()
                rstd_from_ssq(pss_, 128.0, rs, tt)
                kn = ckvn.next()
                op("dve", lambda e: e.scalar_tensor_tensor(out=kn[:, :], in0=ckv[:, tsl], scalar=gkv[:, 0:1], in1=rs[:, :], op0=ALU.mult, op1=ALU.mult), [ckv, gkv, rs], [kn])
                for h in range(4):
                    ps = psr.next()
                    op("pe", lambda e: e.matmul(ps[0:64, :], lhsT=Wukv[:, 0, h * 64:(h + 1) * 64], rhs=kn[:, :], start=True, stop=True), [Wukv, kn], [ps])
                    s_ = sbf.next()
                    op("dve", lambda e: e.tensor_copy(out=s_[0:64, :], in_=ps[0:64, :]), [ps], [s_])
                    dma("pool", kmT[h, 0:64, tsl], s_[0:64, :], [s_], [kmT])
                for i4 in range(4):
                    i = tb * 4 + i4
                    ps = psr.next()
                    op("pe", lambda e: e.matmul(ps[:, :], lhsT=kn[:, i4 * 128:(i4 + 1) * 128], rhs=Wukv[:, 0, 256:768], start=True, stop=True), [kn, Wukv], [ps])
                    s_ = sbf.next()
                    op("act", lambda e: e.copy(out=s_[:, :], in_=ps[:, :]), [ps], [s_])
                    dma("pool", vM[i * 128:(i + 1) * 128, :], s_[:, :], [s_], [vM])
            fw.barrier()
        if upto == "A1":
            return nc, ext, out_d, dump_bufs

        LAM_INIT0 = 0.8 - 0.6 * math.exp(-0.3 * 0)
        with ExitStack() as ph:
            negd = fw.sb("negd", [128, NEGD_W], F32, ph)
            dma("sp", negd[:], negd_d.ap(), [negd_d], [negd])
            lamt = fw.sb("lamt", [128, 4, 64], F32, ph)
            dma("sp", lamt[:], lam4.ap().rearrange("(o a) b -> o a b", o=1).broadcast_to([128, 4, 64]), [lam4], [lamt])
            lw = fw.sb("lw", [128, 8], F32, ph)
            lprod = fw.sb("lprod", [128, 2, 64], F32, ph)
            op("dve", lambda e: e.tensor_tensor(out=lprod[:, 0, :], in0=lamt[:, 0, :], in1=lamt[:, 1, :], op=ALU.mult), [lamt], [lprod])
            op("dve", lambda e: e.tensor_tensor(out=lprod[:, 1, :], in0=lamt[:, 2, :], in1=lamt[:, 3, :], op=ALU.mult), [lamt], [lprod])
            op("dve", lambda e: e.reduce_sum(out=lw[:, 0:2], in_=lprod[:, :, :], axis=AX.X), [lprod], [lw])
            op("act", lambda e: e.activation(out=lw[:, 2:4], in_=lw[:, 0:2], func=AF.Exp), [lw], [lw])
            op("dve", lambda e: e.tensor_tensor(out=lw[:, 4:5], in0=lw[:, 3:4], in1=lw[:, 2:3], op=ALU.subtract), [lw], [lw])
            op("dve", lambda e: e.tensor_scalar_add(out=lw[:, 5:6], in0=lw[:, 4:5], scalar1=-LAM_INIT0), [lw], [lw])
            neg_lam = lw[:, 5:6]
            dg = fw.sb("dg", [128, 1], F32, ph)
            dma("sp", dg[:], diff_g.ap(), [diff_g], [dg])
            dg2 = fw.sb("dg2", [128, 1], F32, ph)
            op("dve", lambda e: e.tensor_scalar_mul(out=dg2[:, :], in0=dg[:, :], scalar1=(1.0 - LAM_INIT0)), [dg], [dg2])

            QTs = Rot([fw.sb(f"b_q{i}", [128, S], BF16, ph) for i in range(2)])
            QZ = [Rot([fw.sb(f"b_qz{m}{i}", [128, S], BF16, ph) for i in range(2)]) for m in range(2)]
            for m in range(2):
                for qz in QZ[m].bufs:
                    op("dve", lambda e: e.memset(qz[:, :], 0.0), [], [qz])
            KTs = Rot([fw.sb(f"b_k{i}", [128, S], BF16, ph) for i in range(2)])
            Vs = Rot([fw.sb(f"b_v{i}", [128, NT, 128], BF16, ph) for i in range(2)])
            tmps = Rot([fw.sb(f"b_t{i}", [128, 512], BF16, ph) for i in range(5)])
            es = Rot([fw.sb(f"b_e{i}", [128, 512], BF16, ph) for i in range(6)])
            ETs = Rot([fw.sb(f"b_et{i}", [128, NEGD_W], BF16, ph) for i in range(2)])
            pscore = Rot(psb[0:4])
            pacc = Rot([(psb[4], psb[5]), (psb[6], psb[7])])
            of = Rot([fw.sb(f"b_of{i}", [128, 512], F32, ph) for i in range(6)])
            rsf = Rot([fw.sb(f"b_rs{i}", [128, 512], F32, ph) for i in range(8)])
            ob = Rot([fw.sb(f"b_ob{i}", [128, 512], BF16, ph) for i in range(3)])
            slopes = alibi_slopes(4)
            LA = 3
            jobs = []

            def mk_loader(kind, h, QT, KT, V):
                def ld():
                    if kind == "diff":
                        ET = cur_et[h]
                        for c4 in range(4):
                            csl = slice(c4 * 2016, (c4 + 1) * 2016)
                            op("act", lambda e: e.activation(out=ET[:, csl], in_=negd[:, csl], func=AF.Exp, scale=float(slopes[h])), [negd], [ET])
                        for m in range(2):
                            dma("sp", QT[m][m * 64:(m + 1) * 64, :], qkA[h, m * 64:(m + 1) * 64, :], [qkA], [QT[m]])
                        dma("sp", KT[:, :], qkA[4 + h, :, :], [qkA], [KT])
                        dma("sp", V[:, :, :], vA[:, h * 128:(h + 1) * 128].rearrange("(t p) e -> p t e", p=128), [vA], [V])
                    else:
                        dma("sp", QT[0:96, :], qmT[h, :, :], [qmT], [QT])
                        dma("sp", KT[0:96, :], kmT[h, :, :], [kmT], [KT])
                        dma("sp", V[:, :, :], vM[:, h * 128:(h + 1) * 128].rearrange("(t p) e -> p t e", p=128), [vM], [V])
                return ld

            def fin_diff(h, q0, st_):
                def fin_map(po, pss_):
                    rs, r2 = rsf.next(), rsf.next()
                    op("act", lambda e: e.activation(out=r2[:, :], in_=pss_[:, :], func=AF.Ln), [pss_], [r2])
                    op("act", lambda e: e.activation(out=rs[:, :], in_=r2[:, :], func=AF.Exp, scale=-1.0), [r2], [rs])
                    yield
                    op("dve", lambda e: e.tensor_tensor(out=r2[:, :], in0=rs[:, :], in1=pss_[:, :], op=ALU.mult), [rs, pss_], [r2])
                    op("dve", lambda e: e.tensor_scalar(out=r2[:, :], in0=r2[:, :], scalar1=-1.0, scalar2=2.0, op0=ALU.mult, op1=ALU.add), [r2], [r2])
                    yield
                    op("dve", lambda e: e.tensor_tensor(out=rs[:, :], in0=rs[:, :], in1=r2[:, :], op=ALU.mult), [rs, r2], [rs])
                    o_ = of.next()
                    op("dve", lambda e: e.tensor_tensor(out=o_[:, :], in0=po[:, :], in1=rs[:, :], op=ALU.mult), [po, rs], [o_])
                    st_.append(o_)
                    if len(st_) == 2:
                        yield
                        om = st_
                        oa = of.next()
                        op("dve", lambda e: e.scalar_tensor_tensor(out=oa[:, :], in0=om[1][:, :], scalar=neg_lam, in1=om[0][:, :], op0=ALU.mult, op1=ALU.add), [om[0], om[1], lw], [oa])
                        yield
                        sq_ = of.next()
                        op("act", lambda e: e.activation(out=sq_[:, :], in_=oa[:, :], func=AF.Square), [oa], [sq_])
                        yield
                        pssq = pscore.next()
                        op("pe", lambda e: e.matmul(pssq[:, :], lhsT=ones_f[:, :], rhs=sq_[:, :], start=True, stop=True), [ones_f, sq_], [pssq])
                        yield
                        rs2, tt = rsf.next(), rsf.next()
                        op("act", lambda e: e.activation(out=tt[:, :], in_=pssq[:, :], func=AF.Ln, bias=eps_t[:, 0:1], scale=1.0 / 128.0), [pssq, eps_t], [tt])
                        op("act", lambda e: e.activation(out=rs2[:, :], in_=tt[:, :], func=AF.Exp, scale=-0.5), [tt], [rs2])
                        yield
                        o_b = ob.next()
                        op("dve", lambda e: e.scalar_tensor_tensor(out=o_b[:, :], in0=oa[:, :], scalar=dg2[:, 0:1], in1=rs2[:, :], op0=ALU.mult, op1=ALU.mult), [oa, dg2, rs2], [o_b])
                        dma("pool", oT_d[h, :, q0:q0 + 512], o_b[:, :], [o_b], [oT_d])
                return fin_map

            def fin_mla(h, q0):
                def fin_map(po, pss_):
                    rs = rsf.next()
                    op("dve", lambda e: e.reciprocal(out=rs[:, :], in_=pss_[:, :]), [pss_], [rs])
                    yield
                    o_b = ob.next()
                    op("dve", lambda e: e.tensor_tensor(out=o_b[:, :], in0=po[:, :], in1=rs[:, :], op=ALU.mult), [po, rs], [o_b])
                    dma("pool", oT_d[4 + h, :, q0:q0 + 512], o_b[:, :], [o_b], [oT_d])
                return fin_map

            cur_et = {}
            SKIP_THR = {0: 512, 1: 2048}
            for h in range(4):
                QT, KT, V = (QZ[0].next(), QZ[1].next()), KTs.next(), Vs.next()
                cur_et[h] = ETs.next()
                first_of_head = True
                for qb in range(8):
                    st_ = []
                    fin = fin_diff(h, qb * 512, st_)
                    kbs = []
                    for kb in range(NT):
                        md = max(0, kb * 128 - (qb * 512 + 511), qb * 512 - (kb * 128 + 127))
                        if h in SKIP_THR and md >= SKIP_THR[h]:
                            continue
                        kbs.append(kb)
                    for m in range(2):
                        for kb in kbs:
                            jobs.append(dict(QT=QT[m], KT=KT, V=V, r0=0, kr=128, q0=qb * 512, kb=kb, slope=slopes[h], ET=cur_et[h], first=(kb == kbs[0]), last=(kb == kbs[-1]), fin=fin,
                                             pre=(mk_loader("diff", h, QT, KT, V) if first_of_head else None)))
                            first_of_head = False
            for h in range(4):
                QT, KT, V = QTs.next(), KTs.next(), Vs.next()
                first_of_head = True
                for qb in range(8):
                    fin = fin_mla(h, qb * 512)
                    for kb in range(NT):
                        jobs.append(dict(QT=QT, KT=KT, V=V, r0=0, kr=96, q0=qb * 512, kb=kb, slope=None, first=(kb == 0), last=(kb == NT - 1), fin=fin,
                                         pre=(mk_loader("mla", h, QT, KT, V) if first_of_head else None)))
                        first_of_head = False

            PREF = 150
            first_seen = False
            for idx_, j_ in enumerate(jobs):
                if j_["pre"] is not None:
                    if first_seen:
                        tgt = max(0, idx_ - PREF)
                        ld_ = j_["pre"]
                        j_["pre"] = None
                        prev = jobs[tgt].get("pre2")
                        jobs[tgt]["pre2"] = ld_ if prev is None else (lambda a=prev, b=ld_: (a(), b()))
                    first_seen = True

            def stage1(j):
                if j["pre"] is not None:
                    j["pre"]()
                if j.get("pre2") is not None:
                    j["pre2"]()
                QT, KT, r0, kr, q0, kb = j["QT"], j["KT"], j["r0"], j["kr"], j["q0"], j["kb"]
                ps = pscore.next()
                op("pe", lambda e: e.matmul(ps[:, :], lhsT=KT[r0:r0 + kr, kb * 128:(kb + 1) * 128], rhs=QT[r0:r0 + kr, q0:q0 + 512], start=True, stop=True), [KT, QT], [ps])
                E = es.next()
                if j["slope"] is not None:
                    tmp = tmps.next()
                    n0 = q0 - kb * 128 + NEGD_C
                    ET = j["ET"]
                    op("act", lambda e: e.activation(out=tmp[:, :], in_=ps[:, :], func=AF.Exp), [ps], [tmp])
                    op("dve", lambda e: e.tensor_tensor(out=E[:, :], in0=tmp[:, :], in1=ET[:, n0:n0 + 512], op=ALU.mult), [tmp, ET], [E])
                else:
                    op("act", lambda e: e.activation(out=E[:, :], in_=ps[:, :], func=AF.Exp), [ps], [E])
                j["E"] = E

            cur = [None]

            def stage2(j):
                if j["first"]:
                    cur[0] = pacc.next()
                po, pss_ = cur[0]
                V, kb, E = j["V"], j["kb"], j["E"]
                op("pe", lambda e: e.matmul(po[:, :], lhsT=V[:, kb, :], rhs=E[:, :], start=j["first"], stop=j["last"]), [V, E], [po])
                op("pe", lambda e: e.matmul(pss_[:, :], lhsT=ones_bf[:, :], rhs=E[:, :], start=j["first"], stop=j["last"]), [ones_bf, E], [pss_])
                if j["last"]:
                    deferred.append([FIN_DELAY, j["fin"](po, pss_)])

            deferred = []
            FIN_DELAY = 4
            FIN_STEP = 2

            def run_deferred(force=False):
                for d_ in list(deferred):
                    d_[0] -= 1
                    while d_[0] <= 0 or force:
                        try:
                            next(d_[1])
                            d_[0] = FIN_STEP
                        except StopIteration:
                            deferred.remove(d_)
                            break
                        if not force:
                            break

            for idx in range(len(jobs) + LA):
                run_deferred()
                if idx % 50 == 10:
                    issue_cast()
                if idx < len(jobs):
                    stage1(jobs[idx])
                if idx >= LA:
                    stage2(jobs[idx - LA])
            while deferred:
                run_deferred(force=True)
            issue_cast(len(cast_list))
            fw.barrier()
        if upto == "B":
            return nc, ext, out_d, dump_bufs

        LOG = fw.sb("LOG", [128, NT, 36], F32)

        def ln_stage1(y, stt, junk):
            op("act", lambda e: e.activation(out=junk[:, :], in_=y[:, :], func=AF.Identity, accum_out=stt[:, 0:1]), [y], [junk, stt])
            op("act", lambda e: e.activation(out=junk[:, :], in_=y[:, :], func=AF.Square, accum_out=stt[:, 1:2]), [y], [junk, stt])

        def ln_stage2(y, g_t, b_t, stt, out_t, junk):
            ln_front(y, stt, junk)
            ln_back(g_t, b_t, out_t, junk)

        def ln_back(g_t, b_t, out_t, junk):
            op("dve", lambda e: e.tensor_tensor(out=junk[:, :], in0=junk[:, :], in1=g_t[:, :], op=ALU.mult), [junk, g_t], [junk])
            op("pool", lambda e: e.tensor_tensor(out=out_t[:, :], in0=junk[:, :], in1=b_t[:, :], op=ALU.add), [junk, b_t], [out_t])

        def ln_front(y, stt, junk):
            op("dve", lambda e: e.tensor_scalar_mul(out=stt[:, 2:3], in0=stt[:, 0:1], scalar1=1.0 / D), [stt], [stt])
            op("dve", lambda e: e.tensor_tensor(out=stt[:, 3:4], in0=stt[:, 2:3], in1=stt[:, 2:3], op=ALU.mult), [stt], [stt])
            op("dve", lambda e: e.scalar_tensor_tensor(out=stt[:, 4:5], in0=stt[:, 1:2], scalar=1.0 / D, in1=stt[:, 3:4], op0=ALU.mult, op1=ALU.subtract), [stt], [stt])
            op("act", lambda e: e.activation(out=stt[:, 5:6], in_=stt[:, 4:5], func=AF.Sqrt, bias=eps_t[:, 0:1], scale=1.0), [stt, eps_t], [stt])
            op("dve", lambda e: e.reciprocal(out=stt[:, 6:7], in_=stt[:, 5:6]), [stt], [stt])
            op("dve", lambda e: e.scalar_tensor_tensor(out=stt[:, 7:8], in0=stt[:, 2:3], scalar=-1.0, in1=stt[:, 6:7], op0=ALU.mult, op1=ALU.mult), [stt], [stt])
            op("act", lambda e: e.activation(out=junk[:, :], in_=y[:, :], func=AF.Identity, bias=stt[:, 7:8], scale=stt[:, 6:7]), [y, stt], [junk])

        def phase_C(layer, w_out_dram, xsrc):
            with ExitStack() as ph:
                oT = fw.sb("c_oT", [128, 8, S], BF16, ph, multi=True)
                for c in range(8):
                    dma("sp", oT[:, c, :], oT_d[c, :, :], [oT_d], [oT])
                Wo = fw.sb("c_Wo", [128, 8, D], BF16, ph, multi=True)
                for c in range(8):
                    dma("pool", Wo[:, c, :], w_out_dram[c * 128:(c + 1) * 128, :], [w_out_dram], [Wo])
                Wr = fw.sb("c_Wr", [128, 8, 36], F32, ph)
                dma("sp", Wr[:], w_router[layer].rearrange("(c p) n -> p c n", p=128), [w_router], [Wr])
                g_t = fw.sb("c_g", [128, D], F32, ph)
                b_t = fw.sb("c_b", [128, D], F32, ph)
                dma("sp", g_t[:], ln_mix_g[layer:layer + 1, :].broadcast_to([128, D]), [ln_mix_g], [g_t])
                dma("sp", b_t[:], ln_mix_b[layer:layer + 1, :].broadcast_to([128, D]), [ln_mix_b], [b_t])
                brt = fw.sb("c_brt", [128, 36], F32, ph)
                dma("sp", brt[:], b_router[layer:layer + 1, :].broadcast_to([128, 36]), [b_router], [brt])
                zr = fw.sb("c_zr", [1, D], BF16, ph)
                op("dve", lambda e: e.memset(zr[:], 0.0), [], [zr])
                dma("sp", x1b_d[S:S + 1, :], zr[:], [zr], [x1b_d])
                xts = Rot([fw.sb(f"c_x{i}", [128, D], F32, ph) for i in range(3)])
                ys = Rot([fw.sb(f"c_y{i}", [128, D], F32, ph) for i in range(4)])
                junks = Rot([fw.sb(f"c_j{i}", [128, D], F32, ph) for i in range(4)])
                x1s = Rot([fw.sb(f"c_x1{i}", [128, D], F32, ph) for i in range(4)])
                x1bs = Rot([fw.sb(f"c_x1b{i}", [128, D], BF16, ph) for i in range(2)])
                x1Ts = Rot([fw.sb(f"c_x1T{i}", [128, 8, 128], F32, ph) for i in range(2)])
                stts = Rot([fw.sb(f"c_st{i}", [128, 8], F32, ph) for i in range(4)])
                pmm = Rot(psb[0:4])
                ptr = Rot(psb[4:6])
                prt = Rot(psb[6:8])
                def c_A(i):
                    isl = slice(i * 128, (i + 1) * 128)
                    xt = xts.next()
                    dma("sp", xt[:], xsrc[isl, :], [xsrc], [xt])
                    y = ys.next()
                    for n in range(2):
                        ps = pmm.next()
                        for c in range(8):
                            op("pe", lambda e: e.matmul(ps[:, :], lhsT=oT[:, c, isl], rhs=Wo[:, c, n * 512:(n + 1) * 512], start=(c == 0), stop=(c == 7)), [oT, Wo], [ps])
                        op("dve", lambda e: e.scalar_tensor_tensor(out=y[:, n * 512:(n + 1) * 512], in0=xt[:, n * 512:(n + 1) * 512], scalar=float(ALPHA), in1=ps[:, :], op0=ALU.mult, op1=ALU.add), [xt, ps], [y])
                    junk, stt = junks.next(), stts.next()
                    ln_stage1(y, stt, junk)
                    return (y, junk, stt)

                def c_B1f(i, st3):
                    y, junk, stt = st3
                    ln_front(y, stt, junk)

                def c_B1b(i, st3):
                    isl = slice(i * 128, (i + 1) * 128)
                    y, junk, stt = st3
                    x1t = x1s.next()
                    ln_back(g_t, b_t, x1t, junk)
                    dma("pool", x1_d[isl, :], x1t[:, :], [x1t], [x1_d])
                    x1b = x1bs.next()
                    op("act", lambda e: e.copy(out=x1b[:, :], in_=x1t[:, :]), [x1t], [x1b])
                    dma("pool", x1b_d[isl, :], x1b[:, :], [x1b], [x1b_d])
                    return x1t

                def c_B2(i, x1t):
                    x1T = x1Ts.next()
                    for hf in range(2):
                        ps = ptr.next()
                        for c4 in range(4):
                            c = hf * 4 + c4
                            op("pe", lambda e: e.transpose(out=ps[:, c4 * 128:(c4 + 1) * 128], in_=x1t[:, c * 128:(c + 1) * 128], identity=ident[:]), [x1t, ident], [ps])
                        o_ap = x1T[:, hf * 4:(hf + 1) * 4, :]
                        i_ap = ps[:, :].rearrange("p (c n) -> p c n", c=4)
                        if hf == 0:
                            op("act", lambda e: e.copy(out=o_ap, in_=i_ap), [ps], [x1T])
                        else:
                            op("dve", lambda e: e.tensor_copy(out=o_ap, in_=i_ap), [ps], [x1T])
                    ps = prt.next()
                    for c in range(8):
                        op("pe", lambda e: e.matmul(ps[:, 0:36], lhsT=x1T[:, c, :], rhs=Wr[:, c, :], start=(c == 0), stop=(c == 7)), [x1T, Wr], [ps])
                    op("dve", lambda e: e.tensor_tensor(out=LOG[:, i, :], in0=ps[:, 0:36], in1=brt[:, :], op=ALU.add), [ps, brt], [LOG])

                stA = {}
                x1t_of = {}
                for i in range(-2, NT + 1):
                    if 0 <= i + 2 < NT:
                        stA[i + 2] = c_A(i + 2)
                    if 0 <= i + 1 < NT:
                        c_B1f(i + 1, stA[i + 1])
                    if 0 <= i - 1 < NT:
                        c_B2(i - 1, x1t_of.pop(i - 1))
                    if 0 <= i + 1 < NT:
                        x1t_of[i + 1] = c_B1b(i + 1, stA.pop(i + 1))
                fw.barrier()

        def phase_M(layer, dst):
            with ExitStack() as ph:
                bs = ExitStack()
                tstack = [ph]

                def T(name, shape, dt=F32):
                    return fw.sb("m_" + name, shape, dt, tstack[0])
                mc = T("mc", [128, 4, 64])
                dma("sp", mc[:], moe_c.ap(), [moe_c], [mc])
                tokid = T("tokid", [128, NT])
                dma("sp", tokid[:], tokid_d.ap(), [tokid_d], [tokid])
                triu = T("triu", [128, 128])
                dma("sp", triu[:], triu_d.ap(), [triu_d], [triu])
                triu_b = T("triu_b", [128, 128], BF16)
                op("dve", lambda e: e.tensor_copy(out=triu_b[:, :], in_=triu[:, :]), [triu], [triu_b])
                coarse = LOG[:, :, 0:4]
                fine4 = LOG[:, :, 4:36].rearrange("p j (g i) -> p j g i", g=4)
                gmax = T("gmax", [128, NT])
                op("dve", lambda e: e.tensor_reduce(out=gmax[:, :], in_=coarse, axis=AX.X, op=ALU.max), [LOG], [gmax])
                ohg = T("ohg", [128, NT, 4])
                op("dve", lambda e: e.tensor_tensor(out=ohg[:, :, :], in0=coarse, in1=gmax[:, :].unsqueeze(2).to_broadcast([128, NT, 4]), op=ALU.is_equal), [LOG, gmax], [ohg])
                ex = T("ex", [128, NT, 4])
                op("dve", lambda e: e.tensor_tensor(out=ex[:, :, :], in0=coarse, in1=gmax[:, :].unsqueeze(2).to_broadcast([128, NT, 4]), op=ALU.subtract), [LOG, gmax], [ex])
                op("act", lambda e: e.activation(out=ex[:, :, :], in_=ex[:, :, :], func=AF.Exp), [ex], [ex])
                pg = T("pg", [128, NT])
                op("dve", lambda e: e.reduce_sum(out=pg[:, :], in_=ex[:, :, :], axis=AX.X), [ex], [pg])
                op("dve", lambda e: e.reciprocal(out=pg[:, :], in_=pg[:, :]), [pg], [pg])
                t48 = T("t48", [128, NT, 4, 8])
                op("dve", lambda e: e.tensor_tensor(out=t48[:, :, :, :], in0=fine4, in1=ohg[:, :, :].unsqueeze(3).to_broadcast([128, NT, 4, 8]), op=ALU.mult), [LOG, ohg], [t48])
                fsel = T("fsel", [128, NT, 8])
                op("dve", lambda e: e.reduce_sum(out=fsel[:, :, :], in_=t48[:, :, :, :].rearrange("p j g i -> p j i g"), axis=AX.X), [t48], [fsel])
                v1 = T("v1", [128, NT])
                op("dve", lambda e: e.tensor_reduce(out=v1[:, :], in_=fsel[:, :, :], axis=AX.X, op=ALU.max), [fsel], [v1])
                oh1 = T("oh1", [128, NT, 8])
                op("dve", lambda e: e.tensor_tensor(out=oh1[:, :, :], in0=fsel[:, :, :], in1=v1[:, :].unsqueeze(2).to_broadcast([128, NT, 8]), op=ALU.is_equal), [fsel, v1], [oh1])
                msk = T("msk", [128, NT, 8])
                op("dve", lambda e: e.scalar_tensor_tensor(out=msk[:, :, :], in0=oh1[:, :, :], scalar=-1.0e30, in1=fsel[:, :, :], op0=ALU.mult, op1=ALU.add), [oh1, fsel], [msk])
                v2 = T("v2", [128, NT])
                op("dve", lambda e: e.tensor_reduce(out=v2[:, :], in_=msk[:, :, :], axis=AX.X, op=ALU.max), [msk], [v2])
                oh2 = T("oh2", [128, NT, 8])
                op("dve", lambda e: e.tensor_tensor(out=oh2[:, :, :], in0=msk[:, :, :], in1=v2[:, :].unsqueeze(2).to_broadcast([128, NT, 8]), op=ALU.is_equal), [msk, v2], [oh2])
                ed = T("ed", [128, NT])
                op("dve", lambda e: e.tensor_tensor(out=ed[:, :], in0=v2[:, :], in1=v1[:, :], op=ALU.subtract), [v1, v2], [ed])
                op("act", lambda e: e.activation(out=ed[:, :], in_=ed[:, :], func=AF.Exp), [ed], [ed])
                w1 = T("w1", [128, NT])
                op("dve", lambda e: e.tensor_scalar_add(out=w1[:, :], in0=ed[:, :], scalar1=1.0), [ed], [w1])
                op("dve", lambda e: e.reciprocal(out=w1[:, :], in_=w1[:, :]), [w1], [w1])
                gates = T("gates", [128, 2, NT])
                op("dve", lambda e: e.tensor_tensor(out=gates[:, 0, :], in0=w1[:, :], in1=pg[:, :], op=ALU.mult), [w1, pg], [gates])
                op("dve", lambda e: e.tensor_tensor(out=w1[:, :], in0=w1[:, :], in1=ed[:, :], op=ALU.mult), [w1, ed], [w1])
                op("dve", lambda e: e.tensor_tensor(out=gates[:, 1, :], in0=w1[:, :], in1=pg[:, :], op=ALU.mult), [w1, pg], [gates])
                Ak = [T(f"A{k}", [128, NT, 4, 8]) for k in range(2)]
                for k, ohk in enumerate((oh1, oh2)):
                    op("dve", lambda e: e.tensor_tensor(out=Ak[k][:, :, :, :], in0=ohg[:, :, :].unsqueeze(3).to_broadcast([128, NT, 4, 8]), in1=ohk[:, :, :].unsqueeze(2).to_broadcast([128, NT, 4, 8]), op=ALU.mult), [ohg, ohk], [Ak[k]])
                A_bf = T("A_bf", [128, NT * 32], BF16)
                op("dve", lambda e: e.tensor_tensor(out=A_bf[:, :], in0=Ak[0][:, :, :, :].rearrange("p j g i -> p (j g i)"), in1=Ak[1][:, :, :, :].rearrange("p j g i -> p (j g i)"), op=ALU.add), [Ak[0], Ak[1]], [A_bf])
                sa = T("sa", [128, NT, 32])
                sb_ = T("sb", [128, NT, 32])
                tots = T("tots", [128, NT, 32])
                rank = T("rank", [128, NT, 32])
                for hf in range(2):
                    ps = psb[hf]
                    op("pe", lambda e: e.matmul(ps[:, :], lhsT=ones_bf[:, :], rhs=A_bf[:, hf * 512:(hf + 1) * 512], start=True, stop=True), [ones_bf, A_bf], [ps])
                    op("dve", lambda e: e.tensor_copy(out=tots[:, hf * 16:(hf + 1) * 16, :], in_=ps[:, :].rearrange("p (j e) -> p j e", e=32)), [ps], [tots])
                    ps2 = psb[2 + hf]
                    op("pe", lambda e: e.matmul(ps2[:, :], lhsT=triu_b[:, :], rhs=A_bf[:, hf * 512:(hf + 1) * 512], start=True, stop=True), [triu_b, A_bf], [ps2])
                    op("dve", lambda e: e.tensor_copy(out=rank[:, hf * 16:(hf + 1) * 16, :], in_=ps2[:, :].rearrange("p (j e) -> p j e", e=32)), [ps2], [rank])
                op("dve", lambda e: e.tensor_copy(out=sa[:, :, :], in_=tots[:, :, :]), [tots], [sa])
                a_, b_ = sa, sb_
                for s_ in (1, 2, 4, 8, 16):
                    op("dve", lambda e: e.tensor_tensor(out=b_[:, s_:, :], in0=a_[:, s_:, :], in1=a_[:, :NT - s_, :], op=ALU.add), [a_], [b_])
                    op("dve", lambda e: e.tensor_copy(out=b_[:, :s_, :], in_=a_[:, :s_, :]), [a_], [b_])
                    a_, b_ = b_, a_
                inc = a_
                cnt = T("cnt", [128, 32])
                op("dve", lambda e: e.tensor_copy(out=cnt[:, :], in_=inc[:, NT - 1, :]), [inc], [cnt])
                op("dve", lambda e: e.tensor_tensor(out=rank[:, :, :], in0=rank[:, :, :], in1=inc[:, :, :], op=ALU.add), [rank, inc], [rank])
                op("dve", lambda e: e.tensor_tensor(out=rank[:, :, :], in0=rank[:, :, :], in1=tots[:, :, :], op=ALU.subtract), [rank, tots], [rank])
                cmp_ = T("cmp", [128, 64, 32])
                op("dve", lambda e: e.tensor_tensor(out=cmp_[:, 0:32, :], in0=cnt[:, :].unsqueeze(2).to_broadcast([128, 32, 32]), in1=mc[:, 1, 0:32].unsqueeze(1).to_broadcast([128, 32, 32]), op=ALU.is_gt), [cnt, mc], [cmp_])
                nblk = T("nblk", [128, 32])
                op("dve", lambda e: e.reduce_sum(out=nblk[:, :], in_=cmp_[:, 0:32, :], axis=AX.X), [cmp_], [nblk])
                na = T("na", [128, 32])
                nb_ = T("nb", [128, 32])
                op("dve", lambda e: e.tensor_copy(out=na[:, :], in_=nblk[:, :]), [nblk], [na])
                a_, b_ = na, nb_
                for s_ in (1, 2, 4, 8, 16):
                    op("dve", lambda e: e.tensor_tensor(out=b_[:, s_:], in0=a_[:, s_:], in1=a_[:, :32 - s_], op=ALU.add), [a_], [b_])
                    op("dve", lambda e: e.tensor_copy(out=b_[:, :s_], in_=a_[:, :s_]), [a_], [b_])
                    a_, b_ = b_, a_
                pend = T("pend", [128, 32])
                pstart = T("pstart", [128, 32])
                op("dve", lambda e: e.tensor_scalar_mul(out=pend[:, :], in0=a_[:, :], scalar1=float(BLK)), [a_], [pend])
                op("dve", lambda e: e.scalar_tensor_tensor(out=pstart[:, :], in0=nblk[:, :], scalar=-float(BLK), in1=pend[:, :], op0=ALU.mult, op1=ALU.add), [nblk, pend], [pstart])
                op("dve", lambda e: e.tensor_tensor(out=rank[:, :, :], in0=rank[:, :, :], in1=pstart[:, :].unsqueeze(1).to_broadcast([128, NT, 32]), op=ALU.add), [rank, pstart], [rank])
                dest = T("dest", [128, 2, NT])
                for k in range(2):
                    op("dve", lambda e: e.tensor_tensor(out=sa[:, :, :], in0=Ak[k][:, :, :, :].rearrange("p j g i -> p j (g i)"), in1=rank[:, :, :], op=ALU.mult), [Ak[k], rank], [sa])
                    op("dve", lambda e: e.reduce_sum(out=dest[:, k, :], in_=sa[:, :, :], axis=AX.X), [sa], [dest])
                dest_i = T("dest_i", [128, 2, NT], I32)
                op("dve", lambda e: e.tensor_copy(out=dest_i[:, :, :], in_=dest[:, :, :]), [dest], [dest_i])
                op("dve", lambda e: e.tensor_tensor(out=cmp_[:, 0:NB, :], in0=pend[:, :].unsqueeze(1).to_broadcast([128, NB, 32]), in1=mc[:, 2, 0:NB].unsqueeze(2).to_broadcast([128, NB, 32]), op=ALU.is_le), [pend, mc], [cmp_])
                beid = T("beid", [128, 64])
                op("dve", lambda e: e.reduce_sum(out=beid[:, 0:NB], in_=cmp_[:, 0:NB, :], axis=AX.X), [cmp_], [beid])
                op("dve", lambda e: e.tensor_scalar_min(out=beid[:, 0:NB], in0=beid[:, 0:NB], scalar1=31.0), [beid], [beid])
                op("dve", lambda e: e.tensor_scalar(out=beid[:, 0:NB], in0=beid[:, 0:NB], scalar1=128.0, scalar2=float(layer * E_ * 128), op0=ALU.mult, op1=ALU.add), [beid], [beid])
                op("dve", lambda e: e.tensor_tensor(out=beid[:, 0:NB], in0=beid[:, 0:NB], in1=mc[:, 3, 0:1].to_broadcast([128, NB]), op=ALU.add), [beid, mc], [beid])
                widx = T("widx", [128, 64], I32)
                op("dve", lambda e: e.tensor_copy(out=widx[:, 0:NB], in_=beid[:, 0:NB]), [beid], [widx])
                NA = NSLOT // 128 + 1
                padrec = T("padrec", [128, NA, 4])
                op("dve", lambda e: e.memset(padrec[:, :, :], 0.0), [], [padrec])
                op("dve", lambda e: e.memset(padrec[:, :, 0:1], float(S)), [], [padrec])
                op("dve", lambda e: e.tensor_scalar_add(out=padrec[:, :, 2], in0=mc[:, 3, 0:1].to_broadcast([128, NA]), scalar1=float(2 * S)), [mc], [padrec])
                dma("sp", slot_d.ap().rearrange("(a p) c -> p a c", p=128), padrec[:, :, :], [padrec], [slot_d])
                rec = T("rec", [128, 2, NT, 4])
                op("dve", lambda e: e.memset(rec[:, :, :, :], 0.0), [], [rec])
                for k in range(2):
                    op("dve", lambda e: e.tensor_copy(out=rec[:, k, :, 0], in_=tokid[:, :]), [tokid], [rec])
                    op("dve", lambda e: e.tensor_copy(out=rec[:, k, :, 1], in_=gates[:, k, :]), [gates], [rec])
                    op("dve", lambda e: e.tensor_scalar_add(out=rec[:, k, :, 2], in0=tokid[:, :], scalar1=float(k * S)), [tokid], [rec])
                sc_bufs = []
                for k in range(2):
                    for j in range(NT):
                        sc_b = Buf(None, "slotscatter")
                        sc_bufs.append(sc_b)
                        op("pool", lambda e: e.indirect_dma_start(out=slot_d[:, :], out_offset=bass.IndirectOffsetOnAxis(ap=dest_i[:, k, j:j + 1], axis=0), in_=rec[:, k, j, :], in_offset=None), [rec, dest_i, slot_d], [sc_b], dma=True)
                SL = T("SL", [128, NA - 1, 4])
                dma("sp", SL[:, :, :], slot_d[0:NSLOT, :].rearrange("(a p) c -> p a c", p=128), [slot_d] + sc_bufs, [SL])
                tok_i = T("tok_i", [128, NA - 1], I32)
                row_i = T("row_i", [128, NA - 1], I32)
                gate_s = T("gate_s", [128, NA - 1])
                op("dve", lambda e: e.tensor_copy(out=tok_i[:, :], in_=SL[:, :, 0]), [SL], [tok_i])
                op("dve", lambda e: e.tensor_copy(out=row_i[:, :], in_=SL[:, :, 2]), [SL], [row_i])
                op("dve", lambda e: e.tensor_copy(out=gate_s[:, :], in_=SL[:, :, 1]), [SL], [gate_s])
                if "moe_dbg" in dumps:
                    dump("m_dest", dest[:, :, :], [128, 2, NT], F32, [dest])
                    dump("m_gates", gates[:, :, :], [128, 2, NT], F32, [gates])
                    dump("m_beid", beid[:, :], [128, 64], F32, [beid])
                    dump("m_SL", SL[:, :, :], [128, NA - 1, 4], F32, [SL])
                    dump("m_cnt", cnt[:, :], [128, 32], F32, [cnt])
                tstack[0] = bs
                Wgs = Rot([T(f"Wg{i}", [128, 8 * HID], BF16) for i in range(2)])
                Wus = Rot([T(f"Wu{i}", [128, 8 * HID], BF16) for i in range(2)])
                Wds = Rot([T(f"Wd{i}", [128, 4 * D], BF16) for i in range(2)])
                xgs = Rot([T(f"xg{i}", [128, D], BF16) for i in range(4)])
                xgTs = Rot([T(f"xgT{i}", [128, 8, BLK], BF16) for i in range(2)])
                acts = Rot([T(f"act{i}", [128, 4, BLK], BF16) for i in range(2)])
                sgs = Rot([T(f"sg{i}", [128, BLK], F32) for i in range(3)])
                ysbs = Rot([T(f"ysb{i}", [128, D], F32) for i in range(3)])
                identb = T("identb", [128, 128], BF16)
                op("dve", lambda e: e.tensor_copy(out=identb[:, :], in_=ident[:, :]), [ident], [identb])
                ptr = Rot(psb[0:2])
                pgu = Rot(psb[2:6])
                pyy = Rot(psb[6:8])
                def gathers(b):
                    Wg, Wu, Wd = Wgs.next(), Wus.next(), Wds.next()
                    for Wt, src in ((Wg, wgb), (Wu, wub), (Wd, wdb)):
                        op("pool", lambda e: e.indirect_dma_start(out=Wt[:, :], out_offset=None, in_=src[:, :], in_offset=bass.IndirectOffsetOnAxis(ap=widx[:, b:b + 1], axis=0)), [src, widx], [Wt], dma=True)
                    xg2 = []
                    for hf in range(2):
                        a = 2 * b + hf
                        xg = xgs.next()
                        op("pool", lambda e: e.indirect_dma_start(out=xg[:, :], out_offset=None, in_=x1b_d[:, :], in_offset=bass.IndirectOffsetOnAxis(ap=tok_i[:, a:a + 1], axis=0)), [x1b_d, tok_i], [xg], dma=True)
                        xg2.append(xg)
                    return Wg, Wu, Wd, xg2

                nxt = gathers(0)
                for b in range(NB):
                    Wg, Wu, Wd, xg2 = nxt
                    if b + 1 < NB:
                        nxt = gathers(b + 1)
                    xgT = xgTs.next()
                    for hf in range(2):
                        xg = xg2[hf]
                        ps = ptr.next()
                        psv = ps[:, :].bitcast(BF16)
                        for c in range(8):
                            op("pe", lambda e: e.transpose(out=psv[:, c * 128:(c + 1) * 128], in_=xg[:, c * 128:(c + 1) * 128], identity=identb[:]), [xg, identb], [ps])
                        o_ap = xgT[:, :, hf * 128:(hf + 1) * 128]
                        i_ap = psv.rearrange("p (c n) -> p c n", c=8)
                        if hf == 0:
                            op("act", lambda e: e.copy(out=o_ap, in_=i_ap), [ps], [xgT])
                        else:
                            op("dve", lambda e: e.tensor_copy(out=o_ap, in_=i_ap), [ps], [xgT])
                    act_ = acts.next()
                    for m in range(4):
                        pg_, pu_ = pgu.next(), pgu.next()
                        for c in range(8):
                            op("pe", lambda e: e.matmul(pg_[:, 0:BLK], lhsT=Wg[:, c * HID + m * 128:c * HID + (m + 1) * 128], rhs=xgT[:, c, :], start=(c == 0), stop=(c == 7)), [Wg, xgT], [pg_])
                        for c in range(8):
                            op("pe", lambda e: e.matmul(pu_[:, 0:BLK], lhsT=Wu[:, c * HID + m * 128:c * HID + (m + 1) * 128], rhs=xgT[:, c, :], start=(c == 0), stop=(c == 7)), [Wu, xgT], [pu_])
                        sg = sgs.next()
                        op("act", lambda e: e.activation(out=sg[:, :], in_=pg_[:, 0:BLK], func=AF.Silu), [pg_], [sg])
                        op("dve", lambda e: e.tensor_tensor(out=act_[:, m, :], in0=sg[:, :], in1=pu_[:, 0:BLK], op=ALU.mult), [sg, pu_], [act_])
                    for hf in range(2):
                        a = 2 * b + hf
                        ysb = ysbs.next()
                        for n in range(2):
                            py = pyy.next()
                            for m in range(4):
                                op("pe", lambda e: e.matmul(py[:, :], lhsT=act_[:, m, hf * 128:(hf + 1) * 128], rhs=Wd[:, m * D + n * 512:m * D + (n + 1) * 512], start=(m == 0), stop=(m == 3)), [act_, Wd], [py])
                            if n == 0:
                                op("act", lambda e: e.activation(out=ysb[:, 0:512], in_=py[:, :], func=AF.Copy, scale=gate_s[:, a:a + 1]), [py, gate_s], [ysb])
                            else:
                                op("dve", lambda e: e.tensor_scalar_mul(out=ysb[:, 512:1024], in0=py[:, :], scalar1=gate_s[:, a:a + 1]), [py, gate_s], [ysb])
                        op("pool", lambda e: e.indirect_dma_start(out=y_d[:, :], out_offset=bass.IndirectOffsetOnAxis(ap=row_i[:, a:a + 1], axis=0), in_=ysb[:, :], in_offset=None), [ysb, row_i], [y_d], dma=True)
                fw.barrier()
                bs.close()
                tstack[0] = ph
                g_t = T("g", [128, D])
                b_t = T("b", [128, D])
                dma("sp", g_t[:], ln_ffn_g[layer:layer + 1, :].broadcast_to([128, D]), [ln_ffn_g], [g_t])
                dma("sp", b_t[:], ln_ffn_b[layer:layer + 1, :].broadcast_to([128, D]), [ln_ffn_b], [b_t])
                xts = Rot([T(f"cx{i}", [128, D]) for i in range(4)])
                y0s = Rot([T(f"cy0{i}", [128, D]) for i in range(4)])
                y1s = Rot([T(f"cy1{i}", [128, D]) for i in range(4)])
                junks = Rot([T(f"cj{i}", [128, D]) for i in range(4)])
                outs = Rot([T(f"co{i}", [128, D]) for i in range(4)])
                stts = Rot([T(f"cst{i}", [128, 8]) for i in range(4)])
                def m_A(i):
                    isl = slice(i * 128, (i + 1) * 128)
                    xt, y0, y1 = xts.next(), y0s.next(), y1s.next()
                    dma("sp", xt[:], x1_d[isl, :], [x1_d], [xt])
                    dma("sp", y0[:], y_d[isl, :], [y_d], [y0])
                    dma("sp", y1[:], y_d[S + i * 128:S + (i + 1) * 128, :], [y_d], [y1])
                    op("pool", lambda e: e.tensor_tensor(out=y1[:, :], in0=y0[:, :], in1=y1[:, :], op=ALU.add), [y0, y1], [y1])
                    op("dve", lambda e: e.scalar_tensor_tensor(out=y0[:, :], in0=xt[:, :], scalar=float(ALPHA), in1=y1[:, :], op0=ALU.mult, op1=ALU.add), [xt, y1], [y0])
                    junk, stt = junks.next(), stts.next()
                    ln_stage1(y0, stt, junk)
                    return (y0, junk, stt)

                def m_B1f(i, st3):
                    y0, junk, stt = st3
                    ln_front(y0, stt, junk)

                def m_B1b(i, st3):
                    isl = slice(i * 128, (i + 1) * 128)
                    y0, junk, stt = st3
                    o_t = outs.next()
                    ln_back(g_t, b_t, o_t, junk)
                    dma("pool", dst[isl, :], o_t[:, :], [o_t], [dst])

                stA = {}
                for i in range(-2, NT):
                    if 0 <= i + 1 < NT:
                        m_B1f(i + 1, stA[i + 1])
                    if 0 <= i + 2 < NT:
                        stA[i + 2] = m_A(i + 2)
                    if 0 <= i + 1 < NT:
                        m_B1b(i + 1, stA.pop(i + 1))
                fw.barrier()

        phase_C(0, w_out_ab, x_in)
        if upto == "C0":
            return nc, ext, out_d, dump_bufs
        phase_M(0, x2_d)
        if upto == "M0":
            return nc, ext, out_d, dump_bufs
        PATS = (1, 4, 16)
        with ExitStack() as ph:
            xT = fw.sb("l1_xT", [128, 8, S], BF16, ph)
            Wc = fw.sb("l1_W", [128, 8, 3 * D], BF16, ph, multi=True)
            for c in range(8):
                dma("pool", Wc[:, c, :], w_in_c[c * 128:(c + 1) * 128, :], [w_in_c], [Wc])
            stg = Rot([fw.sb(f"l1_x{i}", [128, D], F32, ph) for i in range(2)])
            build_xT(x2_d, xT, Rot(psb[0:2]), stg)
            psr = Rot(psb[2:8])
            sbf = Rot([fw.sb(f"l1_sb{i}", [128, 512], BF16, ph) for i in range(4)])
            vst = Rot([fw.sb(f"l1_v{i}", [128, 16, 65], BF16, ph) for i in range(6)])
            for v_ in vst.bufs:
                op("dve", lambda e: e.memset(v_[:, :, :], 1.0), [], [v_])
            for tb in range(8):
                tsl = slice(tb * 512, (tb + 1) * 512)
                for ch in range(16):
                    ps = psr.next()
                    for c in range(8):
                        op("pe", lambda e: e.matmul(ps[:, :], lhsT=Wc[:, c, ch * 128:(ch + 1) * 128], rhs=xT[:, c, tsl], start=(c == 0), stop=(c == 7)), [Wc, xT], [ps])
                    s_ = sbf.next()
                    if ch < 8:
                        op("act", lambda e: e.activation(out=s_[:, :], in_=ps[:, :], func=AF.Copy, scale=0.125), [ps], [s_])
                    else:
                        op("dve", lambda e: e.tensor_copy(out=s_[:, :], in_=ps[:, :]), [ps], [s_])
                    dma("pool", qkC[ch, :, tsl], s_[:, :], [s_], [qkC])
            for ri, r in enumerate(PATS):
                nqb = S // r // 128
                for res in range(r):
                    for kt in range(nqb):
                        t = res * nqb + kt
                        tok = ssl(res + r * 128 * kt, 128, r)
                        v_ = vst.next()
                        for n in range(2):
                            ps = psr.next()
                            for c in range(8):
                                op("pe", lambda e: e.matmul(ps[:, :], lhsT=xT[:, c, tok], rhs=Wc[:, c, 2048 + n * 512:2048 + (n + 1) * 512], start=(c == 0), stop=(c == 7)), [xT, Wc], [ps])
                            o_ap = v_[:, n * 8:(n + 1) * 8, 0:64]
                            i_ap = ps[:, :].rearrange("p (h e) -> p h e", e=64)
                            if n == 0:
                                op("act", lambda e: e.copy(out=o_ap, in_=i_ap), [ps], [v_])
                            else:
                                op("dve", lambda e: e.tensor_copy(out=o_ap, in_=i_ap), [ps], [v_])
                        for g in range(4):
                            dma("sp" if g % 2 == 0 else "pool", vC[ri, g, :, t, :], v_[:, g * 4:(g + 1) * 4, :].rearrange("p h e -> p (h e)"), [v_], [vC])
            fw.barrier()
        if upto == "A1x":
            return nc, ext, out_d, dump_bufs

        with ExitStack() as ph:
            dng = fw.sb("d_negd", [128, 3, 512], F32, ph)
            dma("sp", dng[:], dil_negd_d.ap(), [dil_negd_d], [dng])
            sel = fw.sb("d_sel", [128, 64], F32, ph)
            dma("sp", sel[:], sel_d.ap(), [sel_d], [sel])
            QZ = [fw.sb(f"d_QZ{par}", [128, 2, S], BF16, ph) for par in range(2)]
            for par in range(2):
                op("pool", lambda e: e.memset(QZ[par][:, :, :], 0.0), [], [QZ[par]])
            KT = fw.sb("d_KT", [128, 2, S], BF16, ph, multi=True)
            Vr = [fw.sb(f"d_V{ri}", [128, NT, 260], BF16, ph) for ri in range(3)]
            OaccL = [fw.sb(f"d_O{i}", [65, S], F32, ph) for i in range(4)]
            tmps = Rot([fw.sb(f"d_t{i}", [128, 512], F32, ph) for i in range(4)])
            Es = Rot([fw.sb(f"d_e{i}", [128, 512], BF16, ph) for i in range(8)])
            rcp = Rot([fw.sb(f"d_r{i}", [64, 512], F32, ph) for i in range(4)])
            obs = Rot([fw.sb(f"d_ob{i}", [64, 512], BF16, ph) for i in range(2)])
            pscore = Rot(psb[0:6])
            pov = Rot(psb[6:8])
            dslopes = alibi_slopes(16)
            LAG = 1
            gjobs = []
            for g in range(4):
                for hl in range(4):
                    h = 4 * g + hl
                    for ri, r in enumerate(PATS):
                        nqb = S // r // 128
                        G = min(4, nqb)
                        for res in range(r):
                            for qg in range(0, nqb, G):
                                gjobs.append(dict(g=g, hl=hl, h=h, ri=ri, r=r, nqb=nqb, G=G, res=res, qg=qg, first_g=False, last_h=False))
                    gjobs[-1]["last_h"] = True
            seen_g = set()
            for j in gjobs:
                if j["g"] not in seen_g:
                    seen_g.add(j["g"])
                    j["first_g"] = True

            def load_group(g):
                for cl in range(2):
                    for par in range(2):
                        dma("sp", QZ[par][par * 64:(par + 1) * 64, cl, :], qkC[2 * g + cl, par * 64:(par + 1) * 64, :], [qkC], [QZ[par]])
                    dma("sp", KT[:, cl, :], qkC[8 + 2 * g + cl, :, :], [qkC], [KT])
                for ri in range(3):
                    dma("sp", Vr[ri][:, :, :], vC[ri, g, :, :, :], [vC], [Vr[ri]])

            def d_stage1(j):
                if j["first_g"]:
                    load_group(j["g"])
                hl, h, r, nqb, G, res, qg = j["hl"], j["h"], j["r"], j["nqb"], j["G"], j["res"], j["qg"]
                cl, r0 = hl // 2, (hl % 2) * 64
                blocks = list(range(qg, qg + G))
                Et, rng = [], []
                for typ in range(3):
                    ps = pscore.next()
                    val = [il for il, i in enumerate(blocks) if 0 <= i + typ - 1 < nqb]
                    lo, hi = val[0], val[-1] + 1
                    for il in val:
                        i = blocks[il]
                        kt = i + typ - 1
                        ksl = ssl(res + r * 128 * kt, 128, r)
                        qsl = ssl(res + r * 128 * i, 128, r)
                        op("pe", lambda e: e.matmul(ps[:, il * 128:(il + 1) * 128], lhsT=KT[:, cl, ksl], rhs=QZ[hl % 2][:, cl, qsl], start=True, stop=True), [KT, QZ[hl % 2]], [ps])
                    tmp, E = tmps.next(), Es.next()
                    op("dve", lambda e: e.scalar_tensor_tensor(out=tmp[:, lo * 128:hi * 128], in0=dng[:, typ, lo * 128:hi * 128], scalar=float(dslopes[h] * r), in1=ps[:, lo * 128:hi * 128], op0=ALU.mult, op1=ALU.add), [dng, ps], [tmp])
                    op("act", lambda e: e.activation(out=E[:, lo * 128:hi * 128], in_=tmp[:, lo * 128:hi * 128], func=AF.Exp), [tmp], [E])
                    Et.append(E)
                    rng.append(val)
                j["Et"], j["rng"], j["blocks"] = Et, rng, blocks

            def d_stage2(j):
                hl, h, ri, r, nqb, G, res, qg = j["hl"], j["h"], j["ri"], j["r"], j["nqb"], j["G"], j["res"], j["qg"]
                r0 = (hl % 2) * 64
                Et, rng, blocks = j["Et"], j["rng"], j["blocks"]
                po = pov.next()
                for il, i in enumerate(blocks):
                    typs = [typ for typ in range(3) if il in rng[typ]]
                    for n_, typ in enumerate(typs):
                        kt = i + typ - 1
                        t = res * nqb + kt
                        op("pe", lambda e: e.matmul(po[0:65, il * 128:(il + 1) * 128], lhsT=Vr[ri][:, t, hl * 65:(hl + 1) * 65], rhs=Et[typ][:, il * 128:(il + 1) * 128], start=(n_ == 0), stop=(n_ == len(typs) - 1)), [Vr[ri], Et[typ]], [po])
                osl = ssl(res + r * 128 * qg, 128 * G, r)
                Oacc = OaccL[hl]
                if ri == 0:
                    op("act", lambda e: e.copy(out=Oacc[0:65, osl], in_=po[0:65, 0:G * 128]), [po], [Oacc])
                else:
                    op("dve", lambda e: e.tensor_tensor(out=Oacc[0:65, osl], in0=Oacc[0:65, osl], in1=po[0:65, 0:G * 128], op=ALU.add), [Oacc, po], [Oacc])
                if j["last_h"]:
                    for _ in norm_gen(hl, h, r0):
                        pass

            def norm_gen(hl, h, r0):
                    Oacc = OaccL[hl]
                    for tb in range(8):
                        tsl = slice(tb * 512, (tb + 1) * 512)
                        pn = pov.next()
                        op("pe", lambda e: e.matmul(pn[0:64, :], lhsT=sel[0:65, :], rhs=Oacc[0:65, tsl], start=True, stop=True), [sel, Oacc], [pn])
                        rc, rc2, o_b = rcp.next(), rcp.next(), obs.next()
                        op("act", lambda e: e.activation(out=rc2[:, :], in_=pn[0:64, :], func=AF.Ln), [pn], [rc2])
                        op("act", lambda e: e.activation(out=rc[:, :], in_=rc2[:, :], func=AF.Exp, scale=-1.0), [rc2], [rc])
                        op("dve", lambda e: e.tensor_tensor(out=rc2[:, :], in0=rc[:, :], in1=pn[0:64, :], op=ALU.mult), [rc, pn], [rc2])
                        op("dve", lambda e: e.tensor_scalar(out=rc2[:, :], in0=rc2[:, :], scalar1=-1.0, scalar2=2.0, op0=ALU.mult, op1=ALU.add), [rc2], [rc2])
                        op("dve", lambda e: e.tensor_tensor(out=rc[:, :], in0=rc[:, :], in1=rc2[:, :], op=ALU.mult), [rc, rc2], [rc])
                        op("dve", lambda e: e.tensor_tensor(out=o_b[:, :], in0=Oacc[0:64, tsl], in1=rc[:, :], op=ALU.mult), [Oacc, rc], [o_b])
                        dma("pool", oT_d[h // 2, r0:r0 + 64, tsl], o_b[:, :], [o_b], [oT_d])
                        yield

            dnorm = []

            def step_norm(drain=False):
                for gn in list(dnorm):
                    while True:
                        try:
                            next(gn)
                        except StopIteration:
                            dnorm.remove(gn)
                            break
                        if not drain:
                            break

            pend = []
            for j in gjobs:
                if j["first_g"]:
                    while pend:
                        d_stage2(pend.pop(0))
                d_stage1(j)
                pend.append(j)
                if len(pend) > LAG:
                    d_stage2(pend.pop(0))
                step_norm()
            while pend:
                d_stage2(pend.pop(0))
            step_norm(drain=True)
            fw.barrier()
        if upto == "B1":
            return nc, ext, out_d, dump_bufs
        phase_C(1, w_out_c, x2_d)
        phase_M(1, out_d)
    return nc, ext, out_d, dump_bufs


def host_consts():
    c = {}
    c["ident"] = np.eye(128, dtype=np.float32)
    k = np.arange(128, dtype=np.float32)[:, None]
    n = np.arange(NEGD_W, dtype=np.float32)[None, :]
    c["negd"] = (-np.abs(n - k - NEGD_C)).astype(np.float32)
    half = 16
    inv_freq = np.power(np.float32(10000.0), -np.arange(half, dtype=np.float32) * np.float32(2.0) / np.float32(32)).astype(np.float32)
    ang = (np.arange(S, dtype=np.float32)[:, None] * inv_freq[None, :]).astype(np.float32)
    cos = np.cos(ang).astype(np.float32).T
    sin = np.sin(ang).astype(np.float32).T
    c32 = np.concatenate([cos, cos], 0)
    s32 = np.concatenate([-sin, sin], 0)
    c["ropecos"] = np.ascontiguousarray(np.tile(c32, (4, 1)))
    c["ropesin"] = np.ascontiguousarray(np.tile(s32, (4, 1)))
    kk = np.arange(128, dtype=np.float32)[:, None]
    qq = np.arange(128, dtype=np.float32)[None, :]
    tabs = []
    for typ in range(3):
        d = kk - qq + 128.0 * (typ - 1)
        t = np.where(np.abs(d) <= 64, -np.abs(d), -1.0e9).astype(np.float32)
        tabs.append(np.tile(t, (1, 4)))
    c["dil_negd"] = np.ascontiguousarray(np.stack(tabs, 1))
    mc = np.zeros((128, 4, 64), np.float32)
    mc[:, 0, :32] = np.arange(32, dtype=np.float32)[None, :]
    mc[:, 1, :32] = 256.0 * np.arange(32, dtype=np.float32)[None, :]
    mc[:, 2, :63] = 256.0 * np.arange(63, dtype=np.float32)[None, :]
    c["moe_c"] = mc
    mc[:, 3, :] = np.arange(128, dtype=np.float32)[:, None]
    c["tokid"] = (np.arange(NT, dtype=np.float32)[None, :] * 128 + np.arange(128, dtype=np.float32)[:, None]).astype(np.float32)
    sl = np.zeros((128, 64), np.float32)
    sl[64, :] = 1.0
    c["sel"] = sl
    c["triu"] = np.triu(np.ones((128, 128), np.float32), 1)
    return c


def host_weights(inp):
    w = {}
    wi = inp["w_in_ab"][0]
    w["w_in_ab"] = np.ascontiguousarray(np.concatenate([wi, wi[:, 1936:1952], wi[:, 1920:1936]], 1))
    wq = inp["w_uq"][0]
    nope = [wq[:, h * 96:h * 96 + 64] for h in range(4)]
    rope = [wq[:, h * 96 + 64:h * 96 + 96] for h in range(4)]
    rsw = [np.concatenate([wq[:, h * 96 + 80:h * 96 + 96], wq[:, h * 96 + 64:h * 96 + 80]], 1) for h in range(4)]
    w["w_uq"] = np.ascontiguousarray(np.concatenate(nope + rope + rsw, 1))
    wk = inp["w_ukv"][0]
    kn = [wk[:, h * 192:h * 192 + 64] for h in range(4)]
    vv = [wk[:, h * 192 + 64:h * 192 + 192] for h in range(4)]
    w["w_ukv"] = np.ascontiguousarray(np.concatenate(kn + vv, 1))
    w["w_out_ab"] = np.ascontiguousarray(inp["w_out_ab"][0])
    w["lam4"] = np.ascontiguousarray(np.stack([inp["lam_q1"][0], inp["lam_k1"][0], inp["lam_q2"][0], inp["lam_k2"][0]], 0))
    w["diff_g"] = np.ascontiguousarray(inp["diff_norm_g"][0].reshape(128, 1))
    w["qn_g"] = np.ascontiguousarray(inp["mla_q_norm_g"][0].reshape(2, 128).T)
    w["kvn_g"] = np.ascontiguousarray(inp["mla_kv_norm_g"][0].reshape(128, 1))
    for k in ("ln_mix_g", "ln_mix_b", "ln_ffn_g", "ln_ffn_b"):
        w[k] = np.ascontiguousarray(inp[k])
    w["w_router"] = np.ascontiguousarray(np.concatenate([inp["moe_w_group"], inp["moe_w_route"]], 2))
    w["b_router"] = np.ascontiguousarray(np.concatenate([inp["moe_b_group"], inp["moe_b_route"]], 1))
    g = inp["moe_w_gate"].reshape(2, E_, 8, 128, HID).transpose(0, 1, 3, 2, 4)
    w["wg_l"] = np.ascontiguousarray(g).reshape(2 * E_ * 128, 8 * HID)
    u = inp["moe_w_up"].reshape(2, E_, 8, 128, HID).transpose(0, 1, 3, 2, 4)
    w["wu_l"] = np.ascontiguousarray(u).reshape(2 * E_ * 128, 8 * HID)
    dd = inp["moe_w_down"].reshape(2, E_, 4, 128, D).transpose(0, 1, 3, 2, 4)
    w["wd_l"] = np.ascontiguousarray(dd).reshape(2 * E_ * 128, 4 * D)
    w["w_in_c"] = np.ascontiguousarray(inp["w_in_c"][0])
    w["w_out_c"] = np.ascontiguousarray(inp["w_out_c"][0])
    return w


def kernel(**inputs):
    inp = {k: np.asarray(v) for k, v in inputs.items()}
    nc, ext, out_d, _ = build_program()
    shared = host_consts()
    shared.update(host_weights(inp))
    x = np.ascontiguousarray(inp["x"], dtype=np.float32)
    in_maps = []
    for c in range(8):
        m = dict(shared)
        m["x"] = x[c]
        in_maps.append(m)
    res = run_bass_kernel_spmd(nc, in_maps, core_ids=list(range(8)))
    return np.stack([np.asarray(r["out"]) for r in res.results], 0).astype(np.float32)
```

```python
import math
import numpy as np
from contextlib import ExitStack
import concourse.bass as bass
import concourse.mybir as mybir
from concourse.bass_utils import run_bass_kernel_spmd

F32 = mybir.dt.float32
BF16 = mybir.dt.bfloat16
I32 = mybir.dt.int32
ALU = mybir.AluOpType
AF = mybir.ActivationFunctionType
AX = mybir.AxisListType

S = 4096
D = 1024
NT = S // 128
DEPTH = 2
ALPHA = (2 * DEPTH) ** 0.25
EPS = 1e-5
NPOOL = 16
E_ = 32
HID = 512
BLK = 256
NB = 63
NSLOT = NB * BLK
NEGD_C = 3968
NEGD_W = 8064


class Buf:
    __slots__ = ("t", "w", "r", "name", "multi", "wm")

    def __init__(self, t, name="", multi=False):
        self.t = t
        self.w = None
        self.r = {}
        self.name = name
        self.multi = multi
        self.wm = {}

    def __getitem__(self, k):
        return self.t[k]

    def ap(self):
        return self.t.ap()


class FW:
    def __init__(self, nc, stack):
        self.nc = nc
        self.stack = stack
        self.engs = {"pe": nc.tensor, "act": nc.scalar, "dve": nc.vector, "pool": nc.gpsimd, "sp": nc.sync}
        self.esem, self.cnt, self.seen, self.dq = {}, {}, {}, {}
        for e in self.engs:
            self.esem[e] = stack.enter_context(nc.semaphore("es_" + e))
            self.cnt[e] = 0
            self.seen[e] = {}
        for q in ("sp", "pool", "act"):
            sems = [stack.enter_context(nc.semaphore(f"dq_{q}_{i}")) for i in range(NPOOL)]
            self.dq[q] = {"sems": sems, "n": 0}
        self.n_inst = 0
        self.uid = 0

    def sb(self, name, shape, dtype, stack=None, multi=False):
        st = stack if stack is not None else self.stack
        self.uid += 1
        return Buf(st.enter_context(self.nc.sbuf_tensor(f"s{self.uid}_{name}", list(shape), dtype)), name, multi)

    def ps(self, name, shape, dtype=F32, stack=None):
        st = stack if stack is not None else self.stack
        return Buf(st.enter_context(self.nc.psum_tensor("p_" + name, list(shape), dtype)), name)

    def dram(self, name, shape, dtype, kind="Internal"):
        return Buf(self.nc.dram_tensor(name, list(shape), dtype, kind=kind), name, True)

    def op(self, eng, fn, reads=(), writes=(), dma=False):
        waits = {}
        own = self.esem[eng]
        seen = self.seen[eng]

        def need(sem, val):
            if eng == "pe" and sem is own:
                return
            if seen.get(sem, 0) >= val:
                return
            if waits.get(sem, 0) < val:
                waits[sem] = val

        for b in reads:
            if b.multi:
                for s_, v_ in b.wm.items():
                    need(s_, v_)
            elif b.w is not None:
                need(*b.w)
        for b in writes:
            if not b.multi and b.w is not None:
                need(*b.w)
            for s_, v_ in b.r.items():
                need(s_, v_)
        if dma:
            q = self.dq[eng]
            j = q["n"]
            s = q["sems"][j % NPOOL]
            if j >= NPOOL:
                need(s, 16 * (j // NPOOL))
        e = self.engs[eng]
        for sem, val in waits.items():
            e.wait_ge(sem, val)
            seen[sem] = val
        ins = fn(e)
        self.n_inst += 1
        if dma:
            ins.then_inc(s, 16)
            ev = (s, 16 * (j // NPOOL + 1))
            q["n"] += 1
        else:
            self.cnt[eng] += 1
            ins.then_inc(own, 1)
            ev = (own, self.cnt[eng])
        for b in reads:
            if b.r.get(ev[0], 0) < ev[1]:
                b.r[ev[0]] = ev[1]
        for b in writes:
            if b.multi:
                if b.wm.get(ev[0], 0) < ev[1]:
                    b.wm[ev[0]] = ev[1]
            else:
                b.w = ev
                b.r = {}
        return ev

    def barrier(self):
        evs = []
        for e in self.engs:
            if self.cnt[e] > 0:
                evs.append((self.esem[e], self.cnt[e]))
        for q in self.dq.values():
            n = q["n"]
            for i, s in enumerate(q["sems"]):
                k = (n - i + NPOOL - 1) // NPOOL
                if k > 0:
                    evs.append((s, 16 * k))
        for e, eo in self.engs.items():
            for sem, val in evs:
                if sem is self.esem[e]:
                    continue
                if self.seen[e].get(sem, 0) >= val:
                    continue
                eo.wait_ge(sem, val)
                self.seen[e][sem] = val


class Rot:
    def __init__(self, bufs):
        self.bufs = bufs
        self.i = 0

    def next(self):
        b = self.bufs[self.i % len(self.bufs)]
        self.i += 1
        return b


def ssl(start, n, step):
    return slice(start, start + step * (n - 1) + 1, step)


def alibi_slopes(n):
    start = 2.0 ** (-8.0 / n)
    return [start ** (i + 1) for i in range(n)]


def build_program(upto=None, dumps=()):
    nc = bass.Bass("TRN2", target_bir_lowering=False)
    ext = {}

    def ein(name, shape, dtype=F32):
        ext[name] = Buf(nc.dram_tensor(name, list(shape), dtype, kind="ExternalInput"), name)
        return ext[name]

    x_in = ein("x", [S, D])
    ident_d = ein("ident", [128, 128])
    negd_d = ein("negd", [128, NEGD_W])
    cos_d = ein("ropecos", [128, S])
    sin_d = ein("ropesin", [128, S])
    w_in_ab = ein("w_in_ab", [D, 1984])
    w_uq = ein("w_uq", [256, 512])
    w_ukv = ein("w_ukv", [128, 768])
    w_out_ab = ein("w_out_ab", [D, D])
    lam4 = ein("lam4", [4, 64])
    diff_g = ein("diff_g", [128, 1])
    qn_g = ein("qn_g", [128, 2])
    kvn_g = ein("kvn_g", [128, 1])
    ln_mix_g = ein("ln_mix_g", [2, D])
    ln_mix_b = ein("ln_mix_b", [2, D])
    ln_ffn_g = ein("ln_ffn_g", [2, D])
    ln_ffn_b = ein("ln_ffn_b", [2, D])
    w_router = ein("w_router", [2, D, 36])
    b_router = ein("b_router", [2, 36])
    wg_l = ein("wg_l", [2 * E_ * 128, 8 * HID])
    wu_l = ein("wu_l", [2 * E_ * 128, 8 * HID])
    wd_l = ein("wd_l", [2 * E_ * 128, 4 * D])
    w_in_c = ein("w_in_c", [D, 3 * D])
    w_out_c = ein("w_out_c", [D, D])
    dil_negd_d = ein("dil_negd", [128, 3, 512])
    moe_c = ein("moe_c", [128, 4, 64])
    triu_d = ein("triu", [128, 128])
    tokid_d = ein("tokid", [128, NT])
    sel_d = ein("sel", [128, 64])
    out_d = Buf(nc.dram_tensor("out", [S, D], F32, kind="ExternalOutput"), "out", True)

    dump_bufs = {}

    with ExitStack() as st:
        fw = FW(nc, st)
        op = fw.op

        def dma(q, out_ap, in_ap, reads, writes):
            return op(q, lambda e: e.dma_start(out=out_ap, in_=in_ap), reads, writes, dma=True)

        _dram0 = fw.dram
        fw.dram = lambda name, shape, dtype: _dram0(name, shape, dtype, kind=("ExternalOutput" if name in dumps else "Internal"))
        qkA = fw.dram("qkA", [8, 128, S], BF16)
        vA = fw.dram("vA", [S, 512], BF16)
        cq_d = fw.dram("cq_d", [256, S], F32)
        ckv_d = fw.dram("ckv_d", [128, S], F32)
        qmT = fw.dram("qmT", [4, 96, S], BF16)
        kmT = fw.dram("kmT", [4, 96, S], BF16)
        vM = fw.dram("vM", [S, 512], BF16)
        oT_d = fw.dram("oT_d", [8, 128, S], BF16)
        x1_d = fw.dram("x1_d", [S, D], F32)
        x1b_d = fw.dram("x1b_d", [S + 1, D], BF16)
        x2_d = fw.dram("x2_d", [S, D], F32)
        slot_d = fw.dram("slot_d", [NSLOT + 128, 4], F32)
        y_d = fw.dram("y_d", [2 * S + 128, D], F32)
        wgb = fw.dram("wgb", [2 * E_ * 128, 8 * HID], BF16)
        wub = fw.dram("wub", [2 * E_ * 128, 8 * HID], BF16)
        wdb = fw.dram("wdb", [2 * E_ * 128, 4 * D], BF16)
        cast_list = []
        CR = 512
        for l_ in range(2):
            for src_, dst_ in ((wg_l, wgb), (wu_l, wub), (wd_l, wdb)):
                for r_ in range(l_ * E_ * 128, (l_ + 1) * E_ * 128, CR):
                    cast_list.append((src_, dst_, r_))
        cast_bufs = []

        def issue_cast(n=1):
            for _ in range(n):
                if not cast_list:
                    return
                src_, dst_, r_ = cast_list.pop(0)
                cb = Buf(None, "castchunk")
                cast_bufs.append(cb)
                dma("pool", dst_[r_:r_ + CR, :], src_[r_:r_ + CR, :], [src_], [cb])
        qkC = fw.dram("qkC", [16, 128, S], BF16)
        vC = fw.dram("vC", [3, 4, 128, NT, 260], BF16)

        ident = fw.sb("ident", [128, 128], F32)
        dma("sp", ident[:], ident_d.ap(), [ident_d], [ident])
        ones_bf = fw.sb("ones_bf", [128, 128], BF16)
        op("dve", lambda e: e.memset(ones_bf[:], 1.0), [], [ones_bf])
        ones_f = fw.sb("ones_f", [128, 128], F32)
        op("dve", lambda e: e.memset(ones_f[:], 1.0), [], [ones_f])
        eps_t = fw.sb("eps_t", [128, 1], F32)
        op("dve", lambda e: e.memset(eps_t[:], EPS), [], [eps_t])
        psb = [fw.ps(f"psb{i}", [128, 512], F32) for i in range(8)]

        def dump(name, buf_ap, shape, dtype, reads):
            if name in dumps:
                d = Buf(nc.dram_tensor("dbg_" + name, list(shape), dtype, kind="ExternalOutput"), name)
                dump_bufs[name] = d
                dma("sp", d.ap(), buf_ap, reads, [d])

        def build_xT(src_d, xT, pst, stg):
            for i in range(NT):
                xt = stg.next()
                dma("sp", xt[:], src_d[i * 128:(i + 1) * 128, :], [src_d], [xt])
                for hf in range(2):
                    ps = pst.next()
                    for c4 in range(4):
                        c = hf * 4 + c4
                        op("pe", lambda e: e.transpose(out=ps[:, c4 * 128:(c4 + 1) * 128], in_=xt[:, c * 128:(c + 1) * 128], identity=ident[:]), [xt, ident], [ps])
                    eng = "act" if hf == 0 else "dve"
                    o_ap = xT[:, hf * 4:(hf + 1) * 4, i * 128:(i + 1) * 128]
                    i_ap = ps[:, :].rearrange("p (c n) -> p c n", c=4)
                    if eng == "act":
                        op("act", lambda e: e.copy(out=o_ap, in_=i_ap), [ps], [xT])
                    else:
                        op("dve", lambda e: e.tensor_copy(out=o_ap, in_=i_ap), [ps], [xT])

        def attn_core(QT, KT, Vsb, krows, q0, po, pss, pscore, slope, negd, tmps, es, band=None):
            qb_, qr = QT
            kb_, kr = KT
            kbs = list(range(NT))
            for n, kb in enumerate(kbs):
                ps = pscore.next()
                op("pe", lambda e: e.matmul(ps[:, :], lhsT=kb_[kr:kr + krows, kb * 128:(kb + 1) * 128], rhs=qb_[qr:qr + krows, q0:q0 + 512], start=True, stop=True), [kb_, qb_], [ps])
                E = es.next()
                if slope is not None:
                    tmp = tmps.next()
                    n0 = q0 - kb * 128 + NEGD_C
                    op("dve", lambda e: e.scalar_tensor_tensor(out=tmp[:, :], in0=negd[:, n0:n0 + 512], scalar=float(slope), in1=ps[:, :], op0=ALU.mult, op1=ALU.add), [negd, ps], [tmp])
                    op("act", lambda e: e.activation(out=E[:, :], in_=tmp[:, :], func=AF.Exp), [tmp], [E])
                else:
                    op("act", lambda e: e.activation(out=E[:, :], in_=ps[:, :], func=AF.Exp), [ps], [E])
                first, last = (n == 0), (n == len(kbs) - 1)
                op("pe", lambda e: e.matmul(po[:, :], lhsT=Vsb[:, kb, :], rhs=E[:, :], start=first, stop=last), [Vsb, E], [po])
                op("pe", lambda e: e.matmul(pss[:, :], lhsT=ones_bf[:, :], rhs=E[:, :], start=first, stop=last), [ones_bf, E], [pss])

        def rstd_from_ssq(ps_ssq, n, out_t, tmp_t):
            op("act", lambda e: e.activation(out=tmp_t[:, :], in_=ps_ssq[:, :], func=AF.Sqrt, bias=eps_t[:, 0:1], scale=1.0 / n), [ps_ssq, eps_t], [tmp_t])
            op("dve", lambda e: e.reciprocal(out=out_t[:, :], in_=tmp_t[:, :]), [tmp_t], [out_t])

        with ExitStack() as ph:
            xT = fw.sb("xT", [128, 8, S], BF16, ph)
            Win = fw.sb("Win", [128, 8, 1984], BF16, ph, multi=True)
            for c in range(8):
                dma("pool", Win[:, c, :], w_in_ab[c * 128:(c + 1) * 128, :], [w_in_ab], [Win])
            cosT = fw.sb("cosT", [128, S], F32, ph)
            sinT = fw.sb("sinT", [128, S], F32, ph)
            dma("sp", cosT[:], cos_d.ap(), [cos_d], [cosT])
            dma("sp", sinT[:], sin_d.ap(), [sin_d], [sinT])
            stg = Rot([fw.sb(f"a0_x{i}", [128, D], F32, ph) for i in range(2)])
            pst = Rot(psb[0:2])
            build_xT(x_in, xT, pst, stg)
            dump("xT", xT[:, :, :], [128, 8, S], BF16, [xT])
            psr = Rot(psb[2:8])
            sbf = Rot([fw.sb(f"a0_sb{i}", [128, 512], BF16, ph) for i in range(4)])
            sf = Rot([fw.sb(f"a0_sf{i}", [128, 512], F32, ph) for i in range(4)])
            tog = [0]

            def evac(out_ap, in_ap, reads, writes, scale=None):
                tog[0] ^= 1
                if tog[0] or scale is not None:
                    if scale is None:
                        op("act", lambda e: e.copy(out=out_ap, in_=in_ap), reads, writes)
                    else:
                        op("act", lambda e: e.activation(out=out_ap, in_=in_ap, func=AF.Copy, scale=float(scale)), reads, writes)
                else:
                    op("dve", lambda e: e.tensor_copy(out=out_ap, in_=in_ap), reads, writes)

            def proj_fm(Wsb, nchunk, col0, M, rhs_fn, rhs_reads, tb):
                ps = psr.next()
                for c in range(nchunk):
                    op("pe", lambda e: e.matmul(ps[0:M, :], lhsT=Wsb[:, c, col0:col0 + M], rhs=rhs_fn(c), start=(c == 0), stop=(c == nchunk - 1)), [Wsb] + rhs_reads, [ps])
                return ps

            for tb in range(8):
                tsl = slice(tb * 512, (tb + 1) * 512)
                rf = lambda c: xT[:, c, tsl]
                for h in range(8):
                    ps = proj_fm(Win, 8, h * 128, 128, rf, [xT], tb)
                    s_ = sbf.next()
                    evac(s_[:, :], ps[:, :], [ps], [s_], scale=(0.125 if h < 4 else None))
                    dma("pool", qkA[h, :, tsl], s_[:, :], [s_], [qkA])
                for j in range(3):
                    ps = proj_fm(Win, 8, 1536 + j * 128, 128, rf, [xT], tb)
                    s_ = sf.next()
                    evac(s_[:, :], ps[:, :], [ps], [s_])
                    if j < 2:
                        dma("pool", cq_d[j * 128:(j + 1) * 128, tsl], s_[:, :], [s_], [cq_d])
                    else:
                        dma("pool", ckv_d[:, tsl], s_[:, :], [s_], [ckv_d])
                ps1 = proj_fm(Win, 8, 1920, 32, rf, [xT], tb)
                ps2 = proj_fm(Win, 8, 1952, 32, rf, [xT], tb)
                t1, t2 = sf.next(), sf.next()
                op("dve", lambda e: e.tensor_tensor(out=t1[0:32, :], in0=ps1[0:32, :], in1=cosT[0:32, tsl], op=ALU.mult), [ps1, cosT], [t1])
                op("dve", lambda e: e.tensor_tensor(out=t2[0:32, :], in0=ps2[0:32, :], in1=sinT[0:32, tsl], op=ALU.mult), [ps2, sinT], [t2])
                s_ = sbf.next()
                op("dve", lambda e: e.tensor_tensor(out=s_[0:32, :], in0=t1[0:32, :], in1=t2[0:32, :], op=ALU.add), [t1, t2], [s_])
                for h in range(4):
                    dma("pool", kmT[h, 64:96, tsl], s_[0:32, :], [s_], [kmT])
            for i in range(NT):
                ps = psr.next()
                for c in range(8):
                    op("pe", lambda e: e.matmul(ps[:, :], lhsT=xT[:, c, i * 128:(i + 1) * 128], rhs=Win[:, c, 1024:1536], start=(c == 0), stop=(c == 7)), [xT, Win], [ps])
                s_ = sbf.next()
                evac(s_[:, :], ps[:, :], [ps], [s_])
                dma("pool", vA[i * 128:(i + 1) * 128, :], s_[:, :], [s_], [vA])
            fw.barrier()
        if upto == "A0":
            return nc, ext, out_d, dump_bufs

        with ExitStack() as ph:
            cq = fw.sb("cq", [128, 2, S], F32, ph, multi=True)
            ckv = fw.sb("ckv", [128, S], F32, ph)
            for j in range(2):
                dma("sp", cq[:, j, :], cq_d[j * 128:(j + 1) * 128, :], [cq_d], [cq])
            dma("sp", ckv[:, :], ckv_d.ap(), [ckv_d], [ckv])
            Wuq = fw.sb("Wuq", [128, 2, 512], BF16, ph, multi=True)
            for j in range(2):
                dma("pool", Wuq[:, j, :], w_uq[j * 128:(j + 1) * 128, :], [w_uq], [Wuq])
            Wukv = fw.sb("Wukv", [128, 1, 768], BF16, ph)
            dma("pool", Wukv[:, 0, :], w_ukv.ap(), [w_ukv], [Wukv])
            gq = fw.sb("gq", [128, 2], F32, ph)
            gkv = fw.sb("gkv", [128, 1], F32, ph)
            dma("sp", gq[:], qn_g.ap(), [qn_g], [gq])
            dma("sp", gkv[:], kvn_g.ap(), [kvn_g], [gkv])
            cosT = fw.sb("cosT1", [128, S], F32, ph)
            sinT = fw.sb("sinT1", [128, S], F32, ph)
            dma("sp", cosT[:], cos_d.ap(), [cos_d], [cosT])
            dma("sp", sinT[:], sin_d.ap(), [sin_d], [sinT])
            sq = Rot([fw.sb(f"a1_sq{i}", [128, 512], F32, ph) for i in range(2)])
            tmpf = Rot([fw.sb(f"a1_t{i}", [128, 512], F32, ph) for i in range(4)])
            cqn = Rot([fw.sb(f"a1_cqn{i}", [128, 2, 512], BF16, ph) for i in range(2)])
            ckvn = Rot([fw.sb(f"a1_ckvn{i}", [128, 512], BF16, ph) for i in range(2)])
            sbf = Rot([fw.sb(f"a1_sb{i}", [128, 512], BF16, ph) for i in range(4)])
            psr = Rot(psb[0:8])
            SCALE_M = 96.0 ** -0.5
            for tb in range(8):
                tsl = slice(tb * 512, (tb + 1) * 512)
                pss_ = psr.next()
                for j in range(2):
                    s_ = sq.next()
                    op("act", lambda e: e.activation(out=s_[:, :], in_=cq[:, j, tsl], func=AF.Square), [cq], [s_])
                    op("pe", lambda e: e.matmul(pss_[:, :], lhsT=ones_f[:, :], rhs=s_[:, :], start=(j == 0), stop=(j == 1)), [ones_f, s_], [pss_])
                rs, tt = tmpf.next(), tmpf.next()
                rstd_from_ssq(pss_, 256.0, rs, tt)
                cn = cqn.next()
                for j in range(2):
                    op("dve", lambda e: e.scalar_tensor_tensor(out=cn[:, j, :], in0=cq[:, j, tsl], scalar=gq[:, j:j + 1], in1=rs[:, :], op0=ALU.mult, op1=ALU.mult), [cq, gq, rs], [cn])
                for h in range(4):
                    ps = psr.next()
                    for j in range(2):
                        op("pe", lambda e: e.matmul(ps[0:64, :], lhsT=Wuq[:, j, h * 64:(h + 1) * 64], rhs=cn[:, j, :], start=(j == 0), stop=(j == 1)), [Wuq, cn], [ps])
                    s_ = sbf.next()
                    op("act", lambda e: e.activation(out=s_[0:64, :], in_=ps[0:64, :], func=AF.Copy, scale=SCALE_M), [ps], [s_])
                    dma("pool", qmT[h, 0:64, tsl], s_[0:64, :], [s_], [qmT])
                ps1, ps2 = psr.next(), psr.next()
                for j in range(2):
                    op("pe", lambda e: e.matmul(ps1[:, :], lhsT=Wuq[:, j, 256:384], rhs=cn[:, j, :], start=(j == 0), stop=(j == 1)), [Wuq, cn], [ps1])
                for j in range(2):
                    op("pe", lambda e: e.matmul(ps2[:, :], lhsT=Wuq[:, j, 384:512], rhs=cn[:, j, :], start=(j == 0), stop=(j == 1)), [Wuq, cn], [ps2])
                t1, t2 = tmpf.next(), tmpf.next()
                op("dve", lambda e: e.tensor_tensor(out=t1[:, :], in0=ps1[:, :], in1=cosT[:, tsl], op=ALU.mult), [ps1, cosT], [t1])
                op("dve", lambda e: e.tensor_tensor(out=t2[:, :], in0=ps2[:, :], in1=sinT[:, tsl], op=ALU.mult), [ps2, sinT], [t2])
                op("dve", lambda e: e.tensor_tensor(out=t1[:, :], in0=t1[:, :], in1=t2[:, :], op=ALU.add), [t1, t2], [t1])
                s_ = sbf.next()
                op("act", lambda e: e.activation(out=s_[:, :], in_=t1[:, :], func=AF.Copy, scale=SCALE_M), [t1], [s_])
                for h in range(4):
                    dma("pool", qmT[h, 64:96, tsl], s_[h * 32:(h + 1) * 32, :], [s_], [qmT])
                pss_ = psr.next()
                s_ = sq.next()
                op("act", lambda e: e.activation(out=s_[:, :], in_=ckv[:, tsl], func=AF.Square), [ckv], [s_])
                op("pe", lambda e: e.matmul(pss_[:, :], lhsT=ones_f[:, :], rhs=s_[:, :], start=True, stop=True), [ones_f, s_], [pss_])
                rs, tt = tmpf.next(), tmpf.next()
                rstd_from_ssq(pss_, 128.0, rs, tt)
                kn = ckvn.next()
                op("dve", lambda e: e.scalar_tensor_tensor(out=kn[:, :], in0=ckv[:, tsl], scalar=gkv[:, 0:1], in1=rs[:, :], op0=ALU.mult, op1=ALU.mult), [ckv, gkv, rs], [kn])
                for h in range(4):
                    ps = psr.next()
                    op("pe", lambda e: e.matmul(ps[0:64, :], lhsT=Wukv[:, 0, h * 64:(h + 1) * 64], rhs=kn[:, :], start=True, stop=True), [Wukv, kn], [ps])
                    s_ = sbf.next()
                    op("dve", lambda e: e.tensor_copy(out=s_[0:64, :], in_=ps[0:64, :]), [ps], [s_])
                    dma("pool", kmT[h, 0:64, tsl], s_[0:64, :], [s_], [kmT])
                for i4 in range(4):
                    i = tb * 4 + i4
                    ps = psr.next()
                    op("pe", lambda e: e.matmul(ps[:, :], lhsT=kn[:, i4 * 128:(i4 + 1) * 128], rhs=Wukv[:, 0, 256:768], start=True, stop=True), [kn, Wukv], [ps])
                    s_ = sbf.next()
                    op("act", lambda e: e.copy(out=s_[:, :], in_=ps[:, :]), [ps], [s_])
                    dma("pool", vM[i * 128:(i + 1) * 128, :], s_[:, :], [s_], [vM])
            fw.barrier()
        if upto == "A1":
            return nc, ext, out_d, dump_bufs

        LAM_INIT0 = 0.8 - 0.6 * math.exp(-0.3 * 0)
        with ExitStack() as ph:
            negd = fw.sb("negd", [128, NEGD_W], F32, ph)
            dma("sp", negd[:], negd_d.ap(), [negd_d], [negd])
            lamt = fw.sb("lamt", [128, 4, 64], F32, ph)
            dma("sp", lamt[:], lam4.ap().rearrange("(o a) b -> o a b", o=1).broadcast_to([128, 4, 64]), [lam4], [lamt])
            lw = fw.sb("lw", [128, 8], F32, ph)
            lprod = fw.sb("lprod", [128, 2, 64], F32, ph)
            op("dve", lambda e: e.tensor_tensor(out=lprod[:, 0, :], in0=lamt[:, 0, :], in1=lamt[:, 1, :], op=ALU.mult), [lamt], [lprod])
            op("dve", lambda e: e.tensor_tensor(out=lprod[:, 1, :], in0=lamt[:, 2, :], in1=lamt[:, 3, :], op=ALU.mult), [lamt], [lprod])
            op("dve", lambda e: e.reduce_sum(out=lw[:, 0:2], in_=lprod[:, :, :], axis=AX.X), [lprod], [lw])
            op("act", lambda e: e.activation(out=lw[:, 2:4], in_=lw[:, 0:2], func=AF.Exp), [lw], [lw])
            op("dve", lambda e: e.tensor_tensor(out=lw[:, 4:5], in0=lw[:, 3:4], in1=lw[:, 2:3], op=ALU.subtract), [lw], [lw])
            op("dve", lambda e: e.tensor_scalar_add(out=lw[:, 5:6], in0=lw[:, 4:5], scalar1=-LAM_INIT0), [lw], [lw])
            neg_lam = lw[:, 5:6]
            dg = fw.sb("dg", [128, 1], F32, ph)
            dma("sp", dg[:], diff_g.ap(), [diff_g], [dg])
            dg2 = fw.sb("dg2", [128, 1], F32, ph)
            op("dve", lambda e: e.tensor_scalar_mul(out=dg2[:, :], in0=dg[:, :], scalar1=(1.0 - LAM_INIT0)), [dg], [dg2])

            QTs = Rot([fw.sb(f"b_q{i}", [128, S], BF16, ph) for i in range(2)])
            QZ = [Rot([fw.sb(f"b_qz{m}{i}", [128, S], BF16, ph) for i in range(2)]) for m in range(2)]
            for m in range(2):
                for qz in QZ[m].bufs:
                    op("dve", lambda e: e.memset(qz[:, :], 0.0), [], [qz])
            KTs = Rot([fw.sb(f"b_k{i}", [128, S], BF16, ph) for i in range(2)])
            Vs = Rot([fw.sb(f"b_v{i}", [128, NT, 128], BF16, ph) for i in range(2)])
            tmps = Rot([fw.sb(f"b_t{i}", [128, 512], BF16, ph) for i in range(5)])
            es = Rot([fw.sb(f"b_e{i}", [128, 512], BF16, ph) for i in range(6)])
            ETs = Rot([fw.sb(f"b_et{i}", [128, NEGD_W], BF16, ph) for i in range(2)])
            pscore = Rot(psb[0:4])
            pacc = Rot([(psb[4], psb[5]), (psb[6], psb[7])])
            of = Rot([fw.sb(f"b_of{i}", [128, 512], F32, ph) for i in range(6)])
            rsf = Rot([fw.sb(f"b_rs{i}", [128, 512], F32, ph) for i in range(8)])
            ob = Rot([fw.sb(f"b_ob{i}", [128, 512], BF16, ph) for i in range(3)])
            slopes = alibi_slopes(4)
            LA = 3
            jobs = []

            def mk_loader(kind, h, QT, KT, V):
                def ld():
                    if kind == "diff":
                        ET = cur_et[h]
                        for c4 in range(4):
                            csl = slice(c4 * 2016, (c4 + 1) * 2016)
                            op("act", lambda e: e.activation(out=ET[:, csl], in_=negd[:, csl], func=AF.Exp, scale=float(slopes[h])), [negd], [ET])
                        for m in range(2):
                            dma("sp", QT[m][m * 64:(m + 1) * 64, :], qkA[h, m * 64:(m + 1) * 64, :], [qkA], [QT[m]])
                        dma("sp", KT[:, :], qkA[4 + h, :, :], [qkA], [KT])
                        dma("sp", V[:, :, :], vA[:, h * 128:(h + 1) * 128].rearrange("(t p) e -> p t e", p=128), [vA], [V])
                    else:
                        dma("sp", QT[0:96, :], qmT[h, :, :], [qmT], [QT])
                        dma("sp", KT[0:96, :], kmT[h, :, :], [kmT], [KT])
                        dma("sp", V[:, :, :], vM[:, h * 128:(h + 1) * 128].rearrange("(t p) e -> p t e", p=128), [vM], [V])
                return ld

            def fin_diff(h, q0, st_):
                def fin_map(po, pss_):
                    rs, r2 = rsf.next(), rsf.next()
                    op("act", lambda e: e.activation(out=r2[:, :], in_=pss_[:, :], func=AF.Ln), [pss_], [r2])
                    op("act", lambda e: e.activation(out=rs[:, :], in_=r2[:, :], func=AF.Exp, scale=-1.0), [r2], [rs])
                    yield
                    op("dve", lambda e: e.tensor_tensor(out=r2[:, :], in0=rs[:, :], in1=pss_[:, :], op=ALU.mult), [rs, pss_], [r2])
                    op("dve", lambda e: e.tensor_scalar(out=r2[:, :], in0=r2[:, :], scalar1=-1.0, scalar2=2.0, op0=ALU.mult, op1=ALU.add), [r2], [r2])
                    yield
                    op("dve", lambda e: e.tensor_tensor(out=rs[:, :], in0=rs[:, :], in1=r2[:, :], op=ALU.mult), [rs, r2], [rs])
                    o_ = of.next()
                    op("dve", lambda e: e.tensor_tensor(out=o_[:, :], in0=po[:, :], in1=rs[:, :], op=ALU.mult), [po, rs], [o_])
                    st_.append(o_)
                    if len(st_) == 2:
                        yield
                        om = st_
                        oa = of.next()
                        op("dve", lambda e: e.scalar_tensor_tensor(out=oa[:, :], in0=om[1][:, :], scalar=neg_lam, in1=om[0][:, :], op0=ALU.mult, op1=ALU.add), [om[0], om[1], lw], [oa])
                        yield
                        sq_ = of.next()
                        op("act", lambda e: e.activation(out=sq_[:, :], in_=oa[:, :], func=AF.Square), [oa], [sq_])
                        yield
                        pssq = pscore.next()
                        op("pe", lambda e: e.matmul(pssq[:, :], lhsT=ones_f[:, :], rhs=sq_[:, :], start=True, stop=True), [ones_f, sq_], [pssq])
                        yield
                        rs2, tt = rsf.next(), rsf.next()
                        op("act", lambda e: e.activation(out=tt[:, :], in_=pssq[:, :], func=AF.Ln, bias=eps_t[:, 0:1], scale=1.0 / 128.0), [pssq, eps_t], [tt])
                        op("act", lambda e: e.activation(out=rs2[:, :], in_=tt[:, :], func=AF.Exp, scale=-0.5), [tt], [rs2])
                        yield
                        o_b = ob.next()
                        op("dve", lambda e: e.scalar_tensor_tensor(out=o_b[:, :], in0=oa[:, :], scalar=dg2[:, 0:1], in1=rs2[:, :], op0=ALU.mult, op1=ALU.mult), [oa, dg2, rs2], [o_b])
                        dma("pool", oT_d[h, :, q0:q0 + 512], o_b[:, :], [o_b], [oT_d])
                return fin_map

            def fin_mla(h, q0):
                def fin_map(po, pss_):
                    rs = rsf.next()
                    op("dve", lambda e: e.reciprocal(out=rs[:, :], in_=pss_[:, :]), [pss_], [rs])
                    yield
                    o_b = ob.next()
                    op("dve", lambda e: e.tensor_tensor(out=o_b[:, :], in0=po[:, :], in1=rs[:, :], op=ALU.mult), [po, rs], [o_b])
                    dma("pool", oT_d[4 + h, :, q0:q0 + 512], o_b[:, :], [o_b], [oT_d])
                return fin_map

            cur_et = {}
            SKIP_THR = {0: 512, 1: 2048}
            for h in range(4):
                QT, KT, V = (QZ[0].next(), QZ[1].next()), KTs.next(), Vs.next()
                cur_et[h] = ETs.next()
                first_of_head = True
                for qb in range(8):
                    st_ = []
                    fin = fin_diff(h, qb * 512, st_)
                    kbs = []
                    for kb in range(NT):
                        md = max(0, kb * 128 - (qb * 512 + 511), qb * 512 - (kb * 128 + 127))
                        if h in SKIP_THR and md >= SKIP_THR[h]:
                            continue
                        kbs.append(kb)
                    for m in range(2):
                        for kb in kbs:
                            jobs.append(dict(QT=QT[m], KT=KT, V=V, r0=0, kr=128, q0=qb * 512, kb=kb, slope=slopes[h], ET=cur_et[h], first=(kb == kbs[0]), last=(kb == kbs[-1]), fin=fin,
                                             pre=(mk_loader("diff", h, QT, KT, V) if first_of_head else None)))
                            first_of_head = False
            for h in range(4):
                QT, KT, V = QTs.next(), KTs.next(), Vs.next()
                first_of_head = True
                for qb in range(8):
                    fin = fin_mla(h, qb * 512)
                    for kb in range(NT):
                        jobs.append(dict(QT=QT, KT=KT, V=V, r0=0, kr=96, q0=qb * 512, kb=kb, slope=None, first=(kb == 0), last=(kb == NT - 1), fin=fin,
                                         pre=(mk_loader("mla", h, QT, KT, V) if first_of_head else None)))
                        first_of_head = False

            PREF = 150
            first_seen = False
            for idx_, j_ in enumerate(jobs):
                if j_["pre"] is not None:
                    if first_seen:
                        tgt = max(0, idx_ - PREF)
                        ld_ = j_["pre"]
                        j_["pre"] = None
                        prev = jobs[tgt].get("pre2")
                        jobs[tgt]["pre2"] = ld_ if prev is None else (lambda a=prev, b=ld_: (a(), b()))
                    first_seen = True

            def stage1(j):
                if j["pre"] is not None:
                    j["pre"]()
                if j.get("pre2") is not None:
                    j["pre2"]()
                QT, KT, r0, kr, q0, kb = j["QT"], j["KT"], j["r0"], j["kr"], j["q0"], j["kb"]
                ps = pscore.next()
                op("pe", lambda e: e.matmul(ps[:, :], lhsT=KT[r0:r0 + kr, kb * 128:(kb + 1) * 128], rhs=QT[r0:r0 + kr, q0:q0 + 512], start=True, stop=True), [KT, QT], [ps])
                E = es.next()
                if j["slope"] is not None:
                    tmp = tmps.next()
                    n0 = q0 - kb * 128 + NEGD_C
                    ET = j["ET"]
                    op("act", lambda e: e.activation(out=tmp[:, :], in_=ps[:, :], func=AF.Exp), [ps], [tmp])
                    op("dve", lambda e: e.tensor_tensor(out=E[:, :], in0=tmp[:, :], in1=ET[:, n0:n0 + 512], op=ALU.mult), [tmp, ET], [E])
                else:
                    op("act", lambda e: e.activation(out=E[:, :], in_=ps[:, :], func=AF.Exp), [ps], [E])
                j["E"] = E

            cur = [None]

            def stage2(j):
                if j["first"]:
                    cur[0] = pacc.next()
                po, pss_ = cur[0]
                V, kb, E = j["V"], j["kb"], j["E"]
                op("pe", lambda e: e.matmul(po[:, :], lhsT=V[:, kb, :], rhs=E[:, :], start=j["first"], stop=j["last"]), [V, E], [po])
                op("pe", lambda e: e.matmul(pss_[:, :], lhsT=ones_bf[:, :], rhs=E[:, :], start=j["first"], stop=j["last"]), [ones_bf, E], [pss_])
                if j["last"]:
                    deferred.append([FIN_DELAY, j["fin"](po, pss_)])

            deferred = []
            FIN_DELAY = 4
            FIN_STEP = 2

            def run_deferred(force=False):
                for d_ in list(deferred):
                    d_[0] -= 1
                    while d_[0] <= 0 or force:
                        try:
                            next(d_[1])
                            d_[0] = FIN_STEP
                        except StopIteration:
                            deferred.remove(d_)
                            break
                        if not force:
                            break

            for idx in range(len(jobs) + LA):
                run_deferred()
                if idx % 50 == 10:
                    issue_cast()
                if idx < len(jobs):
                    stage1(jobs[idx])
                if idx >= LA:
                    stage2(jobs[idx - LA])
            while deferred:
                run_deferred(force=True)
            issue_cast(len(cast_list))
            fw.barrier()
        if upto == "B":
            return nc, ext, out_d, dump_bufs

        LOG = fw.sb("LOG", [128, NT, 36], F32)

        def ln_stage1(y, stt, junk):
            op("act", lambda e: e.activation(out=junk[:, :], in_=y[:, :], func=AF.Identity, accum_out=stt[:, 0:1]), [y], [junk, stt])
            op("act", lambda e: e.activation(out=junk[:, :], in_=y[:, :], func=AF.Square, accum_out=stt[:, 1:2]), [y], [junk, stt])

        def ln_stage2(y, g_t, b_t, stt, out_t, junk):
            ln_front(y, stt, junk)
            ln_back(g_t, b_t, out_t, junk)

        def ln_back(g_t, b_t, out_t, junk):
            op("dve", lambda e: e.tensor_tensor(out=junk[:, :], in0=junk[:, :], in1=g_t[:, :], op=ALU.mult), [junk, g_t], [junk])
            op("pool", lambda e: e.tensor_tensor(out=out_t[:, :], in0=junk[:, :], in1=b_t[:, :], op=ALU.add), [junk, b_t], [out_t])

        def ln_f1(stt):
            op("dve", lambda e: e.tensor_scalar_mul(out=stt[:, 2:3], in0=stt[:, 0:1], scalar1=1.0 / D), [stt], [stt])
            op("dve", lambda e: e.tensor_tensor(out=stt[:, 3:4], in0=stt[:, 2:3], in1=stt[:, 2:3], op=ALU.mult), [stt], [stt])
            op("dve", lambda e: e.scalar_tensor_tensor(out=stt[:, 4:5], in0=stt[:, 1:2], scalar=1.0 / D, in1=stt[:, 3:4], op0=ALU.mult, op1=ALU.subtract), [stt], [stt])

        def ln_f2(stt):
            op("act", lambda e: e.activation(out=stt[:, 5:6], in_=stt[:, 4:5], func=AF.Sqrt, bias=eps_t[:, 0:1], scale=1.0), [stt, eps_t], [stt])

        def ln_f3(stt):
            op("dve", lambda e: e.reciprocal(out=stt[:, 6:7], in_=stt[:, 5:6]), [stt], [stt])
            op("dve", lambda e: e.scalar_tensor_tensor(out=stt[:, 7:8], in0=stt[:, 2:3], scalar=-1.0, in1=stt[:, 6:7], op0=ALU.mult, op1=ALU.mult), [stt], [stt])

        def ln_f4(y, stt, junk):
            op("act", lambda e: e.activation(out=junk[:, :], in_=y[:, :], func=AF.Identity, bias=stt[:, 7:8], scale=stt[:, 6:7]), [y, stt], [junk])

        def ln_front(y, stt, junk):
            ln_f1(stt)
            ln_f2(stt)
            ln_f3(stt)
            ln_f4(y, stt, junk)

        def phase_C(layer, w_out_dram, xsrc):
            with ExitStack() as ph:
                oT = fw.sb("c_oT", [128, 8, S], BF16, ph, multi=True)
                for c in range(8):
                    dma("sp", oT[:, c, :], oT_d[c, :, :], [oT_d], [oT])
                Wo = fw.sb("c_Wo", [128, 8, D], BF16, ph, multi=True)
                for c in range(8):
                    dma("pool", Wo[:, c, :], w_out_dram[c * 128:(c + 1) * 128, :], [w_out_dram], [Wo])
                Wr = fw.sb("c_Wr", [128, 8, 36], F32, ph)
                dma("sp", Wr[:], w_router[layer].rearrange("(c p) n -> p c n", p=128), [w_router], [Wr])
                g_t = fw.sb("c_g", [128, D], F32, ph)
                b_t = fw.sb("c_b", [128, D], F32, ph)
                dma("sp", g_t[:], ln_mix_g[layer:layer + 1, :].broadcast_to([128, D]), [ln_mix_g], [g_t])
                dma("sp", b_t[:], ln_mix_b[layer:layer + 1, :].broadcast_to([128, D]), [ln_mix_b], [b_t])
                brt = fw.sb("c_brt", [128, 36], F32, ph)
                dma("sp", brt[:], b_router[layer:layer + 1, :].broadcast_to([128, 36]), [b_router], [brt])
                zr = fw.sb("c_zr", [1, D], BF16, ph)
                op("dve", lambda e: e.memset(zr[:], 0.0), [], [zr])
                dma("sp", x1b_d[S:S + 1, :], zr[:], [zr], [x1b_d])
                xts = Rot([fw.sb(f"c_x{i}", [128, D], F32, ph) for i in range(3)])
                ys = Rot([fw.sb(f"c_y{i}", [128, D], F32, ph) for i in range(4)])
                junks = Rot([fw.sb(f"c_j{i}", [128, D], F32, ph) for i in range(4)])
                x1s = Rot([fw.sb(f"c_x1{i}", [128, D], F32, ph) for i in range(4)])
                x1bs = Rot([fw.sb(f"c_x1b{i}", [128, D], BF16, ph) for i in range(2)])
                x1Ts = Rot([fw.sb(f"c_x1T{i}", [128, 8, 128], F32, ph) for i in range(2)])
                stts = Rot([fw.sb(f"c_st{i}", [128, 8], F32, ph) for i in range(4)])
                pmm = Rot(psb[0:4])
                ptr = Rot(psb[4:6])
                prt = Rot(psb[6:8])
                stA, x1t_of, b2s = {}, {}, {}
                for i in range(-2, NT + 1):
                    ia, ib, ic = i + 2, i + 1, i - 1
                    if 0 <= ic < NT:
                        x1t = x1t_of.pop(ic)
                        x1T = x1Ts.next()
                        pst2 = [ptr.next(), ptr.next()]
                        for hf in range(2):
                            for c4 in range(4):
                                c = hf * 4 + c4
                                op("pe", lambda e: e.transpose(out=pst2[hf][:, c4 * 128:(c4 + 1) * 128], in_=x1t[:, c * 128:(c + 1) * 128], identity=ident[:]), [x1t, ident], [pst2[hf]])
                    if 0 <= ia < NT:
                        isl = slice(ia * 128, (ia + 1) * 128)
                        xt = xts.next()
                        dma("sp", xt[:], xsrc[isl, :], [xsrc], [xt])
                        y = ys.next()
                        pmm2 = [pmm.next(), pmm.next()]
                        for n in range(2):
                            for c in range(8):
                                op("pe", lambda e: e.matmul(pmm2[n][:, :], lhsT=oT[:, c, isl], rhs=Wo[:, c, n * 512:(n + 1) * 512], start=(c == 0), stop=(c == 7)), [oT, Wo], [pmm2[n]])
                    if 0 <= ib < NT:
                        yb, junkb, sttb = stA[ib]
                        ln_f1(sttb)
                        ln_f2(sttb)
                    if 0 <= ic < NT:
                        for hf in range(2):
                            o_ap = x1T[:, hf * 4:(hf + 1) * 4, :]
                            i_ap = pst2[hf][:, :].rearrange("p (c n) -> p c n", c=4)
                            if hf == 0:
                                op("act", lambda e: e.copy(out=o_ap, in_=i_ap), [pst2[hf]], [x1T])
                            else:
                                op("dve", lambda e: e.tensor_copy(out=o_ap, in_=i_ap), [pst2[hf]], [x1T])
                    if 0 <= ib < NT:
                        ln_f3(sttb)
                    if 0 <= ic < NT:
                        psr_ = prt.next()
                        for c in range(8):
                            op("pe", lambda e: e.matmul(psr_[:, 0:36], lhsT=x1T[:, c, :], rhs=Wr[:, c, :], start=(c == 0), stop=(c == 7)), [x1T, Wr], [psr_])
                    if 0 <= ib < NT:
                        ln_f4(yb, sttb, junkb)
                    if 0 <= ia < NT:
                        for n in range(2):
                            op("dve", lambda e: e.scalar_tensor_tensor(out=y[:, n * 512:(n + 1) * 512], in0=xt[:, n * 512:(n + 1) * 512], scalar=float(ALPHA), in1=pmm2[n][:, :], op0=ALU.mult, op1=ALU.add), [xt, pmm2[n]], [y])
                        junk, stt = junks.next(), stts.next()
                        ln_stage1(y, stt, junk)
                        stA[ia] = (y, junk, stt)
                    if 0 <= ib < NT:
                        isl = slice(ib * 128, (ib + 1) * 128)
                        x1n = x1s.next()
                        ln_back(g_t, b_t, x1n, junkb)
                        dma("pool", x1_d[isl, :], x1n[:, :], [x1n], [x1_d])
                        x1b = x1bs.next()
                        op("act", lambda e: e.copy(out=x1b[:, :], in_=x1n[:, :]), [x1n], [x1b])
                        dma("pool", x1b_d[isl, :], x1b[:, :], [x1b], [x1b_d])
                        x1t_of[ib] = x1n
                        stA.pop(ib)
                    if 0 <= ic < NT:
                        op("dve", lambda e: e.tensor_tensor(out=LOG[:, ic, :], in0=psr_[:, 0:36], in1=brt[:, :], op=ALU.add), [psr_, brt], [LOG])
                fw.barrier()

        def phase_M(layer, dst):
            with ExitStack() as ph:
                bs = ExitStack()
                tstack = [ph]

                def T(name, shape, dt=F32):
                    return fw.sb("m_" + name, shape, dt, tstack[0])
                mc = T("mc", [128, 4, 64])
                dma("sp", mc[:], moe_c.ap(), [moe_c], [mc])
                tokid = T("tokid", [128, NT])
                dma("sp", tokid[:], tokid_d.ap(), [tokid_d], [tokid])
                triu = T("triu", [128, 128])
                dma("sp", triu[:], triu_d.ap(), [triu_d], [triu])
                triu_b = T("triu_b", [128, 128], BF16)
                op("dve", lambda e: e.tensor_copy(out=triu_b[:, :], in_=triu[:, :]), [triu], [triu_b])
                coarse = LOG[:, :, 0:4]
                fine4 = LOG[:, :, 4:36].rearrange("p j (g i) -> p j g i", g=4)
                gmax = T("gmax", [128, NT])
                op("dve", lambda e: e.tensor_reduce(out=gmax[:, :], in_=coarse, axis=AX.X, op=ALU.max), [LOG], [gmax])
                ohg = T("ohg", [128, NT, 4])
                op("dve", lambda e: e.tensor_tensor(out=ohg[:, :, :], in0=coarse, in1=gmax[:, :].unsqueeze(2).to_broadcast([128, NT, 4]), op=ALU.is_equal), [LOG, gmax], [ohg])
                ex = T("ex", [128, NT, 4])
                op("dve", lambda e: e.tensor_tensor(out=ex[:, :, :], in0=coarse, in1=gmax[:, :].unsqueeze(2).to_broadcast([128, NT, 4]), op=ALU.subtract), [LOG, gmax], [ex])
                op("act", lambda e: e.activation(out=ex[:, :, :], in_=ex[:, :, :], func=AF.Exp), [ex], [ex])
                pg = T("pg", [128, NT])
                op("dve", lambda e: e.reduce_sum(out=pg[:, :], in_=ex[:, :, :], axis=AX.X), [ex], [pg])
                op("dve", lambda e: e.reciprocal(out=pg[:, :], in_=pg[:, :]), [pg], [pg])
                t48 = T("t48", [128, NT, 4, 8])
                op("dve", lambda e: e.tensor_tensor(out=t48[:, :, :, :], in0=fine4, in1=ohg[:, :, :].unsqueeze(3).to_broadcast([128, NT, 4, 8]), op=ALU.mult), [LOG, ohg], [t48])
                fsel = T("fsel", [128, NT, 8])
                op("dve", lambda e: e.reduce_sum(out=fsel[:, :, :], in_=t48[:, :, :, :].rearrange("p j g i -> p j i g"), axis=AX.X), [t48], [fsel])
                v1 = T("v1", [128, NT])
                op("dve", lambda e: e.tensor_reduce(out=v1[:, :], in_=fsel[:, :, :], axis=AX.X, op=ALU.max), [fsel], [v1])
                oh1 = T("oh1", [128, NT, 8])
                op("dve", lambda e: e.tensor_tensor(out=oh1[:, :, :], in0=fsel[:, :, :], in1=v1[:, :].unsqueeze(2).to_broadcast([128, NT, 8]), op=ALU.is_equal), [fsel, v1], [oh1])
                msk = T("msk", [128, NT, 8])
                op("dve", lambda e: e.scalar_tensor_tensor(out=msk[:, :, :], in0=oh1[:, :, :], scalar=-1.0e30, in1=fsel[:, :, :], op0=ALU.mult, op1=ALU.add), [oh1, fsel], [msk])
                v2 = T("v2", [128, NT])
                op("dve", lambda e: e.tensor_reduce(out=v2[:, :], in_=msk[:, :, :], axis=AX.X, op=ALU.max), [msk], [v2])
                oh2 = T("oh2", [128, NT, 8])
                op("dve", lambda e: e.tensor_tensor(out=oh2[:, :, :], in0=msk[:, :, :], in1=v2[:, :].unsqueeze(2).to_broadcast([128, NT, 8]), op=ALU.is_equal), [msk, v2], [oh2])
                ed = T("ed", [128, NT])
                op("dve", lambda e: e.tensor_tensor(out=ed[:, :], in0=v2[:, :], in1=v1[:, :], op=ALU.subtract), [v1, v2], [ed])
                op("act", lambda e: e.activation(out=ed[:, :], in_=ed[:, :], func=AF.Exp), [ed], [ed])
                w1 = T("w1", [128, NT])
                op("dve", lambda e: e.tensor_scalar_add(out=w1[:, :], in0=ed[:, :], scalar1=1.0), [ed], [w1])
                op("dve", lambda e: e.reciprocal(out=w1[:, :], in_=w1[:, :]), [w1], [w1])
                gates = T("gates", [128, 2, NT])
                op("dve", lambda e: e.tensor_tensor(out=gates[:, 0, :], in0=w1[:, :], in1=pg[:, :], op=ALU.mult), [w1, pg], [gates])
                op("dve", lambda e: e.tensor_tensor(out=w1[:, :], in0=w1[:, :], in1=ed[:, :], op=ALU.mult), [w1, ed], [w1])
                op("dve", lambda e: e.tensor_tensor(out=gates[:, 1, :], in0=w1[:, :], in1=pg[:, :], op=ALU.mult), [w1, pg], [gates])
                Ak = [T(f"A{k}", [128, NT, 4, 8]) for k in range(2)]
                for k, ohk in enumerate((oh1, oh2)):
                    op("dve", lambda e: e.tensor_tensor(out=Ak[k][:, :, :, :], in0=ohg[:, :, :].unsqueeze(3).to_broadcast([128, NT, 4, 8]), in1=ohk[:, :, :].unsqueeze(2).to_broadcast([128, NT, 4, 8]), op=ALU.mult), [ohg, ohk], [Ak[k]])
                A_bf = T("A_bf", [128, NT * 32], BF16)
                op("dve", lambda e: e.tensor_tensor(out=A_bf[:, :], in0=Ak[0][:, :, :, :].rearrange("p j g i -> p (j g i)"), in1=Ak[1][:, :, :, :].rearrange("p j g i -> p (j g i)"), op=ALU.add), [Ak[0], Ak[1]], [A_bf])
                sa = T("sa", [128, NT, 32])
                sb_ = T("sb", [128, NT, 32])
                tots = T("tots", [128, NT, 32])
                rank = T("rank", [128, NT, 32])
                for hf in range(2):
                    ps = psb[hf]
                    op("pe", lambda e: e.matmul(ps[:, :], lhsT=ones_bf[:, :], rhs=A_bf[:, hf * 512:(hf + 1) * 512], start=True, stop=True), [ones_bf, A_bf], [ps])
                    op("dve", lambda e: e.tensor_copy(out=tots[:, hf * 16:(hf + 1) * 16, :], in_=ps[:, :].rearrange("p (j e) -> p j e", e=32)), [ps], [tots])
                    ps2 = psb[2 + hf]
                    op("pe", lambda e: e.matmul(ps2[:, :], lhsT=triu_b[:, :], rhs=A_bf[:, hf * 512:(hf + 1) * 512], start=True, stop=True), [triu_b, A_bf], [ps2])
                    op("dve", lambda e: e.tensor_copy(out=rank[:, hf * 16:(hf + 1) * 16, :], in_=ps2[:, :].rearrange("p (j e) -> p j e", e=32)), [ps2], [rank])
                op("dve", lambda e: e.tensor_copy(out=sa[:, :, :], in_=tots[:, :, :]), [tots], [sa])
                a_, b_ = sa, sb_
                for s_ in (1, 2, 4, 8, 16):
                    op("dve", lambda e: e.tensor_tensor(out=b_[:, s_:, :], in0=a_[:, s_:, :], in1=a_[:, :NT - s_, :], op=ALU.add), [a_], [b_])
                    op("dve", lambda e: e.tensor_copy(out=b_[:, :s_, :], in_=a_[:, :s_, :]), [a_], [b_])
                    a_, b_ = b_, a_
                inc = a_
                cnt = T("cnt", [128, 32])
                op("dve", lambda e: e.tensor_copy(out=cnt[:, :], in_=inc[:, NT - 1, :]), [inc], [cnt])
                op("dve", lambda e: e.tensor_tensor(out=rank[:, :, :], in0=rank[:, :, :], in1=inc[:, :, :], op=ALU.add), [rank, inc], [rank])
                op("dve", lambda e: e.tensor_tensor(out=rank[:, :, :], in0=rank[:, :, :], in1=tots[:, :, :], op=ALU.subtract), [rank, tots], [rank])
                cmp_ = T("cmp", [128, 64, 32])
                op("dve", lambda e: e.tensor_tensor(out=cmp_[:, 0:32, :], in0=cnt[:, :].unsqueeze(2).to_broadcast([128, 32, 32]), in1=mc[:, 1, 0:32].unsqueeze(1).to_broadcast([128, 32, 32]), op=ALU.is_gt), [cnt, mc], [cmp_])
                nblk = T("nblk", [128, 32])
                op("dve", lambda e: e.reduce_sum(out=nblk[:, :], in_=cmp_[:, 0:32, :], axis=AX.X), [cmp_], [nblk])
                na = T("na", [128, 32])
                nb_ = T("nb", [128, 32])
                op("dve", lambda e: e.tensor_copy(out=na[:, :], in_=nblk[:, :]), [nblk], [na])
                a_, b_ = na, nb_
                for s_ in (1, 2, 4, 8, 16):
                    op("dve", lambda e: e.tensor_tensor(out=b_[:, s_:], in0=a_[:, s_:], in1=a_[:, :32 - s_], op=ALU.add), [a_], [b_])
                    op("dve", lambda e: e.tensor_copy(out=b_[:, :s_], in_=a_[:, :s_]), [a_], [b_])
                    a_, b_ = b_, a_
                pend = T("pend", [128, 32])
                pstart = T("pstart", [128, 32])
                op("dve", lambda e: e.tensor_scalar_mul(out=pend[:, :], in0=a_[:, :], scalar1=float(BLK)), [a_], [pend])
                op("dve", lambda e: e.scalar_tensor_tensor(out=pstart[:, :], in0=nblk[:, :], scalar=-float(BLK), in1=pend[:, :], op0=ALU.mult, op1=ALU.add), [nblk, pend], [pstart])
                op("dve", lambda e: e.tensor_tensor(out=rank[:, :, :], in0=rank[:, :, :], in1=pstart[:, :].unsqueeze(1).to_broadcast([128, NT, 32]), op=ALU.add), [rank, pstart], [rank])
                dest = T("dest", [128, 2, NT])
                for k in range(2):
                    op("dve", lambda e: e.tensor_tensor(out=sa[:, :, :], in0=Ak[k][:, :, :, :].rearrange("p j g i -> p j (g i)"), in1=rank[:, :, :], op=ALU.mult), [Ak[k], rank], [sa])
                    op("dve", lambda e: e.reduce_sum(out=dest[:, k, :], in_=sa[:, :, :], axis=AX.X), [sa], [dest])
                dest_i = T("dest_i", [128, 2, NT], I32)
                op("dve", lambda e: e.tensor_copy(out=dest_i[:, :, :], in_=dest[:, :, :]), [dest], [dest_i])
                op("dve", lambda e: e.tensor_tensor(out=cmp_[:, 0:NB, :], in0=pend[:, :].unsqueeze(1).to_broadcast([128, NB, 32]), in1=mc[:, 2, 0:NB].unsqueeze(2).to_broadcast([128, NB, 32]), op=ALU.is_le), [pend, mc], [cmp_])
                beid = T("beid", [128, 64])
                op("dve", lambda e: e.reduce_sum(out=beid[:, 0:NB], in_=cmp_[:, 0:NB, :], axis=AX.X), [cmp_], [beid])
                op("dve", lambda e: e.tensor_scalar_min(out=beid[:, 0:NB], in0=beid[:, 0:NB], scalar1=31.0), [beid], [beid])
                op("dve", lambda e: e.tensor_scalar(out=beid[:, 0:NB], in0=beid[:, 0:NB], scalar1=128.0, scalar2=float(layer * E_ * 128), op0=ALU.mult, op1=ALU.add), [beid], [beid])
                op("dve", lambda e: e.tensor_tensor(out=beid[:, 0:NB], in0=beid[:, 0:NB], in1=mc[:, 3, 0:1].to_broadcast([128, NB]), op=ALU.add), [beid, mc], [beid])
                widx = T("widx", [128, 64], I32)
                op("dve", lambda e: e.tensor_copy(out=widx[:, 0:NB], in_=beid[:, 0:NB]), [beid], [widx])
                NA = NSLOT // 128 + 1
                padrec = T("padrec", [128, NA, 4])
                op("dve", lambda e: e.memset(padrec[:, :, :], 0.0), [], [padrec])
                op("dve", lambda e: e.memset(padrec[:, :, 0:1], float(S)), [], [padrec])
                op("dve", lambda e: e.tensor_scalar_add(out=padrec[:, :, 2], in0=mc[:, 3, 0:1].to_broadcast([128, NA]), scalar1=float(2 * S)), [mc], [padrec])
                dma("sp", slot_d.ap().rearrange("(a p) c -> p a c", p=128), padrec[:, :, :], [padrec], [slot_d])
                rec = T("rec", [128, 2, NT, 4])
                op("dve", lambda e: e.memset(rec[:, :, :, :], 0.0), [], [rec])
                for k in range(2):
                    op("dve", lambda e: e.tensor_copy(out=rec[:, k, :, 0], in_=tokid[:, :]), [tokid], [rec])
                    op("dve", lambda e: e.tensor_copy(out=rec[:, k, :, 1], in_=gates[:, k, :]), [gates], [rec])
                    op("dve", lambda e: e.tensor_scalar_add(out=rec[:, k, :, 2], in0=tokid[:, :], scalar1=float(k * S)), [tokid], [rec])
                sc_bufs = []
                for k in range(2):
                    for j in range(NT):
                        sc_b = Buf(None, "slotscatter")
                        sc_bufs.append(sc_b)
                        op("pool", lambda e: e.indirect_dma_start(out=slot_d[:, :], out_offset=bass.IndirectOffsetOnAxis(ap=dest_i[:, k, j:j + 1], axis=0), in_=rec[:, k, j, :], in_offset=None), [rec, dest_i, slot_d], [sc_b], dma=True)
                SL = T("SL", [128, NA - 1, 4])
                dma("sp", SL[:, :, :], slot_d[0:NSLOT, :].rearrange("(a p) c -> p a c", p=128), [slot_d] + sc_bufs, [SL])
                tok_i = T("tok_i", [128, NA - 1], I32)
                row_i = T("row_i", [128, NA - 1], I32)
                gate_s = T("gate_s", [128, NA - 1])
                op("dve", lambda e: e.tensor_copy(out=tok_i[:, :], in_=SL[:, :, 0]), [SL], [tok_i])
                op("dve", lambda e: e.tensor_copy(out=row_i[:, :], in_=SL[:, :, 2]), [SL], [row_i])
                op("dve", lambda e: e.tensor_copy(out=gate_s[:, :], in_=SL[:, :, 1]), [SL], [gate_s])
                if "moe_dbg" in dumps:
                    dump("m_dest", dest[:, :, :], [128, 2, NT], F32, [dest])
                    dump("m_gates", gates[:, :, :], [128, 2, NT], F32, [gates])
                    dump("m_beid", beid[:, :], [128, 64], F32, [beid])
                    dump("m_SL", SL[:, :, :], [128, NA - 1, 4], F32, [SL])
                    dump("m_cnt", cnt[:, :], [128, 32], F32, [cnt])
                tstack[0] = bs
                Wgs = Rot([T(f"Wg{i}", [128, 8 * HID], BF16) for i in range(2)])
                Wus = Rot([T(f"Wu{i}", [128, 8 * HID], BF16) for i in range(2)])
                Wds = Rot([T(f"Wd{i}", [128, 4 * D], BF16) for i in range(2)])
                xgs = Rot([T(f"xg{i}", [128, D], BF16) for i in range(4)])
                xgTs = Rot([T(f"xgT{i}", [128, 8, BLK], BF16) for i in range(2)])
                acts = Rot([T(f"act{i}", [128, 4, BLK], BF16) for i in range(2)])
                sgs = Rot([T(f"sg{i}", [128, BLK], F32) for i in range(3)])
                ysbs = Rot([T(f"ysb{i}", [128, D], F32) for i in range(3)])
                identb = T("identb", [128, 128], BF16)
                op("dve", lambda e: e.tensor_copy(out=identb[:, :], in_=ident[:, :]), [ident], [identb])
                ptr = Rot(psb[0:2])
                pgu = Rot(psb[2:6])
                pyy = Rot(psb[6:8])
                def gathers(b):
                    Wg, Wu, Wd = Wgs.next(), Wus.next(), Wds.next()
                    for Wt, src in ((Wg, wgb), (Wu, wub), (Wd, wdb)):
                        op("pool", lambda e: e.indirect_dma_start(out=Wt[:, :], out_offset=None, in_=src[:, :], in_offset=bass.IndirectOffsetOnAxis(ap=widx[:, b:b + 1], axis=0)), [src, widx], [Wt], dma=True)
                    xg2 = []
                    for hf in range(2):
                        a = 2 * b + hf
                        xg = xgs.next()
                        op("pool", lambda e: e.indirect_dma_start(out=xg[:, :], out_offset=None, in_=x1b_d[:, :], in_offset=bass.IndirectOffsetOnAxis(ap=tok_i[:, a:a + 1], axis=0)), [x1b_d, tok_i], [xg], dma=True)
                        xg2.append(xg)
                    return Wg, Wu, Wd, xg2

                nxt = gathers(0)
                for b in range(NB):
                    Wg, Wu, Wd, xg2 = nxt
                    if b + 1 < NB:
                        nxt = gathers(b + 1)
                    xgT = xgTs.next()
                    for hf in range(2):
                        xg = xg2[hf]
                        ps = ptr.next()
                        psv = ps[:, :].bitcast(BF16)
                        for c in range(8):
                            op("pe", lambda e: e.transpose(out=psv[:, c * 128:(c + 1) * 128], in_=xg[:, c * 128:(c + 1) * 128], identity=identb[:]), [xg, identb], [ps])
                        o_ap = xgT[:, :, hf * 128:(hf + 1) * 128]
                        i_ap = psv.rearrange("p (c n) -> p c n", c=8)
                        if hf == 0:
                            op("act", lambda e: e.copy(out=o_ap, in_=i_ap), [ps], [xgT])
                        else:
                            op("dve", lambda e: e.tensor_copy(out=o_ap, in_=i_ap), [ps], [xgT])
                    act_ = acts.next()
                    for m in range(4):
                        pg_, pu_ = pgu.next(), pgu.next()
                        for c in range(8):
                            op("pe", lambda e: e.matmul(pg_[:, 0:BLK], lhsT=Wg[:, c * HID + m * 128:c * HID + (m + 1) * 128], rhs=xgT[:, c, :], start=(c == 0), stop=(c == 7)), [Wg, xgT], [pg_])
                        for c in range(8):
                            op("pe", lambda e: e.matmul(pu_[:, 0:BLK], lhsT=Wu[:, c * HID + m * 128:c * HID + (m + 1) * 128], rhs=xgT[:, c, :], start=(c == 0), stop=(c == 7)), [Wu, xgT], [pu_])
                        sg = sgs.next()
                        op("act", lambda e: e.activation(out=sg[:, :], in_=pg_[:, 0:BLK], func=AF.Silu), [pg_], [sg])
                        op("dve", lambda e: e.tensor_tensor(out=act_[:, m, :], in0=sg[:, :], in1=pu_[:, 0:BLK], op=ALU.mult), [sg, pu_], [act_])
                    for hf in range(2):
                        a = 2 * b + hf
                        ysb = ysbs.next()
                        for n in range(2):
                            py = pyy.next()
                            for m in range(4):
                                op("pe", lambda e: e.matmul(py[:, :], lhsT=act_[:, m, hf * 128:(hf + 1) * 128], rhs=Wd[:, m * D + n * 512:m * D + (n + 1) * 512], start=(m == 0), stop=(m == 3)), [act_, Wd], [py])
                            if n == 0:
                                op("act", lambda e: e.activation(out=ysb[:, 0:512], in_=py[:, :], func=AF.Copy, scale=gate_s[:, a:a + 1]), [py, gate_s], [ysb])
                            else:
                                op("dve", lambda e: e.tensor_scalar_mul(out=ysb[:, 512:1024], in0=py[:, :], scalar1=gate_s[:, a:a + 1]), [py, gate_s], [ysb])
                        op("pool", lambda e: e.indirect_dma_start(out=y_d[:, :], out_offset=bass.IndirectOffsetOnAxis(ap=row_i[:, a:a + 1], axis=0), in_=ysb[:, :], in_offset=None), [ysb, row_i], [y_d], dma=True)
                fw.barrier()
                bs.close()
                tstack[0] = ph
                g_t = T("g", [128, D])
                b_t = T("b", [128, D])
                dma("sp", g_t[:], ln_ffn_g[layer:layer + 1, :].broadcast_to([128, D]), [ln_ffn_g], [g_t])
                dma("sp", b_t[:], ln_ffn_b[layer:layer + 1, :].broadcast_to([128, D]), [ln_ffn_b], [b_t])
                xts = Rot([T(f"cx{i}", [128, D]) for i in range(4)])
                y0s = Rot([T(f"cy0{i}", [128, D]) for i in range(4)])
                y1s = Rot([T(f"cy1{i}", [128, D]) for i in range(4)])
                junks = Rot([T(f"cj{i}", [128, D]) for i in range(4)])
                outs = Rot([T(f"co{i}", [128, D]) for i in range(4)])
                stts = Rot([T(f"cst{i}", [128, 8]) for i in range(4)])
                def m_A(i):
                    isl = slice(i * 128, (i + 1) * 128)
                    xt, y0, y1 = xts.next(), y0s.next(), y1s.next()
                    dma("sp", xt[:], x1_d[isl, :], [x1_d], [xt])
                    dma("sp", y0[:], y_d[isl, :], [y_d], [y0])
                    dma("sp", y1[:], y_d[S + i * 128:S + (i + 1) * 128, :], [y_d], [y1])
                    op("pool", lambda e: e.tensor_tensor(out=y1[:, :], in0=y0[:, :], in1=y1[:, :], op=ALU.add), [y0, y1], [y1])
                    op("dve", lambda e: e.scalar_tensor_tensor(out=y0[:, :], in0=xt[:, :], scalar=float(ALPHA), in1=y1[:, :], op0=ALU.mult, op1=ALU.add), [xt, y1], [y0])
                    junk, stt = junks.next(), stts.next()
                    ln_stage1(y0, stt, junk)
                    return (y0, junk, stt)

                def m_B1f(i, st3):
                    y0, junk, stt = st3
                    ln_front(y0, stt, junk)

                def m_B1b(i, st3):
                    isl = slice(i * 128, (i + 1) * 128)
                    y0, junk, stt = st3
                    o_t = outs.next()
                    ln_back(g_t, b_t, o_t, junk)
                    dma("pool", dst[isl, :], o_t[:, :], [o_t], [dst])

                stA = {}
                for i in range(-2, NT):
                    if 0 <= i + 1 < NT:
                        m_B1f(i + 1, stA[i + 1])
                    if 0 <= i + 2 < NT:
                        stA[i + 2] = m_A(i + 2)
                    if 0 <= i + 1 < NT:
                        m_B1b(i + 1, stA.pop(i + 1))
                fw.barrier()

        phase_C(0, w_out_ab, x_in)
        if upto == "C0":
            return nc, ext, out_d, dump_bufs
        phase_M(0, x2_d)
        if upto == "M0":
            return nc, ext, out_d, dump_bufs
        PATS = (1, 4, 16)
        with ExitStack() as ph:
            xT = fw.sb("l1_xT", [128, 8, S], BF16, ph)
            Wc = fw.sb("l1_W", [128, 8, 3 * D], BF16, ph, multi=True)
            for c in range(8):
                dma("pool", Wc[:, c, :], w_in_c[c * 128:(c + 1) * 128, :], [w_in_c], [Wc])
            stg = Rot([fw.sb(f"l1_x{i}", [128, D], F32, ph) for i in range(2)])
            build_xT(x2_d, xT, Rot(psb[0:2]), stg)
            psr = Rot(psb[2:8])
            sbf = Rot([fw.sb(f"l1_sb{i}", [128, 512], BF16, ph) for i in range(4)])
            vst = Rot([fw.sb(f"l1_v{i}", [128, 16, 65], BF16, ph) for i in range(6)])
            for v_ in vst.bufs:
                op("dve", lambda e: e.memset(v_[:, :, :], 1.0), [], [v_])
            for tb in range(8):
                tsl = slice(tb * 512, (tb + 1) * 512)
                for ch in range(16):
                    ps = psr.next()
                    for c in range(8):
                        op("pe", lambda e: e.matmul(ps[:, :], lhsT=Wc[:, c, ch * 128:(ch + 1) * 128], rhs=xT[:, c, tsl], start=(c == 0), stop=(c == 7)), [Wc, xT], [ps])
                    s_ = sbf.next()
                    if ch < 8:
                        op("act", lambda e: e.activation(out=s_[:, :], in_=ps[:, :], func=AF.Copy, scale=0.125), [ps], [s_])
                    else:
                        op("dve", lambda e: e.tensor_copy(out=s_[:, :], in_=ps[:, :]), [ps], [s_])
                    dma("pool", qkC[ch, :, tsl], s_[:, :], [s_], [qkC])
            for ri, r in enumerate(PATS):
                nqb = S // r // 128
                for res in range(r):
                    for kt in range(nqb):
                        t = res * nqb + kt
                        tok = ssl(res + r * 128 * kt, 128, r)
                        v_ = vst.next()
                        for n in range(2):
                            ps = psr.next()
                            for c in range(8):
                                op("pe", lambda e: e.matmul(ps[:, :], lhsT=xT[:, c, tok], rhs=Wc[:, c, 2048 + n * 512:2048 + (n + 1) * 512], start=(c == 0), stop=(c == 7)), [xT, Wc], [ps])
                            o_ap = v_[:, n * 8:(n + 1) * 8, 0:64]
                            i_ap = ps[:, :].rearrange("p (h e) -> p h e", e=64)
                            if n == 0:
                                op("act", lambda e: e.copy(out=o_ap, in_=i_ap), [ps], [v_])
                            else:
                                op("dve", lambda e: e.tensor_copy(out=o_ap, in_=i_ap), [ps], [v_])
                        for g in range(4):
                            dma("sp" if g % 2 == 0 else "pool", vC[ri, g, :, t, :], v_[:, g * 4:(g + 1) * 4, :].rearrange("p h e -> p (h e)"), [v_], [vC])
            fw.barrier()
        if upto == "A1x":
            return nc, ext, out_d, dump_bufs

        with ExitStack() as ph:
            dng = fw.sb("d_negd", [128, 3, 512], F32, ph)
            dma("sp", dng[:], dil_negd_d.ap(), [dil_negd_d], [dng])
            sel = fw.sb("d_sel", [128, 64], F32, ph)
            dma("sp", sel[:], sel_d.ap(), [sel_d], [sel])
            QZ = [fw.sb(f"d_QZ{par}", [128, 2, S], BF16, ph) for par in range(2)]
            for par in range(2):
                op("pool", lambda e: e.memset(QZ[par][:, :, :], 0.0), [], [QZ[par]])
            KT = fw.sb("d_KT", [128, 2, S], BF16, ph, multi=True)
            Vr = [fw.sb(f"d_V{ri}", [128, NT, 260], BF16, ph) for ri in range(3)]
            OaccL = [fw.sb(f"d_O{i}", [65, S], F32, ph) for i in range(4)]
            tmps = Rot([fw.sb(f"d_t{i}", [128, 512], F32, ph) for i in range(4)])
            Es = Rot([fw.sb(f"d_e{i}", [128, 512], BF16, ph) for i in range(8)])
            rcp = Rot([fw.sb(f"d_r{i}", [64, 512], F32, ph) for i in range(4)])
            obs = Rot([fw.sb(f"d_ob{i}", [64, 512], BF16, ph) for i in range(2)])
            pscore = Rot(psb[0:6])
            pov = Rot(psb[6:8])
            dslopes = alibi_slopes(16)
            LAG = 1
            gjobs = []
            for g in range(4):
                for hl in range(4):
                    h = 4 * g + hl
                    for ri, r in enumerate(PATS):
                        nqb = S // r // 128
                        G = min(4, nqb)
                        for res in range(r):
                            for qg in range(0, nqb, G):
                                gjobs.append(dict(g=g, hl=hl, h=h, ri=ri, r=r, nqb=nqb, G=G, res=res, qg=qg, first_g=False, last_h=False))
                    gjobs[-1]["last_h"] = True
            seen_g = set()
            for j in gjobs:
                if j["g"] not in seen_g:
                    seen_g.add(j["g"])
                    j["first_g"] = True

            def load_group(g):
                for cl in range(2):
                    for par in range(2):
                        dma("sp", QZ[par][par * 64:(par + 1) * 64, cl, :], qkC[2 * g + cl, par * 64:(par + 1) * 64, :], [qkC], [QZ[par]])
                    dma("sp", KT[:, cl, :], qkC[8 + 2 * g + cl, :, :], [qkC], [KT])
                for ri in range(3):
                    dma("sp", Vr[ri][:, :, :], vC[ri, g, :, :, :], [vC], [Vr[ri]])

            def d_stage1(j):
                if j["first_g"]:
                    load_group(j["g"])
                hl, h, r, nqb, G, res, qg = j["hl"], j["h"], j["r"], j["nqb"], j["G"], j["res"], j["qg"]
                cl, r0 = hl // 2, (hl % 2) * 64
                blocks = list(range(qg, qg + G))
                Et, rng = [], []
                for typ in range(3):
                    ps = pscore.next()
                    val = [il for il, i in enumerate(blocks) if 0 <= i + typ - 1 < nqb]
                    lo, hi = val[0], val[-1] + 1
                    for il in val:
                        i = blocks[il]
                        kt = i + typ - 1
                        ksl = ssl(res + r * 128 * kt, 128, r)
                        qsl = ssl(res + r * 128 * i, 128, r)
                        op("pe", lambda e: e.matmul(ps[:, il * 128:(il + 1) * 128], lhsT=KT[:, cl, ksl], rhs=QZ[hl % 2][:, cl, qsl], start=True, stop=True), [KT, QZ[hl % 2]], [ps])
                    tmp, E = tmps.next(), Es.next()
                    op("dve", lambda e: e.scalar_tensor_tensor(out=tmp[:, lo * 128:hi * 128], in0=dng[:, typ, lo * 128:hi * 128], scalar=float(dslopes[h] * r), in1=ps[:, lo * 128:hi * 128], op0=ALU.mult, op1=ALU.add), [dng, ps], [tmp])
                    op("act", lambda e: e.activation(out=E[:, lo * 128:hi * 128], in_=tmp[:, lo * 128:hi * 128], func=AF.Exp), [tmp], [E])
                    Et.append(E)
                    rng.append(val)
                j["Et"], j["rng"], j["blocks"] = Et, rng, blocks

            def d_stage2(j):
                hl, h, ri, r, nqb, G, res, qg = j["hl"], j["h"], j["ri"], j["r"], j["nqb"], j["G"], j["res"], j["qg"]
                r0 = (hl % 2) * 64
                Et, rng, blocks = j["Et"], j["rng"], j["blocks"]
                po = pov.next()
                for il, i in enumerate(blocks):
                    typs = [typ for typ in range(3) if il in rng[typ]]
                    for n_, typ in enumerate(typs):
                        kt = i + typ - 1
                        t = res * nqb + kt
                        op("pe", lambda e: e.matmul(po[0:65, il * 128:(il + 1) * 128], lhsT=Vr[ri][:, t, hl * 65:(hl + 1) * 65], rhs=Et[typ][:, il * 128:(il + 1) * 128], start=(n_ == 0), stop=(n_ == len(typs) - 1)), [Vr[ri], Et[typ]], [po])
                osl = ssl(res + r * 128 * qg, 128 * G, r)
                Oacc = OaccL[hl]
                if ri == 0:
                    op("act", lambda e: e.copy(out=Oacc[0:65, osl], in_=po[0:65, 0:G * 128]), [po], [Oacc])
                else:
                    op("dve", lambda e: e.tensor_tensor(out=Oacc[0:65, osl], in0=Oacc[0:65, osl], in1=po[0:65, 0:G * 128], op=ALU.add), [Oacc, po], [Oacc])
                if j["last_h"]:
                    for _ in norm_gen(hl, h, r0):
                        pass

            def norm_gen(hl, h, r0):
                    Oacc = OaccL[hl]
                    for tb in range(8):
                        tsl = slice(tb * 512, (tb + 1) * 512)
                        pn = pov.next()
                        op("pe", lambda e: e.matmul(pn[0:64, :], lhsT=sel[0:65, :], rhs=Oacc[0:65, tsl], start=True, stop=True), [sel, Oacc], [pn])
                        rc, rc2, o_b = rcp.next(), rcp.next(), obs.next()
                        op("act", lambda e: e.activation(out=rc2[:, :], in_=pn[0:64, :], func=AF.Ln), [pn], [rc2])
                        op("act", lambda e: e.activation(out=rc[:, :], in_=rc2[:, :], func=AF.Exp, scale=-1.0), [rc2], [rc])
                        op("dve", lambda e: e.tensor_tensor(out=rc2[:, :], in0=rc[:, :], in1=pn[0:64, :], op=ALU.mult), [rc, pn], [rc2])
                        op("dve", lambda e: e.tensor_scalar(out=rc2[:, :], in0=rc2[:, :], scalar1=-1.0, scalar2=2.0, op0=ALU.mult, op1=ALU.add), [rc2], [rc2])
                        op("dve", lambda e: e.tensor_tensor(out=rc[:, :], in0=rc[:, :], in1=rc2[:, :], op=ALU.mult), [rc, rc2], [rc])
                        op("dve", lambda e: e.tensor_tensor(out=o_b[:, :], in0=Oacc[0:64, tsl], in1=rc[:, :], op=ALU.mult), [Oacc, rc], [o_b])
                        dma("pool", oT_d[h // 2, r0:r0 + 64, tsl], o_b[:, :], [o_b], [oT_d])
                        yield

            dnorm = []

            def step_norm(drain=False):
                for gn in list(dnorm):
                    while True:
                        try:
                            next(gn)
                        except StopIteration:
                            dnorm.remove(gn)
                            break
                        if not drain:
                            break

            pend = []
            for j in gjobs:
                if j["first_g"]:
                    while pend:
                        d_stage2(pend.pop(0))
                d_stage1(j)
                pend.append(j)
                if len(pend) > LAG:
                    d_stage2(pend.pop(0))
                step_norm()
            while pend:
                d_stage2(pend.pop(0))
            step_norm(drain=True)
            fw.barrier()
        if upto == "B1":
            return nc, ext, out_d, dump_bufs
        phase_C(1, w_out_c, x2_d)
        phase_M(1, out_d)
    return nc, ext, out_d, dump_bufs


def host_consts():
    c = {}
    c["ident"] = np.eye(128, dtype=np.float32)
    k = np.arange(128, dtype=np.float32)[:, None]
    n = np.arange(NEGD_W, dtype=np.float32)[None, :]
    c["negd"] = (-np.abs(n - k - NEGD_C)).astype(np.float32)
    half = 16
    inv_freq = np.power(np.float32(10000.0), -np.arange(half, dtype=np.float32) * np.float32(2.0) / np.float32(32)).astype(np.float32)
    ang = (np.arange(S, dtype=np.float32)[:, None] * inv_freq[None, :]).astype(np.float32)
    cos = np.cos(ang).astype(np.float32).T
    sin = np.sin(ang).astype(np.float32).T
    c32 = np.concatenate([cos, cos], 0)
    s32 = np.concatenate([-sin, sin], 0)
    c["ropecos"] = np.ascontiguousarray(np.tile(c32, (4, 1)))
    c["ropesin"] = np.ascontiguousarray(np.tile(s32, (4, 1)))
    kk = np.arange(128, dtype=np.float32)[:, None]
    qq = np.arange(128, dtype=np.float32)[None, :]
    tabs = []
    for typ in range(3):
        d = kk - qq + 128.0 * (typ - 1)
        t = np.where(np.abs(d) <= 64, -np.abs(d), -1.0e9).astype(np.float32)
        tabs.append(np.tile(t, (1, 4)))
    c["dil_negd"] = np.ascontiguousarray(np.stack(tabs, 1))
    mc = np.zeros((128, 4, 64), np.float32)
    mc[:, 0, :32] = np.arange(32, dtype=np.float32)[None, :]
    mc[:, 1, :32] = 256.0 * np.arange(32, dtype=np.float32)[None, :]
    mc[:, 2, :63] = 256.0 * np.arange(63, dtype=np.float32)[None, :]
    c["moe_c"] = mc
    mc[:, 3, :] = np.arange(128, dtype=np.float32)[:, None]
    c["tokid"] = (np.arange(NT, dtype=np.float32)[None, :] * 128 + np.arange(128, dtype=np.float32)[:, None]).astype(np.float32)
    sl = np.zeros((128, 64), np.float32)
    sl[64, :] = 1.0
    c["sel"] = sl
    c["triu"] = np.triu(np.ones((128, 128), np.float32), 1)
    return c


def host_weights(inp):
    w = {}
    wi = inp["w_in_ab"][0]
    w["w_in_ab"] = np.ascontiguousarray(np.concatenate([wi, wi[:, 1936:1952], wi[:, 1920:1936]], 1))
    wq = inp["w_uq"][0]
    nope = [wq[:, h * 96:h * 96 + 64] for h in range(4)]
    rope = [wq[:, h * 96 + 64:h * 96 + 96] for h in range(4)]
    rsw = [np.concatenate([wq[:, h * 96 + 80:h * 96 + 96], wq[:, h * 96 + 64:h * 96 + 80]], 1) for h in range(4)]
    w["w_uq"] = np.ascontiguousarray(np.concatenate(nope + rope + rsw, 1))
    wk = inp["w_ukv"][0]
    kn = [wk[:, h * 192:h * 192 + 64] for h in range(4)]
    vv = [wk[:, h * 192 + 64:h * 192 + 192] for h in range(4)]
    w["w_ukv"] = np.ascontiguousarray(np.concatenate(kn + vv, 1))
    w["w_out_ab"] = np.ascontiguousarray(inp["w_out_ab"][0])
    w["lam4"] = np.ascontiguousarray(np.stack([inp["lam_q1"][0], inp["lam_k1"][0], inp["lam_q2"][0], inp["lam_k2"][0]], 0))
    w["diff_g"] = np.ascontiguousarray(inp["diff_norm_g"][0].reshape(128, 1))
    w["qn_g"] = np.ascontiguousarray(inp["mla_q_norm_g"][0].reshape(2, 128).T)
    w["kvn_g"] = np.ascontiguousarray(inp["mla_kv_norm_g"][0].reshape(128, 1))
    for k in ("ln_mix_g", "ln_mix_b", "ln_ffn_g", "ln_ffn_b"):
        w[k] = np.ascontiguousarray(inp[k])
    w["w_router"] = np.ascontiguousarray(np.concatenate([inp["moe_w_group"], inp["moe_w_route"]], 2))
    w["b_router"] = np.ascontiguousarray(np.concatenate([inp["moe_b_group"], inp["moe_b_route"]], 1))
    g = inp["moe_w_gate"].reshape(2, E_, 8, 128, HID).transpose(0, 1, 3, 2, 4)
    w["wg_l"] = np.ascontiguousarray(g).reshape(2 * E_ * 128, 8 * HID)
    u = inp["moe_w_up"].reshape(2, E_, 8, 128, HID).transpose(0, 1, 3, 2, 4)
    w["wu_l"] = np.ascontiguousarray(u).reshape(2 * E_ * 128, 8 * HID)
    dd = inp["moe_w_down"].reshape(2, E_, 4, 128, D).transpose(0, 1, 3, 2, 4)
    w["wd_l"] = np.ascontiguousarray(dd).reshape(2 * E_ * 128, 4 * D)
    w["w_in_c"] = np.ascontiguousarray(inp["w_in_c"][0])
    w["w_out_c"] = np.ascontiguousarray(inp["w_out_c"][0])
    return w


def kernel(**inputs):
    inp = {k: np.asarray(v) for k, v in inputs.items()}
    nc, ext, out_d, _ = build_program()
    shared = host_consts()
    shared.update(host_weights(inp))
    x = np.ascontiguousarray(inp["x"], dtype=np.float32)
    in_maps = []
    for c in range(8):
        m = dict(shared)
        m["x"] = x[c]
        in_maps.append(m)
    res = run_bass_kernel_spmd(nc, in_maps, core_ids=list(range(8)))
    return np.stack([np.asarray(r["out"]) for r in res.results], 0).astype(np.float32)
```

```python
import math
import numpy as np
from contextlib import ExitStack
import concourse.bass as bass
import concourse.mybir as mybir
from concourse.bass_utils import run_bass_kernel_spmd

F32 = mybir.dt.float32
BF16 = mybir.dt.bfloat16
I32 = mybir.dt.int32
ALU = mybir.AluOpType
AF = mybir.ActivationFunctionType
AX = mybir.AxisListType

S = 4096
D = 1024
NT = S // 128
DEPTH = 2
ALPHA = (2 * DEPTH) ** 0.25
EPS = 1e-5
NPOOL = 16
E_ = 32
HID = 512
BLK = 256
NB = 63
NSLOT = NB * BLK
NEGD_C = 3968
NEGD_W = 8064


class Buf:
    __slots__ = ("t", "w", "r", "name", "multi", "wm")

    def __init__(self, t, name="", multi=False):
        self.t = t
        self.w = None
        self.r = {}
        self.name = name
        self.multi = multi
        self.wm = {}

    def __getitem__(self, k):
        return self.t[k]

    def ap(self):
        return self.t.ap()


class FW:
    def __init__(self, nc, stack):
        self.nc = nc
        self.stack = stack
        self.engs = {"pe": nc.tensor, "act": nc.scalar, "dve": nc.vector, "pool": nc.gpsimd, "sp": nc.sync}
        self.esem, self.cnt, self.seen, self.dq = {}, {}, {}, {}
        for e in self.engs:
            self.esem[e] = stack.enter_context(nc.semaphore("es_" + e))
            self.cnt[e] = 0
            self.seen[e] = {}
        for q in ("sp", "pool", "act"):
            sems = [stack.enter_context(nc.semaphore(f"dq_{q}_{i}")) for i in range(NPOOL)]
            self.dq[q] = {"sems": sems, "n": 0}
        self.n_inst = 0
        self.uid = 0

    def sb(self, name, shape, dtype, stack=None, multi=False):
        st = stack if stack is not None else self.stack
        self.uid += 1
        return Buf(st.enter_context(self.nc.sbuf_tensor(f"s{self.uid}_{name}", list(shape), dtype)), name, multi)

    def ps(self, name, shape, dtype=F32, stack=None):
        st = stack if stack is not None else self.stack
        return Buf(st.enter_context(self.nc.psum_tensor("p_" + name, list(shape), dtype)), name)

    def dram(self, name, shape, dtype, kind="Internal"):
        return Buf(self.nc.dram_tensor(name, list(shape), dtype, kind=kind), name, True)

    def op(self, eng, fn, reads=(), writes=(), dma=False):
        waits = {}
        own = self.esem[eng]
        seen = self.seen[eng]

        def need(sem, val):
            if eng == "pe" and sem is own:
                return
            if seen.get(sem, 0) >= val:
                return
            if waits.get(sem, 0) < val:
                waits[sem] = val

        for b in reads:
            if b.multi:
                for s_, v_ in b.wm.items():
                    need(s_, v_)
            elif b.w is not None:
                need(*b.w)
        for b in writes:
            if not b.multi and b.w is not None:
                need(*b.w)
            for s_, v_ in b.r.items():
                need(s_, v_)
        if dma:
            q = self.dq[eng]
            j = q["n"]
            s = q["sems"][j % NPOOL]
            if j >= NPOOL:
                need(s, 16 * (j // NPOOL))
        e = self.engs[eng]
        for sem, val in waits.items():
            e.wait_ge(sem, val)
            seen[sem] = val
        ins = fn(e)
        self.n_inst += 1
        if dma:
            ins.then_inc(s, 16)
            ev = (s, 16 * (j // NPOOL + 1))
            q["n"] += 1
        else:
            self.cnt[eng] += 1
            ins.then_inc(own, 1)
            ev = (own, self.cnt[eng])
        for b in reads:
            if b.r.get(ev[0], 0) < ev[1]:
                b.r[ev[0]] = ev[1]
        for b in writes:
            if b.multi:
                if b.wm.get(ev[0], 0) < ev[1]:
                    b.wm[ev[0]] = ev[1]
            else:
                b.w = ev
                b.r = {}
        return ev

    def barrier(self):
        evs = []
        for e in self.engs:
            if self.cnt[e] > 0:
                evs.append((self.esem[e], self.cnt[e]))
        for q in self.dq.values():
            n = q["n"]
            for i, s in enumerate(q["sems"]):
                k = (n - i + NPOOL - 1) // NPOOL
                if k > 0:
                    evs.append((s, 16 * k))
        for e, eo in self.engs.items():
            for sem, val in evs:
                if sem is self.esem[e]:
                    continue
                if self.seen[e].get(sem, 0) >= val:
                    continue
                eo.wait_ge(sem, val)
                self.seen[e][sem] = val


class Rot:
    def __init__(self, bufs):
        self.bufs = bufs
        self.i = 0

    def next(self):
        b = self.bufs[self.i % len(self.bufs)]
        self.i += 1
        return b


def ssl(start, n, step):
    return slice(start, start + step * (n - 1) + 1, step)


def alibi_slopes(n):
    start = 2.0 ** (-8.0 / n)
    return [start ** (i + 1) for i in range(n)]


def build_program(upto=None, dumps=()):
    nc = bass.Bass("TRN2", target_bir_lowering=False)
    ext = {}

    def ein(name, shape, dtype=F32):
        ext[name] = Buf(nc.dram_tensor(name, list(shape), dtype, kind="ExternalInput"), name)
        return ext[name]

    x_in = ein("x", [S, D])
    ident_d = ein("ident", [128, 128])
    negd_d = ein("negd", [128, NEGD_W])
    cos_d = ein("ropecos", [128, S])
    sin_d = ein("ropesin", [128, S])
    w_in_ab = ein("w_in_ab", [D, 1984])
    w_uq = ein("w_uq", [256, 512])
    w_ukv = ein("w_ukv", [128, 768])
    w_out_ab = ein("w_out_ab", [D, D])
    lam4 = ein("lam4", [4, 64])
    diff_g = ein("diff_g", [128, 1])
    qn_g = ein("qn_g", [128, 2])
    kvn_g = ein("kvn_g", [128, 1])
    ln_mix_g = ein("ln_mix_g", [2, D])
    ln_mix_b = ein("ln_mix_b", [2, D])
    ln_ffn_g = ein("ln_ffn_g", [2, D])
    ln_ffn_b = ein("ln_ffn_b", [2, D])
    w_router = ein("w_router", [2, D, 36])
    b_router = ein("b_router", [2, 36])
    wg_l = ein("wg_l", [2 * E_ * 128, 8 * HID])
    wu_l = ein("wu_l", [2 * E_ * 128, 8 * HID])
    wd_l = ein("wd_l", [2 * E_ * 128, 4 * D])
    w_in_c = ein("w_in_c", [D, 3 * D])
    w_out_c = ein("w_out_c", [D, D])
    dil_negd_d = ein("dil_negd", [128, 3, 512])
    moe_c = ein("moe_c", [128, 4, 64])
    triu_d = ein("triu", [128, 128])
    tokid_d = ein("tokid", [128, NT])
    sel_d = ein("sel", [128, 64])
    out_d = Buf(nc.dram_tensor("out", [S, D], F32, kind="ExternalOutput"), "out", True)

    dump_bufs = {}

    with ExitStack() as st:
        fw = FW(nc, st)
        op = fw.op

        def dma(q, out_ap, in_ap, reads, writes):
            return op(q, lambda e: e.dma_start(out=out_ap, in_=in_ap), reads, writes, dma=True)

        _dram0 = fw.dram
        fw.dram = lambda name, shape, dtype: _dram0(name, shape, dtype, kind=("ExternalOutput" if name in dumps else "Internal"))
        qkA = fw.dram("qkA", [8, 128, S], BF16)
        vA = fw.dram("vA", [S, 512], BF16)
        cq_d = fw.dram("cq_d", [256, S], F32)
        ckv_d = fw.dram("ckv_d", [128, S], F32)
        qmT = fw.dram("qmT", [4, 96, S], BF16)
        kmT = fw.dram("kmT", [4, 96, S], BF16)
        vM = fw.dram("vM", [S, 512], BF16)
        oT_d = fw.dram("oT_d", [8, 128, S], BF16)
        x1_d = fw.dram("x1_d", [S, D], F32)
        x1b_d = fw.dram("x1b_d", [S + 1, D], BF16)
        x2_d = fw.dram("x2_d", [S, D], F32)
        slot_d = fw.dram("slot_d", [NSLOT + 128, 4], F32)
        y_d = fw.dram("y_d", [2 * S + 128, D], F32)
        wgb = fw.dram("wgb", [2 * E_ * 128, 8 * HID], BF16)
        wub = fw.dram("wub", [2 * E_ * 128, 8 * HID], BF16)
        wdb = fw.dram("wdb", [2 * E_ * 128, 4 * D], BF16)
        cast_list = []
        CR = 512
        for l_ in range(2):
            for src_, dst_ in ((wg_l, wgb), (wu_l, wub), (wd_l, wdb)):
                for r_ in range(l_ * E_ * 128, (l_ + 1) * E_ * 128, CR):
                    cast_list.append((src_, dst_, r_))
        cast_bufs = []

        def issue_cast(n=1):
            for _ in range(n):
                if not cast_list:
                    return
                src_, dst_, r_ = cast_list.pop(0)
                cb = Buf(None, "castchunk")
                cast_bufs.append(cb)
                dma("pool", dst_[r_:r_ + CR, :], src_[r_:r_ + CR, :], [src_], [cb])
        qkC = fw.dram("qkC", [16, 128, S], BF16)
        vC = fw.dram("vC", [3, 4, 128, NT, 260], BF16)

        ident = fw.sb("ident", [128, 128], F32)
        dma("sp", ident[:], ident_d.ap(), [ident_d], [ident])
        ones_bf = fw.sb("ones_bf", [128, 128], BF16)
        op("dve", lambda e: e.memset(ones_bf[:], 1.0), [], [ones_bf])
        ones_f = fw.sb("ones_f", [128, 128], F32)
        op("dve", lambda e: e.memset(ones_f[:], 1.0), [], [ones_f])
        eps_t = fw.sb("eps_t", [128, 1], F32)
        op("dve", lambda e: e.memset(eps_t[:], EPS), [], [eps_t])
        psb = [fw.ps(f"psb{i}", [128, 512], F32) for i in range(8)]

        def dump(name, buf_ap, shape, dtype, reads):
            if name in dumps:
                d = Buf(nc.dram_tensor("dbg_" + name, list(shape), dtype, kind="ExternalOutput"), name)
                dump_bufs[name] = d
                dma("sp", d.ap(), buf_ap, reads, [d])

        def build_xT(src_d, xT, pst, stg):
            for i in range(NT):
                xt = stg.next()
                dma("sp", xt[:], src_d[i * 128:(i + 1) * 128, :], [src_d], [xt])
                for hf in range(2):
                    ps = pst.next()
                    for c4 in range(4):
                        c = hf * 4 + c4
                        op("pe", lambda e: e.transpose(out=ps[:, c4 * 128:(c4 + 1) * 128], in_=xt[:, c * 128:(c + 1) * 128], identity=ident[:]), [xt, ident], [ps])
                    eng = "act" if hf == 0 else "dve"
                    o_ap = xT[:, hf * 4:(hf + 1) * 4, i * 128:(i + 1) * 128]
                    i_ap = ps[:, :].rearrange("p (c n) -> p c n", c=4)
                    if eng == "act":
                        op("act", lambda e: e.copy(out=o_ap, in_=i_ap), [ps], [xT])
                    else:
                        op("dve", lambda e: e.tensor_copy(out=o_ap, in_=i_ap), [ps], [xT])

        def attn_core(QT, KT, Vsb, krows, q0, po, pss, pscore, slope, negd, tmps, es, band=None):
            qb_, qr = QT
            kb_, kr = KT
            kbs = list(range(NT))
            for n, kb in enumerate(kbs):
                ps = pscore.next()
                op("pe", lambda e: e.matmul(ps[:, :], lhsT=kb_[kr:kr + krows, kb * 128:(kb + 1) * 128], rhs=qb_[qr:qr + krows, q0:q0 + 512], start=True, stop=True), [kb_, qb_], [ps])
                E = es.next()
                if slope is not None:
                    tmp = tmps.next()
                    n0 = q0 - kb * 128 + NEGD_C
                    op("dve", lambda e: e.scalar_tensor_tensor(out=tmp[:, :], in0=negd[:, n0:n0 + 512], scalar=float(slope), in1=ps[:, :], op0=ALU.mult, op1=ALU.add), [negd, ps], [tmp])
                    op("act", lambda e: e.activation(out=E[:, :], in_=tmp[:, :], func=AF.Exp), [tmp], [E])
                else:
                    op("act", lambda e: e.activation(out=E[:, :], in_=ps[:, :], func=AF.Exp), [ps], [E])
                first, last = (n == 0), (n == len(kbs) - 1)
                op("pe", lambda e: e.matmul(po[:, :], lhsT=Vsb[:, kb, :], rhs=E[:, :], start=first, stop=last), [Vsb, E], [po])
                op("pe", lambda e: e.matmul(pss[:, :], lhsT=ones_bf[:, :], rhs=E[:, :], start=first, stop=last), [ones_bf, E], [pss])

        def rstd_from_ssq(ps_ssq, n, out_t, tmp_t):
            op("act", lambda e: e.activation(out=tmp_t[:, :], in_=ps_ssq[:, :], func=AF.Sqrt, bias=eps_t[:, 0:1], scale=1.0 / n), [ps_ssq, eps_t], [tmp_t])
            op("dve", lambda e: e.reciprocal(out=out_t[:, :], in_=tmp_t[:, :]), [tmp_t], [out_t])

        with ExitStack() as ph:
            xT = fw.sb("xT", [128, 8, S], BF16, ph)
            Win = fw.sb("Win", [128, 8, 1984], BF16, ph, multi=True)
            for c in range(8):
                dma("pool", Win[:, c, :], w_in_ab[c * 128:(c + 1) * 128, :], [w_in_ab], [Win])
            cosT = fw.sb("cosT", [128, S], F32, ph)
            sinT = fw.sb("sinT", [128, S], F32, ph)
            dma("sp", cosT[:], cos_d.ap(), [cos_d], [cosT])
            dma("sp", sinT[:], sin_d.ap(), [sin_d], [sinT])
            stg = Rot([fw.sb(f"a0_x{i}", [128, D], F32, ph) for i in range(2)])
            pst = Rot(psb[0:2])
            build_xT(x_in, xT, pst, stg)
            dump("xT", xT[:, :, :], [128, 8, S], BF16, [xT])
            psr = Rot(psb[2:8])
            sbf = Rot([fw.sb(f"a0_sb{i}", [128, 512], BF16, ph) for i in range(4)])
            sf = Rot([fw.sb(f"a0_sf{i}", [128, 512], F32, ph) for i in range(4)])
            tog = [0]

            def evac(out_ap, in_ap, reads, writes, scale=None):
                tog[0] ^= 1
                if tog[0] or scale is not None:
                    if scale is None:
                        op("act", lambda e: e.copy(out=out_ap, in_=in_ap), reads, writes)
                    else:
                        op("act", lambda e: e.activation(out=out_ap, in_=in_ap, func=AF.Copy, scale=float(scale)), reads, writes)
                else:
                    op("dve", lambda e: e.tensor_copy(out=out_ap, in_=in_ap), reads, writes)

            def proj_fm(Wsb, nchunk, col0, M, rhs_fn, rhs_reads, tb):
                ps = psr.next()
                for c in range(nchunk):
                    op("pe", lambda e: e.matmul(ps[0:M, :], lhsT=Wsb[:, c, col0:col0 + M], rhs=rhs_fn(c), start=(c == 0), stop=(c == nchunk - 1)), [Wsb] + rhs_reads, [ps])
                return ps

            for tb in range(8):
                tsl = slice(tb * 512, (tb + 1) * 512)
                rf = lambda c: xT[:, c, tsl]
                for h in range(8):
                    ps = proj_fm(Win, 8, h * 128, 128, rf, [xT], tb)
                    s_ = sbf.next()
                    evac(s_[:, :], ps[:, :], [ps], [s_], scale=(0.125 if h < 4 else None))
                    dma("pool", qkA[h, :, tsl], s_[:, :], [s_], [qkA])
                for j in range(3):
                    ps = proj_fm(Win, 8, 1536 + j * 128, 128, rf, [xT], tb)
                    s_ = sf.next()
                    evac(s_[:, :], ps[:, :], [ps], [s_])
                    if j < 2:
                        dma("pool", cq_d[j * 128:(j + 1) * 128, tsl], s_[:, :], [s_], [cq_d])
                    else:
                        dma("pool", ckv_d[:, tsl], s_[:, :], [s_], [ckv_d])
                ps1 = proj_fm(Win, 8, 1920, 32, rf, [xT], tb)
                ps2 = proj_fm(Win, 8, 1952, 32, rf, [xT], tb)
                t1, t2 = sf.next(), sf.next()
                op("dve", lambda e: e.tensor_tensor(out=t1[0:32, :], in0=ps1[0:32, :], in1=cosT[0:32, tsl], op=ALU.mult), [ps1, cosT], [t1])
                op("dve", lambda e: e.tensor_tensor(out=t2[0:32, :], in0=ps2[0:32, :], in1=sinT[0:32, tsl], op=ALU.mult), [ps2, sinT], [t2])
                s_ = sbf.next()
                op("dve", lambda e: e.tensor_tensor(out=s_[0:32, :], in0=t1[0:32, :], in1=t2[0:32, :], op=ALU.add), [t1, t2], [s_])
                for h in range(4):
                    dma("pool", kmT[h, 64:96, tsl], s_[0:32, :], [s_], [kmT])
            for i in range(NT):
                ps = psr.next()
                for c in range(8):
                    op("pe", lambda e: e.matmul(ps[:, :], lhsT=xT[:, c, i * 128:(i + 1) * 128], rhs=Win[:, c, 1024:1536], start=(c == 0), stop=(c == 7)), [xT, Win], [ps])
                s_ = sbf.next()
                evac(s_[:, :], ps[:, :], [ps], [s_])
                dma("pool", vA[i * 128:(i + 1) * 128, :], s_[:, :], [s_], [vA])
            fw.barrier()
        if upto == "A0":
            return nc, ext, out_d, dump_bufs

        with ExitStack() as ph:
            cq = fw.sb("cq", [128, 2, S], F32, ph, multi=True)
            ckv = fw.sb("ckv", [128, S], F32, ph)
            for j in range(2):
                dma("sp", cq[:, j, :], cq_d[j * 128:(j + 1) * 128, :], [cq_d], [cq])
            dma("sp", ckv[:, :], ckv_d.ap(), [ckv_d], [ckv])
            Wuq = fw.sb("Wuq", [128, 2, 512], BF16, ph, multi=True)
            for j in range(2):
                dma("pool", Wuq[:, j, :], w_uq[j * 128:(j + 1) * 128, :], [w_uq], [Wuq])
            Wukv = fw.sb("Wukv", [128, 1, 768], BF16, ph)
            dma("pool", Wukv[:, 0, :], w_ukv.ap(), [w_ukv], [Wukv])
            gq = fw.sb("gq", [128, 2], F32, ph)
            gkv = fw.sb("gkv", [128, 1], F32, ph)
            dma("sp", gq[:], qn_g.ap(), [qn_g], [gq])
            dma("sp", gkv[:], kvn_g.ap(), [kvn_g], [gkv])
            cosT = fw.sb("cosT1", [128, S], F32, ph)
            sinT = fw.sb("sinT1", [128, S], F32, ph)
            dma("sp", cosT[:], cos_d.ap(), [cos_d], [cosT])
            dma("sp", sinT[:], sin_d.ap(), [sin_d], [sinT])
            sq = Rot([fw.sb(f"a1_sq{i}", [128, 512], F32, ph) for i in range(2)])
            tmpf = Rot([fw.sb(f"a1_t{i}", [128, 512], F32, ph) for i in range(4)])
            cqn = Rot([fw.sb(f"a1_cqn{i}", [128, 2, 512], BF16, ph) for i in range(2)])
            ckvn = Rot([fw.sb(f"a1_ckvn{i}", [128, 512], BF16, ph) for i in range(2)])
            sbf = Rot([fw.sb(f"a1_sb{i}", [128, 512], BF16, ph) for i in range(4)])
            psr = Rot(psb[0:8])
            SCALE_M = 96.0 ** -0.5
            for tb in range(8):
                tsl = slice(tb * 512, (tb + 1) * 512)
                pss_ = psr.next()
                for j in range(2):
                    s_ = sq.next()
                    op("act", lambda e: e.activation(out=s_[:, :], in_=cq[:, j, tsl], func=AF.Square), [cq], [s_])
                    op("pe", lambda e: e.matmul(pss_[:, :], lhsT=ones_f[:, :], rhs=s_[:, :], start=(j == 0), stop=(j == 1)), [ones_f, s_], [pss_])
                rs, tt = tmpf.next(), tmpf.next()
                rstd_from_ssq(pss_, 256.0, rs, tt)
                cn = cqn.next()
                for j in range(2):
                    op("dve", lambda e: e.scalar_tensor_tensor(out=cn[:, j, :], in0=cq[:, j, tsl], scalar=gq[:, j:j + 1], in1=rs[:, :], op0=ALU.mult, op1=ALU.mult), [cq, gq, rs], [cn])
                for h in range(4):
                    ps = psr.next()
                    for j in range(2):
                        op("pe", lambda e: e.matmul(ps[0:64, :], lhsT=Wuq[:, j, h * 64:(h + 1) * 64], rhs=cn[:, j, :], start=(j == 0), stop=(j == 1)), [Wuq, cn], [ps])
                    s_ = sbf.next()
                    op("act", lambda e: e.activation(out=s_[0:64, :], in_=ps[0:64, :], func=AF.Copy, scale=SCALE_M), [ps], [s_])
                    dma("pool", qmT[h, 0:64, tsl], s_[0:64, :], [s_], [qmT])
                ps1, ps2 = psr.next(), psr.next()
                for j in range(2):
                    op("pe", lambda e: e.matmul(ps1[:, :], lhsT=Wuq[:, j, 256:384], rhs=cn[:, j, :], start=(j == 0), stop=(j == 1)), [Wuq, cn], [ps1])
                for j in range(2):
                    op("pe", lambda e: e.matmul(ps2[:, :], lhsT=Wuq[:, j, 384:512], rhs=cn[:, j, :], start=(j == 0), stop=(j == 1)), [Wuq, cn], [ps2])
                t1, t2 = tmpf.next(), tmpf.next()
                op("dve", lambda e: e.tensor_tensor(out=t1[:, :], in0=ps1[:, :], in1=cosT[:, tsl], op=ALU.mult), [ps1, cosT], [t1])
                op("dve", lambda e: e.tensor_tensor(out=t2[:, :], in0=ps2[:, :], in1=sinT[:, tsl], op=ALU.mult), [ps2, sinT], [t2])
                op("dve", lambda e: e.tensor_tensor(out=t1[:, :], in0=t1[:, :], in1=t2[:, :], op=ALU.add), [t1, t2], [t1])
                s_ = sbf.next()
                op("act", lambda e: e.activation(out=s_[:, :], in_=t1[:, :], func=AF.Copy, scale=SCALE_M), [t1], [s_])
                for h in range(4):
                    dma("pool", qmT[h, 64:96, tsl], s_[h * 32:(h + 1) * 32, :], [s_], [qmT])
                pss_ = psr.next()
                s_ = sq.next()
                op("act", lambda e: e.activation(out=s_[:, :], in_=ckv[:, tsl], func=AF.Square), [ckv], [s_])
                op("pe", lambda e: e.matmul(pss_[:, :], lhsT=ones_f[:, :], rhs=s_[:, :], start=True, stop=True), [ones_f, s_], [pss_])
                rs, tt = tmpf.next(), tmpf.next()
                rstd_from_ssq(pss_, 128.0, rs, tt)
                kn = ckvn.next()
                op("dve", lambda e: e.scalar_tensor_tensor(out=kn[:, :], in0=ckv[:, tsl], scalar=gkv[:, 0:1], in1=rs[:, :], op0=ALU.mult, op1=ALU.mult), [ckv, gkv, rs], [kn])
                for h in range(4):
                    ps = psr.next()
                    op("pe", lambda e: e.matmul(ps[0:64, :], lhsT=Wukv[:, 0, h * 64:(h + 1) * 64], rhs=kn[:, :], start=True, stop=True), [Wukv, kn], [ps])
                    s_ = sbf.next()
                    op("dve", lambda e: e.tensor_copy(out=s_[0:64, :], in_=ps[0:64, :]), [ps], [s_])
                    dma("pool", kmT[h, 0:64, tsl], s_[0:64, :], [s_], [kmT])
                for i4 in range(4):
                    i = tb * 4 + i4
                    ps = psr.next()
                    op("pe", lambda e: e.matmul(ps[:, :], lhsT=kn[:, i4 * 128:(i4 + 1) * 128], rhs=Wukv[:, 0, 256:768], start=True, stop=True), [kn, Wukv], [ps])
                    s_ = sbf.next()
                    op("act", lambda e: e.copy(out=s_[:, :], in_=ps[:, :]), [ps], [s_])
                    dma("pool", vM[i * 128:(i + 1) * 128, :], s_[:, :], [s_], [vM])
            fw.barrier()
        if upto == "A1":
            return nc, ext, out_d, dump_bufs

        LAM_INIT0 = 0.8 - 0.6 * math.exp(-0.3 * 0)
        with ExitStack() as ph:
            negd = fw.sb("negd", [128, NEGD_W], F32, ph)
            dma("sp", negd[:], negd_d.ap(), [negd_d], [negd])
            lamt = fw.sb("lamt", [128, 4, 64], F32, ph)
            dma("sp", lamt[:], lam4.ap().rearrange("(o a) b -> o a b", o=1).broadcast_to([128, 4, 64]), [lam4], [lamt])
            lw = fw.sb("lw", [128, 8], F32, ph)
            lprod = fw.sb("lprod", [128, 2, 64], F32, ph)
            op("dve", lambda e: e.tensor_tensor(out=lprod[:, 0, :], in0=lamt[:, 0, :], in1=lamt[:, 1, :], op=ALU.mult), [lamt], [lprod])
            op("dve", lambda e: e.tensor_tensor(out=lprod[:, 1, :], in0=lamt[:, 2, :], in1=lamt[:, 3, :], op=ALU.mult), [lamt], [lprod])
            op("dve", lambda e: e.reduce_sum(out=lw[:, 0:2], in_=lprod[:, :, :], axis=AX.X), [lprod], [lw])
            op("act", lambda e: e.activation(out=lw[:, 2:4], in_=lw[:, 0:2], func=AF.Exp), [lw], [lw])
            op("dve", lambda e: e.tensor_tensor(out=lw[:, 4:5], in0=lw[:, 3:4], in1=lw[:, 2:3], op=ALU.subtract), [lw], [lw])
            op("dve", lambda e: e.tensor_scalar_add(out=lw[:, 5:6], in0=lw[:, 4:5], scalar1=-LAM_INIT0), [lw], [lw])
            neg_lam = lw[:, 5:6]
            dg = fw.sb("dg", [128, 1], F32, ph)
            dma("sp", dg[:], diff_g.ap(), [diff_g], [dg])
            dg2 = fw.sb("dg2", [128, 1], F32, ph)
            op("dve", lambda e: e.tensor_scalar_mul(out=dg2[:, :], in0=dg[:, :], scalar1=(1.0 - LAM_INIT0)), [dg], [dg2])

            QTs = Rot([fw.sb(f"b_q{i}", [128, S], BF16, ph) for i in range(2)])
            QZ = [Rot([fw.sb(f"b_qz{m}{i}", [128, S], BF16, ph) for i in range(2)]) for m in range(2)]
            for m in range(2):
                for qz in QZ[m].bufs:
                    op("dve", lambda e: e.memset(qz[:, :], 0.0), [], [qz])
            KTs = Rot([fw.sb(f"b_k{i}", [128, S], BF16, ph) for i in range(2)])
            Vs = Rot([fw.sb(f"b_v{i}", [128, NT, 128], BF16, ph) for i in range(2)])
            tmps = Rot([fw.sb(f"b_t{i}", [128, 512], BF16, ph) for i in range(5)])
            es = Rot([fw.sb(f"b_e{i}", [128, 512], BF16, ph) for i in range(6)])
            ETs = Rot([fw.sb(f"b_et{i}", [128, NEGD_W], BF16, ph) for i in range(2)])
            pscore = Rot(psb[0:4])
            pacc = Rot([(psb[4], psb[5]), (psb[6], psb[7])])
            of = Rot([fw.sb(f"b_of{i}", [128, 512], F32, ph) for i in range(6)])
            rsf = Rot([fw.sb(f"b_rs{i}", [128, 512], F32, ph) for i in range(8)])
            ob = Rot([fw.sb(f"b_ob{i}", [128, 512], BF16, ph) for i in range(3)])
            slopes = alibi_slopes(4)
            LA = 3
            jobs = []

            def mk_loader(kind, h, QT, KT, V):
                def ld():
                    if kind == "diff":
                        ET = cur_et[h]
                        for c4 in range(4):
                            csl = slice(c4 * 2016, (c4 + 1) * 2016)
                            op("act", lambda e: e.activation(out=ET[:, csl], in_=negd[:, csl], func=AF.Exp, scale=float(slopes[h])), [negd], [ET])
                        for m in range(2):
                            dma("sp", QT[m][m * 64:(m + 1) * 64, :], qkA[h, m * 64:(m + 1) * 64, :], [qkA], [QT[m]])
                        dma("sp", KT[:, :], qkA[4 + h, :, :], [qkA], [KT])
                        dma("sp", V[:, :, :], vA[:, h * 128:(h + 1) * 128].rearrange("(t p) e -> p t e", p=128), [vA], [V])
                    else:
                        dma("sp", QT[0:96, :], qmT[h, :, :], [qmT], [QT])
                        dma("sp", KT[0:96, :], kmT[h, :, :], [kmT], [KT])
                        dma("sp", V[:, :, :], vM[:, h * 128:(h + 1) * 128].rearrange("(t p) e -> p t e", p=128), [vM], [V])
                return ld

            def fin_diff(h, q0, st_):
                def fin_map(po, pss_):
                    rs, r2 = rsf.next(), rsf.next()
                    op("act", lambda e: e.activation(out=r2[:, :], in_=pss_[:, :], func=AF.Ln), [pss_], [r2])
                    op("act", lambda e: e.activation(out=rs[:, :], in_=r2[:, :], func=AF.Exp, scale=-1.0), [r2], [rs])
                    yield
                    op("dve", lambda e: e.tensor_tensor(out=r2[:, :], in0=rs[:, :], in1=pss_[:, :], op=ALU.mult), [rs, pss_], [r2])
                    op("dve", lambda e: e.tensor_scalar(out=r2[:, :], in0=r2[:, :], scalar1=-1.0, scalar2=2.0, op0=ALU.mult, op1=ALU.add), [r2], [r2])
                    yield
                    op("dve", lambda e: e.tensor_tensor(out=rs[:, :], in0=rs[:, :], in1=r2[:, :], op=ALU.mult), [rs, r2], [rs])
                    o_ = of.next()
                    op("dve", lambda e: e.tensor_tensor(out=o_[:, :], in0=po[:, :], in1=rs[:, :], op=ALU.mult), [po, rs], [o_])
                    st_.append(o_)
                    if len(st_) == 2:
                        yield
                        om = st_
                        oa = of.next()
                        op("dve", lambda e: e.scalar_tensor_tensor(out=oa[:, :], in0=om[1][:, :], scalar=neg_lam, in1=om[0][:, :], op0=ALU.mult, op1=ALU.add), [om[0], om[1], lw], [oa])
                        yield
                        sq_ = of.next()
                        op("act", lambda e: e.activation(out=sq_[:, :], in_=oa[:, :], func=AF.Square), [oa], [sq_])
                        yield
                        pssq = pscore.next()
                        op("pe", lambda e: e.matmul(pssq[:, :], lhsT=ones_f[:, :], rhs=sq_[:, :], start=True, stop=True), [ones_f, sq_], [pssq])
                        yield
                        rs2, tt = rsf.next(), rsf.next()
                        op("act", lambda e: e.activation(out=tt[:, :], in_=pssq[:, :], func=AF.Ln, bias=eps_t[:, 0:1], scale=1.0 / 128.0), [pssq, eps_t], [tt])
                        op("act", lambda e: e.activation(out=rs2[:, :], in_=tt[:, :], func=AF.Exp, scale=-0.5), [tt], [rs2])
                        yield
                        o_b = ob.next()
                        op("dve", lambda e: e.scalar_tensor_tensor(out=o_b[:, :], in0=oa[:, :], scalar=dg2[:, 0:1], in1=rs2[:, :], op0=ALU.mult, op1=ALU.mult), [oa, dg2, rs2], [o_b])
                        dma("pool", oT_d[h, :, q0:q0 + 512], o_b[:, :], [o_b], [oT_d])
                return fin_map

            def fin_mla(h, q0):
                def fin_map(po, pss_):
                    rs = rsf.next()
                    op("dve", lambda e: e.reciprocal(out=rs[:, :], in_=pss_[:, :]), [pss_], [rs])
                    yield
                    o_b = ob.next()
                    op("dve", lambda e: e.tensor_tensor(out=o_b[:, :], in0=po[:, :], in1=rs[:, :], op=ALU.mult), [po, rs], [o_b])
                    dma("pool", oT_d[4 + h, :, q0:q0 + 512], o_b[:, :], [o_b], [oT_d])
                return fin_map

            cur_et = {}
            SKIP_THR = {0: 512, 1: 2048}
            for h in range(4):
                QT, KT, V = (QZ[0].next(), QZ[1].next()), KTs.next(), Vs.next()
                cur_et[h] = ETs.next()
                first_of_head = True
                for qb in range(8):
                    st_ = []
                    fin = fin_diff(h, qb * 512, st_)
                    kbs = []
                    for kb in range(NT):
                        md = max(0, kb * 128 - (qb * 512 + 511), qb * 512 - (kb * 128 + 127))
                        if h in SKIP_THR and md >= SKIP_THR[h]:
                            continue
                        kbs.append(kb)
                    for m in range(2):
                        for kb in kbs:
                            jobs.append(dict(QT=QT[m], KT=KT, V=V, r0=0, kr=128, q0=qb * 512, kb=kb, slope=slopes[h], ET=cur_et[h], first=(kb == kbs[0]), last=(kb == kbs[-1]), fin=fin,
                                             pre=(mk_loader("diff", h, QT, KT, V) if first_of_head else None)))
                            first_of_head = False
            for h in range(4):
                QT, KT, V = QTs.next(), KTs.next(), Vs.next()
                first_of_head = True
                for qb in range(8):
                    fin = fin_mla(h, qb * 512)
                    for kb in range(NT):
                        jobs.append(dict(QT=QT, KT=KT, V=V, r0=0, kr=96, q0=qb * 512, kb=kb, slope=None, first=(kb == 0), last=(kb == NT - 1), fin=fin,
                                         pre=(mk_loader("mla", h, QT, KT, V) if first_of_head else None)))
                        first_of_head = False

            PREF = 150
            first_seen = False
            for idx_, j_ in enumerate(jobs):
                if j_["pre"] is not None:
                    if first_seen:
                        tgt = max(0, idx_ - PREF)
                        ld_ = j_["pre"]
                        j_["pre"] = None
                        prev = jobs[tgt].get("pre2")
                        jobs[tgt]["pre2"] = ld_ if prev is None else (lambda a=prev, b=ld_: (a(), b()))
                    first_seen = True

            def stage1(j):
                if j["pre"] is not None:
                    j["pre"]()
                if j.get("pre2") is not None:
                    j["pre2"]()
                QT, KT, r0, kr, q0, kb = j["QT"], j["KT"], j["r0"], j["kr"], j["q0"], j["kb"]
                ps = pscore.next()
                op("pe", lambda e: e.matmul(ps[:, :], lhsT=KT[r0:r0 + kr, kb * 128:(kb + 1) * 128], rhs=QT[r0:r0 + kr, q0:q0 + 512], start=True, stop=True), [KT, QT], [ps])
                E = es.next()
                if j["slope"] is not None:
                    tmp = tmps.next()
                    n0 = q0 - kb * 128 + NEGD_C
                    ET = j["ET"]
                    op("act", lambda e: e.activation(out=tmp[:, :], in_=ps[:, :], func=AF.Exp), [ps], [tmp])
                    op("dve", lambda e: e.tensor_tensor(out=E[:, :], in0=tmp[:, :], in1=ET[:, n0:n0 + 512], op=ALU.mult), [tmp, ET], [E])
                else:
                    op("act", lambda e: e.activation(out=E[:, :], in_=ps[:, :], func=AF.Exp), [ps], [E])
                j["E"] = E

            cur = [None]

            def stage2(j):
                if j["first"]:
                    cur[0] = pacc.next()
                po, pss_ = cur[0]
                V, kb, E = j["V"], j["kb"], j["E"]
                op("pe", lambda e: e.matmul(po[:, :], lhsT=V[:, kb, :], rhs=E[:, :], start=j["first"], stop=j["last"]), [V, E], [po])
                op("pe", lambda e: e.matmul(pss_[:, :], lhsT=ones_bf[:, :], rhs=E[:, :], start=j["first"], stop=j["last"]), [ones_bf, E], [pss_])
                if j["last"]:
                    deferred.append([FIN_DELAY, j["fin"](po, pss_)])

            deferred = []
            FIN_DELAY = 4
            FIN_STEP = 2

            def run_deferred(force=False):
                for d_ in list(deferred):
                    d_[0] -= 1
                    while d_[0] <= 0 or force:
                        try:
                            next(d_[1])
                            d_[0] = FIN_STEP
                        except StopIteration:
                            deferred.remove(d_)
                            break
                        if not force:
                            break

            for idx in range(len(jobs) + LA):
                run_deferred()
                if idx % 50 == 10:
                    issue_cast()
                if idx < len(jobs):
                    stage1(jobs[idx])
                if idx >= LA:
                    stage2(jobs[idx - LA])
            while deferred:
                run_deferred(force=True)
            issue_cast(len(cast_list))
            fw.barrier()
        if upto == "B":
            return nc, ext, out_d, dump_bufs

        LOG = fw.sb("LOG", [128, NT, 36], F32)

        def ln_stage1(y, stt, junk):
            op("act", lambda e: e.activation(out=junk[:, :], in_=y[:, :], func=AF.Identity, accum_out=stt[:, 0:1]), [y], [junk, stt])
            op("act", lambda e: e.activation(out=junk[:, :], in_=y[:, :], func=AF.Square, accum_out=stt[:, 1:2]), [y], [junk, stt])

        def ln_stage2(y, g_t, b_t, stt, out_t, junk):
            ln_front(y, stt, junk)
            ln_back(g_t, b_t, out_t, junk)

        def ln_back(g_t, b_t, out_t, junk):
            op("dve", lambda e: e.tensor_tensor(out=junk[:, :], in0=junk[:, :], in1=g_t[:, :], op=ALU.mult), [junk, g_t], [junk])
            op("pool", lambda e: e.tensor_tensor(out=out_t[:, :], in0=junk[:, :], in1=b_t[:, :], op=ALU.add), [junk, b_t], [out_t])

        def ln_f1(stt):
            op("dve", lambda e: e.tensor_scalar_mul(out=stt[:, 2:3], in0=stt[:, 0:1], scalar1=1.0 / D), [stt], [stt])
            op("dve", lambda e: e.tensor_tensor(out=stt[:, 3:4], in0=stt[:, 2:3], in1=stt[:, 2:3], op=ALU.mult), [stt], [stt])
            op("dve", lambda e: e.scalar_tensor_tensor(out=stt[:, 4:5], in0=stt[:, 1:2], scalar=1.0 / D, in1=stt[:, 3:4], op0=ALU.mult, op1=ALU.subtract), [stt], [stt])

        def ln_f2(stt):
            op("act", lambda e: e.activation(out=stt[:, 5:6], in_=stt[:, 4:5], func=AF.Sqrt, bias=eps_t[:, 0:1], scale=1.0), [stt, eps_t], [stt])

        def ln_f3(stt):
            op("dve", lambda e: e.reciprocal(out=stt[:, 6:7], in_=stt[:, 5:6]), [stt], [stt])
            op("dve", lambda e: e.scalar_tensor_tensor(out=stt[:, 7:8], in0=stt[:, 2:3], scalar=-1.0, in1=stt[:, 6:7], op0=ALU.mult, op1=ALU.mult), [stt], [stt])

        def ln_f4(y, stt, junk):
            op("act", lambda e: e.activation(out=junk[:, :], in_=y[:, :], func=AF.Identity, bias=stt[:, 7:8], scale=stt[:, 6:7]), [y, stt], [junk])

        def ln_front(y, stt, junk):
            ln_f1(stt)
            ln_f2(stt)
            ln_f3(stt)
            ln_f4(y, stt, junk)

        def phase_C(layer, w_out_dram, xsrc):
            with ExitStack() as ph:
                oT = fw.sb("c_oT", [128, 8, S], BF16, ph, multi=True)
                for c in range(8):
                    dma("sp", oT[:, c, :], oT_d[c, :, :], [oT_d], [oT])
                Wo = fw.sb("c_Wo", [128, 8, D], BF16, ph, multi=True)
                for c in range(8):
                    dma("pool", Wo[:, c, :], w_out_dram[c * 128:(c + 1) * 128, :], [w_out_dram], [Wo])
                Wr = fw.sb("c_Wr", [128, 8, 36], F32, ph)
                dma("sp", Wr[:], w_router[layer].rearrange("(c p) n -> p c n", p=128), [w_router], [Wr])
                g_t = fw.sb("c_g", [128, D], F32, ph)
                b_t = fw.sb("c_b", [128, D], F32, ph)
                dma("sp", g_t[:], ln_mix_g[layer:layer + 1, :].broadcast_to([128, D]), [ln_mix_g], [g_t])
                dma("sp", b_t[:], ln_mix_b[layer:layer + 1, :].broadcast_to([128, D]), [ln_mix_b], [b_t])
                brt = fw.sb("c_brt", [128, 36], F32, ph)
                dma("sp", brt[:], b_router[layer:layer + 1, :].broadcast_to([128, 36]), [b_router], [brt])
                zr = fw.sb("c_zr", [1, D], BF16, ph)
                op("dve", lambda e: e.memset(zr[:], 0.0), [], [zr])
                dma("sp", x1b_d[S:S + 1, :], zr[:], [zr], [x1b_d])
                xts = Rot([fw.sb(f"c_x{i}", [128, D], F32, ph) for i in range(3)])
                ys = Rot([fw.sb(f"c_y{i}", [128, D], F32, ph) for i in range(4)])
                junks = Rot([fw.sb(f"c_j{i}", [128, D], F32, ph) for i in range(4)])
                x1s = Rot([fw.sb(f"c_x1{i}", [128, D], F32, ph) for i in range(4)])
                x1bs = Rot([fw.sb(f"c_x1b{i}", [128, D], BF16, ph) for i in range(2)])
                x1Ts = Rot([fw.sb(f"c_x1T{i}", [128, 8, 128], F32, ph) for i in range(2)])
                stts = Rot([fw.sb(f"c_st{i}", [128, 8], F32, ph) for i in range(4)])
                pmm = Rot(psb[0:4])
                ptr = Rot(psb[4:6])
                prt = Rot(psb[6:8])
                stA, x1t_of, b2s = {}, {}, {}
                for i in range(-2, NT + 1):
                    ia, ib, ic = i + 2, i + 1, i - 1
                    if 0 <= ic < NT:
                        x1t = x1t_of.pop(ic)
                        x1T = x1Ts.next()
                        pst2 = [ptr.next(), ptr.next()]
                        for hf in range(2):
                            for c4 in range(4):
                                c = hf * 4 + c4
                                op("pe", lambda e: e.transpose(out=pst2[hf][:, c4 * 128:(c4 + 1) * 128], in_=x1t[:, c * 128:(c + 1) * 128], identity=ident[:]), [x1t, ident], [pst2[hf]])
                    if 0 <= ia < NT:
                        isl = slice(ia * 128, (ia + 1) * 128)
                        xt = xts.next()
                        dma("sp", xt[:], xsrc[isl, :], [xsrc], [xt])
                        y = ys.next()
                        pmm2 = [pmm.next(), pmm.next()]
                        for n in range(2):
                            for c in range(8):
                                op("pe", lambda e: e.matmul(pmm2[n][:, :], lhsT=oT[:, c, isl], rhs=Wo[:, c, n * 512:(n + 1) * 512], start=(c == 0), stop=(c == 7)), [oT, Wo], [pmm2[n]])
                    if 0 <= ib < NT:
                        yb, junkb, sttb = stA[ib]
                        ln_f1(sttb)
                        ln_f2(sttb)
                    if 0 <= ic < NT:
                        for hf in range(2):
                            o_ap = x1T[:, hf * 4:(hf + 1) * 4, :]
                            i_ap = pst2[hf][:, :].rearrange("p (c n) -> p c n", c=4)
                            if hf == 0:
                                op("act", lambda e: e.copy(out=o_ap, in_=i_ap), [pst2[hf]], [x1T])
                            else:
                                op("dve", lambda e: e.tensor_copy(out=o_ap, in_=i_ap), [pst2[hf]], [x1T])
                    if 0 <= ib < NT:
                        ln_f3(sttb)
                    if 0 <= ic < NT:
                        psr_ = prt.next()
                        for c in range(8):
                            op("pe", lambda e: e.matmul(psr_[:, 0:36], lhsT=x1T[:, c, :], rhs=Wr[:, c, :], start=(c == 0), stop=(c == 7)), [x1T, Wr], [psr_])
                    if 0 <= ib < NT:
                        ln_f4(yb, sttb, junkb)
                    if 0 <= ia < NT:
                        for n in range(2):
                            op("dve", lambda e: e.scalar_tensor_tensor(out=y[:, n * 512:(n + 1) * 512], in0=xt[:, n * 512:(n + 1) * 512], scalar=float(ALPHA), in1=pmm2[n][:, :], op0=ALU.mult, op1=ALU.add), [xt, pmm2[n]], [y])
                        junk, stt = junks.next(), stts.next()
                        ln_stage1(y, stt, junk)
                        stA[ia] = (y, junk, stt)
                    if 0 <= ib < NT:
                        isl = slice(ib * 128, (ib + 1) * 128)
                        x1n = x1s.next()
                        ln_back(g_t, b_t, x1n, junkb)
                        dma("pool", x1_d[isl, :], x1n[:, :], [x1n], [x1_d])
                        x1b = x1bs.next()
                        op("act", lambda e: e.copy(out=x1b[:, :], in_=x1n[:, :]), [x1n], [x1b])
                        dma("pool", x1b_d[isl, :], x1b[:, :], [x1b], [x1b_d])
                        x1t_of[ib] = x1n
                        stA.pop(ib)
                    if 0 <= ic < NT:
                        op("dve", lambda e: e.tensor_tensor(out=LOG[:, ic, :], in0=psr_[:, 0:36], in1=brt[:, :], op=ALU.add), [psr_, brt], [LOG])
                fw.barrier()

        def phase_M(layer, dst):
            with ExitStack() as ph:
                bs = ExitStack()
                tstack = [ph]

                def T(name, shape, dt=F32):
                    return fw.sb("m_" + name, shape, dt, tstack[0])
                mc = T("mc", [128, 4, 64])
                dma("sp", mc[:], moe_c.ap(), [moe_c], [mc])
                tokid = T("tokid", [128, NT])
                dma("sp", tokid[:], tokid_d.ap(), [tokid_d], [tokid])
                triu = T("triu", [128, 128])
                dma("sp", triu[:], triu_d.ap(), [triu_d], [triu])
                triu_b = T("triu_b", [128, 128], BF16)
                op("dve", lambda e: e.tensor_copy(out=triu_b[:, :], in_=triu[:, :]), [triu], [triu_b])
                coarse = LOG[:, :, 0:4]
                fine4 = LOG[:, :, 4:36].rearrange("p j (g i) -> p j g i", g=4)
                gmax = T("gmax", [128, NT])
                op("dve", lambda e: e.tensor_reduce(out=gmax[:, :], in_=coarse, axis=AX.X, op=ALU.max), [LOG], [gmax])
                ohg = T("ohg", [128, NT, 4])
                op("dve", lambda e: e.tensor_tensor(out=ohg[:, :, :], in0=coarse, in1=gmax[:, :].unsqueeze(2).to_broadcast([128, NT, 4]), op=ALU.is_equal), [LOG, gmax], [ohg])
                ex = T("ex", [128, NT, 4])
                op("dve", lambda e: e.tensor_tensor(out=ex[:, :, :], in0=coarse, in1=gmax[:, :].unsqueeze(2).to_broadcast([128, NT, 4]), op=ALU.subtract), [LOG, gmax], [ex])
                op("act", lambda e: e.activation(out=ex[:, :, :], in_=ex[:, :, :], func=AF.Exp), [ex], [ex])
                pg = T("pg", [128, NT])
                op("dve", lambda e: e.reduce_sum(out=pg[:, :], in_=ex[:, :, :], axis=AX.X), [ex], [pg])
                op("dve", lambda e: e.reciprocal(out=pg[:, :], in_=pg[:, :]), [pg], [pg])
                t48 = T("t48", [128, NT, 4, 8])
                op("dve", lambda e: e.tensor_tensor(out=t48[:, :, :, :], in0=fine4, in1=ohg[:, :, :].unsqueeze(3).to_broadcast([128, NT, 4, 8]), op=ALU.mult), [LOG, ohg], [t48])
                fsel = T("fsel", [128, NT, 8])
                op("dve", lambda e: e.reduce_sum(out=fsel[:, :, :], in_=t48[:, :, :, :].rearrange("p j g i -> p j i g"), axis=AX.X), [t48], [fsel])
                v1 = T("v1", [128, NT])
                op("dve", lambda e: e.tensor_reduce(out=v1[:, :], in_=fsel[:, :, :], axis=AX.X, op=ALU.max), [fsel], [v1])
                oh1 = T("oh1", [128, NT, 8])
                op("dve", lambda e: e.tensor_tensor(out=oh1[:, :, :], in0=fsel[:, :, :], in1=v1[:, :].unsqueeze(2).to_broadcast([128, NT, 8]), op=ALU.is_equal), [fsel, v1], [oh1])
                msk = T("msk", [128, NT, 8])
                op("dve", lambda e: e.scalar_tensor_tensor(out=msk[:, :, :], in0=oh1[:, :, :], scalar=-1.0e30, in1=fsel[:, :, :], op0=ALU.mult, op1=ALU.add), [oh1, fsel], [msk])
                v2 = T("v2", [128, NT])
                op("dve", lambda e: e.tensor_reduce(out=v2[:, :], in_=msk[:, :, :], axis=AX.X, op=ALU.max), [msk], [v2])
                oh2 = T("oh2", [128, NT, 8])
                op("dve", lambda e: e.tensor_tensor(out=oh2[:, :, :], in0=msk[:, :, :], in1=v2[:, :].unsqueeze(2).to_broadcast([128, NT, 8]), op=ALU.is_equal), [msk, v2], [oh2])
                ed = T("ed", [128, NT])
                op("dve", lambda e: e.tensor_tensor(out=ed[:, :], in0=v2[:, :], in1=v1[:, :], op=ALU.subtract), [v1, v2], [ed])
                op("act", lambda e: e.activation(out=ed[:, :], in_=ed[:, :], func=AF.Exp), [ed], [ed])
                w1 = T("w1", [128, NT])
                op("dve", lambda e: e.tensor_scalar_add(out=w1[:, :], in0=ed[:, :], scalar1=1.0), [ed], [w1])
                op("dve", lambda e: e.reciprocal(out=w1[:, :], in_=w1[:, :]), [w1], [w1])
                gates = T("gates", [128, 2, NT])
                op("dve", lambda e: e.tensor_tensor(out=gates[:, 0, :], in0=w1[:, :], in1=pg[:, :], op=ALU.mult), [w1, pg], [gates])
                op("dve", lambda e: e.tensor_tensor(out=w1[:, :], in0=w1[:, :], in1=ed[:, :], op=ALU.mult), [w1, ed], [w1])
                op("dve", lambda e: e.tensor_tensor(out=gates[:, 1, :], in0=w1[:, :], in1=pg[:, :], op=ALU.mult), [w1, pg], [gates])
                Ak = [T(f"A{k}", [128, NT, 4, 8]) for k in range(2)]
                for k, ohk in enumerate((oh1, oh2)):
                    op("dve", lambda e: e.tensor_tensor(out=Ak[k][:, :, :, :], in0=ohg[:, :, :].unsqueeze(3).to_broadcast([128, NT, 4, 8]), in1=ohk[:, :, :].unsqueeze(2).to_broadcast([128, NT, 4, 8]), op=ALU.mult), [ohg, ohk], [Ak[k]])
                A_bf = T("A_bf", [128, NT * 32], BF16)
                op("dve", lambda e: e.tensor_tensor(out=A_bf[:, :], in0=Ak[0][:, :, :, :].rearrange("p j g i -> p (j g i)"), in1=Ak[1][:, :, :, :].rearrange("p j g i -> p (j g i)"), op=ALU.add), [Ak[0], Ak[1]], [A_bf])
                sa = T("sa", [128, NT, 32])
                sb_ = T("sb", [128, NT, 32])
                tots = T("tots", [128, NT, 32])
                rank = T("rank", [128, NT, 32])
                for hf in range(2):
                    ps = psb[hf]
                    op("pe", lambda e: e.matmul(ps[:, :], lhsT=ones_bf[:, :], rhs=A_bf[:, hf * 512:(hf + 1) * 512], start=True, stop=True), [ones_bf, A_bf], [ps])
                    op("dve", lambda e: e.tensor_copy(out=tots[:, hf * 16:(hf + 1) * 16, :], in_=ps[:, :].rearrange("p (j e) -> p j e", e=32)), [ps], [tots])
                    ps2 = psb[2 + hf]
                    op("pe", lambda e: e.matmul(ps2[:, :], lhsT=triu_b[:, :], rhs=A_bf[:, hf * 512:(hf + 1) * 512], start=True, stop=True), [triu_b, A_bf], [ps2])
                    op("dve", lambda e: e.tensor_copy(out=rank[:, hf * 16:(hf + 1) * 16, :], in_=ps2[:, :].rearrange("p (j e) -> p j e", e=32)), [ps2], [rank])
                op("dve", lambda e: e.tensor_copy(out=sa[:, :, :], in_=tots[:, :, :]), [tots], [sa])
                a_, b_ = sa, sb_
                for s_ in (1, 2, 4, 8, 16):
                    op("dve", lambda e: e.tensor_tensor(out=b_[:, s_:, :], in0=a_[:, s_:, :], in1=a_[:, :NT - s_, :], op=ALU.add), [a_], [b_])
                    op("dve", lambda e: e.tensor_copy(out=b_[:, :s_, :], in_=a_[:, :s_, :]), [a_], [b_])
                    a_, b_ = b_, a_
                inc = a_
                cnt = T("cnt", [128, 32])
                op("dve", lambda e: e.tensor_copy(out=cnt[:, :], in_=inc[:, NT - 1, :]), [inc], [cnt])
                op("dve", lambda e: e.tensor_tensor(out=rank[:, :, :], in0=rank[:, :, :], in1=inc[:, :, :], op=ALU.add), [rank, inc], [rank])
                op("dve", lambda e: e.tensor_tensor(out=rank[:, :, :], in0=rank[:, :, :], in1=tots[:, :, :], op=ALU.subtract), [rank, tots], [rank])
                cmp_ = T("cmp", [128, 64, 32])
                op("dve", lambda e: e.tensor_tensor(out=cmp_[:, 0:32, :], in0=cnt[:, :].unsqueeze(2).to_broadcast([128, 32, 32]), in1=mc[:, 1, 0:32].unsqueeze(1).to_broadcast([128, 32, 32]), op=ALU.is_gt), [cnt, mc], [cmp_])
                nblk = T("nblk", [128, 32])
                op("dve", lambda e: e.reduce_sum(out=nblk[:, :], in_=cmp_[:, 0:32, :], axis=AX.X), [cmp_], [nblk])
                na = T("na", [128, 32])
                nb_ = T("nb", [128, 32])
                op("dve", lambda e: e.tensor_copy(out=na[:, :], in_=nblk[:, :]), [nblk], [na])
                a_, b_ = na, nb_
                for s_ in (1, 2, 4, 8, 16):
                    op("dve", lambda e: e.tensor_tensor(out=b_[:, s_:], in0=a_[:, s_:], in1=a_[:, :32 - s_], op=ALU.add), [a_], [b_])
                    op("dve", lambda e: e.tensor_copy(out=b_[:, :s_], in_=a_[:, :s_]), [a_], [b_])
                    a_, b_ = b_, a_
                pend = T("pend", [128, 32])
                pstart = T("pstart", [128, 32])
                op("dve", lambda e: e.tensor_scalar_mul(out=pend[:, :], in0=a_[:, :], scalar1=float(BLK)), [a_], [pend])
                op("dve", lambda e: e.scalar_tensor_tensor(out=pstart[:, :], in0=nblk[:, :], scalar=-float(BLK), in1=pend[:, :], op0=ALU.mult, op1=ALU.add), [nblk, pend], [pstart])
                op("dve", lambda e: e.tensor_tensor(out=rank[:, :, :], in0=rank[:, :, :], in1=pstart[:, :].unsqueeze(1).to_broadcast([128, NT, 32]), op=ALU.add), [rank, pstart], [rank])
                dest = T("dest", [128, 2, NT])
                for k in range(2):
                    op("dve", lambda e: e.tensor_tensor(out=sa[:, :, :], in0=Ak[k][:, :, :, :].rearrange("p j g i -> p j (g i)"), in1=rank[:, :, :], op=ALU.mult), [Ak[k], rank], [sa])
                    op("dve", lambda e: e.reduce_sum(out=dest[:, k, :], in_=sa[:, :, :], axis=AX.X), [sa], [dest])
                dest_i = T("dest_i", [128, 2, NT], I32)
                op("dve", lambda e: e.tensor_copy(out=dest_i[:, :, :], in_=dest[:, :, :]), [dest], [dest_i])
                op("dve", lambda e: e.tensor_tensor(out=cmp_[:, 0:NB, :], in0=pend[:, :].unsqueeze(1).to_broadcast([128, NB, 32]), in1=mc[:, 2, 0:NB].unsqueeze(2).to_broadcast([128, NB, 32]), op=ALU.is_le), [pend, mc], [cmp_])
                beid = T("beid", [128, 64])
                op("dve", lambda e: e.reduce_sum(out=beid[:, 0:NB], in_=cmp_[:, 0:NB, :], axis=AX.X), [cmp_], [beid])
                op("dve", lambda e: e.tensor_scalar_min(out=beid[:, 0:NB], in0=beid[:, 0:NB], scalar1=31.0), [beid], [beid])
                op("dve", lambda e: e.tensor_scalar(out=beid[:, 0:NB], in0=beid[:, 0:NB], scalar1=128.0, scalar2=float(layer * E_ * 128), op0=ALU.mult, op1=ALU.add), [beid], [beid])
                op("dve", lambda e: e.tensor_tensor(out=beid[:, 0:NB], in0=beid[:, 0:NB], in1=mc[:, 3, 0:1].to_broadcast([128, NB]), op=ALU.add), [beid, mc], [beid])
                widx = T("widx", [128, 64], I32)
                op("dve", lambda e: e.tensor_copy(out=widx[:, 0:NB], in_=beid[:, 0:NB]), [beid], [widx])
                NA = NSLOT // 128 + 1
                padrec = T("padrec", [128, NA, 4])
                op("dve", lambda e: e.memset(padrec[:, :, :], 0.0), [], [padrec])
                op("dve", lambda e: e.memset(padrec[:, :, 0:1], float(S)), [], [padrec])
                op("dve", lambda e: e.tensor_scalar_add(out=padrec[:, :, 2], in0=mc[:, 3, 0:1].to_broadcast([128, NA]), scalar1=float(2 * S)), [mc], [padrec])
                dma("sp", slot_d.ap().rearrange("(a p) c -> p a c", p=128), padrec[:, :, :], [padrec], [slot_d])
                rec = T("rec", [128, 2, NT, 4])
                op("dve", lambda e: e.memset(rec[:, :, :, :], 0.0), [], [rec])
                for k in range(2):
                    op("dve", lambda e: e.tensor_copy(out=rec[:, k, :, 0], in_=tokid[:, :]), [tokid], [rec])
                    op("dve", lambda e: e.tensor_copy(out=rec[:, k, :, 1], in_=gates[:, k, :]), [gates], [rec])
                    op("dve", lambda e: e.tensor_scalar_add(out=rec[:, k, :, 2], in0=tokid[:, :], scalar1=float(k * S)), [tokid], [rec])
                sc_bufs = []
                for k in range(2):
                    for j in range(NT):
                        sc_b = Buf(None, "slotscatter")
                        sc_bufs.append(sc_b)
                        op("pool", lambda e: e.indirect_dma_start(out=slot_d[:, :], out_offset=bass.IndirectOffsetOnAxis(ap=dest_i[:, k, j:j + 1], axis=0), in_=rec[:, k, j, :], in_offset=None), [rec, dest_i, slot_d], [sc_b], dma=True)
                SL = T("SL", [128, NA - 1, 4])
                dma("sp", SL[:, :, :], slot_d[0:NSLOT, :].rearrange("(a p) c -> p a c", p=128), [slot_d] + sc_bufs, [SL])
                tok_i = T("tok_i", [128, NA - 1], I32)
                row_i = T("row_i", [128, NA - 1], I32)
                gate_s = T("gate_s", [128, NA - 1])
                op("dve", lambda e: e.tensor_copy(out=tok_i[:, :], in_=SL[:, :, 0]), [SL], [tok_i])
                op("dve", lambda e: e.tensor_copy(out=row_i[:, :], in_=SL[:, :, 2]), [SL], [row_i])
                op("dve", lambda e: e.tensor_copy(out=gate_s[:, :], in_=SL[:, :, 1]), [SL], [gate_s])
                if "moe_dbg" in dumps:
                    dump("m_dest", dest[:, :, :], [128, 2, NT], F32, [dest])
                    dump("m_gates", gates[:, :, :], [128, 2, NT], F32, [gates])
                    dump("m_beid", beid[:, :], [128, 64], F32, [beid])
                    dump("m_SL", SL[:, :, :], [128, NA - 1, 4], F32, [SL])
                    dump("m_cnt", cnt[:, :], [128, 32], F32, [cnt])
                tstack[0] = bs
                Wgs = Rot([T(f"Wg{i}", [128, 8 * HID], BF16) for i in range(2)])
                Wus = Rot([T(f"Wu{i}", [128, 8 * HID], BF16) for i in range(2)])
                Wds = Rot([T(f"Wd{i}", [128, 4 * D], BF16) for i in range(2)])
                xgs = Rot([T(f"xg{i}", [128, D], BF16) for i in range(4)])
                xgTs = Rot([T(f"xgT{i}", [128, 8, BLK], BF16) for i in range(2)])
                acts = Rot([T(f"act{i}", [128, 4, BLK], BF16) for i in range(2)])
                sgs = Rot([T(f"sg{i}", [128, BLK], F32) for i in range(3)])
                ysbs = Rot([T(f"ysb{i}", [128, D], F32) for i in range(3)])
                identb = T("identb", [128, 128], BF16)
                op("dve", lambda e: e.tensor_copy(out=identb[:, :], in_=ident[:, :]), [ident], [identb])
                ptr = Rot(psb[0:2])
                pgu = Rot(psb[2:6])
                pyy = Rot(psb[6:8])
                def g_gu(b):
                    Wg, Wu = Wgs.next(), Wus.next()
                    for Wt, src in ((Wg, wgb), (Wu, wub)):
                        op("pool", lambda e: e.indirect_dma_start(out=Wt[:, :], out_offset=None, in_=src[:, :], in_offset=bass.IndirectOffsetOnAxis(ap=widx[:, b:b + 1], axis=0)), [src, widx], [Wt], dma=True)
                    xg2 = []
                    for hf in range(2):
                        a = 2 * b + hf
                        xg = xgs.next()
                        op("pool", lambda e: e.indirect_dma_start(out=xg[:, :], out_offset=None, in_=x1b_d[:, :], in_offset=bass.IndirectOffsetOnAxis(ap=tok_i[:, a:a + 1], axis=0)), [x1b_d, tok_i], [xg], dma=True)
                        xg2.append(xg)
                    return Wg, Wu, xg2

                def g_d(b):
                    Wd = Wds.next()
                    op("pool", lambda e: e.indirect_dma_start(out=Wd[:, :], out_offset=None, in_=wdb[:, :], in_offset=bass.IndirectOffsetOnAxis(ap=widx[:, b:b + 1], axis=0)), [wdb, widx], [Wd], dma=True)
                    return Wd

                def blk_X(b, Wg, Wu, xg2):
                    xgT = xgTs.next()
                    for hf in range(2):
                        xg = xg2[hf]
                        ps = ptr.next()
                        psv = ps[:, :].bitcast(BF16)
                        for c in range(8):
                            op("pe", lambda e: e.transpose(out=psv[:, c * 128:(c + 1) * 128], in_=xg[:, c * 128:(c + 1) * 128], identity=identb[:]), [xg, identb], [ps])
                        o_ap = xgT[:, :, hf * 128:(hf + 1) * 128]
                        i_ap = psv.rearrange("p (c n) -> p c n", c=8)
                        if hf == 0:
                            op("act", lambda e: e.copy(out=o_ap, in_=i_ap), [ps], [xgT])
                        else:
                            op("dve", lambda e: e.tensor_copy(out=o_ap, in_=i_ap), [ps], [xgT])
                    act_ = acts.next()
                    for m in range(4):
                        pg_, pu_ = pgu.next(), pgu.next()
                        for c in range(8):
                            op("pe", lambda e: e.matmul(pg_[:, 0:BLK], lhsT=Wg[:, c * HID + m * 128:c * HID + (m + 1) * 128], rhs=xgT[:, c, :], start=(c == 0), stop=(c == 7)), [Wg, xgT], [pg_])
                        for c in range(8):
                            op("pe", lambda e: e.matmul(pu_[:, 0:BLK], lhsT=Wu[:, c * HID + m * 128:c * HID + (m + 1) * 128], rhs=xgT[:, c, :], start=(c == 0), stop=(c == 7)), [Wu, xgT], [pu_])
                        sg = sgs.next()
                        op("act", lambda e: e.activation(out=sg[:, :], in_=pg_[:, 0:BLK], func=AF.Silu), [pg_], [sg])
                        op("dve", lambda e: e.tensor_tensor(out=act_[:, m, :], in0=sg[:, :], in1=pu_[:, 0:BLK], op=ALU.mult), [sg, pu_], [act_])
                    return act_

                def blk_Y(b, act_, Wd):
                    for hf in range(2):
                        a = 2 * b + hf
                        ysb = ysbs.next()
                        for n in range(2):
                            py = pyy.next()
                            for m in range(4):
                                op("pe", lambda e: e.matmul(py[:, :], lhsT=act_[:, m, hf * 128:(hf + 1) * 128], rhs=Wd[:, m * D + n * 512:m * D + (n + 1) * 512], start=(m == 0), stop=(m == 3)), [act_, Wd], [py])
                            if n == 0:
                                op("act", lambda e: e.activation(out=ysb[:, 0:512], in_=py[:, :], func=AF.Copy, scale=gate_s[:, a:a + 1]), [py, gate_s], [ysb])
                            else:
                                op("dve", lambda e: e.tensor_scalar_mul(out=ysb[:, 512:1024], in0=py[:, :], scalar1=gate_s[:, a:a + 1]), [py, gate_s], [ysb])
                        op("pool", lambda e: e.indirect_dma_start(out=y_d[:, :], out_offset=bass.IndirectOffsetOnAxis(ap=row_i[:, a:a + 1], axis=0), in_=ysb[:, :], in_offset=None), [ysb, row_i], [y_d], dma=True)

                gu = {0: g_gu(0)}
                wd_ = {0: g_d(0)}
                if NB > 1:
                    gu[1] = g_gu(1)
                    wd_[1] = g_d(1)
                acts_of = {0: blk_X(0, *gu.pop(0))}
                for b in range(NB):
                    if b + 1 < NB:
                        acts_of[b + 1] = blk_X(b + 1, *gu.pop(b + 1))
                    if b + 2 < NB:
                        gu[b + 2] = g_gu(b + 2)
                    blk_Y(b, acts_of.pop(b), wd_.pop(b))
                    if b + 2 < NB:
                        wd_[b + 2] = g_d(b + 2)
                fw.barrier()
                bs.close()
                tstack[0] = ph
                g_t = T("g", [128, D])
                b_t = T("b", [128, D])
                dma("sp", g_t[:], ln_ffn_g[layer:layer + 1, :].broadcast_to([128, D]), [ln_ffn_g], [g_t])
                dma("sp", b_t[:], ln_ffn_b[layer:layer + 1, :].broadcast_to([128, D]), [ln_ffn_b], [b_t])
                xts = Rot([T(f"cx{i}", [128, D]) for i in range(4)])
                y0s = Rot([T(f"cy0{i}", [128, D]) for i in range(4)])
                y1s = Rot([T(f"cy1{i}", [128, D]) for i in range(4)])
                junks = Rot([T(f"cj{i}", [128, D]) for i in range(4)])
                outs = Rot([T(f"co{i}", [128, D]) for i in range(4)])
                stts = Rot([T(f"cst{i}", [128, 8]) for i in range(4)])
                def m_A(i):
                    isl = slice(i * 128, (i + 1) * 128)
                    xt, y0, y1 = xts.next(), y0s.next(), y1s.next()
                    dma("sp", xt[:], x1_d[isl, :], [x1_d], [xt])
                    dma("sp", y0[:], y_d[isl, :], [y_d], [y0])
                    dma("sp", y1[:], y_d[S + i * 128:S + (i + 1) * 128, :], [y_d], [y1])
                    op("pool", lambda e: e.tensor_tensor(out=y1[:, :], in0=y0[:, :], in1=y1[:, :], op=ALU.add), [y0, y1], [y1])
                    op("dve", lambda e: e.scalar_tensor_tensor(out=y0[:, :], in0=xt[:, :], scalar=float(ALPHA), in1=y1[:, :], op0=ALU.mult, op1=ALU.add), [xt, y1], [y0])
                    junk, stt = junks.next(), stts.next()
                    ln_stage1(y0, stt, junk)
                    return (y0, junk, stt)

                def m_B1f(i, st3):
                    y0, junk, stt = st3
                    ln_front(y0, stt, junk)

                def m_B1b(i, st3):
                    isl = slice(i * 128, (i + 1) * 128)
                    y0, junk, stt = st3
                    o_t = outs.next()
                    ln_back(g_t, b_t, o_t, junk)
                    dma("pool", dst[isl, :], o_t[:, :], [o_t], [dst])

                stA = {}
                for i in range(-2, NT):
                    if 0 <= i + 1 < NT:
                        m_B1f(i + 1, stA[i + 1])
                    if 0 <= i + 2 < NT:
                        stA[i + 2] = m_A(i + 2)
                    if 0 <= i + 1 < NT:
                        m_B1b(i + 1, stA.pop(i + 1))
                fw.barrier()

        phase_C(0, w_out_ab, x_in)
        if upto == "C0":
            return nc, ext, out_d, dump_bufs
        phase_M(0, x2_d)
        if upto == "M0":
            return nc, ext, out_d, dump_bufs
        PATS = (1, 4, 16)
        with ExitStack() as ph:
            xT = fw.sb("l1_xT", [128, 8, S], BF16, ph)
            Wc = fw.sb("l1_W", [128, 8, 3 * D], BF16, ph, multi=True)
            for c in range(8):
                dma("pool", Wc[:, c, :], w_in_c[c * 128:(c + 1) * 128, :], [w_in_c], [Wc])
            stg = Rot([fw.sb(f"l1_x{i}", [128, D], F32, ph) for i in range(2)])
            build_xT(x2_d, xT, Rot(psb[0:2]), stg)
            psr = Rot(psb[2:8])
            sbf = Rot([fw.sb(f"l1_sb{i}", [128, 512], BF16, ph) for i in range(4)])
            vst = Rot([fw.sb(f"l1_v{i}", [128, 16, 65], BF16, ph) for i in range(6)])
            for v_ in vst.bufs:
                op("dve", lambda e: e.memset(v_[:, :, :], 1.0), [], [v_])
            for tb in range(8):
                tsl = slice(tb * 512, (tb + 1) * 512)
                for ch in range(16):
                    ps = psr.next()
                    for c in range(8):
                        op("pe", lambda e: e.matmul(ps[:, :], lhsT=Wc[:, c, ch * 128:(ch + 1) * 128], rhs=xT[:, c, tsl], start=(c == 0), stop=(c == 7)), [Wc, xT], [ps])
                    s_ = sbf.next()
                    if ch < 8:
                        op("act", lambda e: e.activation(out=s_[:, :], in_=ps[:, :], func=AF.Copy, scale=0.125), [ps], [s_])
                    else:
                        op("dve", lambda e: e.tensor_copy(out=s_[:, :], in_=ps[:, :]), [ps], [s_])
                    dma("pool", qkC[ch, :, tsl], s_[:, :], [s_], [qkC])
            for ri, r in enumerate(PATS):
                nqb = S // r // 128
                for res in range(r):
                    for kt in range(nqb):
                        t = res * nqb + kt
                        tok = ssl(res + r * 128 * kt, 128, r)
                        v_ = vst.next()
                        for n in range(2):
                            ps = psr.next()
                            for c in range(8):
                                op("pe", lambda e: e.matmul(ps[:, :], lhsT=xT[:, c, tok], rhs=Wc[:, c, 2048 + n * 512:2048 + (n + 1) * 512], start=(c == 0), stop=(c == 7)), [xT, Wc], [ps])
                            o_ap = v_[:, n * 8:(n + 1) * 8, 0:64]
                            i_ap = ps[:, :].rearrange("p (h e) -> p h e", e=64)
                            if n == 0:
                                op("act", lambda e: e.copy(out=o_ap, in_=i_ap), [ps], [v_])
                            else:
                                op("dve", lambda e: e.tensor_copy(out=o_ap, in_=i_ap), [ps], [v_])
                        for g in range(4):
                            dma("sp" if g % 2 == 0 else "pool", vC[ri, g, :, t, :], v_[:, g * 4:(g + 1) * 4, :].rearrange("p h e -> p (h e)"), [v_], [vC])
            fw.barrier()
        if upto == "A1x":
            return nc, ext, out_d, dump_bufs

        with ExitStack() as ph:
            dng = fw.sb("d_negd", [128, 3, 512], F32, ph)
            dma("sp", dng[:], dil_negd_d.ap(), [dil_negd_d], [dng])
            sel = fw.sb("d_sel", [128, 64], F32, ph)
            dma("sp", sel[:], sel_d.ap(), [sel_d], [sel])
            QZ = [fw.sb(f"d_QZ{par}", [128, 2, S], BF16, ph) for par in range(2)]
            for par in range(2):
                op("pool", lambda e: e.memset(QZ[par][:, :, :], 0.0), [], [QZ[par]])
            KT = fw.sb("d_KT", [128, 2, S], BF16, ph, multi=True)
            Vr = [fw.sb(f"d_V{ri}", [128, NT, 260], BF16, ph) for ri in range(3)]
            OaccL = [fw.sb(f"d_O{i}", [65, S], F32, ph) for i in range(4)]
            tmps = Rot([fw.sb(f"d_t{i}", [128, 512], F32, ph) for i in range(4)])
            Es = Rot([fw.sb(f"d_e{i}", [128, 512], BF16, ph) for i in range(8)])
            rcp = Rot([fw.sb(f"d_r{i}", [64, 512], F32, ph) for i in range(4)])
            obs = Rot([fw.sb(f"d_ob{i}", [64, 512], BF16, ph) for i in range(2)])
            pscore = Rot(psb[0:6])
            pov = Rot(psb[6:8])
            dslopes = alibi_slopes(16)
            LAG = 1
            gjobs = []
            for g in range(4):
                for hl in range(4):
                    h = 4 * g + hl
                    for ri, r in enumerate(PATS):
                        nqb = S // r // 128
                        G = min(4, nqb)
                        for res in range(r):
                            for qg in range(0, nqb, G):
                                gjobs.append(dict(g=g, hl=hl, h=h, ri=ri, r=r, nqb=nqb, G=G, res=res, qg=qg, first_g=False, last_h=False))
                    gjobs[-1]["last_h"] = True
            seen_g = set()
            for j in gjobs:
                if j["g"] not in seen_g:
                    seen_g.add(j["g"])
                    j["first_g"] = True

            def load_group(g):
                for cl in range(2):
                    for par in range(2):
                        dma("sp", QZ[par][par * 64:(par + 1) * 64, cl, :], qkC[2 * g + cl, par * 64:(par + 1) * 64, :], [qkC], [QZ[par]])
                    dma("sp", KT[:, cl, :], qkC[8 + 2 * g + cl, :, :], [qkC], [KT])
                for ri in range(3):
                    dma("sp", Vr[ri][:, :, :], vC[ri, g, :, :, :], [vC], [Vr[ri]])

            def d_stage1(j):
                if j["first_g"]:
                    load_group(j["g"])
                hl, h, r, nqb, G, res, qg = j["hl"], j["h"], j["r"], j["nqb"], j["G"], j["res"], j["qg"]
                cl, r0 = hl // 2, (hl % 2) * 64
                blocks = list(range(qg, qg + G))
                Et, rng = [], []
                for typ in range(3):
                    ps = pscore.next()
                    val = [il for il, i in enumerate(blocks) if 0 <= i + typ - 1 < nqb]
                    lo, hi = val[0], val[-1] + 1
                    for il in val:
                        i = blocks[il]
                        kt = i + typ - 1
                        ksl = ssl(res + r * 128 * kt, 128, r)
                        qsl = ssl(res + r * 128 * i, 128, r)
                        op("pe", lambda e: e.matmul(ps[:, il * 128:(il + 1) * 128], lhsT=KT[:, cl, ksl], rhs=QZ[hl % 2][:, cl, qsl], start=True, stop=True), [KT, QZ[hl % 2]], [ps])
                    tmp, E = tmps.next(), Es.next()
                    op("dve", lambda e: e.scalar_tensor_tensor(out=tmp[:, lo * 128:hi * 128], in0=dng[:, typ, lo * 128:hi * 128], scalar=float(dslopes[h] * r), in1=ps[:, lo * 128:hi * 128], op0=ALU.mult, op1=ALU.add), [dng, ps], [tmp])
                    op("act", lambda e: e.activation(out=E[:, lo * 128:hi * 128], in_=tmp[:, lo * 128:hi * 128], func=AF.Exp), [tmp], [E])
                    Et.append(E)
                    rng.append(val)
                j["Et"], j["rng"], j["blocks"] = Et, rng, blocks

            def d_stage2(j):
                hl, h, ri, r, nqb, G, res, qg = j["hl"], j["h"], j["ri"], j["r"], j["nqb"], j["G"], j["res"], j["qg"]
                r0 = (hl % 2) * 64
                Et, rng, blocks = j["Et"], j["rng"], j["blocks"]
                po = pov.next()
                for il, i in enumerate(blocks):
                    typs = [typ for typ in range(3) if il in rng[typ]]
                    for n_, typ in enumerate(typs):
                        kt = i + typ - 1
                        t = res * nqb + kt
                        op("pe", lambda e: e.matmul(po[0:65, il * 128:(il + 1) * 128], lhsT=Vr[ri][:, t, hl * 65:(hl + 1) * 65], rhs=Et[typ][:, il * 128:(il + 1) * 128], start=(n_ == 0), stop=(n_ == len(typs) - 1)), [Vr[ri], Et[typ]], [po])
                osl = ssl(res + r * 128 * qg, 128 * G, r)
                Oacc = OaccL[hl]
                if ri == 0:
                    op("act", lambda e: e.copy(out=Oacc[0:65, osl], in_=po[0:65, 0:G * 128]), [po], [Oacc])
                else:
                    op("dve", lambda e: e.tensor_tensor(out=Oacc[0:65, osl], in0=Oacc[0:65, osl], in1=po[0:65, 0:G * 128], op=ALU.add), [Oacc, po], [Oacc])
                if j["last_h"]:
                    for _ in norm_gen(hl, h, r0):
                        pass

            def norm_gen(hl, h, r0):
                    Oacc = OaccL[hl]
                    for tb in range(8):
                        tsl = slice(tb * 512, (tb + 1) * 512)
                        pn = pov.next()
                        op("pe", lambda e: e.matmul(pn[0:64, :], lhsT=sel[0:65, :], rhs=Oacc[0:65, tsl], start=True, stop=True), [sel, Oacc], [pn])
                        rc, rc2, o_b = rcp.next(), rcp.next(), obs.next()
                        op("act", lambda e: e.activation(out=rc2[:, :], in_=pn[0:64, :], func=AF.Ln), [pn], [rc2])
                        op("act", lambda e: e.activation(out=rc[:, :], in_=rc2[:, :], func=AF.Exp, scale=-1.0), [rc2], [rc])
                        op("dve", lambda e: e.tensor_tensor(out=rc2[:, :], in0=rc[:, :], in1=pn[0:64, :], op=ALU.mult), [rc, pn], [rc2])
                        op("dve", lambda e: e.tensor_scalar(out=rc2[:, :], in0=rc2[:, :], scalar1=-1.0, scalar2=2.0, op0=ALU.mult, op1=ALU.add), [rc2], [rc2])
                        op("dve", lambda e: e.tensor_tensor(out=rc[:, :], in0=rc[:, :], in1=rc2[:, :], op=ALU.mult), [rc, rc2], [rc])
                        op("dve", lambda e: e.tensor_tensor(out=o_b[:, :], in0=Oacc[0:64, tsl], in1=rc[:, :], op=ALU.mult), [Oacc, rc], [o_b])
                        dma("pool", oT_d[h // 2, r0:r0 + 64, tsl], o_b[:, :], [o_b], [oT_d])
                        yield

            dnorm = []

            def step_norm(drain=False):
                for gn in list(dnorm):
                    while True:
                        try:
                            next(gn)
                        except StopIteration:
                            dnorm.remove(gn)
                            break
                        if not drain:
                            break

            pend = []
            for j in gjobs:
                if j["first_g"]:
                    while pend:
                        d_stage2(pend.pop(0))
                d_stage1(j)
                pend.append(j)
                if len(pend) > LAG:
                    d_stage2(pend.pop(0))
                step_norm()
            while pend:
                d_stage2(pend.pop(0))
            step_norm(drain=True)
            fw.barrier()
        if upto == "B1":
            return nc, ext, out_d, dump_bufs
        phase_C(1, w_out_c, x2_d)
        phase_M(1, out_d)
    return nc, ext, out_d, dump_bufs


def host_consts():
    c = {}
    c["ident"] = np.eye(128, dtype=np.float32)
    k = np.arange(128, dtype=np.float32)[:, None]
    n = np.arange(NEGD_W, dtype=np.float32)[None, :]
    c["negd"] = (-np.abs(n - k - NEGD_C)).astype(np.float32)
    half = 16
    inv_freq = np.power(np.float32(10000.0), -np.arange(half, dtype=np.float32) * np.float32(2.0) / np.float32(32)).astype(np.float32)
    ang = (np.arange(S, dtype=np.float32)[:, None] * inv_freq[None, :]).astype(np.float32)
    cos = np.cos(ang).astype(np.float32).T
    sin = np.sin(ang).astype(np.float32).T
    c32 = np.concatenate([cos, cos], 0)
    s32 = np.concatenate([-sin, sin], 0)
    c["ropecos"] = np.ascontiguousarray(np.tile(c32, (4, 1)))
    c["ropesin"] = np.ascontiguousarray(np.tile(s32, (4, 1)))
    kk = np.arange(128, dtype=np.float32)[:, None]
    qq = np.arange(128, dtype=np.float32)[None, :]
    tabs = []
    for typ in range(3):
        d = kk - qq + 128.0 * (typ - 1)
        t = np.where(np.abs(d) <= 64, -np.abs(d), -1.0e9).astype(np.float32)
        tabs.append(np.tile(t, (1, 4)))
    c["dil_negd"] = np.ascontiguousarray(np.stack(tabs, 1))
    mc = np.zeros((128, 4, 64), np.float32)
    mc[:, 0, :32] = np.arange(32, dtype=np.float32)[None, :]
    mc[:, 1, :32] = 256.0 * np.arange(32, dtype=np.float32)[None, :]
    mc[:, 2, :63] = 256.0 * np.arange(63, dtype=np.float32)[None, :]
    c["moe_c"] = mc
    mc[:, 3, :] = np.arange(128, dtype=np.float32)[:, None]
    c["tokid"] = (np.arange(NT, dtype=np.float32)[None, :] * 128 + np.arange(128, dtype=np.float32)[:, None]).astype(np.float32)
    sl = np.zeros((128, 64), np.float32)
    sl[64, :] = 1.0
    c["sel"] = sl
    c["triu"] = np.triu(np.ones((128, 128), np.float32), 1)
    return c


def host_weights(inp):
    w = {}
    wi = inp["w_in_ab"][0]
    w["w_in_ab"] = np.ascontiguousarray(np.concatenate([wi, wi[:, 1936:1952], wi[:, 1920:1936]], 1))
    wq = inp["w_uq"][0]
    nope = [wq[:, h * 96:h * 96 + 64] for h in range(4)]
    rope = [wq[:, h * 96 + 64:h * 96 + 96] for h in range(4)]
    rsw = [np.concatenate([wq[:, h * 96 + 80:h * 96 + 96], wq[:, h * 96 + 64:h * 96 + 80]], 1) for h in range(4)]
    w["w_uq"] = np.ascontiguousarray(np.concatenate(nope + rope + rsw, 1))
    wk = inp["w_ukv"][0]
    kn = [wk[:, h * 192:h * 192 + 64] for h in range(4)]
    vv = [wk[:, h * 192 + 64:h * 192 + 192] for h in range(4)]
    w["w_ukv"] = np.ascontiguousarray(np.concatenate(kn + vv, 1))
    w["w_out_ab"] = np.ascontiguousarray(inp["w_out_ab"][0])
    w["lam4"] = np.ascontiguousarray(np.stack([inp["lam_q1"][0], inp["lam_k1"][0], inp["lam_q2"][0], inp["lam_k2"][0]], 0))
    w["diff_g"] = np.ascontiguousarray(inp["diff_norm_g"][0].reshape(128, 1))
    w["qn_g"] = np.ascontiguousarray(inp["mla_q_norm_g"][0].reshape(2, 128).T)
    w["kvn_g"] = np.ascontiguousarray(inp["mla_kv_norm_g"][0].reshape(128, 1))
    for k in ("ln_mix_g", "ln_mix_b", "ln_ffn_g", "ln_ffn_b"):
        w[k] = np.ascontiguousarray(inp[k])
    w["w_router"] = np.ascontiguousarray(np.concatenate([inp["moe_w_group"], inp["moe_w_route"]], 2))
    w["b_router"] = np.ascontiguousarray(np.concatenate([inp["moe_b_group"], inp["moe_b_route"]], 1))
    g = inp["moe_w_gate"].reshape(2, E_, 8, 128, HID).transpose(0, 1, 3, 2, 4)
    w["wg_l"] = np.ascontiguousarray(g).reshape(2 * E_ * 128, 8 * HID)
    u = inp["moe_w_up"].reshape(2, E_, 8, 128, HID).transpose(0, 1, 3, 2, 4)
    w["wu_l"] = np.ascontiguousarray(u).reshape(2 * E_ * 128, 8 * HID)
    dd = inp["moe_w_down"].reshape(2, E_, 4, 128, D).transpose(0, 1, 3, 2, 4)
    w["wd_l"] = np.ascontiguousarray(dd).reshape(2 * E_ * 128, 4 * D)
    w["w_in_c"] = np.ascontiguousarray(inp["w_in_c"][0])
    w["w_out_c"] = np.ascontiguousarray(inp["w_out_c"][0])
    return w


def kernel(**inputs):
    inp = {k: np.asarray(v) for k, v in inputs.items()}
    nc, ext, out_d, _ = build_program()
    shared = host_consts()
    shared.update(host_weights(inp))
    x = np.ascontiguousarray(inp["x"], dtype=np.float32)
    in_maps = []
    for c in range(8):
        m = dict(shared)
        m["x"] = x[c]
        in_maps.append(m)
    res = run_bass_kernel_spmd(nc, in_maps, core_ids=list(range(8)))
    return np.stack([np.asarray(r["out"]) for r in res.results], 0).astype(np.float32)
```

```python
import math
import numpy as np
from contextlib import ExitStack
import concourse.bass as bass
import concourse.mybir as mybir
from concourse.bass_utils import run_bass_kernel_spmd

F32 = mybir.dt.float32
BF16 = mybir.dt.bfloat16
I32 = mybir.dt.int32
ALU = mybir.AluOpType
AF = mybir.ActivationFunctionType
AX = mybir.AxisListType

S = 4096
D = 1024
NT = S // 128
DEPTH = 2
ALPHA = (2 * DEPTH) ** 0.25
EPS = 1e-5
NPOOL = 16
E_ = 32
HID = 512
BLK = 256
NB = 63
NSLOT = NB * BLK
NEGD_C = 3968
NEGD_W = 8064


class Buf:
    __slots__ = ("t", "w", "r", "name", "multi", "wm")

    def __init__(self, t, name="", multi=False):
        self.t = t
        self.w = None
        self.r = {}
        self.name = name
        self.multi = multi
        self.wm = {}

    def __getitem__(self, k):
        return self.t[k]

    def ap(self):
        return self.t.ap()


class FW:
    def __init__(self, nc, stack):
        self.nc = nc
        self.stack = stack
        self.engs = {"pe": nc.tensor, "act": nc.scalar, "dve": nc.vector, "pool": nc.gpsimd, "sp": nc.sync}
        self.esem, self.cnt, self.seen, self.dq = {}, {}, {}, {}
        for e in self.engs:
            self.esem[e] = stack.enter_context(nc.semaphore("es_" + e))
            self.cnt[e] = 0
            self.seen[e] = {}
        for q in ("sp", "pool", "act"):
            sems = [stack.enter_context(nc.semaphore(f"dq_{q}_{i}")) for i in range(NPOOL)]
            self.dq[q] = {"sems": sems, "n": 0}
        self.n_inst = 0
        self.uid = 0

    def sb(self, name, shape, dtype, stack=None, multi=False):
        st = stack if stack is not None else self.stack
        self.uid += 1
        return Buf(st.enter_context(self.nc.sbuf_tensor(f"s{self.uid}_{name}", list(shape), dtype)), name, multi)

    def ps(self, name, shape, dtype=F32, stack=None):
        st = stack if stack is not None else self.stack
        return Buf(st.enter_context(self.nc.psum_tensor("p_" + name, list(shape), dtype)), name)

    def dram(self, name, shape, dtype, kind="Internal"):
        return Buf(self.nc.dram_tensor(name, list(shape), dtype, kind=kind), name, True)

    def op(self, eng, fn, reads=(), writes=(), dma=False):
        waits = {}
        own = self.esem[eng]
        seen = self.seen[eng]

        def need(sem, val):
            if eng == "pe" and sem is own:
                return
            if seen.get(sem, 0) >= val:
                return
            if waits.get(sem, 0) < val:
                waits[sem] = val

        for b in reads:
            if b.multi:
                for s_, v_ in b.wm.items():
                    need(s_, v_)
            elif b.w is not None:
                need(*b.w)
        for b in writes:
            if not b.multi and b.w is not None:
                need(*b.w)
            for s_, v_ in b.r.items():
                need(s_, v_)
        if dma:
            q = self.dq[eng]
            j = q["n"]
            s = q["sems"][j % NPOOL]
            if j >= NPOOL:
                need(s, 16 * (j // NPOOL))
        e = self.engs[eng]
        for sem, val in waits.items():
            e.wait_ge(sem, val)
            seen[sem] = val
        ins = fn(e)
        self.n_inst += 1
        if dma:
            ins.then_inc(s, 16)
            ev = (s, 16 * (j // NPOOL + 1))
            q["n"] += 1
        else:
            self.cnt[eng] += 1
            ins.then_inc(own, 1)
            ev = (own, self.cnt[eng])
        for b in reads:
            if b.r.get(ev[0], 0) < ev[1]:
                b.r[ev[0]] = ev[1]
        for b in writes:
            if b.multi:
                if b.wm.get(ev[0], 0) < ev[1]:
                    b.wm[ev[0]] = ev[1]
            else:
                b.w = ev
                b.r = {}
        return ev

    def barrier(self):
        evs = []
        for e in self.engs:
            if self.cnt[e] > 0:
                evs.append((self.esem[e], self.cnt[e]))
        for q in self.dq.values():
            n = q["n"]
            for i, s in enumerate(q["sems"]):
                k = (n - i + NPOOL - 1) // NPOOL
                if k > 0:
                    evs.append((s, 16 * k))
        for e, eo in self.engs.items():
            for sem, val in evs:
                if sem is self.esem[e]:
                    continue
                if self.seen[e].get(sem, 0) >= val:
                    continue
                eo.wait_ge(sem, val)
                self.seen[e][sem] = val


class Rot:
    def __init__(self, bufs):
        self.bufs = bufs
        self.i = 0

    def next(self):
        b = self.bufs[self.i % len(self.bufs)]
        self.i += 1
        return b


def ssl(start, n, step):
    return slice(start, start + step * (n - 1) + 1, step)


def alibi_slopes(n):
    start = 2.0 ** (-8.0 / n)
    return [start ** (i + 1) for i in range(n)]


def build_program(upto=None, dumps=()):
    nc = bass.Bass("TRN2", target_bir_lowering=False)
    ext = {}

    def ein(name, shape, dtype=F32):
        ext[name] = Buf(nc.dram_tensor(name, list(shape), dtype, kind="ExternalInput"), name)
        return ext[name]

    x_in = ein("x", [S, D])
    ident_d = ein("ident", [128, 128])
    negd_d = ein("negd", [128, NEGD_W])
    cos_d = ein("ropecos", [128, S])
    sin_d = ein("ropesin", [128, S])
    w_in_ab = ein("w_in_ab", [D, 1984])
    w_uq = ein("w_uq", [256, 512])
    w_ukv = ein("w_ukv", [128, 768])
    w_out_ab = ein("w_out_ab", [D, D])
    lam4 = ein("lam4", [4, 64])
    diff_g = ein("diff_g", [128, 1])
    qn_g = ein("qn_g", [128, 2])
    kvn_g = ein("kvn_g", [128, 1])
    ln_mix_g = ein("ln_mix_g", [2, D])
    ln_mix_b = ein("ln_mix_b", [2, D])
    ln_ffn_g = ein("ln_ffn_g", [2, D])
    ln_ffn_b = ein("ln_ffn_b", [2, D])
    w_router = ein("w_router", [2, D, 36])
    b_router = ein("b_router", [2, 36])
    wg_l = ein("wg_l", [2 * E_ * 128, 8 * HID])
    wu_l = ein("wu_l", [2 * E_ * 128, 8 * HID])
    wd_l = ein("wd_l", [2 * E_ * 128, 4 * D])
    w_in_c = ein("w_in_c", [D, 3 * D])
    w_out_c = ein("w_out_c", [D, D])
    dil_negd_d = ein("dil_negd", [128, 3, 512])
    moe_c = ein("moe_c", [128, 4, 64])
    triu_d = ein("triu", [128, 128])
    tokid_d = ein("tokid", [128, NT])
    sel_d = ein("sel", [128, 64])
    out_d = Buf(nc.dram_tensor("out", [S, D], F32, kind="ExternalOutput"), "out", True)

    dump_bufs = {}

    with ExitStack() as st:
        fw = FW(nc, st)
        op = fw.op

        def dma(q, out_ap, in_ap, reads, writes):
            return op(q, lambda e: e.dma_start(out=out_ap, in_=in_ap), reads, writes, dma=True)

        _dram0 = fw.dram
        fw.dram = lambda name, shape, dtype: _dram0(name, shape, dtype, kind=("ExternalOutput" if name in dumps else "Internal"))
        qkA = fw.dram("qkA", [8, 128, S], BF16)
        vA = fw.dram("vA", [S, 512], BF16)
        cq_d = fw.dram("cq_d", [256, S], F32)
        ckv_d = fw.dram("ckv_d", [128, S], F32)
        qmT = fw.dram("qmT", [4, 96, S], BF16)
        kmT = fw.dram("kmT", [4, 96, S], BF16)
        vM = fw.dram("vM", [S, 512], BF16)
        oT_d = fw.dram("oT_d", [8, 128, S], BF16)
        x1_d = fw.dram("x1_d", [S, D], F32)
        x1b_d = fw.dram("x1b_d", [S + 1, D], BF16)
        x2_d = fw.dram("x2_d", [S, D], F32)
        slot_d = fw.dram("slot_d", [NSLOT + 128, 4], F32)
        y_d = fw.dram("y_d", [2 * S + 128, D], F32)
        wgb = fw.dram("wgb", [2 * E_ * 128, 8 * HID], BF16)
        wub = fw.dram("wub", [2 * E_ * 128, 8 * HID], BF16)
        wdb = fw.dram("wdb", [2 * E_ * 128, 4 * D], BF16)
        cast_list = []
        CR = 512
        for l_ in range(2):
            for src_, dst_ in ((wg_l, wgb), (wu_l, wub), (wd_l, wdb)):
                for r_ in range(l_ * E_ * 128, (l_ + 1) * E_ * 128, CR):
                    cast_list.append((src_, dst_, r_))
        cast_bufs = []

        def issue_cast(n=1):
            for _ in range(n):
                if not cast_list:
                    return
                src_, dst_, r_ = cast_list.pop(0)
                cb = Buf(None, "castchunk")
                cast_bufs.append(cb)
                dma("pool", dst_[r_:r_ + CR, :], src_[r_:r_ + CR, :], [src_], [cb])
        qkC = fw.dram("qkC", [16, 128, S], BF16)
        vC = fw.dram("vC", [3, 4, 128, NT, 260], BF16)

        ident = fw.sb("ident", [128, 128], F32)
        dma("sp", ident[:], ident_d.ap(), [ident_d], [ident])
        ones_bf = fw.sb("ones_bf", [128, 128], BF16)
        op("dve", lambda e: e.memset(ones_bf[:], 1.0), [], [ones_bf])
        ones_f = fw.sb("ones_f", [128, 128], F32)
        op("dve", lambda e: e.memset(ones_f[:], 1.0), [], [ones_f])
        eps_t = fw.sb("eps_t", [128, 1], F32)
        op("dve", lambda e: e.memset(eps_t[:], EPS), [], [eps_t])
        psb = [fw.ps(f"psb{i}", [128, 512], F32) for i in range(8)]

        def dump(name, buf_ap, shape, dtype, reads):
            if name in dumps:
                d = Buf(nc.dram_tensor("dbg_" + name, list(shape), dtype, kind="ExternalOutput"), name)
                dump_bufs[name] = d
                dma("sp", d.ap(), buf_ap, reads, [d])

        def build_xT(src_d, xT, pst, stg):
            for i in range(NT):
                xt = stg.next()
                dma("sp", xt[:], src_d[i * 128:(i + 1) * 128, :], [src_d], [xt])
                for hf in range(2):
                    ps = pst.next()
                    for c4 in range(4):
                        c = hf * 4 + c4
                        op("pe", lambda e: e.transpose(out=ps[:, c4 * 128:(c4 + 1) * 128], in_=xt[:, c * 128:(c + 1) * 128], identity=ident[:]), [xt, ident], [ps])
                    eng = "act" if hf == 0 else "dve"
                    o_ap = xT[:, hf * 4:(hf + 1) * 4, i * 128:(i + 1) * 128]
                    i_ap = ps[:, :].rearrange("p (c n) -> p c n", c=4)
                    if eng == "act":
                        op("act", lambda e: e.copy(out=o_ap, in_=i_ap), [ps], [xT])
                    else:
                        op("dve", lambda e: e.tensor_copy(out=o_ap, in_=i_ap), [ps], [xT])

        def attn_core(QT, KT, Vsb, krows, q0, po, pss, pscore, slope, negd, tmps, es, band=None):
            qb_, qr = QT
            kb_, kr = KT
            kbs = list(range(NT))
            for n, kb in enumerate(kbs):
                ps = pscore.next()
                op("pe", lambda e: e.matmul(ps[:, :], lhsT=kb_[kr:kr + krows, kb * 128:(kb + 1) * 128], rhs=qb_[qr:qr + krows, q0:q0 + 512], start=True, stop=True), [kb_, qb_], [ps])
                E = es.next()
                if slope is not None:
                    tmp = tmps.next()
                    n0 = q0 - kb * 128 + NEGD_C
                    op("dve", lambda e: e.scalar_tensor_tensor(out=tmp[:, :], in0=negd[:, n0:n0 + 512], scalar=float(slope), in1=ps[:, :], op0=ALU.mult, op1=ALU.add), [negd, ps], [tmp])
                    op("act", lambda e: e.activation(out=E[:, :], in_=tmp[:, :], func=AF.Exp), [tmp], [E])
                else:
                    op("act", lambda e: e.activation(out=E[:, :], in_=ps[:, :], func=AF.Exp), [ps], [E])
                first, last = (n == 0), (n == len(kbs) - 1)
                op("pe", lambda e: e.matmul(po[:, :], lhsT=Vsb[:, kb, :], rhs=E[:, :], start=first, stop=last), [Vsb, E], [po])
                op("pe", lambda e: e.matmul(pss[:, :], lhsT=ones_bf[:, :], rhs=E[:, :], start=first, stop=last), [ones_bf, E], [pss])

        def rstd_from_ssq(ps_ssq, n, out_t, tmp_t):
            op("act", lambda e: e.activation(out=tmp_t[:, :], in_=ps_ssq[:, :], func=AF.Sqrt, bias=eps_t[:, 0:1], scale=1.0 / n), [ps_ssq, eps_t], [tmp_t])
            op("dve", lambda e: e.reciprocal(out=out_t[:, :], in_=tmp_t[:, :]), [tmp_t], [out_t])

        with ExitStack() as ph:
            xT = fw.sb("xT", [128, 8, S], BF16, ph)
            Win = fw.sb("Win", [128, 8, 1984], BF16, ph, multi=True)
            for c in range(8):
                dma("pool", Win[:, c, :], w_in_ab[c * 128:(c + 1) * 128, :], [w_in_ab], [Win])
            cosT = fw.sb("cosT", [128, S], F32, ph)
            sinT = fw.sb("sinT", [128, S], F32, ph)
            dma("sp", cosT[:], cos_d.ap(), [cos_d], [cosT])
            dma("sp", sinT[:], sin_d.ap(), [sin_d], [sinT])
            stg = Rot([fw.sb(f"a0_x{i}", [128, D], F32, ph) for i in range(2)])
            pst = Rot(psb[0:2])
            build_xT(x_in, xT, pst, stg)
            dump("xT", xT[:, :, :], [128, 8, S], BF16, [xT])
            psr = Rot(psb[2:8])
            sbf = Rot([fw.sb(f"a0_sb{i}", [128, 512], BF16, ph) for i in range(4)])
            sf = Rot([fw.sb(f"a0_sf{i}", [128, 512], F32, ph) for i in range(4)])
            tog = [0]

            def evac(out_ap, in_ap, reads, writes, scale=None):
                tog[0] ^= 1
                if tog[0] or scale is not None:
                    if scale is None:
                        op("act", lambda e: e.copy(out=out_ap, in_=in_ap), reads, writes)
                    else:
                        op("act", lambda e: e.activation(out=out_ap, in_=in_ap, func=AF.Copy, scale=float(scale)), reads, writes)
                else:
                    op("dve", lambda e: e.tensor_copy(out=out_ap, in_=in_ap), reads, writes)

            def proj_fm(Wsb, nchunk, col0, M, rhs_fn, rhs_reads, tb):
                ps = psr.next()
                for c in range(nchunk):
                    op("pe", lambda e: e.matmul(ps[0:M, :], lhsT=Wsb[:, c, col0:col0 + M], rhs=rhs_fn(c), start=(c == 0), stop=(c == nchunk - 1)), [Wsb] + rhs_reads, [ps])
                return ps

            for tb in range(8):
                tsl = slice(tb * 512, (tb + 1) * 512)
                rf = lambda c: xT[:, c, tsl]
                for h in range(8):
                    ps = proj_fm(Win, 8, h * 128, 128, rf, [xT], tb)
                    s_ = sbf.next()
                    evac(s_[:, :], ps[:, :], [ps], [s_], scale=(0.125 if h < 4 else None))
                    dma("pool", qkA[h, :, tsl], s_[:, :], [s_], [qkA])
                for j in range(3):
                    ps = proj_fm(Win, 8, 1536 + j * 128, 128, rf, [xT], tb)
                    s_ = sf.next()
                    evac(s_[:, :], ps[:, :], [ps], [s_])
                    if j < 2:
                        dma("pool", cq_d[j * 128:(j + 1) * 128, tsl], s_[:, :], [s_], [cq_d])
                    else:
                        dma("pool", ckv_d[:, tsl], s_[:, :], [s_], [ckv_d])
                ps1 = proj_fm(Win, 8, 1920, 32, rf, [xT], tb)
                ps2 = proj_fm(Win, 8, 1952, 32, rf, [xT], tb)
                t1, t2 = sf.next(), sf.next()
                op("dve", lambda e: e.tensor_tensor(out=t1[0:32, :], in0=ps1[0:32, :], in1=cosT[0:32, tsl], op=ALU.mult), [ps1, cosT], [t1])
                op("dve", lambda e: e.tensor_tensor(out=t2[0:32, :], in0=ps2[0:32, :], in1=sinT[0:32, tsl], op=ALU.mult), [ps2, sinT], [t2])
                s_ = sbf.next()
                op("dve", lambda e: e.tensor_tensor(out=s_[0:32, :], in0=t1[0:32, :], in1=t2[0:32, :], op=ALU.add), [t1, t2], [s_])
                for h in range(4):
                    dma("pool", kmT[h, 64:96, tsl], s_[0:32, :], [s_], [kmT])
            for i in range(NT):
                ps = psr.next()
                for c in range(8):
                    op("pe", lambda e: e.matmul(ps[:, :], lhsT=xT[:, c, i * 128:(i + 1) * 128], rhs=Win[:, c, 1024:1536], start=(c == 0), stop=(c == 7)), [xT, Win], [ps])
                s_ = sbf.next()
                evac(s_[:, :], ps[:, :], [ps], [s_])
                dma("pool", vA[i * 128:(i + 1) * 128, :], s_[:, :], [s_], [vA])
            fw.barrier()
        if upto == "A0":
            return nc, ext, out_d, dump_bufs

        with ExitStack() as ph:
            cq = fw.sb("cq", [128, 2, S], F32, ph, multi=True)
            ckv = fw.sb("ckv", [128, S], F32, ph)
            for j in range(2):
                dma("sp", cq[:, j, :], cq_d[j * 128:(j + 1) * 128, :], [cq_d], [cq])
            dma("sp", ckv[:, :], ckv_d.ap(), [ckv_d], [ckv])
            Wuq = fw.sb("Wuq", [128, 2, 512], BF16, ph, multi=True)
            for j in range(2):
                dma("pool", Wuq[:, j, :], w_uq[j * 128:(j + 1) * 128, :], [w_uq], [Wuq])
            Wukv = fw.sb("Wukv", [128, 1, 768], BF16, ph)
            dma("pool", Wukv[:, 0, :], w_ukv.ap(), [w_ukv], [Wukv])
            gq = fw.sb("gq", [128, 2], F32, ph)
            gkv = fw.sb("gkv", [128, 1], F32, ph)
            dma("sp", gq[:], qn_g.ap(), [qn_g], [gq])
            dma("sp", gkv[:], kvn_g.ap(), [kvn_g], [gkv])
            cosT = fw.sb("cosT1", [128, S], F32, ph)
            sinT = fw.sb("sinT1", [128, S], F32, ph)
            dma("sp", cosT[:], cos_d.ap(), [cos_d], [cosT])
            dma("sp", sinT[:], sin_d.ap(), [sin_d], [sinT])
            sq = Rot([fw.sb(f"a1_sq{i}", [128, 512], F32, ph) for i in range(2)])
            tmpf = Rot([fw.sb(f"a1_t{i}", [128, 512], F32, ph) for i in range(4)])
            cqn = Rot([fw.sb(f"a1_cqn{i}", [128, 2, 512], BF16, ph) for i in range(2)])
            ckvn = Rot([fw.sb(f"a1_ckvn{i}", [128, 512], BF16, ph) for i in range(2)])
            sbf = Rot([fw.sb(f"a1_sb{i}", [128, 512], BF16, ph) for i in range(4)])
            psr = Rot(psb[0:8])
            SCALE_M = 96.0 ** -0.5
            for tb in range(8):
                tsl = slice(tb * 512, (tb + 1) * 512)
                pss_ = psr.next()
                for j in range(2):
                    s_ = sq.next()
                    op("act", lambda e: e.activation(out=s_[:, :], in_=cq[:, j, tsl], func=AF.Square), [cq], [s_])
                    op("pe", lambda e: e.matmul(pss_[:, :], lhsT=ones_f[:, :], rhs=s_[:, :], start=(j == 0), stop=(j == 1)), [ones_f, s_], [pss_])
                rs, tt = tmpf.next(), tmpf.next()
                rstd_from_ssq(pss_, 256.0, rs, tt)
                cn = cqn.next()
                for j in range(2):
                    op("dve", lambda e: e.scalar_tensor_tensor(out=cn[:, j, :], in0=cq[:, j, tsl], scalar=gq[:, j:j + 1], in1=rs[:, :], op0=ALU.mult, op1=ALU.mult), [cq, gq, rs], [cn])
                for h in range(4):
                    ps = psr.next()
                    for j in range(2):
                        op("pe", lambda e: e.matmul(ps[0:64, :], lhsT=Wuq[:, j, h * 64:(h + 1) * 64], rhs=cn[:, j, :], start=(j == 0), stop=(j == 1)), [Wuq, cn], [ps])
                    s_ = sbf.next()
                    op("act", lambda e: e.activation(out=s_[0:64, :], in_=ps[0:64, :], func=AF.Copy, scale=SCALE_M), [ps], [s_])
                    dma("pool", qmT[h, 0:64, tsl], s_[0:64, :], [s_], [qmT])
                ps1, ps2 = psr.next(), psr.next()
                for j in range(2):
                    op("pe", lambda e: e.matmul(ps1[:, :], lhsT=Wuq[:, j, 256:384], rhs=cn[:, j, :], start=(j == 0), stop=(j == 1)), [Wuq, cn], [ps1])
                for j in range(2):
                    op("pe", lambda e: e.matmul(ps2[:, :], lhsT=Wuq[:, j, 384:512], rhs=cn[:, j, :], start=(j == 0), stop=(j == 1)), [Wuq, cn], [ps2])
                t1, t2 = tmpf.next(), tmpf.next()
                op("dve", lambda e: e.tensor_tensor(out=t1[:, :], in0=ps1[:, :], in1=cosT[:, tsl], op=ALU.mult), [ps1, cosT], [t1])
                op("dve", lambda e: e.tensor_tensor(out=t2[:, :], in0=ps2[:, :], in1=sinT[:, tsl], op=ALU.mult), [ps2, sinT], [t2])
                op("dve", lambda e: e.tensor_tensor(out=t1[:, :], in0=t1[:, :], in1=t2[:, :], op=ALU.add), [t1, t2], [t1])
                s_ = sbf.next()
                op("act", lambda e: e.activation(out=s_[:, :], in_=t1[:, :], func=AF.Copy, scale=SCALE_M), [t1], [s_])
                for h in range(4):
                    dma("pool", qmT[h, 64:96, tsl], s_[h * 32:(h + 1) * 32, :], [s_], [qmT])
                pss_ = psr.next()
                s_ = sq.next()
                op("act", lambda e: e.activation(out=s_[:, :], in_=ckv[:, tsl], func=AF.Square), [ckv], [s_])
                op("pe", lambda e: e.matmul(pss_[:, :], lhsT=ones_f[:, :], rhs=s_[:, :], start=True, stop=True), [ones_f, s_], [pss_])
                rs, tt = tmpf.next(), tmpf.next()
                rstd_from_ssq(pss_, 128.0, rs, tt)
                kn = ckvn.next()
                op("dve", lambda e: e.scalar_tensor_tensor(out=kn[:, :], in0=ckv[:, tsl], scalar=gkv[:, 0:1], in1=rs[:, :], op0=ALU.mult, op1=ALU.mult), [ckv, gkv, rs], [kn])
                for h in range(4):
                    ps = psr.next()
                    op("pe", lambda e: e.matmul(ps[0:64, :], lhsT=Wukv[:, 0, h * 64:(h + 1) * 64], rhs=kn[:, :], start=True, stop=True), [Wukv, kn], [ps])
                    s_ = sbf.next()
                    op("dve", lambda e: e.tensor_copy(out=s_[0:64, :], in_=ps[0:64, :]), [ps], [s_])
                    dma("pool", kmT[h, 0:64, tsl], s_[0:64, :], [s_], [kmT])
                for i4 in range(4):
                    i = tb * 4 + i4
                    ps = psr.next()
                    op("pe", lambda e: e.matmul(ps[:, :], lhsT=kn[:, i4 * 128:(i4 + 1) * 128], rhs=Wukv[:, 0, 256:768], start=True, stop=True), [kn, Wukv], [ps])
                    s_ = sbf.next()
                    op("act", lambda e: e.copy(out=s_[:, :], in_=ps[:, :]), [ps], [s_])
                    dma("pool", vM[i * 128:(i + 1) * 128, :], s_[:, :], [s_], [vM])
            fw.barrier()
        if upto == "A1":
            return nc, ext, out_d, dump_bufs

        LAM_INIT0 = 0.8 - 0.6 * math.exp(-0.3 * 0)
        with ExitStack() as ph:
            negd = fw.sb("negd", [128, NEGD_W], F32, ph)
            dma("sp", negd[:], negd_d.ap(), [negd_d], [negd])
            lamt = fw.sb("lamt", [128, 4, 64], F32, ph)
            dma("sp", lamt[:], lam4.ap().rearrange("(o a) b -> o a b", o=1).broadcast_to([128, 4, 64]), [lam4], [lamt])
            lw = fw.sb("lw", [128, 8], F32, ph)
            lprod = fw.sb("lprod", [128, 2, 64], F32, ph)
            op("dve", lambda e: e.tensor_tensor(out=lprod[:, 0, :], in0=lamt[:, 0, :], in1=lamt[:, 1, :], op=ALU.mult), [lamt], [lprod])
            op("dve", lambda e: e.tensor_tensor(out=lprod[:, 1, :], in0=lamt[:, 2, :], in1=lamt[:, 3, :], op=ALU.mult), [lamt], [lprod])
            op("dve", lambda e: e.reduce_sum(out=lw[:, 0:2], in_=lprod[:, :, :], axis=AX.X), [lprod], [lw])
            op("act", lambda e: e.activation(out=lw[:, 2:4], in_=lw[:, 0:2], func=AF.Exp), [lw], [lw])
            op("dve", lambda e: e.tensor_tensor(out=lw[:, 4:5], in0=lw[:, 3:4], in1=lw[:, 2:3], op=ALU.subtract), [lw], [lw])
            op("dve", lambda e: e.tensor_scalar_add(out=lw[:, 5:6], in0=lw[:, 4:5], scalar1=-LAM_INIT0), [lw], [lw])
            neg_lam = lw[:, 5:6]
            dg = fw.sb("dg", [128, 1], F32, ph)
            dma("sp", dg[:], diff_g.ap(), [diff_g], [dg])
            dg2 = fw.sb("dg2", [128, 1], F32, ph)
            op("dve", lambda e: e.tensor_scalar_mul(out=dg2[:, :], in0=dg[:, :], scalar1=(1.0 - LAM_INIT0)), [dg], [dg2])

            QTs = Rot([fw.sb(f"b_q{i}", [128, S], BF16, ph) for i in range(2)])
            QZ = [Rot([fw.sb(f"b_qz{m}{i}", [128, S], BF16, ph) for i in range(2)]) for m in range(2)]
            for m in range(2):
                for qz in QZ[m].bufs:
                    op("dve", lambda e: e.memset(qz[:, :], 0.0), [], [qz])
            KTs = Rot([fw.sb(f"b_k{i}", [128, S], BF16, ph) for i in range(2)])
            Vs = Rot([fw.sb(f"b_v{i}", [128, NT, 128], BF16, ph) for i in range(2)])
            tmps = Rot([fw.sb(f"b_t{i}", [128, 512], BF16, ph) for i in range(5)])
            es = Rot([fw.sb(f"b_e{i}", [128, 512], BF16, ph) for i in range(6)])
            ETs = Rot([fw.sb(f"b_et{i}", [128, NEGD_W], BF16, ph) for i in range(2)])
            pscore = Rot(psb[0:4])
            pacc = Rot([(psb[4], psb[5]), (psb[6], psb[7])])
            of = Rot([fw.sb(f"b_of{i}", [128, 512], F32, ph) for i in range(6)])
            rsf = Rot([fw.sb(f"b_rs{i}", [128, 512], F32, ph) for i in range(8)])
            ob = Rot([fw.sb(f"b_ob{i}", [128, 512], BF16, ph) for i in range(3)])
            slopes = alibi_slopes(4)
            LA = 3
            jobs = []

            def mk_loader(kind, h, QT, KT, V):
                def ld():
                    if kind == "diff":
                        ET = cur_et[h]
                        for c4 in range(4):
                            csl = slice(c4 * 2016, (c4 + 1) * 2016)
                            op("act", lambda e: e.activation(out=ET[:, csl], in_=negd[:, csl], func=AF.Exp, scale=float(slopes[h])), [negd], [ET])
                        for m in range(2):
                            dma("sp", QT[m][m * 64:(m + 1) * 64, :], qkA[h, m * 64:(m + 1) * 64, :], [qkA], [QT[m]])
                        dma("sp", KT[:, :], qkA[4 + h, :, :], [qkA], [KT])
                        dma("sp", V[:, :, :], vA[:, h * 128:(h + 1) * 128].rearrange("(t p) e -> p t e", p=128), [vA], [V])
                    else:
                        dma("sp", QT[0:96, :], qmT[h, :, :], [qmT], [QT])
                        dma("sp", KT[0:96, :], kmT[h, :, :], [kmT], [KT])
                        dma("sp", V[:, :, :], vM[:, h * 128:(h + 1) * 128].rearrange("(t p) e -> p t e", p=128), [vM], [V])
                return ld

            def fin_diff(h, q0, st_):
                def fin_map(po, pss_):
                    rs, r2 = rsf.next(), rsf.next()
                    op("act", lambda e: e.activation(out=r2[:, :], in_=pss_[:, :], func=AF.Ln), [pss_], [r2])
                    op("act", lambda e: e.activation(out=rs[:, :], in_=r2[:, :], func=AF.Exp, scale=-1.0), [r2], [rs])
                    yield
                    op("dve", lambda e: e.tensor_tensor(out=r2[:, :], in0=rs[:, :], in1=pss_[:, :], op=ALU.mult), [rs, pss_], [r2])
                    op("dve", lambda e: e.tensor_scalar(out=r2[:, :], in0=r2[:, :], scalar1=-1.0, scalar2=2.0, op0=ALU.mult, op1=ALU.add), [r2], [r2])
                    yield
                    op("dve", lambda e: e.tensor_tensor(out=rs[:, :], in0=rs[:, :], in1=r2[:, :], op=ALU.mult), [rs, r2], [rs])
                    o_ = of.next()
                    op("dve", lambda e: e.tensor_tensor(out=o_[:, :], in0=po[:, :], in1=rs[:, :], op=ALU.mult), [po, rs], [o_])
                    st_.append(o_)
                    if len(st_) == 2:
                        yield
                        om = st_
                        oa = of.next()
                        op("dve", lambda e: e.scalar_tensor_tensor(out=oa[:, :], in0=om[1][:, :], scalar=neg_lam, in1=om[0][:, :], op0=ALU.mult, op1=ALU.add), [om[0], om[1], lw], [oa])
                        yield
                        sq_ = of.next()
                        op("act", lambda e: e.activation(out=sq_[:, :], in_=oa[:, :], func=AF.Square), [oa], [sq_])
                        yield
                        pssq = pscore.next()
                        op("pe", lambda e: e.matmul(pssq[:, :], lhsT=ones_f[:, :], rhs=sq_[:, :], start=True, stop=True), [ones_f, sq_], [pssq])
                        yield
                        rs2, tt = rsf.next(), rsf.next()
                        op("act", lambda e: e.activation(out=tt[:, :], in_=pssq[:, :], func=AF.Ln, bias=eps_t[:, 0:1], scale=1.0 / 128.0), [pssq, eps_t], [tt])
                        op("act", lambda e: e.activation(out=rs2[:, :], in_=tt[:, :], func=AF.Exp, scale=-0.5), [tt], [rs2])
                        yield
                        o_b = ob.next()
                        op("dve", lambda e: e.scalar_tensor_tensor(out=o_b[:, :], in0=oa[:, :], scalar=dg2[:, 0:1], in1=rs2[:, :], op0=ALU.mult, op1=ALU.mult), [oa, dg2, rs2], [o_b])
                        dma("pool", oT_d[h, :, q0:q0 + 512], o_b[:, :], [o_b], [oT_d])
                return fin_map

            def fin_mla(h, q0):
                def fin_map(po, pss_):
                    rs = rsf.next()
                    op("dve", lambda e: e.reciprocal(out=rs[:, :], in_=pss_[:, :]), [pss_], [rs])
                    yield
                    o_b = ob.next()
                    op("dve", lambda e: e.tensor_tensor(out=o_b[:, :], in0=po[:, :], in1=rs[:, :], op=ALU.mult), [po, rs], [o_b])
                    dma("pool", oT_d[4 + h, :, q0:q0 + 512], o_b[:, :], [o_b], [oT_d])
                return fin_map

            cur_et = {}
            SKIP_THR = {0: 512, 1: 2048}
            for h in range(4):
                QT, KT, V = (QZ[0].next(), QZ[1].next()), KTs.next(), Vs.next()
                cur_et[h] = ETs.next()
                first_of_head = True
                for qb in range(8):
                    st_ = []
                    fin = fin_diff(h, qb * 512, st_)
                    kbs = []
                    for kb in range(NT):
                        md = max(0, kb * 128 - (qb * 512 + 511), qb * 512 - (kb * 128 + 127))
                        if h in SKIP_THR and md >= SKIP_THR[h]:
                            continue
                        kbs.append(kb)
                    for m in range(2):
                        for kb in kbs:
                            jobs.append(dict(QT=QT[m], KT=KT, V=V, r0=0, kr=128, q0=qb * 512, kb=kb, slope=slopes[h], ET=cur_et[h], first=(kb == kbs[0]), last=(kb == kbs[-1]), fin=fin,
                                             pre=(mk_loader("diff", h, QT, KT, V) if first_of_head else None)))
                            first_of_head = False
            for h in range(4):
                QT, KT, V = QTs.next(), KTs.next(), Vs.next()
                first_of_head = True
                for qb in range(8):
                    fin = fin_mla(h, qb * 512)
                    for kb in range(NT):
                        jobs.append(dict(QT=QT, KT=KT, V=V, r0=0, kr=96, q0=qb * 512, kb=kb, slope=None, first=(kb == 0), last=(kb == NT - 1), fin=fin,
                                         pre=(mk_loader("mla", h, QT, KT, V) if first_of_head else None)))
                        first_of_head = False

            PREF = 150
            first_seen = False
            for idx_, j_ in enumerate(jobs):
                if j_["pre"] is not None:
                    if first_seen:
                        tgt = max(0, idx_ - PREF)
                        ld_ = j_["pre"]
                        j_["pre"] = None
                        prev = jobs[tgt].get("pre2")
                        jobs[tgt]["pre2"] = ld_ if prev is None else (lambda a=prev, b=ld_: (a(), b()))
                    first_seen = True

            def stage1(j):
                if j["pre"] is not None:
                    j["pre"]()
                if j.get("pre2") is not None:
                    j["pre2"]()
                QT, KT, r0, kr, q0, kb = j["QT"], j["KT"], j["r0"], j["kr"], j["q0"], j["kb"]
                ps = pscore.next()
                op("pe", lambda e: e.matmul(ps[:, :], lhsT=KT[r0:r0 + kr, kb * 128:(kb + 1) * 128], rhs=QT[r0:r0 + kr, q0:q0 + 512], start=True, stop=True), [KT, QT], [ps])
                E = es.next()
                if j["slope"] is not None:
                    tmp = tmps.next()
                    n0 = q0 - kb * 128 + NEGD_C
                    ET = j["ET"]
                    op("act", lambda e: e.activation(out=tmp[:, :], in_=ps[:, :], func=AF.Exp), [ps], [tmp])
                    op("dve", lambda e: e.tensor_tensor(out=E[:, :], in0=tmp[:, :], in1=ET[:, n0:n0 + 512], op=ALU.mult), [tmp, ET], [E])
                else:
                    op("act", lambda e: e.activation(out=E[:, :], in_=ps[:, :], func=AF.Exp), [ps], [E])
                j["E"] = E

            cur = [None]

            def stage2(j):
                if j["first"]:
                    cur[0] = pacc.next()
                po, pss_ = cur[0]
                V, kb, E = j["V"], j["kb"], j["E"]
                op("pe", lambda e: e.matmul(po[:, :], lhsT=V[:, kb, :], rhs=E[:, :], start=j["first"], stop=j["last"]), [V, E], [po])
                op("pe", lambda e: e.matmul(pss_[:, :], lhsT=ones_bf[:, :], rhs=E[:, :], start=j["first"], stop=j["last"]), [ones_bf, E], [pss_])
                if j["last"]:
                    deferred.append([FIN_DELAY, j["fin"](po, pss_)])

            deferred = []
            FIN_DELAY = 4
            FIN_STEP = 2

            def run_deferred(force=False):
                for d_ in list(deferred):
                    d_[0] -= 1
                    while d_[0] <= 0 or force:
                        try:
                            next(d_[1])
                            d_[0] = FIN_STEP
                        except StopIteration:
                            deferred.remove(d_)
                            break
                        if not force:
                            break

            for idx in range(len(jobs) + LA):
                run_deferred()
                if idx % 50 == 10:
                    issue_cast()
                if idx < len(jobs):
                    stage1(jobs[idx])
                if idx >= LA:
                    stage2(jobs[idx - LA])
            while deferred:
                run_deferred(force=True)
            issue_cast(len(cast_list))
            fw.barrier()
        if upto == "B":
            return nc, ext, out_d, dump_bufs

        LOG = fw.sb("LOG", [128, NT, 36], F32)

        def ln_stage1(y, stt, junk):
            op("act", lambda e: e.activation(out=junk[:, :], in_=y[:, :], func=AF.Identity, accum_out=stt[:, 0:1]), [y], [junk, stt])
            op("act", lambda e: e.activation(out=junk[:, :], in_=y[:, :], func=AF.Square, accum_out=stt[:, 1:2]), [y], [junk, stt])

        def ln_stage2(y, g_t, b_t, stt, out_t, junk):
            ln_front(y, stt, junk)
            ln_back(g_t, b_t, out_t, junk)

        def ln_back(g_t, b_t, out_t, junk):
            op("dve", lambda e: e.tensor_tensor(out=junk[:, :], in0=junk[:, :], in1=g_t[:, :], op=ALU.mult), [junk, g_t], [junk])
            op("pool", lambda e: e.tensor_tensor(out=out_t[:, :], in0=junk[:, :], in1=b_t[:, :], op=ALU.add), [junk, b_t], [out_t])

        def ln_f1(stt):
            op("dve", lambda e: e.tensor_scalar_mul(out=stt[:, 2:3], in0=stt[:, 0:1], scalar1=1.0 / D), [stt], [stt])
            op("dve", lambda e: e.tensor_tensor(out=stt[:, 3:4], in0=stt[:, 2:3], in1=stt[:, 2:3], op=ALU.mult), [stt], [stt])
            op("dve", lambda e: e.scalar_tensor_tensor(out=stt[:, 4:5], in0=stt[:, 1:2], scalar=1.0 / D, in1=stt[:, 3:4], op0=ALU.mult, op1=ALU.subtract), [stt], [stt])

        def ln_f2(stt):
            op("act", lambda e: e.activation(out=stt[:, 5:6], in_=stt[:, 4:5], func=AF.Sqrt, bias=eps_t[:, 0:1], scale=1.0), [stt, eps_t], [stt])

        def ln_f3(stt):
            op("dve", lambda e: e.reciprocal(out=stt[:, 6:7], in_=stt[:, 5:6]), [stt], [stt])
            op("dve", lambda e: e.scalar_tensor_tensor(out=stt[:, 7:8], in0=stt[:, 2:3], scalar=-1.0, in1=stt[:, 6:7], op0=ALU.mult, op1=ALU.mult), [stt], [stt])

        def ln_f4(y, stt, junk):
            op("act", lambda e: e.activation(out=junk[:, :], in_=y[:, :], func=AF.Identity, bias=stt[:, 7:8], scale=stt[:, 6:7]), [y, stt], [junk])

        def ln_front(y, stt, junk):
            ln_f1(stt)
            ln_f2(stt)
            ln_f3(stt)
            ln_f4(y, stt, junk)

        def phase_C(layer, w_out_dram, xsrc):
            with ExitStack() as ph:
                oT = fw.sb("c_oT", [128, 8, S], BF16, ph, multi=True)
                for c in range(8):
                    dma("sp", oT[:, c, :], oT_d[c, :, :], [oT_d], [oT])
                Wo = fw.sb("c_Wo", [128, 8, D], BF16, ph, multi=True)
                for c in range(8):
                    dma("pool", Wo[:, c, :], w_out_dram[c * 128:(c + 1) * 128, :], [w_out_dram], [Wo])
                Wr = fw.sb("c_Wr", [128, 8, 36], F32, ph)
                dma("sp", Wr[:], w_router[layer].rearrange("(c p) n -> p c n", p=128), [w_router], [Wr])
                g_t = fw.sb("c_g", [128, D], F32, ph)
                b_t = fw.sb("c_b", [128, D], F32, ph)
                dma("sp", g_t[:], ln_mix_g[layer:layer + 1, :].broadcast_to([128, D]), [ln_mix_g], [g_t])
                dma("sp", b_t[:], ln_mix_b[layer:layer + 1, :].broadcast_to([128, D]), [ln_mix_b], [b_t])
                brt = fw.sb("c_brt", [128, 36], F32, ph)
                dma("sp", brt[:], b_router[layer:layer + 1, :].broadcast_to([128, 36]), [b_router], [brt])
                zr = fw.sb("c_zr", [1, D], BF16, ph)
                op("dve", lambda e: e.memset(zr[:], 0.0), [], [zr])
                dma("sp", x1b_d[S:S + 1, :], zr[:], [zr], [x1b_d])
                xts = Rot([fw.sb(f"c_x{i}", [128, D], F32, ph) for i in range(3)])
                ys = Rot([fw.sb(f"c_y{i}", [128, D], F32, ph) for i in range(4)])
                junks = Rot([fw.sb(f"c_j{i}", [128, D], F32, ph) for i in range(4)])
                x1s = Rot([fw.sb(f"c_x1{i}", [128, D], F32, ph) for i in range(4)])
                x1bs = Rot([fw.sb(f"c_x1b{i}", [128, D], BF16, ph) for i in range(2)])
                x1Ts = Rot([fw.sb(f"c_x1T{i}", [128, 8, 128], F32, ph) for i in range(2)])
                stts = Rot([fw.sb(f"c_st{i}", [128, 8], F32, ph) for i in range(4)])
                pmm = Rot(psb[0:4])
                ptr = Rot(psb[4:6])
                prt = Rot(psb[6:8])
                stA, x1t_of, b2s = {}, {}, {}
                for i in range(-2, NT + 1):
                    ia, ib, ic = i + 2, i + 1, i - 1
                    if 0 <= ic < NT:
                        x1t = x1t_of.pop(ic)
                        x1T = x1Ts.next()
                        pst2 = [ptr.next(), ptr.next()]
                        for hf in range(2):
                            for c4 in range(4):
                                c = hf * 4 + c4
                                op("pe", lambda e: e.transpose(out=pst2[hf][:, c4 * 128:(c4 + 1) * 128], in_=x1t[:, c * 128:(c + 1) * 128], identity=ident[:]), [x1t, ident], [pst2[hf]])
                    if 0 <= ia < NT:
                        isl = slice(ia * 128, (ia + 1) * 128)
                        xt = xts.next()
                        dma("sp", xt[:], xsrc[isl, :], [xsrc], [xt])
                        y = ys.next()
                        pmm2 = [pmm.next(), pmm.next()]
                        for n in range(2):
                            for c in range(8):
                                op("pe", lambda e: e.matmul(pmm2[n][:, :], lhsT=oT[:, c, isl], rhs=Wo[:, c, n * 512:(n + 1) * 512], start=(c == 0), stop=(c == 7)), [oT, Wo], [pmm2[n]])
                    if 0 <= ib < NT:
                        yb, junkb, sttb = stA[ib]
                        ln_f1(sttb)
                        ln_f2(sttb)
                    if 0 <= ic < NT:
                        for hf in range(2):
                            o_ap = x1T[:, hf * 4:(hf + 1) * 4, :]
                            i_ap = pst2[hf][:, :].rearrange("p (c n) -> p c n", c=4)
                            if hf == 0:
                                op("act", lambda e: e.copy(out=o_ap, in_=i_ap), [pst2[hf]], [x1T])
                            else:
                                op("dve", lambda e: e.tensor_copy(out=o_ap, in_=i_ap), [pst2[hf]], [x1T])
                    if 0 <= ib < NT:
                        ln_f3(sttb)
                    if 0 <= ic < NT:
                        psr_ = prt.next()
                        for c in range(8):
                            op("pe", lambda e: e.matmul(psr_[:, 0:36], lhsT=x1T[:, c, :], rhs=Wr[:, c, :], start=(c == 0), stop=(c == 7)), [x1T, Wr], [psr_])
                    if 0 <= ib < NT:
                        ln_f4(yb, sttb, junkb)
                    if 0 <= ia < NT:
                        for n in range(2):
                            op("dve", lambda e: e.scalar_tensor_tensor(out=y[:, n * 512:(n + 1) * 512], in0=xt[:, n * 512:(n + 1) * 512], scalar=float(ALPHA), in1=pmm2[n][:, :], op0=ALU.mult, op1=ALU.add), [xt, pmm2[n]], [y])
                        junk, stt = junks.next(), stts.next()
                        ln_stage1(y, stt, junk)
                        stA[ia] = (y, junk, stt)
                    if 0 <= ib < NT:
                        isl = slice(ib * 128, (ib + 1) * 128)
                        x1n = x1s.next()
                        ln_back(g_t, b_t, x1n, junkb)
                        dma("pool", x1_d[isl, :], x1n[:, :], [x1n], [x1_d])
                        x1b = x1bs.next()
                        op("act", lambda e: e.copy(out=x1b[:, :], in_=x1n[:, :]), [x1n], [x1b])
                        dma("pool", x1b_d[isl, :], x1b[:, :], [x1b], [x1b_d])
                        x1t_of[ib] = x1n
                        stA.pop(ib)
                    if 0 <= ic < NT:
                        op("dve", lambda e: e.tensor_tensor(out=LOG[:, ic, :], in0=psr_[:, 0:36], in1=brt[:, :], op=ALU.add), [psr_, brt], [LOG])
                fw.barrier()

        def phase_M(layer, dst):
            with ExitStack() as ph:
                bs = ExitStack()
                tstack = [ph]

                def T(name, shape, dt=F32):
                    return fw.sb("m_" + name, shape, dt, tstack[0])
                mc = T("mc", [128, 4, 64])
                dma("sp", mc[:], moe_c.ap(), [moe_c], [mc])
                tokid = T("tokid", [128, NT])
                dma("sp", tokid[:], tokid_d.ap(), [tokid_d], [tokid])
                triu = T("triu", [128, 128])
                dma("sp", triu[:], triu_d.ap(), [triu_d], [triu])
                triu_b = T("triu_b", [128, 128], BF16)
                op("dve", lambda e: e.tensor_copy(out=triu_b[:, :], in_=triu[:, :]), [triu], [triu_b])
                coarse = LOG[:, :, 0:4]
                fine4 = LOG[:, :, 4:36].rearrange("p j (g i) -> p j g i", g=4)
                gmax = T("gmax", [128, NT])
                op("dve", lambda e: e.tensor_reduce(out=gmax[:, :], in_=coarse, axis=AX.X, op=ALU.max), [LOG], [gmax])
                ohg = T("ohg", [128, NT, 4])
                op("dve", lambda e: e.tensor_tensor(out=ohg[:, :, :], in0=coarse, in1=gmax[:, :].unsqueeze(2).to_broadcast([128, NT, 4]), op=ALU.is_equal), [LOG, gmax], [ohg])
                ex = T("ex", [128, NT, 4])
                op("dve", lambda e: e.tensor_tensor(out=ex[:, :, :], in0=coarse, in1=gmax[:, :].unsqueeze(2).to_broadcast([128, NT, 4]), op=ALU.subtract), [LOG, gmax], [ex])
                op("act", lambda e: e.activation(out=ex[:, :, :], in_=ex[:, :, :], func=AF.Exp), [ex], [ex])
                pg = T("pg", [128, NT])
                op("dve", lambda e: e.reduce_sum(out=pg[:, :], in_=ex[:, :, :], axis=AX.X), [ex], [pg])
                op("dve", lambda e: e.reciprocal(out=pg[:, :], in_=pg[:, :]), [pg], [pg])
                t48 = T("t48", [128, NT, 4, 8])
                op("dve", lambda e: e.tensor_tensor(out=t48[:, :, :, :], in0=fine4, in1=ohg[:, :, :].unsqueeze(3).to_broadcast([128, NT, 4, 8]), op=ALU.mult), [LOG, ohg], [t48])
                fsel = T("fsel", [128, NT, 8])
                op("dve", lambda e: e.reduce_sum(out=fsel[:, :, :], in_=t48[:, :, :, :].rearrange("p j g i -> p j i g"), axis=AX.X), [t48], [fsel])
                v1 = T("v1", [128, NT])
                op("dve", lambda e: e.tensor_reduce(out=v1[:, :], in_=fsel[:, :, :], axis=AX.X, op=ALU.max), [fsel], [v1])
                oh1 = T("oh1", [128, NT, 8])
                op("dve", lambda e: e.tensor_tensor(out=oh1[:, :, :], in0=fsel[:, :, :], in1=v1[:, :].unsqueeze(2).to_broadcast([128, NT, 8]), op=ALU.is_equal), [fsel, v1], [oh1])
                msk = T("msk", [128, NT, 8])
                op("dve", lambda e: e.scalar_tensor_tensor(out=msk[:, :, :], in0=oh1[:, :, :], scalar=-1.0e30, in1=fsel[:, :, :], op0=ALU.mult, op1=ALU.add), [oh1, fsel], [msk])
                v2 = T("v2", [128, NT])
                op("dve", lambda e: e.tensor_reduce(out=v2[:, :], in_=msk[:, :, :], axis=AX.X, op=ALU.max), [msk], [v2])
                oh2 = T("oh2", [128, NT, 8])
                op("dve", lambda e: e.tensor_tensor(out=oh2[:, :, :], in0=msk[:, :, :], in1=v2[:, :].unsqueeze(2).to_broadcast([128, NT, 8]), op=ALU.is_equal), [msk, v2], [oh2])
                ed = T("ed", [128, NT])
                op("dve", lambda e: e.tensor_tensor(out=ed[:, :], in0=v2[:, :], in1=v1[:, :], op=ALU.subtract), [v1, v2], [ed])
                op("act", lambda e: e.activation(out=ed[:, :], in_=ed[:, :], func=AF.Exp), [ed], [ed])
                w1 = T("w1", [128, NT])
                op("dve", lambda e: e.tensor_scalar_add(out=w1[:, :], in0=ed[:, :], scalar1=1.0), [ed], [w1])
                op("dve", lambda e: e.reciprocal(out=w1[:, :], in_=w1[:, :]), [w1], [w1])
                gates = T("gates", [128, 2, NT])
                op("dve", lambda e: e.tensor_tensor(out=gates[:, 0, :], in0=w1[:, :], in1=pg[:, :], op=ALU.mult), [w1, pg], [gates])
                op("dve", lambda e: e.tensor_tensor(out=w1[:, :], in0=w1[:, :], in1=ed[:, :], op=ALU.mult), [w1, ed], [w1])
                op("dve", lambda e: e.tensor_tensor(out=gates[:, 1, :], in0=w1[:, :], in1=pg[:, :], op=ALU.mult), [w1, pg], [gates])
                Ak = [T(f"A{k}", [128, NT, 4, 8]) for k in range(2)]
                for k, ohk in enumerate((oh1, oh2)):
                    op("dve", lambda e: e.tensor_tensor(out=Ak[k][:, :, :, :], in0=ohg[:, :, :].unsqueeze(3).to_broadcast([128, NT, 4, 8]), in1=ohk[:, :, :].unsqueeze(2).to_broadcast([128, NT, 4, 8]), op=ALU.mult), [ohg, ohk], [Ak[k]])
                A_bf = T("A_bf", [128, NT * 32], BF16)
                op("dve", lambda e: e.tensor_tensor(out=A_bf[:, :], in0=Ak[0][:, :, :, :].rearrange("p j g i -> p (j g i)"), in1=Ak[1][:, :, :, :].rearrange("p j g i -> p (j g i)"), op=ALU.add), [Ak[0], Ak[1]], [A_bf])
                sa = T("sa", [128, NT, 32])
                sb_ = T("sb", [128, NT, 32])
                tots = T("tots", [128, NT, 32])
                rank = T("rank", [128, NT, 32])
                for hf in range(2):
                    ps = psb[hf]
                    op("pe", lambda e: e.matmul(ps[:, :], lhsT=ones_bf[:, :], rhs=A_bf[:, hf * 512:(hf + 1) * 512], start=True, stop=True), [ones_bf, A_bf], [ps])
                    op("dve", lambda e: e.tensor_copy(out=tots[:, hf * 16:(hf + 1) * 16, :], in_=ps[:, :].rearrange("p (j e) -> p j e", e=32)), [ps], [tots])
                    ps2 = psb[2 + hf]
                    op("pe", lambda e: e.matmul(ps2[:, :], lhsT=triu_b[:, :], rhs=A_bf[:, hf * 512:(hf + 1) * 512], start=True, stop=True), [triu_b, A_bf], [ps2])
                    op("dve", lambda e: e.tensor_copy(out=rank[:, hf * 16:(hf + 1) * 16, :], in_=ps2[:, :].rearrange("p (j e) -> p j e", e=32)), [ps2], [rank])
                op("dve", lambda e: e.tensor_copy(out=sa[:, :, :], in_=tots[:, :, :]), [tots], [sa])
                a_, b_ = sa, sb_
                for s_ in (1, 2, 4, 8, 16):
                    op("dve", lambda e: e.tensor_tensor(out=b_[:, s_:, :], in0=a_[:, s_:, :], in1=a_[:, :NT - s_, :], op=ALU.add), [a_], [b_])
                    op("dve", lambda e: e.tensor_copy(out=b_[:, :s_, :], in_=a_[:, :s_, :]), [a_], [b_])
                    a_, b_ = b_, a_
                inc = a_
                cnt = T("cnt", [128, 32])
                op("dve", lambda e: e.tensor_copy(out=cnt[:, :], in_=inc[:, NT - 1, :]), [inc], [cnt])
                op("dve", lambda e: e.tensor_tensor(out=rank[:, :, :], in0=rank[:, :, :], in1=inc[:, :, :], op=ALU.add), [rank, inc], [rank])
                op("dve", lambda e: e.tensor_tensor(out=rank[:, :, :], in0=rank[:, :, :], in1=tots[:, :, :], op=ALU.subtract), [rank, tots], [rank])
                cmp_ = T("cmp", [128, 64, 32])
                op("dve", lambda e: e.tensor_tensor(out=cmp_[:, 0:32, :], in0=cnt[:, :].unsqueeze(2).to_broadcast([128, 32, 32]), in1=mc[:, 1, 0:32].unsqueeze(1).to_broadcast([128, 32, 32]), op=ALU.is_gt), [cnt, mc], [cmp_])
                nblk = T("nblk", [128, 32])
                op("dve", lambda e: e.reduce_sum(out=nblk[:, :], in_=cmp_[:, 0:32, :], axis=AX.X), [cmp_], [nblk])
                na = T("na", [128, 32])
                nb_ = T("nb", [128, 32])
                op("dve", lambda e: e.tensor_copy(out=na[:, :], in_=nblk[:, :]), [nblk], [na])
                a_, b_ = na, nb_
                for s_ in (1, 2, 4, 8, 16):
                    op("dve", lambda e: e.tensor_tensor(out=b_[:, s_:], in0=a_[:, s_:], in1=a_[:, :32 - s_], op=ALU.add), [a_], [b_])
                    op("dve", lambda e: e.tensor_copy(out=b_[:, :s_], in_=a_[:, :s_]), [a_], [b_])
                    a_, b_ = b_, a_
                pend = T("pend", [128, 32])
                pstart = T("pstart", [128, 32])
                op("dve", lambda e: e.tensor_scalar_mul(out=pend[:, :], in0=a_[:, :], scalar1=float(BLK)), [a_], [pend])
                op("dve", lambda e: e.scalar_tensor_tensor(out=pstart[:, :], in0=nblk[:, :], scalar=-float(BLK), in1=pend[:, :], op0=ALU.mult, op1=ALU.add), [nblk, pend], [pstart])
                op("dve", lambda e: e.tensor_tensor(out=rank[:, :, :], in0=rank[:, :, :], in1=pstart[:, :].unsqueeze(1).to_broadcast([128, NT, 32]), op=ALU.add), [rank, pstart], [rank])
                dest = T("dest", [128, 2, NT])
                for k in range(2):
                    op("dve", lambda e: e.tensor_tensor(out=sa[:, :, :], in0=Ak[k][:, :, :, :].rearrange("p j g i -> p j (g i)"), in1=rank[:, :, :], op=ALU.mult), [Ak[k], rank], [sa])
                    op("dve", lambda e: e.reduce_sum(out=dest[:, k, :], in_=sa[:, :, :], axis=AX.X), [sa], [dest])
                dest_i = T("dest_i", [128, 2, NT], I32)
                op("dve", lambda e: e.tensor_copy(out=dest_i[:, :, :], in_=dest[:, :, :]), [dest], [dest_i])
                op("dve", lambda e: e.tensor_tensor(out=cmp_[:, 0:NB, :], in0=pend[:, :].unsqueeze(1).to_broadcast([128, NB, 32]), in1=mc[:, 2, 0:NB].unsqueeze(2).to_broadcast([128, NB, 32]), op=ALU.is_le), [pend, mc], [cmp_])
                beid = T("beid", [128, 64])
                op("dve", lambda e: e.reduce_sum(out=beid[:, 0:NB], in_=cmp_[:, 0:NB, :], axis=AX.X), [cmp_], [beid])
                op("dve", lambda e: e.tensor_scalar_min(out=beid[:, 0:NB], in0=beid[:, 0:NB], scalar1=31.0), [beid], [beid])
                op("dve", lambda e: e.tensor_scalar(out=beid[:, 0:NB], in0=beid[:, 0:NB], scalar1=128.0, scalar2=float(layer * E_ * 128), op0=ALU.mult, op1=ALU.add), [beid], [beid])
                op("dve", lambda e: e.tensor_tensor(out=beid[:, 0:NB], in0=beid[:, 0:NB], in1=mc[:, 3, 0:1].to_broadcast([128, NB]), op=ALU.add), [beid, mc], [beid])
                widx = T("widx", [128, 64], I32)
                op("dve", lambda e: e.tensor_copy(out=widx[:, 0:NB], in_=beid[:, 0:NB]), [beid], [widx])
                NA = NSLOT // 128 + 1
                padrec = T("padrec", [128, NA, 4])
                op("dve", lambda e: e.memset(padrec[:, :, :], 0.0), [], [padrec])
                op("dve", lambda e: e.memset(padrec[:, :, 0:1], float(S)), [], [padrec])
                op("dve", lambda e: e.tensor_scalar_add(out=padrec[:, :, 2], in0=mc[:, 3, 0:1].to_broadcast([128, NA]), scalar1=float(2 * S)), [mc], [padrec])
                dma("sp", slot_d.ap().rearrange("(a p) c -> p a c", p=128), padrec[:, :, :], [padrec], [slot_d])
                rec = T("rec", [128, 2, NT, 4])
                op("dve", lambda e: e.memset(rec[:, :, :, :], 0.0), [], [rec])
                for k in range(2):
                    op("dve", lambda e: e.tensor_copy(out=rec[:, k, :, 0], in_=tokid[:, :]), [tokid], [rec])
                    op("dve", lambda e: e.tensor_copy(out=rec[:, k, :, 1], in_=gates[:, k, :]), [gates], [rec])
                    op("dve", lambda e: e.tensor_scalar_add(out=rec[:, k, :, 2], in0=tokid[:, :], scalar1=float(k * S)), [tokid], [rec])
                sc_bufs = []
                for k in range(2):
                    for j in range(NT):
                        sc_b = Buf(None, "slotscatter")
                        sc_bufs.append(sc_b)
                        op("pool", lambda e: e.indirect_dma_start(out=slot_d[:, :], out_offset=bass.IndirectOffsetOnAxis(ap=dest_i[:, k, j:j + 1], axis=0), in_=rec[:, k, j, :], in_offset=None), [rec, dest_i, slot_d], [sc_b], dma=True)
                SL = T("SL", [128, NA - 1, 4])
                dma("sp", SL[:, :, :], slot_d[0:NSLOT, :].rearrange("(a p) c -> p a c", p=128), [slot_d] + sc_bufs, [SL])
                tok_i = T("tok_i", [128, NA - 1], I32)
                row_i = T("row_i", [128, NA - 1], I32)
                gate_s = T("gate_s", [128, NA - 1])
                op("dve", lambda e: e.tensor_copy(out=tok_i[:, :], in_=SL[:, :, 0]), [SL], [tok_i])
                op("dve", lambda e: e.tensor_copy(out=row_i[:, :], in_=SL[:, :, 2]), [SL], [row_i])
                op("dve", lambda e: e.tensor_copy(out=gate_s[:, :], in_=SL[:, :, 1]), [SL], [gate_s])
                if "moe_dbg" in dumps:
                    dump("m_dest", dest[:, :, :], [128, 2, NT], F32, [dest])
                    dump("m_gates", gates[:, :, :], [128, 2, NT], F32, [gates])
                    dump("m_beid", beid[:, :], [128, 64], F32, [beid])
                    dump("m_SL", SL[:, :, :], [128, NA - 1, 4], F32, [SL])
                    dump("m_cnt", cnt[:, :], [128, 32], F32, [cnt])
                tstack[0] = bs
                Wgs = Rot([T(f"Wg{i}", [128, 8 * HID], BF16) for i in range(2)])
                Wus = Rot([T(f"Wu{i}", [128, 8 * HID], BF16) for i in range(2)])
                Wds = Rot([T(f"Wd{i}", [128, 4 * D], BF16) for i in range(2)])
                xgs = Rot([T(f"xg{i}", [128, D], BF16) for i in range(4)])
                xgTs = Rot([T(f"xgT{i}", [128, 8, BLK], BF16) for i in range(2)])
                acts = Rot([T(f"act{i}", [128, 4, BLK], BF16) for i in range(2)])
                sgs = Rot([T(f"sg{i}", [128, BLK], F32) for i in range(3)])
                ysbs = Rot([T(f"ysb{i}", [128, D], F32) for i in range(3)])
                identb = T("identb", [128, 128], BF16)
                op("dve", lambda e: e.tensor_copy(out=identb[:, :], in_=ident[:, :]), [ident], [identb])
                ptr = Rot(psb[0:2])
                pgu = Rot(psb[2:6])
                pyy = Rot(psb[6:8])
                def g_gu(b):
                    Wg, Wu = Wgs.next(), Wus.next()
                    for Wt, src in ((Wg, wgb), (Wu, wub)):
                        op("pool", lambda e: e.indirect_dma_start(out=Wt[:, :], out_offset=None, in_=src[:, :], in_offset=bass.IndirectOffsetOnAxis(ap=widx[:, b:b + 1], axis=0)), [src, widx], [Wt], dma=True)
                    xg2 = []
                    for hf in range(2):
                        a = 2 * b + hf
                        xg = xgs.next()
                        op("pool", lambda e: e.indirect_dma_start(out=xg[:, :], out_offset=None, in_=x1b_d[:, :], in_offset=bass.IndirectOffsetOnAxis(ap=tok_i[:, a:a + 1], axis=0)), [x1b_d, tok_i], [xg], dma=True)
                        xg2.append(xg)
                    return Wg, Wu, xg2

                def g_d(b):
                    Wd = Wds.next()
                    op("pool", lambda e: e.indirect_dma_start(out=Wd[:, :], out_offset=None, in_=wdb[:, :], in_offset=bass.IndirectOffsetOnAxis(ap=widx[:, b:b + 1], axis=0)), [wdb, widx], [Wd], dma=True)
                    return Wd

                def blk_X(b, Wg, Wu, xg2):
                    xgT = xgTs.next()
                    for hf in range(2):
                        xg = xg2[hf]
                        ps = ptr.next()
                        psv = ps[:, :].bitcast(BF16)
                        for c in range(8):
                            op("pe", lambda e: e.transpose(out=psv[:, c * 128:(c + 1) * 128], in_=xg[:, c * 128:(c + 1) * 128], identity=identb[:]), [xg, identb], [ps])
                        o_ap = xgT[:, :, hf * 128:(hf + 1) * 128]
                        i_ap = psv.rearrange("p (c n) -> p c n", c=8)
                        if hf == 0:
                            op("act", lambda e: e.copy(out=o_ap, in_=i_ap), [ps], [xgT])
                        else:
                            op("dve", lambda e: e.tensor_copy(out=o_ap, in_=i_ap), [ps], [xgT])
                    act_ = acts.next()
                    for m in range(4):
                        pg_, pu_ = pgu.next(), pgu.next()
                        for c in range(8):
                            op("pe", lambda e: e.matmul(pg_[:, 0:BLK], lhsT=Wg[:, c * HID + m * 128:c * HID + (m + 1) * 128], rhs=xgT[:, c, :], start=(c == 0), stop=(c == 7)), [Wg, xgT], [pg_])
                        for c in range(8):
                            op("pe", lambda e: e.matmul(pu_[:, 0:BLK], lhsT=Wu[:, c * HID + m * 128:c * HID + (m + 1) * 128], rhs=xgT[:, c, :], start=(c == 0), stop=(c == 7)), [Wu, xgT], [pu_])
                        sg = sgs.next()
                        op("act", lambda e: e.activation(out=sg[:, :], in_=pg_[:, 0:BLK], func=AF.Silu), [pg_], [sg])
                        op("dve", lambda e: e.tensor_tensor(out=act_[:, m, :], in0=sg[:, :], in1=pu_[:, 0:BLK], op=ALU.mult), [sg, pu_], [act_])
                    return act_

                def blk_Y(b, act_, Wd):
                    for hf in range(2):
                        a = 2 * b + hf
                        ysb = ysbs.next()
                        for n in range(2):
                            py = pyy.next()
                            for m in range(4):
                                op("pe", lambda e: e.matmul(py[:, :], lhsT=act_[:, m, hf * 128:(hf + 1) * 128], rhs=Wd[:, m * D + n * 512:m * D + (n + 1) * 512], start=(m == 0), stop=(m == 3)), [act_, Wd], [py])
                            if n == 0:
                                op("act", lambda e: e.activation(out=ysb[:, 0:512], in_=py[:, :], func=AF.Copy, scale=gate_s[:, a:a + 1]), [py, gate_s], [ysb])
                            else:
                                op("dve", lambda e: e.tensor_scalar_mul(out=ysb[:, 512:1024], in0=py[:, :], scalar1=gate_s[:, a:a + 1]), [py, gate_s], [ysb])
                        op("pool", lambda e: e.indirect_dma_start(out=y_d[:, :], out_offset=bass.IndirectOffsetOnAxis(ap=row_i[:, a:a + 1], axis=0), in_=ysb[:, :], in_offset=None), [ysb, row_i], [y_d], dma=True)

                gu = {0: g_gu(0)}
                wd_ = {0: g_d(0)}
                if NB > 1:
                    gu[1] = g_gu(1)
                    wd_[1] = g_d(1)
                acts_of = {0: blk_X(0, *gu.pop(0))}
                for b in range(NB):
                    if b + 1 < NB:
                        acts_of[b + 1] = blk_X(b + 1, *gu.pop(b + 1))
                    if b + 2 < NB:
                        gu[b + 2] = g_gu(b + 2)
                    blk_Y(b, acts_of.pop(b), wd_.pop(b))
                    if b + 2 < NB:
                        wd_[b + 2] = g_d(b + 2)
                fw.barrier()
                bs.close()
                tstack[0] = ph
                g_t = T("g", [128, D])
                b_t = T("b", [128, D])
                dma("sp", g_t[:], ln_ffn_g[layer:layer + 1, :].broadcast_to([128, D]), [ln_ffn_g], [g_t])
                dma("sp", b_t[:], ln_ffn_b[layer:layer + 1, :].broadcast_to([128, D]), [ln_ffn_b], [b_t])
                xts = Rot([T(f"cx{i}", [128, D]) for i in range(4)])
                y0s = Rot([T(f"cy0{i}", [128, D]) for i in range(4)])
                y1s = Rot([T(f"cy1{i}", [128, D]) for i in range(4)])
                junks = Rot([T(f"cj{i}", [128, D]) for i in range(4)])
                outs = Rot([T(f"co{i}", [128, D]) for i in range(4)])
                stts = Rot([T(f"cst{i}", [128, 8]) for i in range(4)])
                stA = {}
                for i in range(-2, NT):
                    ia, ib = i + 2, i + 1
                    if 0 <= ia < NT:
                        isl = slice(ia * 128, (ia + 1) * 128)
                        xt, y0, y1 = xts.next(), y0s.next(), y1s.next()
                        dma("sp", xt[:], x1_d[isl, :], [x1_d], [xt])
                        dma("sp", y0[:], y_d[isl, :], [y_d], [y0])
                        dma("sp", y1[:], y_d[S + ia * 128:S + (ia + 1) * 128, :], [y_d], [y1])
                    if 0 <= ib < NT:
                        yb, junkb, sttb = stA[ib]
                        ln_f1(sttb)
                        ln_f2(sttb)
                    if 0 <= ia < NT:
                        op("dve", lambda e: e.tensor_tensor(out=y1[:, :], in0=y0[:, :], in1=y1[:, :], op=ALU.add), [y0, y1], [y1])
                    if 0 <= ib < NT:
                        ln_f3(sttb)
                        ln_f4(yb, sttb, junkb)
                    if 0 <= ia < NT:
                        op("dve", lambda e: e.scalar_tensor_tensor(out=y0[:, :], in0=xt[:, :], scalar=float(ALPHA), in1=y1[:, :], op0=ALU.mult, op1=ALU.add), [xt, y1], [y0])
                        junk, stt = junks.next(), stts.next()
                        ln_stage1(y0, stt, junk)
                        stA[ia] = (y0, junk, stt)
                    if 0 <= ib < NT:
                        isl = slice(ib * 128, (ib + 1) * 128)
                        o_t = outs.next()
                        ln_back(g_t, b_t, o_t, junkb)
                        dma("pool", dst[isl, :], o_t[:, :], [o_t], [dst])
                        stA.pop(ib)
                fw.barrier()

        phase_C(0, w_out_ab, x_in)
        if upto == "C0":
            return nc, ext, out_d, dump_bufs
        phase_M(0, x2_d)
        if upto == "M0":
            return nc, ext, out_d, dump_bufs
        PATS = (1, 4, 16)
        with ExitStack() as ph:
            xT = fw.sb("l1_xT", [128, 8, S], BF16, ph)
            Wc = fw.sb("l1_W", [128, 8, 3 * D], BF16, ph, multi=True)
            for c in range(8):
                dma("pool", Wc[:, c, :], w_in_c[c * 128:(c + 1) * 128, :], [w_in_c], [Wc])
            stg = Rot([fw.sb(f"l1_x{i}", [128, D], F32, ph) for i in range(2)])
            build_xT(x2_d, xT, Rot(psb[0:2]), stg)
            psr = Rot(psb[2:8])
            sbf = Rot([fw.sb(f"l1_sb{i}", [128, 512], BF16, ph) for i in range(4)])
            vst = Rot([fw.sb(f"l1_v{i}", [128, 16, 65], BF16, ph) for i in range(6)])
            for v_ in vst.bufs:
                op("dve", lambda e: e.memset(v_[:, :, :], 1.0), [], [v_])
            for tb in range(8):
                tsl = slice(tb * 512, (tb + 1) * 512)
                for ch in range(16):
                    ps = psr.next()
                    for c in range(8):
                        op("pe", lambda e: e.matmul(ps[:, :], lhsT=Wc[:, c, ch * 128:(ch + 1) * 128], rhs=xT[:, c, tsl], start=(c == 0), stop=(c == 7)), [Wc, xT], [ps])
                    s_ = sbf.next()
                    if ch < 8:
                        op("act", lambda e: e.activation(out=s_[:, :], in_=ps[:, :], func=AF.Copy, scale=0.125), [ps], [s_])
                    else:
                        op("dve", lambda e: e.tensor_copy(out=s_[:, :], in_=ps[:, :]), [ps], [s_])
                    dma("pool", qkC[ch, :, tsl], s_[:, :], [s_], [qkC])
            for ri, r in enumerate(PATS):
                nqb = S // r // 128
                for res in range(r):
                    for kt in range(nqb):
                        t = res * nqb + kt
                        tok = ssl(res + r * 128 * kt, 128, r)
                        v_ = vst.next()
                        for n in range(2):
                            ps = psr.next()
                            for c in range(8):
                                op("pe", lambda e: e.matmul(ps[:, :], lhsT=xT[:, c, tok], rhs=Wc[:, c, 2048 + n * 512:2048 + (n + 1) * 512], start=(c == 0), stop=(c == 7)), [xT, Wc], [ps])
                            o_ap = v_[:, n * 8:(n + 1) * 8, 0:64]
                            i_ap = ps[:, :].rearrange("p (h e) -> p h e", e=64)
                            if n == 0:
                                op("act", lambda e: e.copy(out=o_ap, in_=i_ap), [ps], [v_])
                            else:
                                op("dve", lambda e: e.tensor_copy(out=o_ap, in_=i_ap), [ps], [v_])
                        for g in range(4):
                            dma("sp" if g % 2 == 0 else "pool", vC[ri, g, :, t, :], v_[:, g * 4:(g + 1) * 4, :].rearrange("p h e -> p (h e)"), [v_], [vC])
            fw.barrier()
        if upto == "A1x":
            return nc, ext, out_d, dump_bufs

        with ExitStack() as ph:
            dng = fw.sb("d_negd", [128, 3, 512], F32, ph)
            dma("sp", dng[:], dil_negd_d.ap(), [dil_negd_d], [dng])
            sel = fw.sb("d_sel", [128, 64], F32, ph)
            dma("sp", sel[:], sel_d.ap(), [sel_d], [sel])
            QZ = [[fw.sb(f"d_QZ{par}{cl}", [128, S], BF16, ph) for cl in range(2)] for par in range(2)]
            for par in range(2):
                for cl in range(2):
                    op("pool", lambda e: e.memset(QZ[par][cl][:, :], 0.0), [], [QZ[par][cl]])
            KT = [fw.sb(f"d_KT{cl}", [128, S], BF16, ph) for cl in range(2)]
            Vr = [fw.sb(f"d_V{ri}", [128, NT, 260], BF16, ph) for ri in range(3)]
            OaccL = [fw.sb(f"d_O{i}", [65, S], F32, ph) for i in range(4)]
            tmps = Rot([fw.sb(f"d_t{i}", [128, 512], F32, ph) for i in range(4)])
            Es = Rot([fw.sb(f"d_e{i}", [128, 512], BF16, ph) for i in range(8)])
            rcp = Rot([fw.sb(f"d_r{i}", [64, 512], F32, ph) for i in range(4)])
            obs = Rot([fw.sb(f"d_ob{i}", [64, 512], BF16, ph) for i in range(2)])
            pscore = Rot(psb[0:6])
            pov = Rot(psb[6:8])
            dslopes = alibi_slopes(16)
            LAG = 1
            gjobs = []
            for g in range(4):
                for hl in range(4):
                    h = 4 * g + hl
                    for ri, r in enumerate(PATS):
                        nqb = S // r // 128
                        G = min(4, nqb)
                        for res in range(r):
                            for qg in range(0, nqb, G):
                                gjobs.append(dict(g=g, hl=hl, h=h, ri=ri, r=r, nqb=nqb, G=G, res=res, qg=qg, first_g=False, last_h=False))
                    gjobs[-1]["last_h"] = True
            seen_g = set()
            for j in gjobs:
                if j["g"] not in seen_g:
                    seen_g.add(j["g"])
                    j["first_g"] = True

            def load_qk(g, cl):
                for par in range(2):
                    dma("sp", QZ[par][cl][par * 64:(par + 1) * 64, :], qkC[2 * g + cl, par * 64:(par + 1) * 64, :], [qkC], [QZ[par][cl]])
                dma("sp", KT[cl][:, :], qkC[8 + 2 * g + cl, :, :], [qkC], [KT[cl]])

            qk_loaded = set()

            def load_group(g):
                if (g, 0) not in qk_loaded:
                    load_qk(g, 0)
                    qk_loaded.add((g, 0))
                dma("sp", Vr[0][:, :, :], vC[0, g, :, :, :], [vC], [Vr[0]])
                load_qk(g, 1)
                qk_loaded.add((g, 1))
                for ri in (1, 2):
                    dma("sp", Vr[ri][:, :, :], vC[ri, g, :, :, :], [vC], [Vr[ri]])

            def d_stage1(j):
                if j["first_g"]:
                    load_group(j["g"])
                if j["hl"] == 2 and j["ri"] == 0 and j["res"] == 0 and j["qg"] == 0 and j["g"] + 1 < 4:
                    load_qk(j["g"] + 1, 0)
                    qk_loaded.add((j["g"] + 1, 0))
                hl, h, r, nqb, G, res, qg = j["hl"], j["h"], j["r"], j["nqb"], j["G"], j["res"], j["qg"]
                cl, r0 = hl // 2, (hl % 2) * 64
                blocks = list(range(qg, qg + G))
                Et, rng = [], []
                for typ in range(3):
                    ps = pscore.next()
                    val = [il for il, i in enumerate(blocks) if 0 <= i + typ - 1 < nqb]
                    lo, hi = val[0], val[-1] + 1
                    for il in val:
                        i = blocks[il]
                        kt = i + typ - 1
                        ksl = ssl(res + r * 128 * kt, 128, r)
                        qsl = ssl(res + r * 128 * i, 128, r)
                        op("pe", lambda e: e.matmul(ps[:, il * 128:(il + 1) * 128], lhsT=KT[cl][:, ksl], rhs=QZ[hl % 2][cl][:, qsl], start=True, stop=True), [KT[cl], QZ[hl % 2][cl]], [ps])
                    tmp, E = tmps.next(), Es.next()
                    op("dve", lambda e: e.scalar_tensor_tensor(out=tmp[:, lo * 128:hi * 128], in0=dng[:, typ, lo * 128:hi * 128], scalar=float(dslopes[h] * r), in1=ps[:, lo * 128:hi * 128], op0=ALU.mult, op1=ALU.add), [dng, ps], [tmp])
                    op("act", lambda e: e.activation(out=E[:, lo * 128:hi * 128], in_=tmp[:, lo * 128:hi * 128], func=AF.Exp), [tmp], [E])
                    Et.append(E)
                    rng.append(val)
                j["Et"], j["rng"], j["blocks"] = Et, rng, blocks

            def d_stage2(j):
                hl, h, ri, r, nqb, G, res, qg = j["hl"], j["h"], j["ri"], j["r"], j["nqb"], j["G"], j["res"], j["qg"]
                r0 = (hl % 2) * 64
                Et, rng, blocks = j["Et"], j["rng"], j["blocks"]
                po = pov.next()
                for il, i in enumerate(blocks):
                    typs = [typ for typ in range(3) if il in rng[typ]]
                    for n_, typ in enumerate(typs):
                        kt = i + typ - 1
                        t = res * nqb + kt
                        op("pe", lambda e: e.matmul(po[0:65, il * 128:(il + 1) * 128], lhsT=Vr[ri][:, t, hl * 65:(hl + 1) * 65], rhs=Et[typ][:, il * 128:(il + 1) * 128], start=(n_ == 0), stop=(n_ == len(typs) - 1)), [Vr[ri], Et[typ]], [po])
                osl = ssl(res + r * 128 * qg, 128 * G, r)
                Oacc = OaccL[hl]
                if ri == 0:
                    op("act", lambda e: e.copy(out=Oacc[0:65, osl], in_=po[0:65, 0:G * 128]), [po], [Oacc])
                else:
                    op("dve", lambda e: e.tensor_tensor(out=Oacc[0:65, osl], in0=Oacc[0:65, osl], in1=po[0:65, 0:G * 128], op=ALU.add), [Oacc, po], [Oacc])
                if j["last_h"]:
                    for _ in norm_gen(hl, h, r0):
                        pass

            def norm_gen(hl, h, r0):
                    Oacc = OaccL[hl]
                    for tb in range(8):
                        tsl = slice(tb * 512, (tb + 1) * 512)
                        pn = pov.next()
                        op("pe", lambda e: e.matmul(pn[0:64, :], lhsT=sel[0:65, :], rhs=Oacc[0:65, tsl], start=True, stop=True), [sel, Oacc], [pn])
                        rc, rc2, o_b = rcp.next(), rcp.next(), obs.next()
                        op("act", lambda e: e.activation(out=rc2[:, :], in_=pn[0:64, :], func=AF.Ln), [pn], [rc2])
                        op("act", lambda e: e.activation(out=rc[:, :], in_=rc2[:, :], func=AF.Exp, scale=-1.0), [rc2], [rc])
                        op("dve", lambda e: e.tensor_tensor(out=rc2[:, :], in0=rc[:, :], in1=pn[0:64, :], op=ALU.mult), [rc, pn], [rc2])
                        op("dve", lambda e: e.tensor_scalar(out=rc2[:, :], in0=rc2[:, :], scalar1=-1.0, scalar2=2.0, op0=ALU.mult, op1=ALU.add), [rc2], [rc2])
                        op("dve", lambda e: e.tensor_tensor(out=rc[:, :], in0=rc[:, :], in1=rc2[:, :], op=ALU.mult), [rc, rc2], [rc])
                        op("dve", lambda e: e.tensor_tensor(out=o_b[:, :], in0=Oacc[0:64, tsl], in1=rc[:, :], op=ALU.mult), [Oacc, rc], [o_b])
                        dma("pool", oT_d[h // 2, r0:r0 + 64, tsl], o_b[:, :], [o_b], [oT_d])
                        yield

            dnorm = []

            def step_norm(drain=False):
                for gn in list(dnorm):
                    while True:
                        try:
                            next(gn)
                        except StopIteration:
                            dnorm.remove(gn)
                            break
                        if not drain:
                            break

            pend = []
            for j in gjobs:
                if j["first_g"]:
                    while pend:
                        d_stage2(pend.pop(0))
                d_stage1(j)
                pend.append(j)
                if len(pend) > LAG:
                    d_stage2(pend.pop(0))
                step_norm()
            while pend:
                d_stage2(pend.pop(0))
            step_norm(drain=True)
            fw.barrier()
        if upto == "B1":
            return nc, ext, out_d, dump_bufs
        phase_C(1, w_out_c, x2_d)
        phase_M(1, out_d)
    return nc, ext, out_d, dump_bufs


def host_consts():
    c = {}
    c["ident"] = np.eye(128, dtype=np.float32)
    k = np.arange(128, dtype=np.float32)[:, None]
    n = np.arange(NEGD_W, dtype=np.float32)[None, :]
    c["negd"] = (-np.abs(n - k - NEGD_C)).astype(np.float32)
    half = 16
    inv_freq = np.power(np.float32(10000.0), -np.arange(half, dtype=np.float32) * np.float32(2.0) / np.float32(32)).astype(np.float32)
    ang = (np.arange(S, dtype=np.float32)[:, None] * inv_freq[None, :]).astype(np.float32)
    cos = np.cos(ang).astype(np.float32).T
    sin = np.sin(ang).astype(np.float32).T
    c32 = np.concatenate([cos, cos], 0)
    s32 = np.concatenate([-sin, sin], 0)
    c["ropecos"] = np.ascontiguousarray(np.tile(c32, (4, 1)))
    c["ropesin"] = np.ascontiguousarray(np.tile(s32, (4, 1)))
    kk = np.arange(128, dtype=np.float32)[:, None]
    qq = np.arange(128, dtype=np.float32)[None, :]
    tabs = []
    for typ in range(3):
        d = kk - qq + 128.0 * (typ - 1)
        t = np.where(np.abs(d) <= 64, -np.abs(d), -1.0e9).astype(np.float32)
        tabs.append(np.tile(t, (1, 4)))
    c["dil_negd"] = np.ascontiguousarray(np.stack(tabs, 1))
    mc = np.zeros((128, 4, 64), np.float32)
    mc[:, 0, :32] = np.arange(32, dtype=np.float32)[None, :]
    mc[:, 1, :32] = 256.0 * np.arange(32, dtype=np.float32)[None, :]
    mc[:, 2, :63] = 256.0 * np.arange(63, dtype=np.float32)[None, :]
    c["moe_c"] = mc
    mc[:, 3, :] = np.arange(128, dtype=np.float32)[:, None]
    c["tokid"] = (np.arange(NT, dtype=np.float32)[None, :] * 128 + np.arange(128, dtype=np.float32)[:, None]).astype(np.float32)
    sl = np.zeros((128, 64), np.float32)
    sl[64, :] = 1.0
    c["sel"] = sl
    c["triu"] = np.triu(np.ones((128, 128), np.float32), 1)
    return c


def host_weights(inp):
    w = {}
    wi = inp["w_in_ab"][0]
    w["w_in_ab"] = np.ascontiguousarray(np.concatenate([wi, wi[:, 1936:1952], wi[:, 1920:1936]], 1))
    wq = inp["w_uq"][0]
    nope = [wq[:, h * 96:h * 96 + 64] for h in range(4)]
    rope = [wq[:, h * 96 + 64:h * 96 + 96] for h in range(4)]
    rsw = [np.concatenate([wq[:, h * 96 + 80:h * 96 + 96], wq[:, h * 96 + 64:h * 96 + 80]], 1) for h in range(4)]
    w["w_uq"] = np.ascontiguousarray(np.concatenate(nope + rope + rsw, 1))
    wk = inp["w_ukv"][0]
    kn = [wk[:, h * 192:h * 192 + 64] for h in range(4)]
    vv = [wk[:, h * 192 + 64:h * 192 + 192] for h in range(4)]
    w["w_ukv"] = np.ascontiguousarray(np.concatenate(kn + vv, 1))
    w["w_out_ab"] = np.ascontiguousarray(inp["w_out_ab"][0])
    w["lam4"] = np.ascontiguousarray(np.stack([inp["lam_q1"][0], inp["lam_k1"][0], inp["lam_q2"][0], inp["lam_k2"][0]], 0))
    w["diff_g"] = np.ascontiguousarray(inp["diff_norm_g"][0].reshape(128, 1))
    w["qn_g"] = np.ascontiguousarray(inp["mla_q_norm_g"][0].reshape(2, 128).T)
    w["kvn_g"] = np.ascontiguousarray(inp["mla_kv_norm_g"][0].reshape(128, 1))
    for k in ("ln_mix_g", "ln_mix_b", "ln_ffn_g", "ln_ffn_b"):
        w[k] = np.ascontiguousarray(inp[k])
    w["w_router"] = np.ascontiguousarray(np.concatenate([inp["moe_w_group"], inp["moe_w_route"]], 2))
    w["b_router"] = np.ascontiguousarray(np.concatenate([inp["moe_b_group"], inp["moe_b_route"]], 1))
    g = inp["moe_w_gate"].reshape(2, E_, 8, 128, HID).transpose(0, 1, 3, 2, 4)
    w["wg_l"] = np.ascontiguousarray(g).reshape(2 * E_ * 128, 8 * HID)
    u = inp["moe_w_up"].reshape(2, E_, 8, 128, HID).transpose(0, 1, 3, 2, 4)
    w["wu_l"] = np.ascontiguousarray(u).reshape(2 * E_ * 128, 8 * HID)
    dd = inp["moe_w_down"].reshape(2, E_, 4, 128, D).transpose(0, 1, 3, 2, 4)
    w["wd_l"] = np.ascontiguousarray(dd).reshape(2 * E_ * 128, 4 * D)
    w["w_in_c"] = np.ascontiguousarray(inp["w_in_c"][0])
    w["w_out_c"] = np.ascontiguousarray(inp["w_out_c"][0])
    return w


def kernel(**inputs):
    inp = {k: np.asarray(v) for k, v in inputs.items()}
    nc, ext, out_d, _ = build_program()
    shared = host_consts()
    shared.update(host_weights(inp))
    x = np.ascontiguousarray(inp["x"], dtype=np.float32)
    in_maps = []
    for c in range(8):
        m = dict(shared)
        m["x"] = x[c]
        in_maps.append(m)
    res = run_bass_kernel_spmd(nc, in_maps, core_ids=list(range(8)))
    return np.stack([np.asarray(r["out"]) for r in res.results], 0).astype(np.float32)
```
